# Optimizing a Trainium2 kernel written in Bass

```python
import math
import jax, jax.numpy as jnp
from jax import lax
import numpy as np

D_MODEL = 1024
BATCH = 8
SEQ = 4096
DEPTH = 4

GRID_W = 64
CTX_LEN = 256
N_MOD = 6
EPS = 1e-6
D_MIX = D_MODEL
HY_CH = D_MIX // 4
S5_CH = D_MIX // 4
ATT_W = D_MIX // 2
HY_CONV = 3
HY_FILTER_DIM = 64
HY_BANDS = 16
HY_EMB = 1 + 2 * HY_BANDS
HY_DECAY_MIN = -math.log(1e-2) / 1.5
HY_DECAY_MAX = -math.log(1e-2) / 0.3
S5_GROUP = 16
S5_GROUPS = S5_CH // S5_GROUP
S5_STATE = 64
S5_DT_MIN = 1e-3
S5_DT_MAX = 1e-1
ATT_HEAD_DIM = 64
ATT_HEADS = ATT_W // (2 * ATT_HEAD_DIM)
ATT_V_DIM = 2 * ATT_HEAD_DIM
ROPE_HALF = ATT_HEAD_DIM // 2
ROPE_PAIRS_AXIS = ROPE_HALF // 2
ROPE_BASE = 10000.0
Q_BLOCK = 128
MOE_GROUPS = 4
MOE_EPG = 8
N_EXPERTS = MOE_GROUPS * MOE_EPG
MOE_TOP_K = 2
EXPERT_HIDDEN = D_MODEL // 2
MOE_BLOCK = 256
IN_COLS = 3 * HY_CH + S5_CH + 3 * ATT_W
IN_SPLITS = (3 * HY_CH, 3 * HY_CH + S5_CH, 3 * HY_CH + S5_CH + ATT_W, 3 * HY_CH + S5_CH + 2 * ATT_W)

kernel_name = 'hybrid_hyena_s5_diffattn_hmoe_dit'


def rms_norm(x, g):
    xf = x.astype(jnp.float32)
    y = xf * lax.rsqrt(jnp.mean(xf * xf, axis=-1, keepdims=True) + EPS)
    return (y * g.astype(jnp.float32)).astype(x.dtype)


def modulate(h, shift, scale):
    return h * (1.0 + scale) + shift


def short_conv(u, w, b):
    up = jnp.pad(u, ((0, 0), (1, 1), (0, 0)))
    return up[:, :-2] * w[0] + up[:, 1:-1] * w[1] + up[:, 2:] * w[2] + b


def hyena_filter(L, w1, b1, w2, b2, w3, freq):
    f32 = jnp.float32
    t = jnp.arange(L, dtype=f32) / L
    ang = (2.0 * math.pi) * t[:, None] * jnp.arange(1, HY_BANDS + 1, dtype=f32)
    feat = jnp.concatenate([t[:, None], jnp.cos(ang), jnp.sin(ang)], axis=-1)
    fr = freq.astype(f32)
    h = jnp.sin(fr * (feat @ w1.astype(f32) + b1.astype(f32)))
    h = jnp.sin(fr * (h @ w2.astype(f32) + b2.astype(f32)))
    h = (h @ w3.astype(f32)).reshape(L, 2, HY_CH)
    window = jnp.exp(-t[:, None] * jnp.linspace(HY_DECAY_MIN, HY_DECAY_MAX, HY_CH, dtype=f32))
    h = h * window[:, None, :]
    filt = jnp.concatenate([h[:, 0], jnp.zeros((1, HY_CH), f32), h[:0:-1, 1]], axis=0)
    return filt / (jnp.sum(jnp.abs(filt), axis=0, keepdims=True) + EPS)


def hyena_mixer(p, conv_w, conv_b, filt_params, skip, norm_g):
    L = p.shape[1]
    u = short_conv(p, conv_w, conv_b).astype(jnp.float32)
    x0, x1, v = jnp.split(u, 3, axis=-1)
    z = x1 * v
    filt = hyena_filter(L, *filt_params)
    zf = jnp.fft.rfft(z, n=2 * L, axis=1)
    ff = jnp.fft.rfft(filt, n=2 * L, axis=0)
    y = jnp.fft.irfft(zf * ff[None], n=2 * L, axis=1)[:, :L]
    return rms_norm(x0 * (y + skip.astype(jnp.float32) * z), norm_g)


def _scan_combine(left, right):
    a_l, b_l = left
    a_r, b_r = right
    return a_l * a_r, a_r * b_l + b_r


def linear_scan(a_bar, bu, reverse):
    a = jnp.broadcast_to(a_bar, (1, bu.shape[1]) + a_bar.shape)
    _, h = lax.associative_scan(_scan_combine, (a, bu), reverse=reverse, axis=1)
    return h


def s5_mixer(u_ctx, u_lat, a_re, a_im, log_dt, b_re, b_im, c_re, c_im, d_skip, glu_w, norm_g, need_ctx):
    f32 = jnp.float32
    B, Lc, _ = u_ctx.shape
    L = u_lat.shape[1]
    uc = u_ctx.astype(f32).reshape(B, Lc, S5_GROUPS, S5_GROUP)
    ul = u_lat.astype(f32).reshape(B, L, S5_GROUPS, S5_GROUP)
    dsk = d_skip.astype(f32).reshape(S5_GROUPS, S5_GROUP)
    y_lat = dsk * ul
    ctx_terms = [dsk * uc] if need_ctx else []
    for direction in range(2):
        rev = direction == 1
        A = lax.complex(a_re[direction].astype(f32), a_im[direction].astype(f32))
        dtA = jnp.exp(log_dt[direction].astype(f32))[:, None] * A
        a_bar = jnp.exp(dtA)
        b_bar = ((a_bar - 1.0) / A)[:, :, None] * lax.complex(b_re[direction].astype(f32), b_im[direction].astype(f32))
        c_r = c_re[direction].astype(f32)
        c_i = c_im[direction].astype(f32)

        def drive(u):
            return lax.complex(jnp.einsum('blgh,gph->blgp', u, b_bar.real), jnp.einsum('blgh,gph->blgp', u, b_bar.imag))

        def readout(h):
            return jnp.einsum('blgp,ghp->blgh', h.real, c_r) - jnp.einsum('blgp,ghp->blgh', h.imag, c_i)

        h_ctx = linear_scan(a_bar, drive(uc), rev)
        h0 = h_ctx[:, 0] if rev else h_ctx[:, -1]
        steps = (L - jnp.arange(L)) if rev else (jnp.arange(L) + 1)
        carry = jnp.exp(steps.astype(f32)[:, None, None] * dtA)
        h_lat = linear_scan(a_bar, drive(ul), rev) + carry[None] * h0[:, None]
        y_lat = y_lat + readout(h_lat)
        if need_ctx:
            ctx_terms.append(readout(h_ctx))

    def glu(y, n):
        g = jax.nn.gelu(y.reshape(B, n, S5_CH))
        return rms_norm(g * jax.nn.sigmoid(g @ glu_w.astype(f32)), norm_g)

    out_ctx = glu(sum(ctx_terms), Lc) if need_ctx else None
    return glu(y_lat, L), out_ctx


def axial_rope(L):
    rows = L // GRID_W
    row = jnp.repeat(jnp.arange(rows, dtype=jnp.float32), GRID_W)
    col = jnp.tile(jnp.arange(GRID_W, dtype=jnp.float32), rows)
    inv = ROPE_BASE ** (-jnp.arange(ROPE_PAIRS_AXIS, dtype=jnp.float32) / ROPE_PAIRS_AXIS)
    ang = jnp.concatenate([row[:, None] * inv, col[:, None] * inv], axis=-1)
    return jnp.cos(ang), jnp.sin(ang)


def apply_rope(t, cos, sin):
    t1, t2 = t[..., :ROPE_HALF], t[..., ROPE_HALF:]
    return jnp.concatenate([t1 * cos - t2 * sin, t1 * sin + t2 * cos], axis=-1)


def diff_attention(ql, kl, vl, qc, kc, vc, cos, sin, lam, lam_init, subln_g, need_ctx):
    f32 = jnp.float32
    B, L, _ = ql.shape
    Lc = qc.shape[1]
    scale = ATT_HEAD_DIM ** -0.5

    def heads_qk(t, n):
        return t.reshape(B, n, ATT_HEADS, 2, ATT_HEAD_DIM).transpose(0, 2, 3, 1, 4)

    def heads_v(t, n):
        return t.reshape(B, n, ATT_HEADS, ATT_V_DIM).transpose(0, 2, 1, 3)

    def diff_map(logits):
        p = jax.nn.softmax(logits.astype(f32) * scale, axis=-1)
        return p[:, :, 0] - lam * p[:, :, 1]

    def finish(o, n):
        o = rms_norm(o, subln_g) * (1.0 - lam_init)
        return o.transpose(0, 2, 1, 3).reshape(B, n, ATT_HEADS * ATT_V_DIM)

    q_lat = heads_qk(ql, L)
    q_lat_rot = apply_rope(q_lat, cos, sin)
    k_lat_rot = apply_rope(heads_qk(kl, L), cos, sin)
    k_ctx = heads_qk(kc, Lc)
    v_ctx = heads_v(vc, Lc)
    v_all = jnp.concatenate([v_ctx, heads_v(vl, L)], axis=2)

    def latent_block(i):
        s = i * Q_BLOCK
        qb = lax.dynamic_slice_in_dim(q_lat, s, Q_BLOCK, axis=3)
        qrb = lax.dynamic_slice_in_dim(q_lat_rot, s, Q_BLOCK, axis=3)
        logits = jnp.concatenate([
            jnp.einsum('bhmqd,bhmkd->bhmqk', qb, k_ctx, preferred_element_type=f32),
            jnp.einsum('bhmqd,bhmkd->bhmqk', qrb, k_lat_rot, preferred_element_type=f32)], axis=-1)
        return jnp.einsum('bhqk,bhkv->bhqv', diff_map(logits), v_all)

    o = lax.map(latent_block, jnp.arange(L // Q_BLOCK))
    o = o.transpose(1, 2, 0, 3, 4).reshape(B, ATT_HEADS, L, ATT_V_DIM)
    out_ctx = None
    if need_ctx:
        a_c = diff_map(jnp.einsum('bhmqd,bhmkd->bhmqk', heads_qk(qc, Lc), k_ctx, preferred_element_type=f32))
        out_ctx = finish(jnp.einsum('bhqk,bhkv->bhqv', a_c, v_ctx), Lc)
    return finish(o, L), out_ctx


def hier_moe(h, w_g, b_g, w_e, b_e, w1, w3, w2):
    f32 = jnp.float32
    T, D = h.shape
    hf = h.astype(f32)
    g_logits = hf @ w_g.astype(f32) + b_g.astype(f32)
    g_idx = jnp.argmax(g_logits, axis=-1)
    p_group = jnp.take_along_axis(jax.nn.softmax(g_logits, axis=-1), g_idx[:, None], axis=1)
    e_logits = (hf @ w_e.astype(f32) + b_e.astype(f32)).reshape(T, MOE_GROUPS, MOE_EPG)
    e_logits = jnp.take_along_axis(e_logits, g_idx[:, None, None], axis=1)[:, 0]
    top_p, top_i = lax.top_k(jax.nn.softmax(e_logits, axis=-1), MOE_TOP_K)
    gate = (p_group * top_p / jnp.sum(top_p, axis=-1, keepdims=True)).reshape(-1)
    expert = (g_idx[:, None] * MOE_EPG + top_i).reshape(-1).astype(jnp.int32)
    tok = jnp.repeat(jnp.arange(T, dtype=jnp.int32), MOE_TOP_K)
    n_assign = T * MOE_TOP_K
    n_blocks = -(-n_assign // MOE_BLOCK) + N_EXPERTS
    n_pad = n_blocks * MOE_BLOCK
    order = jnp.argsort(expert)
    se = expert[order]
    counts = jnp.bincount(expert, length=N_EXPERTS)
    start = jnp.cumsum(counts) - counts
    padded = (counts + MOE_BLOCK - 1) // MOE_BLOCK * MOE_BLOCK
    pad_end = jnp.cumsum(padded)
    pad_start = pad_end - padded
    dest = pad_start[se] + jnp.arange(n_assign, dtype=jnp.int32) - start[se]
    slot_tok = jnp.full((n_pad,), T, jnp.int32).at[dest].set(tok[order])
    slot_gate = jnp.zeros((n_pad,), f32).at[dest].set(gate[order])
    block_e = jnp.minimum(jnp.searchsorted(pad_end, jnp.arange(n_blocks) * MOE_BLOCK, side='right'), N_EXPERTS - 1)
    h_pad = jnp.concatenate([h, jnp.zeros((1, D), h.dtype)], axis=0)
    xb = h_pad[slot_tok].reshape(n_blocks, MOE_BLOCK, D)

    def run_block(args):
        xi, e = args
        return (jax.nn.silu(xi @ w1[e]) * (xi @ w3[e])) @ w2[e]

    yb = lax.map(run_block, (xb, block_e)).reshape(n_pad, D)
    y = jax.ops.segment_sum(yb * slot_gate[:, None].astype(yb.dtype), slot_tok, num_segments=T + 1)
    return y[:T]


def setup_inputs(seed: int = 0) -> dict:
    key = jax.random.key(seed)
    keys = iter(jax.random.split(key, 64))
    f32 = jnp.float32

    def normal(shape, scale):
        return scale * jax.random.normal(next(keys), shape, f32)

    def gain(shape):
        return 1.0 + normal(shape, 0.01)

    G, P, H = S5_GROUPS, S5_STATE, S5_GROUP
    E, F = N_EXPERTS, EXPERT_HIDDEN
    return {
        'x': normal((BATCH, SEQ, D_MODEL), 1.0),
        'c': normal((BATCH, D_MODEL), 1.0),
        'ctx': normal((BATCH, CTX_LEN, D_MODEL), 1.0),
        'c_ctx': normal((D_MODEL,), 1.0),
        'w_mod': normal((DEPTH, D_MODEL, N_MOD * D_MODEL), 0.5 * D_MODEL ** -0.5),
        'b_mod': normal((DEPTH, N_MOD * D_MODEL), 0.02),
        'norm1_g': gain((DEPTH, D_MODEL)),
        'norm2_g': gain((DEPTH, D_MODEL)),
        'final_g': gain((D_MODEL,)),
        'w_in': normal((DEPTH, D_MODEL, IN_COLS), D_MODEL ** -0.5),
        'w_out': normal((DEPTH, D_MIX, D_MODEL), D_MIX ** -0.5),
        'hy_conv_w': normal((DEPTH, HY_CONV, 3 * HY_CH), HY_CONV ** -0.5),
        'hy_conv_b': normal((DEPTH, 3 * HY_CH), 0.01),
        'hy_ffn_w1': normal((DEPTH, HY_EMB, HY_FILTER_DIM), HY_EMB ** -0.5),
        'hy_ffn_b1': normal((DEPTH, HY_FILTER_DIM), 0.1),
        'hy_ffn_w2': normal((DEPTH, HY_FILTER_DIM, HY_FILTER_DIM), HY_FILTER_DIM ** -0.5),
        'hy_ffn_b2': normal((DEPTH, HY_FILTER_DIM), 0.1),
        'hy_ffn_w3': normal((DEPTH, HY_FILTER_DIM, 2 * HY_CH), HY_FILTER_DIM ** -0.5),
        'hy_freq': gain((DEPTH, HY_FILTER_DIM)),
        'hy_skip': normal((DEPTH, HY_CH), 0.5),
        'hy_norm_g': gain((DEPTH, HY_CH)),
        's5_a_re': -0.5 + normal((DEPTH, 2, G, P), 0.01),
        's5_a_im': math.pi * jnp.arange(P, dtype=f32) + normal((DEPTH, 2, G, P), 0.01),
        's5_log_dt': jax.random.uniform(next(keys), (DEPTH, 2, G), f32, math.log(S5_DT_MIN), math.log(S5_DT_MAX)),
        's5_b_re': normal((DEPTH, 2, G, P, H), (2 * H) ** -0.5),
        's5_b_im': normal((DEPTH, 2, G, P, H), (2 * H) ** -0.5),
        's5_c_re': normal((DEPTH, 2, G, H, P), (2 * P) ** -0.5),
        's5_c_im': normal((DEPTH, 2, G, H, P), (2 * P) ** -0.5),
        's5_d': normal((DEPTH, S5_CH), 0.5),
        's5_glu_w': normal((DEPTH, S5_CH, S5_CH), S5_CH ** -0.5),
        's5_norm_g': gain((DEPTH, S5_CH)),
        'att_lq1': normal((DEPTH, ATT_HEAD_DIM), 0.1),
        'att_lk1': normal((DEPTH, ATT_HEAD_DIM), 0.1),
        'att_lq2': normal((DEPTH, ATT_HEAD_DIM), 0.1),
        'att_lk2': normal((DEPTH, ATT_HEAD_DIM), 0.1),
        'att_subln_g': gain((DEPTH, ATT_V_DIM)),
        'moe_wg': normal((DEPTH, D_MODEL, MOE_GROUPS), D_MODEL ** -0.5),
        'moe_bg': normal((DEPTH, MOE_GROUPS), 0.01),
        'moe_we': normal((DEPTH, D_MODEL, E), D_MODEL ** -0.5),
        'moe_be': normal((DEPTH, E), 0.01),
        'moe_w1': normal((DEPTH, E, D_MODEL, F), D_MODEL ** -0.5),
        'moe_w3': normal((DEPTH, E, D_MODEL, F), D_MODEL ** -0.5),
        'moe_w2': normal((DEPTH, E, F, D_MODEL), F ** -0.5),
    }


def reference(x, c, ctx, c_ctx, w_mod, b_mod, norm1_g, norm2_g, final_g, w_in, w_out,
              hy_conv_w, hy_conv_b, hy_ffn_w1, hy_ffn_b1, hy_ffn_w2, hy_ffn_b2, hy_ffn_w3, hy_freq,
              hy_skip, hy_norm_g, s5_a_re, s5_a_im, s5_log_dt, s5_b_re, s5_b_im, s5_c_re, s5_c_im,
              s5_d, s5_glu_w, s5_norm_g, att_lq1, att_lk1, att_lq2, att_lk2, att_subln_g,
              moe_wg, moe_bg, moe_we, moe_be, moe_w1, moe_w3, moe_w2):
    f32 = jnp.float32
    B, L, D = x.shape
    Lc = ctx.shape[1]
    cos, sin = axial_rope(L)
    silu_c = jax.nn.silu(c)
    silu_cc = jax.nn.silu(c_ctx)
    for l in range(DEPTH):
        need_ctx = l < DEPTH - 1
        lam_init = 0.8 - 0.6 * math.exp(-0.3 * l)
        sh1, sc1, g1, sh2, sc2, g2 = jnp.split((silu_c @ w_mod[l] + b_mod[l])[:, None, :], N_MOD, axis=-1)
        csh1, csc1, cg1, csh2, csc2, cg2 = jnp.split(silu_cc @ w_mod[l] + b_mod[l], N_MOD, axis=-1)

        p_lat = modulate(rms_norm(x, norm1_g[l]), sh1, sc1) @ w_in[l]
        p_ctx = modulate(rms_norm(ctx, norm1_g[l]), csh1, csc1) @ w_in[l]
        hy_l, s5_l, q_l, k_l, v_l = jnp.split(p_lat, IN_SPLITS, axis=-1)
        hy_c, s5_c, q_c, k_c, v_c = jnp.split(p_ctx, IN_SPLITS, axis=-1)

        filt_params = (hy_ffn_w1[l], hy_ffn_b1[l], hy_ffn_w2[l], hy_ffn_b2[l], hy_ffn_w3[l], hy_freq[l])
        hy_lat = hyena_mixer(hy_l, hy_conv_w[l], hy_conv_b[l], filt_params, hy_skip[l], hy_norm_g[l])

        s5_lat, s5_ctx = s5_mixer(s5_c, s5_l, s5_a_re[l], s5_a_im[l], s5_log_dt[l], s5_b_re[l], s5_b_im[l],
                                  s5_c_re[l], s5_c_im[l], s5_d[l], s5_glu_w[l], s5_norm_g[l], need_ctx)

        lam = (jnp.exp(jnp.sum(att_lq1[l].astype(f32) * att_lk1[l].astype(f32)))
               - jnp.exp(jnp.sum(att_lq2[l].astype(f32) * att_lk2[l].astype(f32))) + lam_init)
        att_lat, att_ctx = diff_attention(q_l, k_l, v_l, q_c, k_c, v_c, cos, sin, lam, lam_init,
                                          att_subln_g[l], need_ctx)

        mix_lat = jnp.concatenate([hy_lat, s5_lat, att_lat], axis=-1).astype(x.dtype)
        x = x + g1 * (mix_lat @ w_out[l])
        if need_ctx:
            hy_ctx = hyena_mixer(hy_c, hy_conv_w[l], hy_conv_b[l], filt_params, hy_skip[l], hy_norm_g[l])
            mix_ctx = jnp.concatenate([hy_ctx, s5_ctx, att_ctx], axis=-1).astype(ctx.dtype)
            ctx = ctx + cg1 * (mix_ctx @ w_out[l])

        moe_params = (moe_wg[l], moe_bg[l], moe_we[l], moe_be[l], moe_w1[l], moe_w3[l], moe_w2[l])
        h_lat = modulate(rms_norm(x, norm2_g[l]), sh2, sc2).reshape(B * L, D)
        if need_ctx:
            h_ctx = modulate(rms_norm(ctx, norm2_g[l]), csh2, csc2).reshape(B * Lc, D)
            y = hier_moe(jnp.concatenate([h_lat, h_ctx], axis=0), *moe_params)
            x = x + g2 * y[:B * L].reshape(B, L, D)
            ctx = ctx + cg2 * y[B * L:].reshape(B, Lc, D)
        else:
            x = x + g2 * hier_moe(h_lat, *moe_params).reshape(B, L, D)
    return rms_norm(x, final_g)
```

```python
import contextlib
import math
import numpy as np
import ml_dtypes
import concourse.bass as bass
import concourse.mybir as mybir
from concourse.bass_utils import run_bass_kernel_spmd

F32 = mybir.dt.float32
BF16 = mybir.dt.bfloat16
I32 = mybir.dt.int32
U32 = mybir.dt.uint32
AF = mybir.ActivationFunctionType
ALU = mybir.AluOpType
AX = mybir.AxisListType

DEPTH = 4
D = 1024
LAT = 4096
CTX = 256
T = LAT + CTX
NT = T // 128
EPS = 1e-6
TWO_PI = 2.0 * math.pi
NSLOT_BLK = 256
NBLK = (2 * T) // NSLOT_BLK + 32
NPAD = NBLK * NSLOT_BLK


class Buf:
    __slots__ = ("writers", "readers")

    def __init__(self):
        self.writers = {}
        self.readers = {}


class V:
    __slots__ = ("ap", "buf")

    def __init__(self, ap, buf):
        self.ap = ap
        self.buf = buf


class Tile:
    def __init__(self, t, buf=None):
        self.t = t
        self.b = buf or Buf()

    def __getitem__(self, idx):
        return V(self.t[idx], self.b)

    def v(self, ap):
        return V(ap, self.b)


class Eng:
    def __init__(self, name, h, is_pe=False):
        self.name = name
        self.h = h
        self.sem = None
        self.count = 0
        self.seen = {}
        self.is_pe = is_pe
        self.dq = []
        self.dqi = 0


EPOCH = 16000
NDQ = 6


class KB:
    def __init__(self, nc, stack):
        self.nc = nc
        self.stack = stack
        self.pe = Eng("pe", nc.tensor, True)
        self.act = Eng("act", nc.scalar)
        self.dve = Eng("dve", nc.vector)
        self.pool = Eng("pool", nc.gpsimd)
        self.sp = Eng("sp", nc.sync)
        self.nsem = 0
        self.ninst = 0
        self.uid = 0
        for e in (self.pe, self.act, self.dve, self.pool):
            e.sem = self.newsem()
        for e in (self.sp, self.act, self.pool):
            e.dq = [[self.newsem(), 0] for _ in range(NDQ)]
        self.engs = (self.pe, self.act, self.dve, self.pool, self.sp)

    def newsem(self):
        self.nsem += 1
        return self.stack.enter_context(self.nc.semaphore("s%d" % self.nsem))

    def name(self, n):
        self.uid += 1
        return "%s_%d" % (n, self.uid)

    def sb(self, name, shape, dt=F32, stack=None):
        st = stack or self.stack
        return Tile(st.enter_context(self.nc.sbuf_tensor(self.name(name), list(shape), dt)))

    def ps(self, name, shape, dt=F32, stack=None):
        st = stack or self.stack
        return Tile(st.enter_context(self.nc.psum_tensor(self.name(name), list(shape), dt)))

    def _deps(self, reads, writes):
        deps = {}
        for b in reads:
            for s, v in b.writers.items():
                if deps.get(s, 0) < v:
                    deps[s] = v
        for b in writes:
            for d in (b.writers, b.readers):
                for s, v in d.items():
                    if deps.get(s, 0) < v:
                        deps[s] = v
        return deps

    def _wait(self, eng, deps):
        for s, v in deps.items():
            if eng.is_pe and s is eng.sem:
                continue
            if eng.seen.get(s, 0) < v:
                eng.h.wait_ge(s, v)
                eng.seen[s] = v

    def _mark(self, tok, reads, writes):
        s, v = tok
        for b in reads:
            if b.readers.get(s, 0) < v:
                b.readers[s] = v
        for b in writes:
            b.writers = {s: v}
            b.readers = {}

    def op(self, eng, fn, reads=(), writes=()):
        reads = [r.buf if isinstance(r, V) else r for r in reads]
        writes = [w.buf if isinstance(w, V) else w for w in writes]
        self._wait(eng, self._deps(reads, writes))
        if eng.count >= EPOCH:
            eng.sem = self.newsem()
            eng.count = 0
        inst = fn(eng.h)
        eng.count += 1
        inst.then_inc(eng.sem, 1)
        self.ninst += 1
        self._mark((eng.sem, eng.count), reads, writes)
        return inst

    def dma(self, eng, fn, reads=(), writes=()):
        reads = [r.buf if isinstance(r, V) else r for r in reads]
        writes = [w.buf if isinstance(w, V) else w for w in writes]
        self._wait(eng, self._deps(reads, writes))
        slot = eng.dq[eng.dqi % NDQ]
        eng.dqi += 1
        if slot[1] >= 30000:
            slot[0] = self.newsem()
            slot[1] = 0
        s, v = slot
        if v > 0 and eng.seen.get(s, 0) < v:
            eng.h.wait_ge(s, v)
            eng.seen[s] = v
        inst = fn(eng.h)
        inst.then_inc(s, 16)
        slot[1] = v + 16
        self.ninst += 1
        self._mark((s, v + 16), reads, writes)
        return inst

    def barrier(self):
        toks = {}
        for e in (self.pe, self.act, self.dve, self.pool):
            if e.count > 0:
                toks[e.sem] = e.count
        for e in (self.sp, self.act, self.pool):
            for s, v in e.dq:
                if v > 0:
                    toks[s] = v
        for e in self.engs:
            for s, v in toks.items():
                if s is e.sem:
                    continue
                if e.seen.get(s, 0) < v:
                    e.h.wait_ge(s, v)
                    e.seen[s] = v

    def mm(self, o, lhsT, rhs, start=True, stop=True):
        return self.op(self.pe, lambda h: h.matmul(o.ap, lhsT.ap, rhs.ap, start=start, stop=stop),
                       reads=[lhsT, rhs], writes=[o])

    def tr(self, o, i, ident):
        return self.op(self.pe, lambda h: h.transpose(o.ap, i.ap, ident.ap), reads=[i, ident], writes=[o])

    def act_(self, o, i, func, bias=None, scale=None, accum=None, eng=None):
        reads = [i]
        kw = {}
        if bias is not None:
            if isinstance(bias, V):
                reads.append(bias)
                kw["bias"] = bias.ap
            else:
                kw["bias"] = bias
        if scale is not None:
            if isinstance(scale, V):
                reads.append(scale)
                kw["scale"] = scale.ap
            else:
                kw["scale"] = scale
        writes = [o]
        if accum is not None:
            kw["accum_out"] = accum.ap
            writes.append(accum)
        return self.op(self.act, lambda h: h.activation(out=o.ap, in_=i.ap, func=func, **kw), reads=reads, writes=writes)

    def tt(self, eng, o, a, b, op):
        return self.op(eng, lambda h: h.tensor_tensor(o.ap, a.ap, b.ap, op), reads=[a, b], writes=[o])

    def ts(self, eng, o, a, s1, s2, op0, op1=None):
        reads = [a]
        x1 = s1.ap if isinstance(s1, V) else s1
        x2 = s2.ap if isinstance(s2, V) else s2
        if isinstance(s1, V):
            reads.append(s1)
        if isinstance(s2, V):
            reads.append(s2)
        if op1 is None:
            return self.op(eng, lambda h: h.tensor_scalar(o.ap, a.ap, x1, None, op0), reads=reads, writes=[o])
        return self.op(eng, lambda h: h.tensor_scalar(o.ap, a.ap, x1, x2, op0, op1), reads=reads, writes=[o])

    def stt(self, eng, o, a, s, b, op0, op1):
        reads = [a, b]
        xs = s.ap if isinstance(s, V) else s
        if isinstance(s, V):
            reads.append(s)
        return self.op(eng, lambda h: h.scalar_tensor_tensor(o.ap, a.ap, xs, b.ap, op0, op1), reads=reads, writes=[o])

    def cp(self, eng, o, i):
        if eng is self.act:
            return self.op(eng, lambda h: h.copy(o.ap, i.ap), reads=[i], writes=[o])
        return self.op(eng, lambda h: h.tensor_copy(o.ap, i.ap), reads=[i], writes=[o])

    def memset(self, eng, o, val):
        return self.op(eng, lambda h: h.memset(o.ap, val), writes=[o])

    def ld(self, q, o, src_ap, reads=()):
        return self.dma(q, lambda h: h.dma_start(out=o.ap, in_=src_ap), reads=list(reads), writes=[o])

    def st(self, q, dst_ap, i, writes=()):
        return self.dma(q, lambda h: h.dma_start(out=dst_ap, in_=i.ap), reads=[i], writes=list(writes))


def _col(v, nt):
    return np.ascontiguousarray(np.asarray(v, np.float32).reshape(nt, 128).T)


def host_constants():
    c = {}
    f32 = np.float32
    for tag, L in (("L", LAT), ("C", CTX)):
        t = (np.arange(L, dtype=f32) / f32(L)).astype(f32)
        ang = (f32(2.0 * math.pi) * t[:, None] * np.arange(1, 17, dtype=f32)).astype(f32)
        feat = np.concatenate([t[:, None], np.cos(ang), np.sin(ang)], axis=-1).astype(f32)
        c["featT_" + tag] = np.ascontiguousarray(feat.T)
        c["negt_" + tag] = _col(-t, L // 128)
        N = 2 * L
        k = np.arange(L, dtype=np.float64)
        th = 2.0 * np.pi * np.outer(k + 0.5, k + 0.5) / N
        c["dftC_" + tag] = np.cos(th).astype(ml_dtypes.bfloat16)
        c["dftS_" + tag] = np.sin(th).astype(ml_dtypes.bfloat16)
        ph = np.pi * (k + 0.5) / N
        c["ab_" + tag] = np.ascontiguousarray(np.stack([_col(np.cos(ph), L // 128), _col(np.sin(ph), L // 128)], axis=1))
    dmin = -math.log(1e-2) / 1.5
    dmax = -math.log(1e-2) / 0.3
    c["decay_b"] = np.ascontiguousarray(np.broadcast_to(np.linspace(dmin, dmax, 256, dtype=f32)[None, :], (128, 256)))
    rows = LAT // 64
    row = np.repeat(np.arange(rows, dtype=f32), 64)
    colv = np.tile(np.arange(64, dtype=f32), rows)
    inv = (f32(10000.0) ** (-np.arange(16, dtype=f32) / f32(16))).astype(f32)
    ang = np.concatenate([row[:, None] * inv, colv[:, None] * inv], axis=-1).astype(f32)
    j = (np.arange(128) % 64) % 32
    c["ropeT"] = np.ascontiguousarray(np.stack([np.cos(ang)[:, j].T, np.sin(ang)[:, j].T], axis=1).astype(f32))
    return c


def host_common(inp):
    f32 = np.float32
    g = {k: np.asarray(v) for k, v in inp.items()}
    o = {}
    o["w_mod"] = g["w_mod"]
    o["b_mod"] = g["b_mod"].reshape(DEPTH, 1, 6 * D)
    o["gn_rows"] = np.concatenate([g["norm1_g"], g["norm2_g"], g["final_g"][None]], axis=0).reshape(9, 1, D)
    o["w_in"] = g["w_in"]
    o["w_out"] = g["w_out"]
    o["hy_cw"] = np.ascontiguousarray(g["hy_conv_w"].reshape(DEPTH, 3, 6, 128).transpose(0, 3, 2, 1))
    o["hy_cb"] = np.ascontiguousarray(g["hy_conv_b"].reshape(DEPTH, 6, 128).transpose(0, 2, 1))
    o["hy_skip"] = np.ascontiguousarray(g["hy_skip"].reshape(DEPTH, 2, 128).transpose(0, 2, 1))
    o["hy_ng"] = np.ascontiguousarray(g["hy_norm_g"].reshape(DEPTH, 2, 128).transpose(0, 2, 1))
    o["hy_w1"] = g["hy_ffn_w1"]
    o["hy_w2"] = g["hy_ffn_w2"]
    o["hy_w3"] = g["hy_ffn_w3"]
    o["hy_bf"] = np.ascontiguousarray(np.stack([g["hy_ffn_b1"], g["hy_ffn_b2"], g["hy_freq"]], axis=-1))
    G, P, H = 16, 64, 16
    rows = np.stack([g["s5_a_re"].reshape(DEPTH, 2, G * P), g["s5_a_im"].reshape(DEPTH, 2, G * P),
                     np.repeat(g["s5_log_dt"], P, axis=-1)], axis=2)
    o["s5_row"] = np.ascontiguousarray(rows.reshape(DEPTH, 2, 1, 3 * G * P))
    cols = rows.reshape(DEPTH, 2, 3, 8, 128).transpose(0, 1, 4, 2, 3)
    o["s5_col"] = np.ascontiguousarray(cols)
    bT = np.zeros((DEPTH, 2, 2, 2, 128, 512), f32)
    cT = np.zeros((DEPTH, 2, 2, 8, 128, 128), f32)
    for ri, (bsrc, csrc) in enumerate(((g["s5_b_re"], g["s5_c_re"]), (g["s5_b_im"], g["s5_c_im"]))):
        for gg in range(G):
            half, gm = gg // 8, gg % 8
            bT[:, :, ri, half, gm * 16:(gm + 1) * 16, gm * 64:(gm + 1) * 64] = bsrc[:, :, gg].transpose(0, 1, 3, 2)
            pair, gl = gg // 2, gg % 2
            cT[:, :, ri, pair, gl * 64:(gl + 1) * 64, gm * 16:(gm + 1) * 16] = csrc[:, :, gg].transpose(0, 1, 3, 2)
    o["s5_bT"] = bT
    o["s5_cT"] = cT
    o["s5_dn"] = np.ascontiguousarray(np.stack([g["s5_d"].reshape(DEPTH, 2, 128).transpose(0, 2, 1),
                                                 g["s5_norm_g"].reshape(DEPTH, 2, 128).transpose(0, 2, 1)], axis=2))
    o["s5_glu"] = g["s5_glu_w"]
    o["att_l"] = np.concatenate([g["att_lq1"], g["att_lk1"], g["att_lq2"], g["att_lk2"]], axis=-1).reshape(DEPTH, 1, 256)
    o["att_g"] = g["att_subln_g"].reshape(DEPTH, 1, 128)
    o["moe_wr"] = np.ascontiguousarray(np.concatenate([g["moe_wg"], g["moe_we"]], axis=-1))
    o["moe_br"] = np.concatenate([g["moe_bg"], g["moe_be"]], axis=-1).reshape(DEPTH, 1, 36)
    w1 = g["moe_w1"].reshape(DEPTH, 32, 8, 128, 512).transpose(0, 1, 3, 2, 4)
    w3 = g["moe_w3"].reshape(DEPTH, 32, 8, 128, 512).transpose(0, 1, 3, 2, 4)
    o["moe_w13h"] = np.ascontiguousarray(np.stack([w1, w3], axis=3)).reshape(DEPTH, 32 * 128, 2 * 8 * 512)
    o["moe_w2h"] = np.ascontiguousarray(g["moe_w2"].reshape(DEPTH, 32, 4, 128, 1024).transpose(0, 1, 3, 2, 4)).reshape(DEPTH, 32 * 128, 4 * 1024)
    o.update(host_constants())
    return {k: np.ascontiguousarray(v) for k, v in o.items()}


def host_core(inp, b):
    x = np.asarray(inp["x"])[b]
    ctx = np.asarray(inp["ctx"])[b]
    cc = np.stack([_col(np.asarray(inp["c"])[b], 8), _col(np.asarray(inp["c_ctx"]), 8)], axis=-1)
    return {"xin": np.ascontiguousarray(np.concatenate([ctx, x], axis=0)), "cT": np.ascontiguousarray(cc)}


IN_SHAPES = None


def chunks_of(T0, Ttot, W):
    out = []
    t = T0
    while t < Ttot:
        w = min(W, Ttot - t)
        out.append((t, w))
        t += w
    return out


TOK_CHUNKS = [(0, CTX)] + chunks_of(CTX, T, 512)


class Prog:
    def __init__(self, arrays, layer_ids, debug=(), phases=None):
        self.layer_ids = list(layer_ids)
        self.NL = len(self.layer_ids)
        self.debug = set(debug)
        self.phases = phases
        nc = bass.Bass("TRN2", target_bir_lowering=False)
        self.nc = nc
        self.din = {}
        for k, v in arrays.items():
            dt = {np.dtype(np.float32): F32, np.dtype(ml_dtypes.bfloat16): BF16, np.dtype(np.int32): I32}[v.dtype]
            self.din[k] = nc.dram_tensor(k, list(v.shape), dt, kind="ExternalInput").ap()
        self.out = nc.dram_tensor("out", [LAT, D], F32, kind="ExternalOutput").ap()

        def scratch(name, shape, dt):
            kind = "ExternalOutput" if name in self.debug else "Internal"
            return nc.dram_tensor(name, list(shape), dt, kind=kind).ap()

        self.Xres = scratch("Xres", [T, D], F32)
        self.modrow = scratch("modrow", [self.NL, 2, 6 * D], F32)
        self.pT = scratch("pT", [1024, T], F32)
        self.QT = scratch("QT", [512, T], BF16)
        self.QrT = scratch("QrT", [512, LAT], BF16)
        self.KcT = scratch("KcT", [512, T], BF16)
        self.Vaug = scratch("Vaug", [NT, 128, 4 * 129], BF16)
        self.mixT = scratch("mixT", [1024, T], BF16)
        self.Hs = scratch("Hs", [T + 1, D], BF16)
        self.slot_tok = scratch("slot_tok", [NPAD, 1], I32)
        self.yb = scratch("yb", [NPAD, D], F32)

    def build(self):
        nc = self.nc
        with contextlib.ExitStack() as st:
            kb = KB(nc, st)
            self.kb = kb
            self.setup()
            self.modulation()
            kb.barrier()
            for li in range(self.NL):
                self.layer(li)
            self.final_norm()
            kb.barrier()
        return nc

    def ph(self, name):
        return self.phases is None or name in self.phases

    def setup(self):
        kb = self.kb
        self.ident_f = kb.sb("ident_f", [128, 128])
        kb.memset(kb.pool, self.ident_f[:], 0.0)
        kb.op(kb.pool, lambda h: h.affine_select(self.ident_f.t[:], self.ident_f.t[:], [[-1, 128]], ALU.not_equal, 1.0,
                                                 base=0, channel_multiplier=1), reads=[self.ident_f.b], writes=[self.ident_f.b])
        self.ident_b = kb.sb("ident_b", [128, 128], BF16)
        kb.cp(kb.dve, self.ident_b[:], self.ident_f[:])
        self.ones_f = kb.sb("ones_f", [128, 128])
        kb.memset(kb.dve, self.ones_f[:], 1.0)
        self.tri = kb.sb("tri", [128, 128])
        kb.memset(kb.pool, self.tri[:], 1.0)
        kb.op(kb.pool, lambda h: h.affine_select(self.tri.t[:], self.tri.t[:], [[1, 128]], ALU.is_ge, 0.0,
                                                 base=-1, channel_multiplier=-1), reads=[self.tri.b], writes=[self.tri.b])
        self.iota512 = kb.sb("iota512", [128, 512])
        kb.op(kb.pool, lambda h: h.iota(self.iota512.t[:], [[1, 512]], base=0, channel_multiplier=0,
                                        allow_small_or_imprecise_dtypes=True), writes=[self.iota512.b])
        self.blk = []
        for m in range(2):
            b = kb.sb("blk%d" % m, [128, 128], BF16)
            kb.memset(kb.dve, b[:], 0.0)
            kb.memset(kb.dve, b[m * 64:(m + 1) * 64, :], 1.0)
            self.blk.append(b)
        self.r_hy = kb.sb("r_hy", [128, NT])
        self.r_s5 = kb.sb("r_s5", [128, NT])
        self.qkmax = kb.sb("qkmax", [128, 16])
        self.halfpi = kb.sb("halfpi", [128, 1])
        kb.memset(kb.dve, self.halfpi[:], math.pi / 2.0)

    def modulation(self):
        kb = self.kb
        with contextlib.ExitStack() as ph:
            cT = kb.sb("cT", [128, 8, 2], stack=ph)
            kb.ld(kb.sp, cT[:], self.din["cT"])
            sc = kb.sb("sc", [128, 8, 2], stack=ph)
            kb.act_(sc[:], cT[:], AF.Silu)
            wm = [kb.sb("wm%d" % i, [128, 8, 512], stack=ph) for i in range(2)]
            pm = [kb.ps("pm%d" % i, [128, 512], stack=ph) for i in range(2)]
            rows = [kb.sb("mrow%d" % i, [1, 6 * D], stack=ph) for i in range(2)]
            brow = kb.sb("brow", [1, 6 * D], stack=ph)
            n = 0
            for li in range(self.NL):
                kb.ld(kb.sp, brow[:], self.din["b_mod"][li])
                for ch in range(12):
                    w = wm[n % 2]
                    n += 1
                    kb.ld(kb.sp, w[:], self.din["w_mod"][li][:, ch * 512:(ch + 1) * 512].rearrange("(kt p) n -> p kt n", p=128))
                    for which in range(2):
                        p = pm[which]
                        for kt in range(8):
                            kb.mm(p[0:1, :], sc[:, kt, which:which + 1], w[:, kt, :], start=(kt == 0), stop=(kt == 7))
                        kb.tt(kb.dve, rows[which][:, ch * 512:(ch + 1) * 512], p[0:1, :], brow[:, ch * 512:(ch + 1) * 512], ALU.add)
                for which in range(2):
                    kb.st(kb.sp, self.modrow[li, which:which + 1, :], rows[which][:])
            kb.barrier()

    def load_mod_b(self, li, which, seg, stack, name):
        t = self.kb.sb(name, [128, D], stack=stack)
        self.kb.ld(self.kb.sp, t[:], self.modrow[li, which:which + 1, seg * D:(seg + 1) * D].partition_broadcast(128))
        return t

    def norm_mod_tiles(self, li, gidx, seg_sh, seg_sc, stack):
        kb = self.kb
        gb = kb.sb("gnb", [128, D], stack=stack)
        kb.ld(kb.sp, gb[:], self.din["gn_rows"][gidx].partition_broadcast(128))
        res = []
        for which in range(2):
            scb = self.load_mod_b(li, which, seg_sc, stack, "scb%d" % which)
            shb = self.load_mod_b(li, which, seg_sh, stack, "shb%d" % which)
            kb.stt(kb.dve, scb[:], scb[:], 1.0, gb[:], ALU.add, ALU.mult)
            res.append((scb, shb))
        return res

    def norm_tile(self, xt, ab, xn, tmp_ss, tmp_junk):
        kb = self.kb
        A, B = ab
        kb.act_(tmp_junk[:], xt[:], AF.Square, accum=tmp_ss[:, 0:1])
        kb.ts(kb.dve, tmp_ss[:, 1:2], tmp_ss[:, 0:1], 1.0 / D, EPS, ALU.mult, ALU.add)
        kb.act_(tmp_ss[:, 2:3], tmp_ss[:, 1:2], AF.Sqrt)
        kb.op(kb.dve, lambda h: h.reciprocal(tmp_ss.t[:, 3:4], tmp_ss.t[:, 2:3]), reads=[tmp_ss.b], writes=[tmp_ss.b])
        kb.stt(kb.dve, xn[:], xt[:], tmp_ss[:, 3:4], A[:], ALU.mult, ALU.mult)
        kb.tt(kb.dve, xn[:], xn[:], B[:], ALU.add)

    def layer(self, li):
        kb = self.kb
        if self.ph("inproj"):
            self.inproj(li)
            kb.barrier()
        if self.ph("hyena"):
            self.hyena(li, LAT, CTX, "L")
            kb.barrier()
            if self.layer_ids[li] < DEPTH - 1:
                self.hyena(li, CTX, 0, "C")
                kb.barrier()
        if self.ph("s5") or self.ph("attn"):
            self.s5_attn(li)
            kb.barrier()
        if self.ph("outproj"):
            self.outproj(li)
            kb.barrier()
        if self.ph("moe"):
            self.moe(li)
            kb.barrier()

    def xsrc(self, li):
        return self.din["xin"] if li == 0 else self.Xres

    def inproj(self, li):
        kb = self.kb
        src = self.xsrc(li)
        with contextlib.ExitStack() as ph:
            w_in = kb.sb("w_in", [128, 8, 2560], BF16, stack=ph)
            for kt in range(8):
                kb.ld(kb.pool, w_in[:, kt, :], self.din["w_in"][li][kt * 128:(kt + 1) * 128, :])
            wrot = kb.sb("wrot", [128, 8, 1024], BF16, stack=ph)
            wrot5 = wrot.t[:].rearrange("p k (b two j) -> p k b two j", two=2, j=32)
            for kt in range(8):
                srcv = self.din["w_in"][li][kt * 128:(kt + 1) * 128, 1024:2048].rearrange("p (b two j) -> p b two j", two=2, j=32)
                kb.dma(kb.pool, lambda h: h.dma_start(out=wrot5[:, kt, :, 0, :], in_=srcv[:, :, 1, :]), writes=[wrot.b])
                kb.dma(kb.pool, lambda h: h.dma_start(out=wrot5[:, kt, :, 1, :], in_=srcv[:, :, 0, :]), writes=[wrot.b])
            for kt in range(8):
                kb.op(kb.dve, lambda h: h.tensor_scalar(wrot5[:, kt, :, 0, :], wrot5[:, kt, :, 0, :], -1.0, None, ALU.mult),
                      reads=[wrot.b], writes=[wrot.b])
            rope = kb.sb("rope", [128, 2, LAT], stack=ph)
            kb.ld(kb.sp, rope[:], self.din["ropeT"])
            ab = self.norm_mod_tiles(li, self.layer_ids[li], 0, 1, ph)
            kb.memset(kb.dve, self.qkmax[:], 0.0)
            xt = [kb.sb("xt%d" % i, [128, D], stack=ph) for i in range(2)]
            xn = kb.sb("xn", [128, D], stack=ph)
            junk = kb.sb("junk", [128, D], stack=ph)
            ss = kb.sb("ss", [128, 4], stack=ph)
            xnT = [kb.sb("xnT%d" % i, [128, 8, 512], BF16, stack=ph) for i in range(2)]
            trp = kb.ps("trp", [128, D], stack=ph)
            pj = [kb.ps("pj%d" % i, [128, 512], stack=ph) for i in range(4)]
            pv = kb.ps("pv", [128, 512], stack=ph)
            nb = kb.ps("nb", [128, 512], stack=ph)
            stg = [kb.sb("stg%d" % i, [128, 512], stack=ph) for i in range(2)]
            stb = [kb.sb("stb%d" % i, [128, 512], BF16, stack=ph) for i in range(3)]
            sqb = kb.sb("sqb", [128, 512], BF16, stack=ph)
            red = kb.sb("red", [128, 1], stack=ph)
            t1 = kb.sb("t1", [128, 512], stack=ph)
            t2 = kb.sb("t2", [128, 512], stack=ph)
            vst = [kb.sb("vst%d" % i, [128, 4, 129], BF16, stack=ph) for i in range(2)]
            for v in vst:
                kb.memset(kb.dve, v[:], 1.0)
            cnt = {"ev": 0, "stg": 0, "stb": 0, "pj": 0, "v": 0, "x": 0}

            def evac_engine():
                cnt["ev"] += 1
                return kb.act if cnt["ev"] % 2 == 0 else kb.dve

            def next_pj():
                cnt["pj"] += 1
                return pj[cnt["pj"] % 4]

            def proj_fm(wv, c0, xc, W):
                p = next_pj()
                for kt in range(8):
                    kb.mm(p[:, :W], V(wv(kt, c0), wv.buf), xc[:, kt, :W], start=(kt == 0), stop=(kt == 7))
                return p

            def w_in_cols(kt, c0):
                return w_in.t[:, kt, c0:c0 + 128]
            w_in_cols.buf = w_in.b

            def w_rot_cols(kt, c0):
                return wrot.t[:, kt, c0:c0 + 128]
            w_rot_cols.buf = wrot.b

            def norm_stat(sv, W, col):
                kb.tt(kb.dve, sqb[:, :W], sv, sv, ALU.mult)
                for m in range(2):
                    kb.mm(nb[:, :W], self.blk[m][:], sqb[:, :W])
                    kb.op(kb.dve, lambda h: h.reduce_max(red.t[:], nb.t[:, :W], AX.X), reads=[nb.b], writes=[red.b])
                    kb.tt(kb.dve, self.qkmax[:, col + m:col + m + 1], self.qkmax[:, col + m:col + m + 1], red[:], ALU.max)

            for ci, (t0, W) in enumerate(TOK_CHUNKS):
                which = 1 if ci == 0 else 0
                xc = xnT[ci % 2]
                ntile = W // 128
                for ti in range(ntile):
                    x = xt[cnt["x"] % 2]
                    cnt["x"] += 1
                    kb.ld(kb.sp, x[:], src[t0 + ti * 128:t0 + (ti + 1) * 128, :])
                    self.norm_tile(x, ab[which], xn, ss, junk)
                    for kt in range(8):
                        kb.tr(trp[:, kt * 128:(kt + 1) * 128], xn[:, kt * 128:(kt + 1) * 128], self.ident_f[:])
                    for hf in range(2):
                        e = evac_engine()
                        kb.cp(e, V(xc.t[:, hf * 4:(hf + 1) * 4, ti * 128:(ti + 1) * 128], xc.b),
                              V(trp.t[:, hf * 512:(hf + 1) * 512].rearrange("p (k t) -> p k t", k=4), trp.b))
                for j in range(8):
                    p = proj_fm(w_in_cols, j * 128, xc, W)
                    s = stg[cnt["stg"] % 2]
                    cnt["stg"] += 1
                    kb.cp(evac_engine(), s[:, :W], p[:, :W])
                    kb.st(kb.sp, self.pT[j * 128:(j + 1) * 128, t0:t0 + W], s[:, :W])
                lat = ci > 0
                tl = t0 - CTX
                for kind in range(2):
                    for hh in range(4):
                        c0 = 1024 + kind * 512 + hh * 128
                        pa = proj_fm(w_in_cols, c0, xc, W)
                        col = kind * 8 + hh * 2
                        if (kind == 0) or (not lat):
                            sbt = stb[cnt["stb"] % 3]
                            cnt["stb"] += 1
                            kb.act_(sbt[:, :W], pa[:, :W], AF.Copy, scale=(0.125 if kind == 0 else 1.0))
                            dst = self.QT if kind == 0 else self.KcT
                            kb.st(kb.sp, dst[hh * 128:(hh + 1) * 128, t0:t0 + W], sbt[:, :W])
                            if not lat or kind == 0:
                                norm_stat(sbt[:, :W], W, col)
                        if lat:
                            pb = proj_fm(w_rot_cols, kind * 512 + hh * 128, xc, W)
                            kb.tt(kb.dve, t1[:, :W], pa[:, :W], rope[:, 0, tl:tl + W], ALU.mult)
                            kb.tt(kb.dve, t2[:, :W], pb[:, :W], rope[:, 1, tl:tl + W], ALU.mult)
                            sbt = stb[cnt["stb"] % 3]
                            cnt["stb"] += 1
                            kb.stt(kb.dve, sbt[:, :W], t1[:, :W], (0.125 if kind == 0 else 1.0), t2[:, :W], ALU.mult, ALU.add) \
                                if kind == 1 else None
                            if kind == 0:
                                kb.tt(kb.dve, t1[:, :W], t1[:, :W], t2[:, :W], ALU.add)
                                kb.act_(sbt[:, :W], t1[:, :W], AF.Copy, scale=0.125)
                                kb.st(kb.sp, self.QrT[hh * 128:(hh + 1) * 128, tl:tl + W], sbt[:, :W])
                            else:
                                kb.st(kb.sp, self.KcT[hh * 128:(hh + 1) * 128, t0:t0 + W], sbt[:, :W])
                                norm_stat(sbt[:, :W], W, col)
                for ti in range(ntile):
                    for kt in range(8):
                        kb.mm(pv[:], xc[:, kt, ti * 128:(ti + 1) * 128], w_in[:, kt, 2048:2560], start=(kt == 0), stop=(kt == 7))
                    vs = vst[cnt["v"] % 2]
                    cnt["v"] += 1
                    kb.cp(evac_engine(), V(vs.t[:, :, 0:128], vs.b), V(pv.t[:].rearrange("p (h d) -> p h d", h=4), pv.b))
                    kb.st(kb.sp, self.Vaug[(t0 // 128) + ti], V(vs.t[:].rearrange("p h d -> p (h d)"), vs.b))

    def range_reduce_sin(self, out, arg, ki, kf, W, rows):
        kb = self.kb
        a = arg[0:rows, :W]
        kb.ts(kb.dve, ki[0:rows, :W], a, 1.0 / TWO_PI, None, ALU.mult)
        kb.cp(kb.dve, kf[0:rows, :W], ki[0:rows, :W])
        kb.stt(kb.dve, a, kf[0:rows, :W], -TWO_PI, a, ALU.mult, ALU.add)
        kb.ts(kb.dve, a, a, -3.141592, 3.141592, ALU.max, ALU.min)
        kb.act_(out, a, AF.Sin)

    def conv3(self, eng, u, pin, W, cw, cb, j):
        kb = self.kb
        kb.ts(eng, u, pin[:, 1:W + 1], cw[:, j, 1:2], cb[:, j:j + 1], ALU.mult, ALU.add)
        kb.stt(eng, u, pin[:, 0:W], cw[:, j, 0:1], u, ALU.mult, ALU.add)
        kb.stt(eng, u, pin[:, 2:W + 2], cw[:, j, 2:3], u, ALU.mult, ALU.add)

    def dft_passes(self, ph, C, S, nt, rhs_fn, ncols, epilogue):
        kb = self.kb
        with contextlib.ExitStack() as sc:
            accC = [kb.ps("accC%d" % j, [128, 512], stack=sc) for j in range(4)]
            accS = [kb.ps("accS%d" % j, [128, 512], stack=sc) for j in range(4)]
            Cp = [kb.sb("Cp%d" % j, [128, 512], BF16, stack=sc) for j in range(3)]
            Sp = [kb.sb("Sp%d" % j, [128, 512], BF16, stack=sc) for j in range(3)]
            n = 0
            for p0 in range(0, nt, 4):
                kts = min(4, nt - p0)
                for n_t in range(nt):
                    cp_, sp_ = Cp[n % 3], Sp[n % 3]
                    n += 1
                    kb.ld(kb.sp, cp_[:, :kts * 128], C[n_t * 128:(n_t + 1) * 128, p0 * 128:(p0 + kts) * 128])
                    kb.ld(kb.act, sp_[:, :kts * 128], S[n_t * 128:(n_t + 1) * 128, p0 * 128:(p0 + kts) * 128])
                    r = rhs_fn(n_t)
                    for j in range(kts):
                        kb.mm(accC[j][:, :ncols], cp_[:, j * 128:(j + 1) * 128], r, start=(n_t == 0), stop=(n_t == nt - 1))
                        kb.mm(accS[j][:, :ncols], sp_[:, j * 128:(j + 1) * 128], r, start=(n_t == 0), stop=(n_t == nt - 1))
                for j in range(kts):
                    epilogue(p0 + j, accC[j], accS[j])
            kb.barrier()

    def hyena(self, li, L, tok0, tag):
        kb = self.kb
        nt = L // 128
        N = 2 * L
        C = self.din["dftC_" + tag]
        S = self.din["dftS_" + tag]
        with contextlib.ExitStack() as ph:
            G = kb.sb("G", [128, nt, 512], BF16, stack=ph)
            rn = kb.sb("rn", [128, 4], stack=ph)
            ab = kb.sb("ab", [128, 2, nt], stack=ph)
            kb.ld(kb.sp, ab[:], self.din["ab_" + tag])
            with contextlib.ExitStack() as fa:
                hsd = kb.sb("hsd", [128, nt, 512], BF16, stack=fa)
                with contextlib.ExitStack() as f:
                    featT = kb.sb("featT", [33, L], stack=f)
                    kb.ld(kb.sp, featT[:], self.din["featT_" + tag])
                    w1 = kb.sb("hw1", [33, 64], stack=f)
                    kb.ld(kb.sp, w1[:], self.din["hy_w1"][li])
                    w2 = kb.sb("hw2", [64, 64], stack=f)
                    kb.ld(kb.sp, w2[:], self.din["hy_w2"][li])
                    w3 = kb.sb("hw3", [64, 512], stack=f)
                    kb.ld(kb.sp, w3[:], self.din["hy_w3"][li])
                    bf_ = kb.sb("hbf", [64, 3], stack=f)
                    kb.ld(kb.sp, bf_[:], self.din["hy_bf"][li])
                    fb = kb.sb("hfb", [64, 2], stack=f)
                    kb.tt(kb.dve, fb[:, 0:1], bf_[:, 0:1], bf_[:, 2:3], ALU.mult)
                    kb.tt(kb.dve, fb[:, 1:2], bf_[:, 1:2], bf_[:, 2:3], ALU.mult)
                    negt = kb.sb("negt", [128, nt], stack=f)
                    kb.ld(kb.sp, negt[:], self.din["negt_" + tag])
                    decay = kb.sb("decay", [128, 256], stack=f)
                    kb.ld(kb.sp, decay[:], self.din["decay_b"])
                    h1T = kb.sb("h1T", [64, L], stack=f)
                    h2T = kb.sb("h2T", [64, L], stack=f)
                    arg = kb.sb("harg", [64, 512], stack=f)
                    ki = kb.sb("hki", [64, 512], I32, stack=f)
                    kf = kb.sb("hkf", [64, 512], stack=f)
                    pf = [kb.ps("pf%d" % i, [128, 512], stack=f) for i in range(2)]
                    pn = kb.ps("pn", [128, 2], stack=f)
                    n = 0
                    for layer_i, (wv, inT, outT) in enumerate(((w1, featT, h1T), (w2, h1T, h2T))):
                        for (c0, W) in chunks_of(0, L, 512):
                            p = pf[n % 2]
                            n += 1
                            kb.mm(p[0:64, :W], wv[:], inT[:, c0:c0 + W])
                            kb.ts(kb.dve, arg[:, :W], p[0:64, :W], bf_[:, 2:3], fb[:, layer_i:layer_i + 1], ALU.mult, ALU.add)
                            self.range_reduce_sin(outT[:, c0:c0 + W], arg, ki, kf, W, 64)
                    win = kb.sb("win", [128, 256], stack=f)
                    tf = kb.sb("tf", [128, 256], stack=f)
                    tb = kb.sb("tb", [128, 256], stack=f)
                    absacc = kb.sb("absacc", [128, 256], stack=f)
                    for i in range(nt):
                        p = pf[n % 2]
                        n += 1
                        kb.mm(p[:], h2T[:, i * 128:(i + 1) * 128], w3[:])
                        kb.act_(win[:], decay[:], AF.Exp, scale=negt[:, i:i + 1])
                        kb.tt(kb.dve, tf[:], p[:, 0:256], win[:], ALU.mult)
                        kb.tt(kb.dve, tb[:], p[:, 256:512], win[:], ALU.mult)
                        if i == 0:
                            kb.memset(kb.dve, tb[0:1, :], 0.0)
                        kb.tt(kb.pool, hsd[:, i, 0:256], tf[:], tb[:], ALU.add)
                        kb.tt(kb.pool, hsd[:, i, 256:512], tb[:], tf[:], ALU.subtract)
                        kb.stt(kb.dve, tf[:], tf[:], -1.0, tf[:], ALU.mult, ALU.max)
                        kb.stt(kb.dve, tb[:], tb[:], -1.0, tb[:], ALU.mult, ALU.max)
                        kb.tt(kb.dve, tf[:], tf[:], tb[:], ALU.add)
                        if i == 0:
                            kb.cp(kb.dve, absacc[:], tf[:])
                        else:
                            kb.tt(kb.dve, absacc[:], absacc[:], tf[:], ALU.add)
                    for ct in range(2):
                        kb.mm(pn[:, ct:ct + 1], absacc[:, ct * 128:(ct + 1) * 128], self.ones_f[:, 0:1])
                    kb.ts(kb.dve, rn[:, 2:4], pn[:, 0:2], EPS, None, ALU.add)
                    kb.op(kb.dve, lambda h: h.reciprocal(rn.t[:, 0:2], rn.t[:, 2:4]), reads=[rn.b], writes=[rn.b])
                    kb.ts(kb.dve, rn[:, 0:2], rn[:, 0:2], 2.0 / N, None, ALU.mult)
                    kb.barrier()
                with contextlib.ExitStack() as f2:
                    tmpg = [kb.sb("tmpg%d" % i, [128, 256], stack=f2) for i in range(2)]

                    def epi_filter(kt, aC, aS):
                        a = ab[:, 0, kt:kt + 1]
                        b = ab[:, 1, kt:kt + 1]
                        kb.act_(tmpg[0][:], aC[:, 0:256], AF.Copy, scale=a)
                        kb.stt(kb.dve, G[:, kt, 0:256], aS[:, 0:256], b, tmpg[0][:], ALU.mult, ALU.add)
                        kb.act_(tmpg[1][:], aC[:, 256:512], AF.Copy, scale=b)
                        kb.stt(kb.dve, G[:, kt, 256:512], aS[:, 256:512], a, tmpg[1][:], ALU.mult, ALU.subtract)

                    self.dft_passes(f2, C, S, nt, lambda n_t: hsd[:, n_t, :], 512, epi_filter)
            kb.barrier()
            cw = kb.sb("hcw", [128, 6, 3], stack=ph)
            kb.ld(kb.sp, cw[:], self.din["hy_cw"][li])
            cb = kb.sb("hcb", [128, 6], stack=ph)
            kb.ld(kb.sp, cb[:], self.din["hy_cb"][li])
            skip = kb.sb("hskip", [128, 2], stack=ph)
            kb.ld(kb.sp, skip[:], self.din["hy_skip"][li])
            ng = kb.sb("hng", [128, 2], stack=ph)
            kb.ld(kb.sp, ng[:], self.din["hy_ng"][li])
            zfm = [kb.sb("zfm%d" % i, [128, L], stack=ph) for i in range(2)]
            zT = kb.sb("zT", [128, nt, 256], BF16, stack=ph)
            Y = kb.sb("Y", [128, nt, 512], BF16, stack=ph)
            with contextlib.ExitStack() as z1:
                pin1 = kb.sb("pin1", [128, L + 2], stack=z1)
                pin2 = kb.sb("pin2", [128, L + 2], stack=z1)
                u1 = kb.sb("u1", [128, L], stack=z1)
                ptz = [kb.ps("ptz%d" % i, [128, 256], stack=z1) for i in range(2)]
                for pin in (pin1, pin2):
                    kb.memset(kb.dve, pin[:, 0:1], 0.0)
                    kb.memset(kb.dve, pin[:, L + 1:L + 2], 0.0)
                for ct in range(2):
                    kb.ld(kb.sp, pin1[:, 1:L + 1], self.pT[(2 + ct) * 128:(3 + ct) * 128, tok0:tok0 + L])
                    kb.ld(kb.act, pin2[:, 1:L + 1], self.pT[(4 + ct) * 128:(5 + ct) * 128, tok0:tok0 + L])
                    self.conv3(kb.dve, u1[:], pin1, L, cw, cb, 2 + ct)
                    self.conv3(kb.dve, zfm[ct][:], pin2, L, cw, cb, 4 + ct)
                    kb.tt(kb.dve, zfm[ct][:], zfm[ct][:], u1[:], ALU.mult)
                for i in range(nt):
                    p = ptz[i % 2]
                    for ct in range(2):
                        kb.tr(p[:, ct * 128:(ct + 1) * 128], zfm[ct][:, i * 128:(i + 1) * 128], self.ident_f[:])
                    kb.cp(kb.act if i % 2 else kb.dve, zT[:, i, :], p[:])
                kb.barrier()
            with contextlib.ExitStack() as z2:
                tq = [kb.sb("tq%d" % i, [128, 256], stack=z2) for i in range(4)]

                def epi_fwd(kt, aC, aS):
                    Gr = G[:, kt, 0:256]
                    Gi = G[:, kt, 256:512]
                    kb.tt(kb.dve, tq[0][:], aC[:, 0:256], Gr, ALU.mult)
                    kb.tt(kb.dve, tq[1][:], aS[:, 0:256], Gi, ALU.mult)
                    kb.tt(kb.pool, Y[:, kt, 0:256], tq[0][:], tq[1][:], ALU.add)
                    kb.tt(kb.dve, tq[2][:], aS[:, 0:256], Gr, ALU.mult)
                    kb.tt(kb.dve, tq[3][:], aC[:, 0:256], Gi, ALU.mult)
                    kb.tt(kb.pool, Y[:, kt, 256:512], tq[2][:], tq[3][:], ALU.subtract)

                self.dft_passes(z2, C, S, nt, lambda n_t: zT[:, n_t, :], 256, epi_fwd)
            kb.barrier()
            with contextlib.ExitStack() as z3:
                acc = [[kb.ps("iacc%d_%d" % (ct, cj), [128, 512], stack=z3) for cj in range(3)] for ct in range(2)]
                prs = kb.ps("prs", [128, 4], stack=z3)
                Cp = [kb.sb("iCp%d" % j, [128, 1536], BF16, stack=z3) for j in range(2)]
                Sp = [kb.sb("iSp%d" % j, [128, 1536], BF16, stack=z3) for j in range(2)]
                pin = [kb.sb("ipin%d" % j, [128, 514], stack=z3) for j in range(2)]
                x0 = kb.sb("x0", [128, 512], stack=z3)
                tt_ = kb.sb("itt", [128, 512], stack=z3)
                sq = [kb.sb("isq%d" % j, [128, 512], stack=z3) for j in range(2)]
                ob = [kb.sb("iob%d" % j, [128, 512], BF16, stack=z3) for j in range(2)]
                r4 = kb.sb("ir4", [128, 8], stack=z3)
                chunks = chunks_of(0, L, 512)
                n = 0
                for g0 in range(0, len(chunks), 3):
                    grp = chunks[g0:g0 + 3]
                    cA = grp[0][0]
                    cB = grp[-1][0] + grp[-1][1]
                    for kt in range(nt):
                        cp_, sp_ = Cp[n % 2], Sp[n % 2]
                        n += 1
                        kb.ld(kb.sp, cp_[:, :cB - cA], C[kt * 128:(kt + 1) * 128, cA:cB])
                        kb.ld(kb.act, sp_[:, :cB - cA], S[kt * 128:(kt + 1) * 128, cA:cB])
                        for cj, (c0, W) in enumerate(grp):
                            for ct in range(2):
                                kb.mm(acc[ct][cj][:, :W], Y[:, kt, ct * 128:(ct + 1) * 128], cp_[:, c0 - cA:c0 - cA + W],
                                      start=(kt == 0), stop=False)
                                kb.mm(acc[ct][cj][:, :W], Y[:, kt, 256 + ct * 128:256 + (ct + 1) * 128], sp_[:, c0 - cA:c0 - cA + W],
                                      start=False, stop=(kt == nt - 1))
                    for cj, (c0, W) in enumerate(grp):
                        nsub = W // 128
                        for ct in range(2):
                            pi = pin[ct]
                            lo = c0 - 1
                            hi = c0 + W + 1
                            dlo = 0
                            dhi = W + 2
                            if c0 == 0:
                                kb.memset(kb.dve, pi[:, 0:1], 0.0)
                                lo = 0
                                dlo = 1
                            if c0 + W == L:
                                kb.memset(kb.dve, pi[:, W + 1:W + 2], 0.0)
                                hi = L
                                dhi = W + 1
                            kb.ld(kb.sp, pi[:, dlo:dhi], self.pT[ct * 128:(ct + 1) * 128, tok0 + lo:tok0 + hi])
                            self.conv3(kb.dve, x0[:, :W], pi, W, cw, cb, ct)
                            kb.ts(kb.dve, tt_[:, :W], acc[ct][cj][:, :W], rn[:, ct:ct + 1], None, ALU.mult)
                            kb.stt(kb.dve, tt_[:, :W], zfm[ct][:, c0:c0 + W], skip[:, ct:ct + 1], tt_[:, :W], ALU.mult, ALU.add)
                            kb.tt(kb.dve, tt_[:, :W], tt_[:, :W], x0[:, :W], ALU.mult)
                            kb.act_(sq[ct][:, :W], tt_[:, :W], AF.Square)
                            kb.act_(ob[ct][:, :W], tt_[:, :W], AF.Copy, scale=ng[:, ct:ct + 1])
                            kb.st(kb.sp, self.mixT[ct * 128:(ct + 1) * 128, tok0 + c0:tok0 + c0 + W], ob[ct][:, :W])
                        kb.tt(kb.dve, sq[0][:, :W], sq[0][:, :W], sq[1][:, :W], ALU.add)
                        for sub in range(nsub):
                            kb.mm(prs[:, sub:sub + 1], sq[0][:, sub * 128:(sub + 1) * 128], self.ones_f[:, 0:1])
                        ti0 = (tok0 + c0) // 128
                        kb.ts(kb.dve, r4[:, 0:nsub], prs[:, 0:nsub], 1.0 / 256.0, EPS, ALU.mult, ALU.add)
                        kb.act_(r4[:, 4:4 + nsub], r4[:, 0:nsub], AF.Sqrt)
                        kb.op(kb.dve, lambda h: h.reciprocal(self.r_hy.t[:, ti0:ti0 + nsub], r4.t[:, 4:4 + nsub]),
                              reads=[r4.b], writes=[self.r_hy.b])
                kb.barrier()

    S5W = 256

    def s5_alloc(self, li, st):
        kb = self.kb
        W5 = self.S5W
        tl = {}
        tl["yT"] = [kb.sb("yT%d" % h, [128, T], stack=st) for h in range(2)]
        tl["uT"] = [kb.sb("uT%d" % h, [128, T], BF16, stack=st) for h in range(2)]
        tl["dn"] = kb.sb("s5dn", [128, 2, 2], stack=st)
        tl["uf"] = kb.sb("uf", [128, 1088], stack=st)
        tl["Bm"] = [[kb.sb("Bm%d%d" % (h, r), [128, 512], BF16, stack=st) for r in range(2)] for h in range(2)]
        tl["col"] = kb.sb("s5col", [128, 3, 8], stack=st)
        tl["thc"] = kb.sb("thc", [128, 8], stack=st)
        tl["rhoc"] = kb.sb("rhoc", [128, 8], stack=st)
        tl["rowb"] = kb.sb("rowb", [128, 3, 1024], stack=st)
        tl["a8"] = kb.sb("s5a8", [128, 8], stack=st)
        tl["k8"] = kb.sb("s5k8", [128, 8], I32, stack=st)
        tl["f8"] = kb.sb("s5f8", [128, 8], stack=st)
        tl["cs256"] = kb.sb("cs256", [128, 8], stack=st)
        tl["sn256"] = kb.sb("sn256", [128, 8], stack=st)
        tl["pt"] = [kb.sb("ppt%d" % i, [128, 512], stack=st) for i in range(8)]
        tl["pki"] = kb.sb("ppki", [128, 512], I32, stack=st)
        tl["braw"] = [kb.sb("braw%d" % r, [128, 512], stack=st) for r in range(2)]
        names = ["ang", "kf", "sinT", "cosT", "t1", "t2", "t3", "t4", "bre", "bim", "gre", "gim"]
        d = {nm: [kb.sb("s5%s%d" % (nm, i), [128, W5], stack=st) for i in range(2)] for nm in names}
        d["ki"] = [kb.sb("s5ki%d" % i, [128, W5], I32, stack=st) for i in range(2)]
        d["hre"] = [kb.sb("hre%d" % i, [128, W5], BF16, stack=st) for i in range(3)]
        d["him"] = [kb.sb("him%d" % i, [128, W5], BF16, stack=st) for i in range(3)]
        d["car"] = kb.sb("car", [128, 2], stack=st)
        d["th0"] = kb.sb("th0", [128, 1], stack=st)
        d["rhoT"] = kb.sb("rhoT", [128, W5], stack=st)
        d["Cm"] = [[kb.sb("Cm%d%d" % (r, i), [128, 128], BF16, stack=st) for r in range(2)] for i in range(2)]
        d["Cf"] = [kb.sb("Cf%d" % r, [128, 128], stack=st) for r in range(2)]
        d["pp"] = [kb.ps("s5pp%d" % i, [128, 512], stack=st) for i in range(2)]
        d["ppb"] = [[Buf(), Buf()] for i in range(2)]
        tl["d"] = d
        return tl

    def s5_scan_gen(self, li, tl):
        kb = self.kb
        W5 = self.S5W
        yT, uT, dn, uf, Bm = tl["yT"], tl["uT"], tl["dn"], tl["uf"], tl["Bm"]
        col, thc, rhoc, rowb, pt, pki, braw, d = tl["col"], tl["thc"], tl["rhoc"], tl["rowb"], tl["pt"], tl["pki"], tl["braw"], tl["d"]
        self._s5step = 0
        self._s5pend = []
        kb.ld(kb.sp, dn[:], self.din["s5_dn"][li])
        for half in range(2):
            for q in range(4):
                kb.ld(kb.sp, uf[:], self.pT[768 + half * 128:768 + (half + 1) * 128, q * 1088:(q + 1) * 1088])
                kb.ts(kb.dve, yT[half][:, q * 1088:(q + 1) * 1088], uf[:], dn[:, 0, half:half + 1], None, ALU.mult)
                kb.cp(kb.pool, uT[half][:, q * 1088:(q + 1) * 1088], uf[:])
                yield
        seq = [(0, CTX)] + chunks_of(CTX, T, W5)
        for d_ in range(2):
            kb.ld(kb.sp, V(rowb.t[:].rearrange("p a n -> p (a n)"), rowb.b), self.din["s5_row"][li, d_].partition_broadcast(128))
            for half in range(2):
                hs_ = slice(half * 512, (half + 1) * 512)
                a_re = rowb[:, 0, hs_]
                a_im = rowb[:, 1, hs_]
                dt, dre, dim, rho, sn, cs, x1, x2 = [p[:] for p in pt]
                kb.act_(dt, rowb[:, 2, hs_], AF.Exp)
                kb.tt(kb.dve, dre, dt, a_re, ALU.mult)
                kb.tt(kb.dve, dim, dt, a_im, ALU.mult)
                kb.act_(rho, dre, AF.Exp)
                kb.ts(kb.dve, pki[:], dim, 1.0 / TWO_PI, None, ALU.mult)
                kb.cp(kb.dve, x2, pki[:])
                kb.stt(kb.dve, x1, x2, -TWO_PI, dim, ALU.mult, ALU.add)
                kb.ts(kb.dve, x1, x1, -3.141592, 3.141592, ALU.max, ALU.min)
                kb.act_(sn, x1, AF.Sin)
                kb.stt(kb.dve, x2, x1, -1.0, x1, ALU.mult, ALU.max)
                kb.act_(cs, x2, AF.Sin, scale=-1.0, bias=self.halfpi[:])
                kb.tt(kb.dve, cs, cs, rho, ALU.mult)
                kb.ts(kb.dve, cs, cs, -1.0, None, ALU.add)
                kb.tt(kb.dve, sn, sn, rho, ALU.mult)
                kb.tt(kb.dve, dt, a_re, a_re, ALU.mult)
                kb.tt(kb.dve, dre, a_im, a_im, ALU.mult)
                kb.tt(kb.dve, dt, dt, dre, ALU.add)
                kb.op(kb.dve, lambda h: h.reciprocal(pt[0].t[:], pt[0].t[:]), reads=[pt[0].b], writes=[pt[0].b])
                kb.tt(kb.dve, x1, cs, a_re, ALU.mult)
                kb.tt(kb.dve, dre, sn, a_im, ALU.mult)
                kb.tt(kb.dve, x1, x1, dre, ALU.add)
                kb.tt(kb.dve, x1, x1, dt, ALU.mult)
                kb.tt(kb.dve, x2, sn, a_re, ALU.mult)
                kb.tt(kb.dve, dre, cs, a_im, ALU.mult)
                kb.tt(kb.dve, x2, x2, dre, ALU.subtract)
                kb.tt(kb.dve, x2, x2, dt, ALU.mult)
                for r in range(2):
                    kb.ld(kb.sp, braw[r][:], self.din["s5_bT"][li, d_, r, half])
                ta = pt[1][:]
                tb_ = pt[2][:]
                kb.tt(kb.dve, ta, braw[0][:], x1, ALU.mult)
                kb.tt(kb.dve, tb_, braw[1][:], x2, ALU.mult)
                kb.tt(kb.dve, Bm[half][0][:], ta, tb_, ALU.subtract)
                kb.tt(kb.dve, ta, braw[1][:], x1, ALU.mult)
                kb.tt(kb.dve, tb_, braw[0][:], x2, ALU.mult)
                kb.tt(kb.dve, Bm[half][1][:], ta, tb_, ALU.add)
                yield
            kb.ld(kb.sp, col[:], self.din["s5_col"][li, d_])
            kb.act_(col[:, 2, :], col[:, 2, :], AF.Exp)
            kb.tt(kb.dve, thc[:], col[:, 2, :], col[:, 1, :], ALU.mult)
            kb.tt(kb.dve, rhoc[:], col[:, 2, :], col[:, 0, :], ALU.mult)
            kb.act_(rhoc[:], rhoc[:], AF.Exp)
            a8, k8, f8 = tl["a8"], tl["k8"], tl["f8"]
            cs256, sn256 = tl["cs256"], tl["sn256"]
            kb.ts(kb.dve, a8[:], thc[:], float(W5), None, ALU.mult)
            kb.ts(kb.dve, k8[:], a8[:], 1.0 / TWO_PI, None, ALU.mult)
            kb.cp(kb.dve, f8[:], k8[:])
            kb.stt(kb.dve, a8[:], f8[:], -TWO_PI, a8[:], ALU.mult, ALU.add)
            kb.ts(kb.dve, a8[:], a8[:], -3.141592, 3.141592, ALU.max, ALU.min)
            kb.act_(sn256[:], a8[:], AF.Sin)
            kb.stt(kb.dve, f8[:], a8[:], -1.0, a8[:], ALU.mult, ALU.max)
            kb.act_(cs256[:], f8[:], AF.Sin, scale=-1.0, bias=self.halfpi[:])
            if d_ == 0:
                order = [(t0, W, False) for (t0, W) in seq]
            else:
                order = [(0, CTX, True)] + [(t0, W, True) for (t0, W) in reversed(seq[1:])]
            hreL, himL, car, th0, rhoT, CmL, Cf, pp, ppb = d["hre"], d["him"], d["car"], d["th0"], d["rhoT"], d["Cm"], d["Cf"], d["pp"], d["ppb"]
            for pair in range(8):
                half = pair // 4
                c0 = (pair % 4) * 128
                Cm = CmL[pair % 2]
                for r in range(2):
                    kb.ld(kb.sp, Cf[r][:], self.din["s5_cT"][li, d_, r, pair])
                kb.cp(kb.dve, Cm[0][:], Cf[0][:])
                kb.ts(kb.dve, Cm[1][:], Cf[1][:], -1.0, None, ALU.mult)
                kb.ts(kb.dve, rhoT[:], self.iota512[:, :W5], 0.0, rhoc[:, pair:pair + 1], ALU.mult, ALU.add)
                tau0 = 0
                for ci, (t0, W, rev) in enumerate(order):
                    def view(tile):
                        ap = tile.t[:, t0:t0 + W]
                        if rev:
                            ap = ap[:, ::-1]
                        return V(ap, tile.b)
                    stepno = self._s5step
                    self._s5step += 1
                    sp_ = stepno % 2
                    pbr = V(pp[sp_].t[:, 0:W], ppb[sp_][0])
                    pbi = V(pp[sp_].t[:, 256:256 + W], ppb[sp_][1])
                    py = V(pp[1 - sp_].t[:, 0:W], ppb[1 - sp_][0])
                    hre = hreL[stepno % 3]
                    him = himL[stepno % 3]
                    ang, ki, kf, sinT, cosT = d["ang"][sp_], d["ki"][sp_], d["kf"][sp_], d["sinT"][sp_], d["cosT"][sp_]
                    t1, t2, t3, t4 = d["t1"][sp_], d["t2"][sp_], d["t3"][sp_], d["t4"][sp_]
                    bre, bim, gre, gim = d["bre"][sp_], d["bim"][sp_], d["gre"][sp_], d["gim"][sp_]
                    uv = view(uT[half])
                    kb.mm(pbr, Bm[half][0][:, c0:c0 + 128], uv)
                    kb.mm(pbi, Bm[half][1][:, c0:c0 + 128], uv)
                    if ci == 0:
                        kb.ts(kb.dve, ang[:, :W], self.iota512[:, :W], thc[:, pair:pair + 1], None, ALU.mult)
                        kb.ts(kb.dve, ki[:, :W], ang[:, :W], 1.0 / TWO_PI, None, ALU.mult)
                        kb.cp(kb.pool, kf[:, :W], ki[:, :W])
                        kb.stt(kb.dve, ang[:, :W], kf[:, :W], -TWO_PI, ang[:, :W], ALU.mult, ALU.add)
                        kb.ts(kb.dve, ang[:, :W], ang[:, :W], -3.141592, 3.141592, ALU.max, ALU.min)
                        kb.act_(sinT[:, :W], ang[:, :W], AF.Sin)
                        kb.stt(kb.dve, kf[:, :W], ang[:, :W], -1.0, ang[:, :W], ALU.mult, ALU.max)
                        kb.act_(cosT[:, :W], kf[:, :W], AF.Sin, scale=-1.0, bias=self.halfpi[:])
                    else:
                        sinP, cosP = d["sinT"][1 - sp_], d["cosT"][1 - sp_]
                        cD = cs256[:, pair:pair + 1]
                        sD = sn256[:, pair:pair + 1]
                        kb.act_(ang[:, :W], sinP[:, :W], AF.Copy, scale=sD)
                        kb.stt(kb.dve, cosT[:, :W], cosP[:, :W], cD, ang[:, :W], ALU.mult, ALU.subtract)
                        kb.act_(kf[:, :W], cosP[:, :W], AF.Copy, scale=sD)
                        kb.stt(kb.dve, sinT[:, :W], sinP[:, :W], cD, kf[:, :W], ALU.mult, ALU.add)
                    kb.tt(kb.dve, t1[:, :W], pbr, cosT[:, :W], ALU.mult)
                    kb.tt(kb.dve, t2[:, :W], pbi, sinT[:, :W], ALU.mult)
                    kb.tt(kb.pool, bre[:, :W], t1[:, :W], t2[:, :W], ALU.add)
                    kb.tt(kb.dve, t3[:, :W], pbi, cosT[:, :W], ALU.mult)
                    kb.tt(kb.dve, t4[:, :W], pbr, sinT[:, :W], ALU.mult)
                    kb.tt(kb.pool, bim[:, :W], t3[:, :W], t4[:, :W], ALU.subtract)
                    for (g_, b_, cc) in ((gre, bre, 0), (gim, bim, 1)):
                        init = 0.0 if ci == 0 else car.t[:, cc:cc + 1]
                        rd = [rhoT.b, b_.b] + ([] if ci == 0 else [car.b])
                        kb.op(kb.dve, lambda h: h.tensor_tensor_scan(g_.t[:, :W], rhoT.t[:, :W], b_.t[:, :W], init, ALU.mult, ALU.add),
                              reads=rd, writes=[g_.b])
                    kb.cp(kb.act, car[:, 0:1], gre[:, W - 1:W])
                    kb.cp(kb.act, car[:, 1:2], gim[:, W - 1:W])
                    kb.tt(kb.pool, t1[:, :W], gre[:, :W], cosT[:, :W], ALU.mult)
                    kb.tt(kb.pool, t2[:, :W], gim[:, :W], sinT[:, :W], ALU.mult)
                    kb.tt(kb.pool, hre[:, :W], t1[:, :W], t2[:, :W], ALU.subtract)
                    kb.tt(kb.pool, t3[:, :W], gre[:, :W], sinT[:, :W], ALU.mult)
                    kb.tt(kb.pool, t4[:, :W], gim[:, :W], cosT[:, :W], ALU.mult)
                    kb.tt(kb.pool, him[:, :W], t3[:, :W], t4[:, :W], ALU.add)
                    def readout(py=py, Cm=Cm, hre=hre, him=him, W=W, yv=view(yT[half])):
                        kb.mm(py, Cm[0][:], hre[:, :W], start=True, stop=False)
                        kb.mm(py, Cm[1][:], him[:, :W], start=False, stop=True)
                        kb.tt(kb.dve, yv, yv, py, ALU.add)
                    self._s5pend.append(readout)
                    if len(self._s5pend) > 2:
                        self._s5pend.pop(0)()
                    tau0 += W
                    yield
        while self._s5pend:
            self._s5pend.pop(0)()

    def s5_glu(self, li, tl):
        kb = self.kb
        yT, dn = tl["yT"], tl["dn"]
        if "dbg_y" in self.debug:
            dy = self.nc.dram_tensor("dbg_y", [256, T], F32, kind="ExternalOutput").ap()
            for half in range(2):
                kb.st(kb.sp, dy[half * 128:(half + 1) * 128, :], yT[half][:])
        with contextlib.ExitStack() as gl:
            glu = kb.sb("glu", [128, 2, 256], BF16, stack=gl)
            kb.ld(kb.pool, glu[:], self.din["s5_glu"][li].rearrange("(kt p) n -> p kt n", p=128))
            gf = [kb.sb("gf%d" % i, [128, 512], stack=gl) for i in range(2)]
            gb = [kb.sb("gb%d" % i, [128, 512], BF16, stack=gl) for i in range(2)]
            pg = [kb.ps("pg%d" % i, [128, 512], stack=gl) for i in range(2)]
            prs = kb.ps("prs5", [128, 4], stack=gl)
            sg = kb.sb("sg", [128, 512], stack=gl)
            o_ = [kb.sb("s5o%d" % i, [128, 512], stack=gl) for i in range(2)]
            ob = [kb.sb("s5ob%d" % i, [128, 512], BF16, stack=gl) for i in range(2)]
            r4 = kb.sb("s5r4", [128, 8], stack=gl)
            for (t0, W) in TOK_CHUNKS:
                nsub = W // 128
                for half in range(2):
                    kb.act_(gf[half][:, :W], yT[half][:, t0:t0 + W], AF.Gelu)
                    kb.cp(kb.pool, gb[half][:, :W], gf[half][:, :W])
                for mo in range(2):
                    for kt in range(2):
                        kb.mm(pg[mo][:, :W], glu[:, kt, mo * 128:(mo + 1) * 128], gb[kt][:, :W], start=(kt == 0), stop=(kt == 1))
                    kb.act_(sg[:, :W], pg[mo][:, :W], AF.Sigmoid)
                    kb.tt(kb.dve, o_[mo][:, :W], sg[:, :W], gf[mo][:, :W], ALU.mult)
                    kb.act_(ob[mo][:, :W], o_[mo][:, :W], AF.Copy, scale=dn[:, 1, mo:mo + 1])
                    kb.st(kb.sp, self.mixT[256 + mo * 128:256 + (mo + 1) * 128, t0:t0 + W], ob[mo][:, :W])
                    kb.tt(kb.dve, o_[mo][:, :W], o_[mo][:, :W], o_[mo][:, :W], ALU.mult)
                kb.tt(kb.dve, o_[0][:, :W], o_[0][:, :W], o_[1][:, :W], ALU.add)
                for sub in range(nsub):
                    kb.mm(prs[:, sub:sub + 1], o_[0][:, sub * 128:(sub + 1) * 128], self.ones_f[:, 0:1])
                ti0 = t0 // 128
                kb.ts(kb.dve, r4[:, 0:nsub], prs[:, 0:nsub], 1.0 / 256.0, EPS, ALU.mult, ALU.add)
                kb.act_(r4[:, 4:4 + nsub], r4[:, 0:nsub], AF.Sqrt)
                kb.op(kb.dve, lambda h: h.reciprocal(self.r_s5.t[:, ti0:ti0 + nsub], r4.t[:, 4:4 + nsub]),
                      reads=[r4.b], writes=[self.r_s5.b])
            kb.barrier()

    def s5_attn(self, li):
        kb = self.kb
        do_s5 = self.ph("s5")
        do_at = self.ph("attn")
        with contextlib.ExitStack() as st:
            tl = self.s5_alloc(li, st) if do_s5 else None
            gen = self.s5_scan_gen(li, tl) if do_s5 else None
            state = {"n": 0, "done": gen is None}

            def tick():
                if state["done"]:
                    return
                state["n"] += 1
                if state["n"] % self.S5_TICK == 0:
                    try:
                        next(gen)
                    except StopIteration:
                        state["done"] = True

            if do_at:
                self.attn(li, tick)
            if gen is not None:
                for _ in gen:
                    pass
            kb.barrier()
            if do_s5:
                self.s5_glu(li, tl)

    S5_TICK = 7

    def attn(self, li, tick=lambda: None):
        kb = self.kb
        lid = self.layer_ids[li]
        lam_init = 0.8 - 0.6 * math.exp(-0.3 * lid)
        need_ctx = lid < DEPTH - 1
        with contextlib.ExitStack() as ph:
            al = kb.sb("al", [128, 256], stack=ph)
            kb.ld(kb.sp, al[:], self.din["att_l"][li].partition_broadcast(128))
            prod = kb.sb("alp", [128, 128], stack=ph)
            kb.tt(kb.dve, prod[:, 0:64], al[:, 0:64], al[:, 64:128], ALU.mult)
            kb.tt(kb.dve, prod[:, 64:128], al[:, 128:192], al[:, 192:256], ALU.mult)
            s12 = kb.sb("s12", [128, 4], stack=ph)
            for i in range(2):
                kb.op(kb.dve, lambda h: h.reduce_sum(s12.t[:, i:i + 1], prod.t[:, i * 64:(i + 1) * 64], AX.X),
                      reads=[prod.b], writes=[s12.b])
            kb.act_(s12[:, 0:2], s12[:, 0:2], AF.Exp)
            kb.tt(kb.dve, s12[:, 2:3], s12[:, 1:2], s12[:, 0:1], ALU.subtract)
            kb.ts(kb.dve, s12[:, 3:4], s12[:, 2:3], -lam_init, None, ALU.add)
            neglam = s12[:, 3:4]
            gsub = kb.sb("gsub", [128, 128], stack=ph)
            kb.ld(kb.sp, gsub[:], self.din["att_g"][li].partition_broadcast(128))
            kb.ts(kb.dve, gsub[:], gsub[:], 1.0 - lam_init, None, ALU.mult)
            negM = kb.sb("negM", [128, 8], stack=ph)
            kb.tt(kb.dve, negM[:], self.qkmax[:, 0:8], self.qkmax[:, 8:16], ALU.mult)
            kb.act_(negM[:], negM[:], AF.Sqrt)
            kb.ts(kb.dve, negM[:], negM[:], -1.0, None, ALU.mult)
            QTh = [kb.sb("QTh%d" % i, [128, T], BF16, stack=ph) for i in range(1)]
            QrTh = [kb.sb("QrTh%d" % i, [128, LAT], BF16, stack=ph) for i in range(1)]
            KcTh = [kb.sb("KcTh%d" % i, [128, T], BF16, stack=ph) for i in range(1)]
            Vh = [kb.sb("Vh%d" % i, [128, NT, 129], BF16, stack=ph) for i in range(1)]
            pS = [kb.ps("pS%d" % i, [128, 512], stack=ph) for i in range(2)]
            acc = [kb.ps("aacc%d" % i, [128, 512], stack=ph) for i in range(4)]
            ptr = pS[0]
            Pb = [kb.sb("Pb%d" % i, [128, 512], BF16, stack=ph) for i in range(3)]
            om = [kb.sb("om%d" % i, [128, 4, 128], stack=ph) for i in range(2)]
            o_ = kb.sb("ao", [128, 4, 128], stack=ph)
            junk = kb.sb("ajunk", [128, 128], stack=ph)
            ssq = kb.sb("assq", [128, 12], stack=ph)
            rl = kb.sb("arl", [128, 4], stack=ph)
            att = kb.sb("att", [128, 4, 128], stack=ph)
            attT = [kb.sb("attT%d" % i, [128, 512], BF16, stack=ph) for i in range(2)]
            accS = [kb.sb("accS%d" % i, [128, 4, 129], stack=ph) for i in range(2)]
            n = 0
            nchunk = 0
            pend = []
            tk = {"n": 0}

            def tick2(free0):
                tk["n"] += 1
                while pend and pend[0][0] <= tk["n"] and (free0 or not pend[0][2]):
                    pend.pop(0)[1]()
                tick()

            def flush():
                while pend:
                    pend.pop(0)[1]()

            for hh in range(4):
                b_ = 0
                flush()
                kb.ld(kb.sp, QTh[b_][:], self.QT[hh * 128:(hh + 1) * 128, :])
                kb.ld(kb.act, QrTh[b_][:], self.QrT[hh * 128:(hh + 1) * 128, :])
                kb.ld(kb.sp, KcTh[b_][:], self.KcT[hh * 128:(hh + 1) * 128, :])
                kb.ld(kb.act, Vh[b_][:], self.Vaug.rearrange("t p c -> p t c")[:, :, hh * 129:(hh + 1) * 129])
                qchunks = [(CTX + qc * 512, 512, False) for qc in range(8)]
                if need_ctx:
                    qchunks.append((0, CTX, True))
                for (q0, W, isctx) in qchunks:
                    nsub = W // 128
                    kts = [0, 1] if isctx else list(range(NT))
                    for m in range(2):
                        r0 = m * 64

                        def qk(kk):
                            kt = kts[kk]
                            if kt < 2:
                                qv = QTh[b_][r0:r0 + 64, q0:q0 + W]
                            else:
                                qv = QrTh[b_][r0:r0 + 64, q0 - CTX:q0 - CTX + W]
                            kb.mm(pS[(n + kk) % 2][:, :W], KcTh[b_][r0:r0 + 64, kt * 128:(kt + 1) * 128], qv)
                        qk(0)
                        for kk, kt in enumerate(kts):
                            if kk + 1 < len(kts):
                                qk(kk + 1)
                            p = pS[(n + kk) % 2]
                            pb_ = Pb[(n + kk) % 3]
                            kb.act_(pb_[:, :W], p[:, :W], AF.Exp, bias=negM[:, hh * 2 + m:hh * 2 + m + 1])
                            tick2((n + kk + 1) % 2 == 1 or kk + 1 >= len(kts))
                            for sub in range(nsub):
                                kb.mm(acc[sub][:, 0:129], pb_[:, sub * 128:(sub + 1) * 128], Vh[b_][:, kt, :],
                                      start=(kk == 0), stop=(kk == len(kts) - 1))
                        n += len(kts)
                        flush()
                        for sub in range(nsub):
                            kb.cp(kb.act, accS[m][:, sub, :], acc[sub][:, 0:129])

                        def fin_m(m=m, nsub=nsub):
                            for sub in range(nsub):
                                kb.op(kb.dve, lambda h: h.reciprocal(rl.t[:, sub:sub + 1], accS[m].t[:, sub, 128:129]),
                                      reads=[accS[m].b], writes=[rl.b])
                                kb.ts(kb.dve, om[m][:, sub, :], accS[m][:, sub, 0:128], rl[:, sub:sub + 1], None, ALU.mult)
                        pend.append([tk["n"] + 8, fin_m, False])

                    def fin_chunk(nsub=nsub, W=W, q0=q0, hh=hh):
                        nonlocal nchunk
                        for sub in range(nsub):
                            kb.stt(kb.dve, o_[:, sub, :], om[1][:, sub, :], neglam, om[0][:, sub, :], ALU.mult, ALU.add)
                            kb.act_(junk[:], o_[:, sub, :], AF.Square, accum=ssq[:, sub:sub + 1])
                        kb.ts(kb.dve, ssq[:, 4:4 + nsub], ssq[:, 0:nsub], 1.0 / 128.0, EPS, ALU.mult, ALU.add)
                        kb.act_(ssq[:, 8:8 + nsub], ssq[:, 4:4 + nsub], AF.Sqrt)
                        kb.op(kb.dve, lambda h: h.reciprocal(ssq.t[:, 4:4 + nsub], ssq.t[:, 8:8 + nsub]), reads=[ssq.b], writes=[ssq.b])
                        for sub in range(nsub):
                            kb.stt(kb.dve, att[:, sub, :], o_[:, sub, :], ssq[:, 4 + sub:5 + sub], gsub[:], ALU.mult, ALU.mult)
                            kb.tr(ptr[:, sub * 128:(sub + 1) * 128], att[:, sub, :], self.ident_f[:])
                        at = attT[nchunk % 2]
                        nchunk += 1
                        kb.cp(kb.act, at[:, :W], ptr[:, :W])
                        kb.st(kb.sp, self.mixT[512 + hh * 128:512 + (hh + 1) * 128, q0:q0 + W], at[:, :W])
                    pend.append([tk["n"] + 16, fin_chunk, True])
            flush()
            kb.barrier()

    def outproj(self, li):
        kb = self.kb
        lid = self.layer_ids[li]
        need_ctx = lid < DEPTH - 1
        src = self.xsrc(li)
        with contextlib.ExitStack() as ph:
            wo = kb.sb("wo", [128, 8, D], BF16, stack=ph)
            for kt in range(8):
                kb.ld(kb.pool, wo[:, kt, :], self.din["w_out"][li][kt * 128:(kt + 1) * 128, :])
            g1b = [self.load_mod_b(li, which, 2, ph, "g1b%d" % which) for which in range(2)]
            mx = [kb.sb("mx%d" % i, [128, 8, 512], BF16, stack=ph) for i in range(2)]
            xt = [kb.sb("oxt%d" % i, [128, D], stack=ph) for i in range(2)]
            tt_ = [kb.sb("ott%d" % i, [128, D], stack=ph) for i in range(2)]
            pA = kb.ps("pA", [128, D], stack=ph)
            pB = kb.ps("pB", [128, D], stack=ph)
            pC = kb.ps("pC", [128, D], stack=ph)
            n = 0
            for ci, (t0, W) in enumerate(TOK_CHUNKS):
                if ci == 0 and not need_ctx:
                    continue
                which = 1 if ci == 0 else 0
                m_ = mx[ci % 2]
                kb.ld(kb.sp, m_[:, :, :W], self.mixT[:, t0:t0 + W].rearrange("(kt p) t -> p kt t", p=128))
                for ti in range(W // 128):
                    gi = t0 // 128 + ti
                    x = xt[n % 2]
                    t = tt_[n % 2]
                    n += 1
                    kb.ld(kb.act, x[:], src[gi * 128:(gi + 1) * 128, :])
                    for (ps_, kts) in ((pA, (0, 1)), (pB, (2, 3)), (pC, (4, 5, 6, 7))):
                        for hf in range(2):
                            for kt in kts:
                                kb.mm(ps_[:, hf * 512:(hf + 1) * 512], m_[:, kt, ti * 128:(ti + 1) * 128], wo[:, kt, hf * 512:(hf + 1) * 512],
                                      start=(kt == kts[0]), stop=(kt == kts[-1]))
                    kb.ts(kb.dve, t[:], pA[:], self.r_hy[:, gi:gi + 1], None, ALU.mult)
                    kb.stt(kb.dve, t[:], pB[:], self.r_s5[:, gi:gi + 1], t[:], ALU.mult, ALU.add)
                    kb.tt(kb.dve, t[:], t[:], pC[:], ALU.add)
                    kb.tt(kb.pool, t[:], t[:], g1b[which][:], ALU.mult)
                    kb.tt(kb.pool, t[:], t[:], x[:], ALU.add)
                    kb.st(kb.sp, self.Xres[gi * 128:(gi + 1) * 128, :], t[:])
            kb.barrier()

    def moe(self, li):
        kb = self.kb
        nc = self.nc
        lid = self.layer_ids[li]
        need_ctx = lid < DEPTH - 1
        tiles = list(range(NT)) if need_ctx else list(range(2, NT))
        NB = NBLK
        with contextlib.ExitStack() as ph:
            eid = kb.sb("eid", [128, NT, 2], stack=ph)
            gate = kb.sb("gate", [128, NT, 2], stack=ph)
            rsel = kb.sb("rsel", [128, NT, 2], stack=ph)
            dest_i = kb.sb("dest_i", [128, NT, 2], I32, stack=ph)
            cnt_b = kb.sb("cnt_b", [128, 32], stack=ph)
            widx = kb.sb("widx", [128, NB], I32, stack=ph)
            kb.memset(kb.dve, cnt_b[:], 0.0)
            e_iota = kb.sb("e_iota", [128, 32], stack=ph)
            kb.op(kb.pool, lambda h: h.iota(e_iota.t[:], [[1, 32]], base=0, channel_multiplier=0, allow_small_or_imprecise_dtypes=True),
                  writes=[e_iota.b])
            g2b = [self.load_mod_b(li, which, 5, ph, "g2b%d" % which) for which in range(2)]
            bslot = Buf()
            bHs = Buf()
            with contextlib.ExitStack() as sa:
                ab = self.norm_mod_tiles(li, 4 + lid, 3, 4, sa)
                wr = kb.sb("wr", [128, 8, 36], stack=sa)
                kb.ld(kb.sp, wr[:], self.din["moe_wr"][li].rearrange("(kt p) n -> p kt n", p=128))
                brb = kb.sb("brb", [128, 36], stack=sa)
                kb.ld(kb.sp, brb[:], self.din["moe_br"][li].partition_broadcast(128))
                t_lo = tiles[0]
                n_ = NT - t_lo
                lg_all = kb.sb("lg_all", [128, NT, 36], stack=sa)
                zrow = kb.sb("zrow", [1, D], BF16, stack=sa)
                kb.memset(kb.dve, zrow[:], 0.0)
                kb.st(kb.sp, self.Hs[T:T + 1, :], zrow[:], writes=[bHs])
                with contextlib.ExitStack() as p1:
                    xt = [kb.sb("mxt%d" % i, [128, D], stack=p1) for i in range(2)]
                    hn = [kb.sb("mhn%d" % i, [128, D], stack=p1) for i in range(2)]
                    hb = [kb.sb("mhb%d" % i, [128, D], BF16, stack=p1) for i in range(2)]
                    junk = kb.sb("mjunk", [128, D], stack=p1)
                    ss = [kb.sb("mss%d" % i, [128, 4], stack=p1) for i in range(2)]
                    hT = [kb.sb("mhT%d" % i, [128, 8, 128], stack=p1) for i in range(2)]
                    trp = [kb.ps("mtrp%d" % i, [128, D], stack=p1) for i in range(2)]
                    pl = [kb.ps("mpl%d" % i, [128, 64], stack=p1) for i in range(2)]
                    for n, ti in enumerate(tiles):
                        which = 1 if ti < 2 else 0
                        x = xt[n % 2]
                        kb.ld(kb.sp, x[:], self.Xres[ti * 128:(ti + 1) * 128, :])
                        self.norm_tile(x, ab[which], hn[n % 2], ss[n % 2], junk)
                        h16 = hb[n % 2]
                        kb.cp(kb.act, h16[:], hn[n % 2][:])
                        kb.st(kb.sp, self.Hs[ti * 128:(ti + 1) * 128, :], h16[:], writes=[bHs])
                        for kt in range(8):
                            kb.tr(trp[n % 2][:, kt * 128:(kt + 1) * 128], hn[n % 2][:, kt * 128:(kt + 1) * 128], self.ident_f[:])
                        kb.cp(kb.pool if False else kb.dve, V(hT[n % 2].t[:].rearrange("p k t -> p (k t)"), hT[n % 2].b), trp[n % 2][:])
                        for kt in range(8):
                            kb.mm(pl[n % 2][:, 0:36], hT[n % 2][:, kt, :], wr[:, kt, :], start=(kt == 0), stop=(kt == 7))
                        kb.tt(kb.dve, lg_all[:, ti, :], pl[n % 2][:, 0:36], brb[:], ALU.add)
                    kb.barrier()
                TS = slice(t_lo, NT)
                gmax = kb.sb("gmax", [128, NT, 1], stack=sa)
                ohg = kb.sb("ohg", [128, NT, 4], stack=sa)
                gidx = kb.sb("gidx", [128, NT, 1], stack=sa)
                E4 = kb.sb("E4", [128, NT, 4], stack=sa)
                sg = kb.sb("sg", [128, NT, 1], stack=sa)
                pgr = kb.sb("pgr", [128, NT, 1], stack=sa)
                esel = kb.sb("esel", [128, NT, 8], stack=sa)
                etmp = kb.sb("etmp", [128, NT, 8], stack=sa)
                mx8 = kb.sb("mx8", [128, NT, 8], stack=sa)
                ix8 = kb.sb("ix8", [128, NT, 8], U32, stack=sa)
                i12 = kb.sb("i12", [128, NT, 2], stack=sa)
                sm = kb.sb("msm", [128, NT, 4], stack=sa)
                oh = [kb.sb("oh%d" % i, [128, NT, 32], stack=sa) for i in range(2)]
                ohs = kb.sb("ohs", [128, NT, 32], stack=sa)
                cnt_all = kb.sb("cnt_all", [128, NT, 32], stack=sa)
                rk = kb.sb("rk", [128, NT, 32], stack=sa)
                e_io3 = kb.sb("e_io3", [128, 1, 32], stack=sa)
                kb.cp(kb.dve, e_io3[:, 0, :], e_iota[:])

                def B(v, shape):
                    return V(v.ap.to_broadcast(list(shape)), v.buf)

                kb.op(kb.dve, lambda h: h.tensor_reduce(gmax.t[:, TS, 0], lg_all.t[:, TS, 0:4], AX.X, ALU.max), reads=[lg_all.b], writes=[gmax.b])
                kb.tt(kb.dve, ohg[:, TS, :], lg_all[:, TS, 0:4], B(gmax[:, TS, 0:1], (128, n_, 4)), ALU.is_equal)
                kb.ts(kb.dve, gidx[:, TS, :], ohg[:, TS, 3:4], 3.0, None, ALU.mult)
                kb.stt(kb.dve, gidx[:, TS, :], ohg[:, TS, 2:3], 2.0, gidx[:, TS, :], ALU.mult, ALU.add)
                kb.tt(kb.dve, gidx[:, TS, :], gidx[:, TS, :], ohg[:, TS, 1:2], ALU.add)
                kb.tt(kb.dve, E4[:, TS, :], lg_all[:, TS, 0:4], B(gmax[:, TS, 0:1], (128, n_, 4)), ALU.subtract)
                kb.act_(E4[:, TS, :], E4[:, TS, :], AF.Exp)
                kb.op(kb.dve, lambda h: h.tensor_reduce(sg.t[:, TS, 0], E4.t[:, TS, :], AX.X, ALU.add), reads=[E4.b], writes=[sg.b])
                kb.op(kb.dve, lambda h: h.reciprocal(pgr.t[:, TS, :], sg.t[:, TS, :]), reads=[sg.b], writes=[pgr.b])
                for g in range(4):
                    dst = esel if g == 0 else etmp
                    kb.tt(kb.dve, dst[:, TS, :], lg_all[:, TS, 4 + 8 * g:12 + 8 * g], B(ohg[:, TS, g:g + 1], (128, n_, 8)), ALU.mult)
                    if g > 0:
                        kb.tt(kb.dve, esel[:, TS, :], esel[:, TS, :], etmp[:, TS, :], ALU.add)
                for ti in tiles:
                    kb.op(kb.dve, lambda h: h.max(mx8.t[:, ti, :], esel.t[:, ti, :]), reads=[esel.b], writes=[mx8.b])
                for ti in tiles:
                    kb.op(kb.dve, lambda h: h.max_index(ix8.t[:, ti, :], mx8.t[:, ti, :], esel.t[:, ti, :]), reads=[esel.b, mx8.b], writes=[ix8.b])
                kb.cp(kb.dve, i12[:, TS, :], ix8[:, TS, 0:2])
                kb.stt(kb.dve, eid[:, TS, :], B(gidx[:, TS, 0:1], (128, n_, 2)), 8.0, i12[:, TS, :], ALU.mult, ALU.add)
                kb.tt(kb.dve, sm[:, TS, 0:1], mx8[:, TS, 1:2], mx8[:, TS, 0:1], ALU.subtract)
                kb.act_(sm[:, TS, 1:2], sm[:, TS, 0:1], AF.Exp)
                kb.ts(kb.dve, sm[:, TS, 2:3], sm[:, TS, 1:2], 1.0, None, ALU.add)
                kb.op(kb.dve, lambda h: h.reciprocal(sm.t[:, TS, 3:4], sm.t[:, TS, 2:3]), reads=[sm.b], writes=[sm.b])
                kb.tt(kb.dve, gate[:, TS, 0:1], sm[:, TS, 3:4], pgr[:, TS, :], ALU.mult)
                kb.tt(kb.dve, gate[:, TS, 1:2], gate[:, TS, 0:1], sm[:, TS, 1:2], ALU.mult)
                for k in range(2):
                    kb.tt(kb.dve, oh[k][:, TS, :], B(e_io3[:, 0:1, :], (128, n_, 32)), B(eid[:, TS, k:k + 1], (128, n_, 32)), ALU.is_equal)
                kb.tt(kb.dve, ohs[:, TS, :], oh[0][:, TS, :], oh[1][:, TS, :], ALU.add)
                with contextlib.ExitStack() as p2:
                    pr = kb.ps("mpr", [128, 3, 512], stack=p2)
                    pc = kb.ps("mpc", [128, 3, 512], stack=p2)
                    ncols = n_ * 32
                    ohs_f = ohs.t[:, TS, :].rearrange("p t e -> p (t e)")
                    for c3 in range(3):
                        a = c3 * 512
                        b = min(ncols, a + 512)
                        if a >= b:
                            break
                        kb.mm(pr[:, c3, 0:b - a], self.tri[:], V(ohs_f[:, a:b], ohs.b))
                        kb.mm(pc[:, c3, 0:b - a], self.ones_f[:], V(ohs_f[:, a:b], ohs.b))
                    pr_f = pr.t[:].rearrange("p c n -> p (c n)")
                    pc_f = pc.t[:].rearrange("p c n -> p (c n)")
                    kb.memset(kb.dve, cnt_all[:, t_lo, :], 0.0)
                    for ti in range(t_lo + 1, NT):
                        o0 = (ti - 1 - t_lo) * 32
                        kb.tt(kb.dve, cnt_all[:, ti, :], cnt_all[:, ti - 1, :], V(pc_f[:, o0:o0 + 32], pc.b), ALU.add)
                    o0 = (NT - 1 - t_lo) * 32
                    kb.tt(kb.dve, cnt_b[:], cnt_all[:, NT - 1, :], V(pc_f[:, o0:o0 + 32], pc.b), ALU.add)
                    kb.tt(kb.dve, V(rk.t[:, TS, :].rearrange("p t e -> p (t e)"), rk.b), V(cnt_all.t[:, TS, :].rearrange("p t e -> p (t e)"), cnt_all.b),
                          V(pr_f[:, 0:ncols], pr.b), ALU.add)
                    kb.barrier()
                for k in range(2):
                    kb.tt(kb.dve, ohs[:, TS, :], oh[k][:, TS, :], rk[:, TS, :], ALU.mult)
                    kb.op(kb.dve, lambda h: h.tensor_reduce(rsel.t[:, TS, k], ohs.t[:, TS, :], AX.X, ALU.add), reads=[ohs.b], writes=[rsel.b])
                pad = kb.sb("pad", [128, 32], stack=sa)
                pend = kb.sb("pend", [128, 32], stack=sa)
                pstart = kb.sb("pstart", [128, 1, 32], stack=sa)
                padi = kb.sb("padi", [128, 32], I32, stack=sa)
                kb.ts(kb.dve, pad[:], cnt_b[:], float(NSLOT_BLK - 1), 1.0 / NSLOT_BLK, ALU.add, ALU.mult)
                kb.ts(kb.dve, padi[:], pad[:], -(0.5 - 1.0 / 512.0), None, ALU.add)
                kb.cp(kb.dve, pad[:], padi[:])
                kb.ts(kb.dve, pad[:], pad[:], float(NSLOT_BLK), None, ALU.mult)
                kb.op(kb.dve, lambda h: h.tensor_tensor_scan(pend.t[:], self.ones_f.t[:, 0:32], pad.t[:], 0.0, ALU.mult, ALU.add),
                      reads=[pad.b, self.ones_f.b], writes=[pend.b])
                kb.tt(kb.dve, pstart[:, 0, :], pend[:], pad[:], ALU.subtract)
                dsf = kb.sb("dsf", [128, NT, 2], stack=sa)
                for k in range(2):
                    kb.tt(kb.dve, ohs[:, TS, :], oh[k][:, TS, :], B(pstart[:, 0:1, :], (128, n_, 32)), ALU.mult)
                    kb.op(kb.dve, lambda h: h.tensor_reduce(dsf.t[:, TS, k], ohs.t[:, TS, :], AX.X, ALU.add), reads=[ohs.b], writes=[dsf.b])
                kb.tt(kb.dve, dsf[:, TS, :], dsf[:, TS, :], rsel[:, TS, :], ALU.add)
                kb.cp(kb.dve, dest_i[:, TS, :], dsf[:, TS, :])
                NSC = NPAD // 128
                inif = kb.sb("inif", [128, NSC], stack=sa)
                kb.memset(kb.dve, inif[:], float(T))
                inii = kb.sb("inii", [128, NSC], I32, stack=sa)
                kb.cp(kb.dve, inii[:], inif[:])
                kb.st(kb.sp, self.slot_tok.rearrange("(p a) o -> p (a o)", p=128), inii[:], writes=[bslot])
                tokf = kb.sb("tokf", [128, NT], stack=sa)
                kb.op(kb.pool, lambda h: h.iota(tokf.t[:], [[128, NT]], base=0, channel_multiplier=1, allow_small_or_imprecise_dtypes=True),
                      writes=[tokf.b])
                toki = kb.sb("toki", [128, NT], I32, stack=sa)
                kb.cp(kb.dve, toki[:], tokf[:])
                for ti in tiles:
                    for k in range(2):
                        kb.dma(kb.pool, lambda h: h.indirect_dma_start(
                            out=self.slot_tok, out_offset=bass.IndirectOffsetOnAxis(ap=dest_i.t[:, ti, k:k + 1], axis=0),
                            in_=toki.t[:, ti:ti + 1], in_offset=None), reads=[dest_i.b, toki.b, bslot], writes=[bslot])
                jb = kb.sb("jb", [128, NB], stack=sa)
                kb.op(kb.pool, lambda h: h.iota(jb.t[:], [[NSLOT_BLK, NB]], base=0, channel_multiplier=0, allow_small_or_imprecise_dtypes=True),
                      writes=[jb.b])
                be = kb.sb("be", [128, NB], stack=sa)
                cmp_ = kb.sb("cmp", [128, NB], stack=sa)
                kb.memset(kb.dve, be[:], 0.0)
                for e in range(32):
                    kb.ts(kb.dve, cmp_[:], jb[:], pend[:, e:e + 1], None, ALU.is_ge)
                    kb.tt(kb.dve, be[:], be[:], cmp_[:], ALU.add)
                kb.ts(kb.dve, be[:], be[:], 31.0, 128.0, ALU.min, ALU.mult)
                pcol = kb.sb("pcol", [128, 1], stack=sa)
                kb.op(kb.pool, lambda h: h.iota(pcol.t[:], [[1, 1]], base=0, channel_multiplier=1, allow_small_or_imprecise_dtypes=True),
                      writes=[pcol.b])
                kb.ts(kb.dve, be[:], be[:], pcol[:], None, ALU.add)
                kb.cp(kb.dve, widx[:], be[:])
                kb.barrier()
            with contextlib.ExitStack() as sb_:
                W13 = [kb.sb("W13_%d" % i, [128, 2, 8, 512], BF16, stack=sb_) for i in range(2)]
                W2 = [kb.sb("W2_%d" % i, [128, 4, D], BF16, stack=sb_) for i in range(2)]
                sti = [kb.sb("sti%d" % i, [128, 2], I32, stack=sb_) for i in range(2)]
                X = [kb.sb("mX%d" % i, [128, 2, D], BF16, stack=sb_) for i in range(2)]
                XT = kb.sb("mXT", [128, 8, 256], BF16, stack=sb_)
                ptx = [kb.ps("ptx%d" % i, [128, D], BF16, stack=sb_) for i in range(2)]
                pa = kb.ps("mpa", [128, 256], stack=sb_)
                pb = kb.ps("mpb", [128, 256], stack=sb_)
                pd = kb.ps("mpd", [128, D], stack=sb_)
                sa_ = kb.sb("msa", [128, 256], stack=sb_)
                hid = kb.sb("hid", [128, 4, 256], BF16, stack=sb_)
                yo = [kb.sb("yo%d" % i, [128, D], stack=sb_) for i in range(2)]
                w13src = self.din["moe_w13h_%d" % li]
                w2src = self.din["moe_w2h_%d" % li]
                for j in range(NB):
                    w13 = W13[j % 2]
                    w2 = W2[j % 2]
                    kb.dma(kb.pool, lambda h: h.indirect_dma_start(
                        out=w13.t[:].rearrange("p a k f -> p (a k f)"), out_offset=None, in_=w13src,
                        in_offset=bass.IndirectOffsetOnAxis(ap=widx.t[:, j:j + 1], axis=0)), reads=[widx.b], writes=[w13.b])
                    kb.dma(kb.pool, lambda h: h.indirect_dma_start(
                        out=w2.t[:].rearrange("p k d -> p (k d)"), out_offset=None, in_=w2src,
                        in_offset=bass.IndirectOffsetOnAxis(ap=widx.t[:, j:j + 1], axis=0)), reads=[widx.b], writes=[w2.b])
                    si = sti[j % 2]
                    kb.dma(kb.sp, lambda h: h.dma_start(out=si.t[:], in_=self.slot_tok[j * 256:(j + 1) * 256, :].rearrange("(s p) o -> p (s o)", p=128),
                                                         allow_slow_non_contiguous=True),
                           reads=[bslot], writes=[si.b])
                    x_ = X[j % 2]
                    for s_ in range(2):
                        kb.dma(kb.pool, lambda h: h.indirect_dma_start(
                            out=x_.t[:, s_, :], out_offset=None, in_=self.Hs,
                            in_offset=bass.IndirectOffsetOnAxis(ap=si.t[:, s_:s_ + 1], axis=0)), reads=[si.b, bHs], writes=[x_.b])
                    for s_ in range(2):
                        p = ptx[s_]
                        for kt in range(8):
                            kb.tr(p[:, kt * 128:(kt + 1) * 128], x_[:, s_, kt * 128:(kt + 1) * 128], self.ident_b[:])
                        kb.cp(kb.act if s_ else kb.dve, XT[:, :, s_ * 128:(s_ + 1) * 128], V(p.t[:].rearrange("p (k t) -> p k t", k=8), p.b))
                    for ft in range(4):
                        for kt in range(8):
                            kb.mm(pa[:], w13[:, 0, kt, ft * 128:(ft + 1) * 128], XT[:, kt, :], start=(kt == 0), stop=(kt == 7))
                        for kt in range(8):
                            kb.mm(pb[:], w13[:, 1, kt, ft * 128:(ft + 1) * 128], XT[:, kt, :], start=(kt == 0), stop=(kt == 7))
                        kb.act_(sa_[:], pa[:], AF.Silu)
                        kb.tt(kb.dve, hid[:, ft, :], sa_[:], pb[:], ALU.mult)
                    for s_ in range(2):
                        for hf in range(2):
                            for ft in range(4):
                                kb.mm(pd[:, hf * 512:(hf + 1) * 512], hid[:, ft, s_ * 128:(s_ + 1) * 128], w2[:, ft, hf * 512:(hf + 1) * 512],
                                      start=(ft == 0), stop=(ft == 3))
                        y_ = yo[s_]
                        kb.cp(kb.act if s_ else kb.dve, y_[:], pd[:])
                        kb.st(kb.sp, self.yb[j * 256 + s_ * 128:j * 256 + (s_ + 1) * 128, :], y_[:])
                kb.barrier()
            with contextlib.ExitStack() as sc_:
                Y = [[kb.sb("mY%d%d" % (i, k), [128, D], stack=sc_) for k in range(2)] for i in range(2)]
                xt = [kb.sb("cxt%d" % i, [128, D], stack=sc_) for i in range(2)]
                t_ = [kb.sb("ct%d" % i, [128, D], stack=sc_) for i in range(2)]
                for n, ti in enumerate(tiles):
                    which = 1 if ti < 2 else 0
                    x = xt[n % 2]
                    kb.ld(kb.sp, x[:], self.Xres[ti * 128:(ti + 1) * 128, :])
                    for k in range(2):
                        y_ = Y[n % 2][k]
                        kb.dma(kb.pool, lambda h: h.indirect_dma_start(
                            out=y_.t[:], out_offset=None, in_=self.yb,
                            in_offset=bass.IndirectOffsetOnAxis(ap=dest_i.t[:, ti, k:k + 1], axis=0)), reads=[dest_i.b], writes=[y_.b])
                    t = t_[n % 2]
                    kb.ts(kb.dve, t[:], Y[n % 2][0][:], gate[:, ti, 0:1], None, ALU.mult)
                    kb.stt(kb.dve, t[:], Y[n % 2][1][:], gate[:, ti, 1:2], t[:], ALU.mult, ALU.add)
                    kb.tt(kb.pool, t[:], t[:], g2b[which][:], ALU.mult)
                    kb.tt(kb.pool, t[:], t[:], x[:], ALU.add)
                    kb.st(kb.sp, self.Xres[ti * 128:(ti + 1) * 128, :], t[:])
                kb.barrier()

    def final_norm(self):
        kb = self.kb
        with contextlib.ExitStack() as ph:
            gb = kb.sb("fgb", [128, D], stack=ph)
            kb.ld(kb.sp, gb[:], self.din["gn_rows"][8].partition_broadcast(128))
            xt = [kb.sb("fxt%d" % i, [128, D], stack=ph) for i in range(2)]
            xn = [kb.sb("fxn%d" % i, [128, D], stack=ph) for i in range(2)]
            junk = kb.sb("fjunk", [128, D], stack=ph)
            ss = kb.sb("fss", [128, 4], stack=ph)
            for n in range(LAT // 128):
                x = xt[n % 2]
                o = xn[n % 2]
                kb.ld(kb.sp, x[:], self.Xres[CTX + n * 128:CTX + (n + 1) * 128, :])
                kb.act_(junk[:], x[:], AF.Square, accum=ss[:, 0:1])
                kb.ts(kb.dve, ss[:, 1:2], ss[:, 0:1], 1.0 / D, EPS, ALU.mult, ALU.add)
                kb.act_(ss[:, 2:3], ss[:, 1:2], AF.Sqrt)
                kb.op(kb.dve, lambda h: h.reciprocal(ss.t[:, 3:4], ss.t[:, 2:3]), reads=[ss.b], writes=[ss.b])
                kb.stt(kb.dve, o[:], x[:], ss[:, 3:4], gb[:], ALU.mult, ALU.mult)
                kb.st(kb.sp, self.out[n * 128:(n + 1) * 128, :], o[:])
            kb.barrier()


LAYERED = ("w_mod", "b_mod", "w_in", "w_out", "hy_cw", "hy_cb", "hy_skip", "hy_ng", "hy_w1", "hy_w2", "hy_w3", "hy_bf",
           "s5_row", "s5_col", "s5_bT", "s5_cT", "s5_dn", "s5_glu", "att_l", "att_g", "moe_wr", "moe_br", "moe_w13h", "moe_w2h")


def split_layers(a):
    for k in ("moe_w13h", "moe_w2h"):
        v = a.pop(k)
        for i in range(v.shape[0]):
            a["%s_%d" % (k, i)] = v[i]
    return a


def make_core_arrays(common, core, layer_ids):
    a = {}
    for k, v in common.items():
        if k in LAYERED:
            a[k] = np.ascontiguousarray(v[list(layer_ids)])
        else:
            a[k] = v
    a.update(core)
    return split_layers(a)


def kernel(**inputs):
    common = split_layers(host_common(inputs))
    arrs = []
    for b in range(8):
        a = dict(common)
        a.update(host_core(inputs, b))
        arrs.append(a)
    p = Prog(arrs[0], list(range(DEPTH)))
    nc = p.build()
    res = run_bass_kernel_spmd(nc, arrs, core_ids=list(range(8)))
    out = np.stack([np.asarray(res.results[b]["out"]) for b in range(8)], axis=0)
    return out.astype(np.float32)
```

```python
import contextlib
import math
import numpy as np
import ml_dtypes
import concourse.bass as bass
import concourse.mybir as mybir
from concourse.bass_utils import run_bass_kernel_spmd

F32 = mybir.dt.float32
BF16 = mybir.dt.bfloat16
I32 = mybir.dt.int32
U32 = mybir.dt.uint32
AF = mybir.ActivationFunctionType
ALU = mybir.AluOpType
AX = mybir.AxisListType

DEPTH = 4
D = 1024
LAT = 4096
CTX = 256
T = LAT + CTX
NT = T // 128
EPS = 1e-6
TWO_PI = 2.0 * math.pi
NSLOT_BLK = 256
NBLK = (2 * T) // NSLOT_BLK + 32
NPAD = NBLK * NSLOT_BLK


class Buf:
    __slots__ = ("writers", "readers")

    def __init__(self):
        self.writers = {}
        self.readers = {}


class V:
    __slots__ = ("ap", "buf")

    def __init__(self, ap, buf):
        self.ap = ap
        self.buf = buf


class Tile:
    def __init__(self, t, buf=None):
        self.t = t
        self.b = buf or Buf()

    def __getitem__(self, idx):
        return V(self.t[idx], self.b)

    def v(self, ap):
        return V(ap, self.b)


class Eng:
    def __init__(self, name, h, is_pe=False):
        self.name = name
        self.h = h
        self.sem = None
        self.count = 0
        self.seen = {}
        self.is_pe = is_pe
        self.dq = []
        self.dqi = 0


EPOCH = 16000
NDQ = 6


class KB:
    def __init__(self, nc, stack):
        self.nc = nc
        self.stack = stack
        self.pe = Eng("pe", nc.tensor, True)
        self.act = Eng("act", nc.scalar)
        self.dve = Eng("dve", nc.vector)
        self.pool = Eng("pool", nc.gpsimd)
        self.sp = Eng("sp", nc.sync)
        self.nsem = 0
        self.ninst = 0
        self.uid = 0
        for e in (self.pe, self.act, self.dve, self.pool):
            e.sem = self.newsem()
        for e in (self.sp, self.act, self.pool):
            e.dq = [[self.newsem(), 0] for _ in range(NDQ)]
        self.engs = (self.pe, self.act, self.dve, self.pool, self.sp)

    def newsem(self):
        self.nsem += 1
        return self.stack.enter_context(self.nc.semaphore("s%d" % self.nsem))

    def name(self, n):
        self.uid += 1
        return "%s_%d" % (n, self.uid)

    def sb(self, name, shape, dt=F32, stack=None):
        st = stack or self.stack
        return Tile(st.enter_context(self.nc.sbuf_tensor(self.name(name), list(shape), dt)))

    def ps(self, name, shape, dt=F32, stack=None):
        st = stack or self.stack
        return Tile(st.enter_context(self.nc.psum_tensor(self.name(name), list(shape), dt)))

    def _deps(self, reads, writes):
        deps = {}
        for b in reads:
            for s, v in b.writers.items():
                if deps.get(s, 0) < v:
                    deps[s] = v
        for b in writes:
            for d in (b.writers, b.readers):
                for s, v in d.items():
                    if deps.get(s, 0) < v:
                        deps[s] = v
        return deps

    def _wait(self, eng, deps):
        for s, v in deps.items():
            if eng.is_pe and s is eng.sem:
                continue
            if eng.seen.get(s, 0) < v:
                eng.h.wait_ge(s, v)
                eng.seen[s] = v

    def _mark(self, tok, reads, writes):
        s, v = tok
        for b in reads:
            if b.readers.get(s, 0) < v:
                b.readers[s] = v
        for b in writes:
            b.writers = {s: v}
            b.readers = {}

    def op(self, eng, fn, reads=(), writes=()):
        reads = [r.buf if isinstance(r, V) else r for r in reads]
        writes = [w.buf if isinstance(w, V) else w for w in writes]
        self._wait(eng, self._deps(reads, writes))
        if eng.count >= EPOCH:
            eng.sem = self.newsem()
            eng.count = 0
        inst = fn(eng.h)
        eng.count += 1
        inst.then_inc(eng.sem, 1)
        self.ninst += 1
        self._mark((eng.sem, eng.count), reads, writes)
        return inst

    def dma(self, eng, fn, reads=(), writes=()):
        reads = [r.buf if isinstance(r, V) else r for r in reads]
        writes = [w.buf if isinstance(w, V) else w for w in writes]
        self._wait(eng, self._deps(reads, writes))
        slot = eng.dq[eng.dqi % NDQ]
        eng.dqi += 1
        if slot[1] >= 30000:
            slot[0] = self.newsem()
            slot[1] = 0
        s, v = slot
        if v > 0 and eng.seen.get(s, 0) < v:
            eng.h.wait_ge(s, v)
            eng.seen[s] = v
        inst = fn(eng.h)
        inst.then_inc(s, 16)
        slot[1] = v + 16
        self.ninst += 1
        self._mark((s, v + 16), reads, writes)
        return inst

    def barrier(self):
        toks = {}
        for e in (self.pe, self.act, self.dve, self.pool):
            if e.count > 0:
                toks[e.sem] = e.count
        for e in (self.sp, self.act, self.pool):
            for s, v in e.dq:
                if v > 0:
                    toks[s] = v
        for e in self.engs:
            for s, v in toks.items():
                if s is e.sem:
                    continue
                if e.seen.get(s, 0) < v:
                    e.h.wait_ge(s, v)
                    e.seen[s] = v

    def mm(self, o, lhsT, rhs, start=True, stop=True):
        return self.op(self.pe, lambda h: h.matmul(o.ap, lhsT.ap, rhs.ap, start=start, stop=stop),
                       reads=[lhsT, rhs], writes=[o])

    def tr(self, o, i, ident):
        return self.op(self.pe, lambda h: h.transpose(o.ap, i.ap, ident.ap), reads=[i, ident], writes=[o])

    def act_(self, o, i, func, bias=None, scale=None, accum=None, eng=None):
        reads = [i]
        kw = {}
        if bias is not None:
            if isinstance(bias, V):
                reads.append(bias)
                kw["bias"] = bias.ap
            else:
                kw["bias"] = bias
        if scale is not None:
            if isinstance(scale, V):
                reads.append(scale)
                kw["scale"] = scale.ap
            else:
                kw["scale"] = scale
        writes = [o]
        if accum is not None:
            kw["accum_out"] = accum.ap
            writes.append(accum)
        return self.op(self.act, lambda h: h.activation(out=o.ap, in_=i.ap, func=func, **kw), reads=reads, writes=writes)

    def tt(self, eng, o, a, b, op):
        return self.op(eng, lambda h: h.tensor_tensor(o.ap, a.ap, b.ap, op), reads=[a, b], writes=[o])

    def ts(self, eng, o, a, s1, s2, op0, op1=None):
        reads = [a]
        x1 = s1.ap if isinstance(s1, V) else s1
        x2 = s2.ap if isinstance(s2, V) else s2
        if isinstance(s1, V):
            reads.append(s1)
        if isinstance(s2, V):
            reads.append(s2)
        if op1 is None:
            return self.op(eng, lambda h: h.tensor_scalar(o.ap, a.ap, x1, None, op0), reads=reads, writes=[o])
        return self.op(eng, lambda h: h.tensor_scalar(o.ap, a.ap, x1, x2, op0, op1), reads=reads, writes=[o])

    def stt(self, eng, o, a, s, b, op0, op1):
        reads = [a, b]
        xs = s.ap if isinstance(s, V) else s
        if isinstance(s, V):
            reads.append(s)
        return self.op(eng, lambda h: h.scalar_tensor_tensor(o.ap, a.ap, xs, b.ap, op0, op1), reads=reads, writes=[o])

    def cp(self, eng, o, i):
        if eng is self.act:
            return self.op(eng, lambda h: h.copy(o.ap, i.ap), reads=[i], writes=[o])
        return self.op(eng, lambda h: h.tensor_copy(o.ap, i.ap), reads=[i], writes=[o])

    def memset(self, eng, o, val):
        return self.op(eng, lambda h: h.memset(o.ap, val), writes=[o])

    def ld(self, q, o, src_ap, reads=()):
        return self.dma(q, lambda h: h.dma_start(out=o.ap, in_=src_ap), reads=list(reads), writes=[o])

    def st(self, q, dst_ap, i, writes=()):
        return self.dma(q, lambda h: h.dma_start(out=dst_ap, in_=i.ap), reads=[i], writes=list(writes))


def _col(v, nt):
    return np.ascontiguousarray(np.asarray(v, np.float32).reshape(nt, 128).T)


def host_constants():
    c = {}
    f32 = np.float32
    for tag, L in (("L", LAT), ("C", CTX)):
        t = (np.arange(L, dtype=f32) / f32(L)).astype(f32)
        ang = (f32(2.0 * math.pi) * t[:, None] * np.arange(1, 17, dtype=f32)).astype(f32)
        feat = np.concatenate([t[:, None], np.cos(ang), np.sin(ang)], axis=-1).astype(f32)
        c["featT_" + tag] = np.ascontiguousarray(feat.T)
        c["negt_" + tag] = _col(-t, L // 128)
        N = 2 * L
        k = np.arange(L, dtype=np.float64)
        th = 2.0 * np.pi * np.outer(k + 0.5, k + 0.5) / N
        c["dftC_" + tag] = np.cos(th).astype(ml_dtypes.bfloat16)
        c["dftS_" + tag] = np.sin(th).astype(ml_dtypes.bfloat16)
        th1 = 2.0 * np.pi * np.outer(k, k + 0.5) / N
        c["dftC1_" + tag] = np.cos(th1).astype(ml_dtypes.bfloat16)
        c["dftS1_" + tag] = np.sin(th1).astype(ml_dtypes.bfloat16)
        ph = np.pi * (k + 0.5) / N
        c["ab_" + tag] = np.ascontiguousarray(np.stack([_col(np.cos(ph), L // 128), _col(np.sin(ph), L // 128)], axis=1))
    dmin = -math.log(1e-2) / 1.5
    dmax = -math.log(1e-2) / 0.3
    c["decay_b"] = np.ascontiguousarray(np.broadcast_to(np.linspace(dmin, dmax, 256, dtype=f32)[None, :], (128, 256)))
    rows = LAT // 64
    row = np.repeat(np.arange(rows, dtype=f32), 64)
    colv = np.tile(np.arange(64, dtype=f32), rows)
    inv = (f32(10000.0) ** (-np.arange(16, dtype=f32) / f32(16))).astype(f32)
    ang = np.concatenate([row[:, None] * inv, colv[:, None] * inv], axis=-1).astype(f32)
    j = (np.arange(128) % 64) % 32
    c["ropeT"] = np.ascontiguousarray(np.stack([np.cos(ang)[:, j].T, np.sin(ang)[:, j].T], axis=1).astype(f32))
    return c


def host_common(inp):
    f32 = np.float32
    g = {k: np.asarray(v) for k, v in inp.items()}
    o = {}
    o["w_mod"] = g["w_mod"]
    o["b_mod"] = g["b_mod"].reshape(DEPTH, 1, 6 * D)
    o["gn_rows"] = np.concatenate([g["norm1_g"], g["norm2_g"], g["final_g"][None]], axis=0).reshape(9, 1, D)
    o["w_in"] = g["w_in"]
    o["w_out"] = g["w_out"]
    o["hy_cw"] = np.ascontiguousarray(g["hy_conv_w"].reshape(DEPTH, 3, 6, 128).transpose(0, 3, 2, 1))
    o["hy_cb"] = np.ascontiguousarray(g["hy_conv_b"].reshape(DEPTH, 6, 128).transpose(0, 2, 1))
    o["hy_skip"] = np.ascontiguousarray(g["hy_skip"].reshape(DEPTH, 2, 128).transpose(0, 2, 1))
    o["hy_ng"] = np.ascontiguousarray(g["hy_norm_g"].reshape(DEPTH, 2, 128).transpose(0, 2, 1))
    o["hy_w1"] = g["hy_ffn_w1"]
    o["hy_w2"] = g["hy_ffn_w2"]
    o["hy_w3"] = g["hy_ffn_w3"]
    o["hy_bf"] = np.ascontiguousarray(np.stack([g["hy_ffn_b1"], g["hy_ffn_b2"], g["hy_freq"]], axis=-1))
    G, P, H = 16, 64, 16
    rows = np.stack([g["s5_a_re"].reshape(DEPTH, 2, G * P), g["s5_a_im"].reshape(DEPTH, 2, G * P),
                     np.repeat(g["s5_log_dt"], P, axis=-1)], axis=2)
    o["s5_row"] = np.ascontiguousarray(rows.reshape(DEPTH, 2, 1, 3 * G * P))
    cols = rows.reshape(DEPTH, 2, 3, 8, 128).transpose(0, 1, 4, 2, 3)
    o["s5_col"] = np.ascontiguousarray(cols)
    bT = np.zeros((DEPTH, 2, 2, 2, 128, 512), f32)
    cT = np.zeros((DEPTH, 2, 2, 8, 128, 128), f32)
    for ri, (bsrc, csrc) in enumerate(((g["s5_b_re"], g["s5_c_re"]), (g["s5_b_im"], g["s5_c_im"]))):
        for gg in range(G):
            half, gm = gg // 8, gg % 8
            bT[:, :, ri, half, gm * 16:(gm + 1) * 16, gm * 64:(gm + 1) * 64] = bsrc[:, :, gg].transpose(0, 1, 3, 2)
            pair, gl = gg // 2, gg % 2
            cT[:, :, ri, pair, gl * 64:(gl + 1) * 64, gm * 16:(gm + 1) * 16] = csrc[:, :, gg].transpose(0, 1, 3, 2)
    o["s5_bT"] = bT
    o["s5_cT"] = cT
    o["s5_dn"] = np.ascontiguousarray(np.stack([g["s5_d"].reshape(DEPTH, 2, 128).transpose(0, 2, 1),
                                                 g["s5_norm_g"].reshape(DEPTH, 2, 128).transpose(0, 2, 1)], axis=2))
    o["s5_glu"] = g["s5_glu_w"]
    o["att_l"] = np.concatenate([g["att_lq1"], g["att_lk1"], g["att_lq2"], g["att_lk2"]], axis=-1).reshape(DEPTH, 1, 256)
    o["att_g"] = g["att_subln_g"].reshape(DEPTH, 1, 128)
    o["moe_wr"] = np.ascontiguousarray(np.concatenate([g["moe_wg"], g["moe_we"]], axis=-1))
    o["moe_br"] = np.concatenate([g["moe_bg"], g["moe_be"]], axis=-1).reshape(DEPTH, 1, 36)
    w1 = g["moe_w1"].reshape(DEPTH, 32, 8, 128, 512).transpose(0, 1, 3, 2, 4)
    w3 = g["moe_w3"].reshape(DEPTH, 32, 8, 128, 512).transpose(0, 1, 3, 2, 4)
    o["moe_w13h"] = np.ascontiguousarray(np.stack([w1, w3], axis=3)).reshape(DEPTH, 32 * 128, 2 * 8 * 512)
    o["moe_w2h"] = np.ascontiguousarray(g["moe_w2"].reshape(DEPTH, 32, 4, 128, 1024).transpose(0, 1, 3, 2, 4)).reshape(DEPTH, 32 * 128, 4 * 1024)
    o.update(host_constants())
    return {k: np.ascontiguousarray(v) for k, v in o.items()}


def host_core(inp, b):
    x = np.asarray(inp["x"])[b]
    ctx = np.asarray(inp["ctx"])[b]
    cc = np.stack([_col(np.asarray(inp["c"])[b], 8), _col(np.asarray(inp["c_ctx"]), 8)], axis=-1)
    return {"xin": np.ascontiguousarray(np.concatenate([ctx, x], axis=0)), "cT": np.ascontiguousarray(cc)}


IN_SHAPES = None


def chunks_of(T0, Ttot, W):
    out = []
    t = T0
    while t < Ttot:
        w = min(W, Ttot - t)
        out.append((t, w))
        t += w
    return out


TOK_CHUNKS = [(0, CTX)] + chunks_of(CTX, T, 512)


class Prog:
    def __init__(self, arrays, layer_ids, debug=(), phases=None):
        self.layer_ids = list(layer_ids)
        self.NL = len(self.layer_ids)
        self.debug = set(debug)
        self.phases = phases
        nc = bass.Bass("TRN2", target_bir_lowering=False)
        self.nc = nc
        self.din = {}
        for k, v in arrays.items():
            dt = {np.dtype(np.float32): F32, np.dtype(ml_dtypes.bfloat16): BF16, np.dtype(np.int32): I32}[v.dtype]
            self.din[k] = nc.dram_tensor(k, list(v.shape), dt, kind="ExternalInput").ap()
        self.out = nc.dram_tensor("out", [LAT, D], F32, kind="ExternalOutput").ap()

        def scratch(name, shape, dt):
            kind = "ExternalOutput" if name in self.debug else "Internal"
            return nc.dram_tensor(name, list(shape), dt, kind=kind).ap()

        self.Xres = scratch("Xres", [T, D], F32)
        self.modrow = scratch("modrow", [self.NL, 2, 6 * D], F32)
        self.pT = scratch("pT", [1024, T], F32)
        self.QT = scratch("QT", [512, T], BF16)
        self.QrT = scratch("QrT", [512, LAT], BF16)
        self.KcT = scratch("KcT", [512, T], BF16)
        self.Vaug = scratch("Vaug", [NT, 128, 4 * 129], BF16)
        self.mixT = scratch("mixT", [1024, T], BF16)
        self.Hs = scratch("Hs", [T + 1, D], BF16)
        self.slot_tok = scratch("slot_tok", [NPAD, 1], I32)
        self.yb = scratch("yb", [NPAD, D], F32)

    def build(self):
        nc = self.nc
        with contextlib.ExitStack() as st:
            kb = KB(nc, st)
            self.kb = kb
            self.setup()
            self.modulation()
            kb.barrier()
            for li in range(self.NL):
                self.layer(li)
            self.final_norm()
            kb.barrier()
        return nc

    def ph(self, name):
        return self.phases is None or name in self.phases

    def setup(self):
        kb = self.kb
        self.ident_f = kb.sb("ident_f", [128, 128])
        kb.memset(kb.pool, self.ident_f[:], 0.0)
        kb.op(kb.pool, lambda h: h.affine_select(self.ident_f.t[:], self.ident_f.t[:], [[-1, 128]], ALU.not_equal, 1.0,
                                                 base=0, channel_multiplier=1), reads=[self.ident_f.b], writes=[self.ident_f.b])
        self.ident_b = kb.sb("ident_b", [128, 128], BF16)
        kb.cp(kb.dve, self.ident_b[:], self.ident_f[:])
        self.ones_f = kb.sb("ones_f", [128, 128])
        kb.memset(kb.dve, self.ones_f[:], 1.0)
        self.tri = kb.sb("tri", [128, 128])
        kb.memset(kb.pool, self.tri[:], 1.0)
        kb.op(kb.pool, lambda h: h.affine_select(self.tri.t[:], self.tri.t[:], [[1, 128]], ALU.is_ge, 0.0,
                                                 base=-1, channel_multiplier=-1), reads=[self.tri.b], writes=[self.tri.b])
        self.iota512 = kb.sb("iota512", [128, 512])
        kb.op(kb.pool, lambda h: h.iota(self.iota512.t[:], [[1, 512]], base=0, channel_multiplier=0,
                                        allow_small_or_imprecise_dtypes=True), writes=[self.iota512.b])
        self.blk = []
        for m in range(2):
            b = kb.sb("blk%d" % m, [128, 128], BF16)
            kb.memset(kb.dve, b[:], 0.0)
            kb.memset(kb.dve, b[m * 64:(m + 1) * 64, :], 1.0)
            self.blk.append(b)
        self.r_hy = kb.sb("r_hy", [128, NT])
        self.r_s5 = kb.sb("r_s5", [128, NT])
        self.qkmax = kb.sb("qkmax", [128, 16])
        self.halfpi = kb.sb("halfpi", [128, 1])
        kb.memset(kb.dve, self.halfpi[:], math.pi / 2.0)

    def modulation(self):
        kb = self.kb
        with contextlib.ExitStack() as ph:
            cT = kb.sb("cT", [128, 8, 2], stack=ph)
            kb.ld(kb.sp, cT[:], self.din["cT"])
            sc = kb.sb("sc", [128, 8, 2], stack=ph)
            kb.act_(sc[:], cT[:], AF.Silu)
            wm = [kb.sb("wm%d" % i, [128, 8, 512], stack=ph) for i in range(2)]
            pm = [kb.ps("pm%d" % i, [128, 512], stack=ph) for i in range(2)]
            rows = [kb.sb("mrow%d" % i, [1, 6 * D], stack=ph) for i in range(2)]
            brow = kb.sb("brow", [1, 6 * D], stack=ph)
            n = 0
            for li in range(self.NL):
                kb.ld(kb.sp, brow[:], self.din["b_mod"][li])
                for ch in range(12):
                    w = wm[n % 2]
                    n += 1
                    kb.ld(kb.sp, w[:], self.din["w_mod"][li][:, ch * 512:(ch + 1) * 512].rearrange("(kt p) n -> p kt n", p=128))
                    for which in range(2):
                        p = pm[which]
                        for kt in range(8):
                            kb.mm(p[0:1, :], sc[:, kt, which:which + 1], w[:, kt, :], start=(kt == 0), stop=(kt == 7))
                        kb.tt(kb.dve, rows[which][:, ch * 512:(ch + 1) * 512], p[0:1, :], brow[:, ch * 512:(ch + 1) * 512], ALU.add)
                for which in range(2):
                    kb.st(kb.sp, self.modrow[li, which:which + 1, :], rows[which][:])
            kb.barrier()

    def load_mod_b(self, li, which, seg, stack, name):
        t = self.kb.sb(name, [128, D], stack=stack)
        self.kb.ld(self.kb.sp, t[:], self.modrow[li, which:which + 1, seg * D:(seg + 1) * D].partition_broadcast(128))
        return t

    def norm_mod_tiles(self, li, gidx, seg_sh, seg_sc, stack):
        kb = self.kb
        gb = kb.sb("gnb", [128, D], stack=stack)
        kb.ld(kb.sp, gb[:], self.din["gn_rows"][gidx].partition_broadcast(128))
        res = []
        for which in range(2):
            scb = self.load_mod_b(li, which, seg_sc, stack, "scb%d" % which)
            shb = self.load_mod_b(li, which, seg_sh, stack, "shb%d" % which)
            kb.stt(kb.dve, scb[:], scb[:], 1.0, gb[:], ALU.add, ALU.mult)
            res.append((scb, shb))
        return res

    def norm_tile(self, xt, ab, xn, tmp_ss, tmp_junk):
        kb = self.kb
        A, B = ab
        kb.act_(tmp_junk[:], xt[:], AF.Square, accum=tmp_ss[:, 0:1])
        kb.ts(kb.dve, tmp_ss[:, 1:2], tmp_ss[:, 0:1], 1.0 / D, EPS, ALU.mult, ALU.add)
        kb.act_(tmp_ss[:, 2:3], tmp_ss[:, 1:2], AF.Sqrt)
        kb.op(kb.dve, lambda h: h.reciprocal(tmp_ss.t[:, 3:4], tmp_ss.t[:, 2:3]), reads=[tmp_ss.b], writes=[tmp_ss.b])
        kb.stt(kb.dve, xn[:], xt[:], tmp_ss[:, 3:4], A[:], ALU.mult, ALU.mult)
        kb.tt(kb.dve, xn[:], xn[:], B[:], ALU.add)

    def layer(self, li):
        kb = self.kb
        if self.ph("inproj"):
            self.inproj(li)
            kb.barrier()
        if self.ph("hyena"):
            self.hyena(li, LAT, CTX, "L")
            kb.barrier()
            if self.layer_ids[li] < DEPTH - 1:
                self.hyena(li, CTX, 0, "C")
                kb.barrier()
        if self.ph("s5") or self.ph("attn"):
            self.s5_attn(li)
            kb.barrier()
        if self.ph("outproj"):
            self.outproj(li)
            kb.barrier()
        if self.ph("moe"):
            self.moe(li)
            kb.barrier()

    def xsrc(self, li):
        return self.din["xin"] if li == 0 else self.Xres

    def inproj(self, li):
        kb = self.kb
        src = self.xsrc(li)
        with contextlib.ExitStack() as ph:
            w_in = kb.sb("w_in", [128, 8, 2560], BF16, stack=ph)
            for kt in range(8):
                kb.ld(kb.pool, w_in[:, kt, :], self.din["w_in"][li][kt * 128:(kt + 1) * 128, :])
            wrot = kb.sb("wrot", [128, 8, 1024], BF16, stack=ph)
            wrot5 = wrot.t[:].rearrange("p k (b two j) -> p k b two j", two=2, j=32)
            for kt in range(8):
                srcv = self.din["w_in"][li][kt * 128:(kt + 1) * 128, 1024:2048].rearrange("p (b two j) -> p b two j", two=2, j=32)
                kb.dma(kb.pool, lambda h: h.dma_start(out=wrot5[:, kt, :, 0, :], in_=srcv[:, :, 1, :]), writes=[wrot.b])
                kb.dma(kb.pool, lambda h: h.dma_start(out=wrot5[:, kt, :, 1, :], in_=srcv[:, :, 0, :]), writes=[wrot.b])
            for kt in range(8):
                kb.op(kb.dve, lambda h: h.tensor_scalar(wrot5[:, kt, :, 0, :], wrot5[:, kt, :, 0, :], -1.0, None, ALU.mult),
                      reads=[wrot.b], writes=[wrot.b])
            rope = kb.sb("rope", [128, 2, LAT], stack=ph)
            kb.ld(kb.sp, rope[:], self.din["ropeT"])
            ab = self.norm_mod_tiles(li, self.layer_ids[li], 0, 1, ph)
            kb.memset(kb.dve, self.qkmax[:], 0.0)
            xt = [kb.sb("xt%d" % i, [128, D], stack=ph) for i in range(2)]
            xn = kb.sb("xn", [128, D], stack=ph)
            junk = kb.sb("junk", [128, D], stack=ph)
            ss = kb.sb("ss", [128, 4], stack=ph)
            xnT = [kb.sb("xnT%d" % i, [128, 8, 512], BF16, stack=ph) for i in range(2)]
            trp = kb.ps("trp", [128, D], stack=ph)
            pj = [kb.ps("pj%d" % i, [128, 512], stack=ph) for i in range(4)]
            pv = kb.ps("pv", [128, 512], stack=ph)
            nb = kb.ps("nb", [128, 512], stack=ph)
            stg = [kb.sb("stg%d" % i, [128, 512], stack=ph) for i in range(2)]
            stb = [kb.sb("stb%d" % i, [128, 512], BF16, stack=ph) for i in range(3)]
            sqb = kb.sb("sqb", [128, 512], BF16, stack=ph)
            red = kb.sb("red", [128, 1], stack=ph)
            t1 = kb.sb("t1", [128, 512], stack=ph)
            t2 = kb.sb("t2", [128, 512], stack=ph)
            vst = [kb.sb("vst%d" % i, [128, 4, 129], BF16, stack=ph) for i in range(2)]
            for v in vst:
                kb.memset(kb.dve, v[:], 1.0)
            cnt = {"ev": 0, "stg": 0, "stb": 0, "pj": 0, "v": 0, "x": 0}

            def evac_engine():
                cnt["ev"] += 1
                return kb.act if cnt["ev"] % 2 == 0 else kb.dve

            def next_pj():
                cnt["pj"] += 1
                return pj[cnt["pj"] % 4]

            def proj_fm(wv, c0, xc, W):
                p = next_pj()
                for kt in range(8):
                    kb.mm(p[:, :W], V(wv(kt, c0), wv.buf), xc[:, kt, :W], start=(kt == 0), stop=(kt == 7))
                return p

            def w_in_cols(kt, c0):
                return w_in.t[:, kt, c0:c0 + 128]
            w_in_cols.buf = w_in.b

            def w_rot_cols(kt, c0):
                return wrot.t[:, kt, c0:c0 + 128]
            w_rot_cols.buf = wrot.b

            def norm_stat(sv, W, col):
                kb.tt(kb.dve, sqb[:, :W], sv, sv, ALU.mult)
                for m in range(2):
                    kb.mm(nb[:, :W], self.blk[m][:], sqb[:, :W])
                    kb.op(kb.dve, lambda h: h.reduce_max(red.t[:], nb.t[:, :W], AX.X), reads=[nb.b], writes=[red.b])
                    kb.tt(kb.dve, self.qkmax[:, col + m:col + m + 1], self.qkmax[:, col + m:col + m + 1], red[:], ALU.max)

            for ci, (t0, W) in enumerate(TOK_CHUNKS):
                which = 1 if ci == 0 else 0
                xc = xnT[ci % 2]
                ntile = W // 128
                for ti in range(ntile):
                    x = xt[cnt["x"] % 2]
                    cnt["x"] += 1
                    kb.ld(kb.sp, x[:], src[t0 + ti * 128:t0 + (ti + 1) * 128, :])
                    self.norm_tile(x, ab[which], xn, ss, junk)
                    for kt in range(8):
                        kb.tr(trp[:, kt * 128:(kt + 1) * 128], xn[:, kt * 128:(kt + 1) * 128], self.ident_f[:])
                    for hf in range(2):
                        e = evac_engine()
                        kb.cp(e, V(xc.t[:, hf * 4:(hf + 1) * 4, ti * 128:(ti + 1) * 128], xc.b),
                              V(trp.t[:, hf * 512:(hf + 1) * 512].rearrange("p (k t) -> p k t", k=4), trp.b))
                for j in range(8):
                    p = proj_fm(w_in_cols, j * 128, xc, W)
                    s = stg[cnt["stg"] % 2]
                    cnt["stg"] += 1
                    kb.cp(evac_engine(), s[:, :W], p[:, :W])
                    kb.st(kb.sp, self.pT[j * 128:(j + 1) * 128, t0:t0 + W], s[:, :W])
                lat = ci > 0
                tl = t0 - CTX
                for kind in range(2):
                    for hh in range(4):
                        c0 = 1024 + kind * 512 + hh * 128
                        pa = proj_fm(w_in_cols, c0, xc, W)
                        col = kind * 8 + hh * 2
                        if (kind == 0) or (not lat):
                            sbt = stb[cnt["stb"] % 3]
                            cnt["stb"] += 1
                            kb.act_(sbt[:, :W], pa[:, :W], AF.Copy, scale=(0.125 if kind == 0 else 1.0))
                            dst = self.QT if kind == 0 else self.KcT
                            kb.st(kb.sp, dst[hh * 128:(hh + 1) * 128, t0:t0 + W], sbt[:, :W])
                            if not lat or kind == 0:
                                norm_stat(sbt[:, :W], W, col)
                        if lat:
                            pb = proj_fm(w_rot_cols, kind * 512 + hh * 128, xc, W)
                            kb.tt(kb.dve, t1[:, :W], pa[:, :W], rope[:, 0, tl:tl + W], ALU.mult)
                            kb.tt(kb.dve, t2[:, :W], pb[:, :W], rope[:, 1, tl:tl + W], ALU.mult)
                            sbt = stb[cnt["stb"] % 3]
                            cnt["stb"] += 1
                            kb.stt(kb.dve, sbt[:, :W], t1[:, :W], (0.125 if kind == 0 else 1.0), t2[:, :W], ALU.mult, ALU.add) \
                                if kind == 1 else None
                            if kind == 0:
                                kb.tt(kb.dve, t1[:, :W], t1[:, :W], t2[:, :W], ALU.add)
                                kb.act_(sbt[:, :W], t1[:, :W], AF.Copy, scale=0.125)
                                kb.st(kb.sp, self.QrT[hh * 128:(hh + 1) * 128, tl:tl + W], sbt[:, :W])
                            else:
                                kb.st(kb.sp, self.KcT[hh * 128:(hh + 1) * 128, t0:t0 + W], sbt[:, :W])
                                norm_stat(sbt[:, :W], W, col)
                for ti in range(ntile):
                    for kt in range(8):
                        kb.mm(pv[:], xc[:, kt, ti * 128:(ti + 1) * 128], w_in[:, kt, 2048:2560], start=(kt == 0), stop=(kt == 7))
                    vs = vst[cnt["v"] % 2]
                    cnt["v"] += 1
                    kb.cp(evac_engine(), V(vs.t[:, :, 0:128], vs.b), V(pv.t[:].rearrange("p (h d) -> p h d", h=4), pv.b))
                    kb.st(kb.sp, self.Vaug[(t0 // 128) + ti], V(vs.t[:].rearrange("p h d -> p (h d)"), vs.b))

    def range_reduce_sin(self, out, arg, ki, kf, W, rows):
        kb = self.kb
        a = arg[0:rows, :W]
        kb.ts(kb.dve, ki[0:rows, :W], a, 1.0 / TWO_PI, None, ALU.mult)
        kb.cp(kb.dve, kf[0:rows, :W], ki[0:rows, :W])
        kb.stt(kb.dve, a, kf[0:rows, :W], -TWO_PI, a, ALU.mult, ALU.add)
        kb.ts(kb.dve, a, a, -3.141592, 3.141592, ALU.max, ALU.min)
        kb.act_(out, a, AF.Sin)

    def conv3(self, eng, u, pin, W, cw, cb, j):
        kb = self.kb
        kb.ts(eng, u, pin[:, 1:W + 1], cw[:, j, 1:2], cb[:, j:j + 1], ALU.mult, ALU.add)
        kb.stt(eng, u, pin[:, 0:W], cw[:, j, 0:1], u, ALU.mult, ALU.add)
        kb.stt(eng, u, pin[:, 2:W + 2], cw[:, j, 2:3], u, ALU.mult, ALU.add)

    def dft_passes(self, ph, C, S, nt, rhs_fn, ncols, epilogue):
        kb = self.kb
        with contextlib.ExitStack() as sc:
            accC = [kb.ps("accC%d" % j, [128, 512], stack=sc) for j in range(4)]
            accS = [kb.ps("accS%d" % j, [128, 512], stack=sc) for j in range(4)]
            NG = 4 if nt % 4 == 0 else 2
            Cp = [kb.sb("Cp%d" % j, [128, NG, 512], BF16, stack=sc) for j in range(3)]
            Sp = [kb.sb("Sp%d" % j, [128, NG, 512], BF16, stack=sc) for j in range(3)]
            n = 0
            for p0 in range(0, nt, 4):
                kts = min(4, nt - p0)
                for ng in range(0, nt, NG):
                    cp_, sp_ = Cp[n % 3], Sp[n % 3]
                    n += 1
                    kb.ld(kb.sp, cp_[:, :, :kts * 128],
                          C[ng * 128:(ng + NG) * 128, p0 * 128:(p0 + kts) * 128].rearrange("(a p) k -> p a k", p=128))
                    kb.ld(kb.act, sp_[:, :, :kts * 128],
                          S[ng * 128:(ng + NG) * 128, p0 * 128:(p0 + kts) * 128].rearrange("(a p) k -> p a k", p=128))
                    for a_ in range(NG):
                        n_t = ng + a_
                        r = rhs_fn(n_t)
                        rC, rS = r if isinstance(r, tuple) else (r, r)
                        for j in range(kts):
                            kb.mm(accC[j][:, :ncols], cp_[:, a_, j * 128:(j + 1) * 128], rC, start=(n_t == 0), stop=(n_t == nt - 1))
                            kb.mm(accS[j][:, :ncols], sp_[:, a_, j * 128:(j + 1) * 128], rS, start=(n_t == 0), stop=(n_t == nt - 1))
                for j in range(kts):
                    epilogue(p0 + j, accC[j], accS[j])
            kb.barrier()

    def hyena(self, li, L, tok0, tag):
        kb = self.kb
        nt = L // 128
        N = 2 * L
        C = self.din["dftC_" + tag]
        S = self.din["dftS_" + tag]
        with contextlib.ExitStack() as ph:
            G = kb.sb("G", [128, nt, 512], BF16, stack=ph)
            rn = kb.sb("rn", [128, 4], stack=ph)
            ab = kb.sb("ab", [128, 2, nt], stack=ph)
            kb.ld(kb.sp, ab[:], self.din["ab_" + tag])
            with contextlib.ExitStack() as fa:
                hsd = kb.sb("hsd", [128, nt, 512], BF16, stack=fa)
                with contextlib.ExitStack() as f:
                    featT = kb.sb("featT", [33, L], stack=f)
                    kb.ld(kb.sp, featT[:], self.din["featT_" + tag])
                    w1 = kb.sb("hw1", [33, 64], stack=f)
                    kb.ld(kb.sp, w1[:], self.din["hy_w1"][li])
                    w2 = kb.sb("hw2", [64, 64], stack=f)
                    kb.ld(kb.sp, w2[:], self.din["hy_w2"][li])
                    w3 = kb.sb("hw3", [64, 512], stack=f)
                    kb.ld(kb.sp, w3[:], self.din["hy_w3"][li])
                    bf_ = kb.sb("hbf", [64, 3], stack=f)
                    kb.ld(kb.sp, bf_[:], self.din["hy_bf"][li])
                    fb = kb.sb("hfb", [64, 2], stack=f)
                    kb.tt(kb.dve, fb[:, 0:1], bf_[:, 0:1], bf_[:, 2:3], ALU.mult)
                    kb.tt(kb.dve, fb[:, 1:2], bf_[:, 1:2], bf_[:, 2:3], ALU.mult)
                    negt = kb.sb("negt", [128, nt], stack=f)
                    kb.ld(kb.sp, negt[:], self.din["negt_" + tag])
                    decay = kb.sb("decay", [128, 256], stack=f)
                    kb.ld(kb.sp, decay[:], self.din["decay_b"])
                    h1T = kb.sb("h1T", [64, L], stack=f)
                    h2T = kb.sb("h2T", [64, L], stack=f)
                    arg = kb.sb("harg", [64, 512], stack=f)
                    ki = kb.sb("hki", [64, 512], I32, stack=f)
                    kf = kb.sb("hkf", [64, 512], stack=f)
                    pf = [kb.ps("pf%d" % i, [128, 512], stack=f) for i in range(2)]
                    pn = kb.ps("pn", [128, 2], stack=f)
                    n = 0
                    for layer_i, (wv, inT, outT) in enumerate(((w1, featT, h1T), (w2, h1T, h2T))):
                        for (c0, W) in chunks_of(0, L, 512):
                            p = pf[n % 2]
                            n += 1
                            kb.mm(p[0:64, :W], wv[:], inT[:, c0:c0 + W])
                            kb.ts(kb.dve, arg[:, :W], p[0:64, :W], bf_[:, 2:3], fb[:, layer_i:layer_i + 1], ALU.mult, ALU.add)
                            self.range_reduce_sin(outT[:, c0:c0 + W], arg, ki, kf, W, 64)
                    win = kb.sb("win", [128, 256], stack=f)
                    tf = kb.sb("tf", [128, 256], stack=f)
                    tb = kb.sb("tb", [128, 256], stack=f)
                    absacc = kb.sb("absacc", [128, 256], stack=f)
                    for i in range(nt):
                        p = pf[n % 2]
                        n += 1
                        kb.mm(p[:], h2T[:, i * 128:(i + 1) * 128], w3[:])
                        kb.act_(win[:], decay[:], AF.Exp, scale=negt[:, i:i + 1])
                        kb.tt(kb.dve, tf[:], p[:, 0:256], win[:], ALU.mult)
                        kb.tt(kb.dve, tb[:], p[:, 256:512], win[:], ALU.mult)
                        if i == 0:
                            kb.memset(kb.dve, tb[0:1, :], 0.0)
                        kb.tt(kb.pool, hsd[:, i, 0:256], tf[:], tb[:], ALU.add)
                        kb.tt(kb.pool, hsd[:, i, 256:512], tb[:], tf[:], ALU.subtract)
                        kb.stt(kb.dve, tf[:], tf[:], -1.0, tf[:], ALU.mult, ALU.max)
                        kb.stt(kb.dve, tb[:], tb[:], -1.0, tb[:], ALU.mult, ALU.max)
                        kb.tt(kb.dve, tf[:], tf[:], tb[:], ALU.add)
                        if i == 0:
                            kb.cp(kb.dve, absacc[:], tf[:])
                        else:
                            kb.tt(kb.dve, absacc[:], absacc[:], tf[:], ALU.add)
                    for ct in range(2):
                        kb.mm(pn[:, ct:ct + 1], absacc[:, ct * 128:(ct + 1) * 128], self.ones_f[:, 0:1])
                    kb.ts(kb.dve, rn[:, 2:4], pn[:, 0:2], EPS, None, ALU.add)
                    kb.op(kb.dve, lambda h: h.reciprocal(rn.t[:, 0:2], rn.t[:, 2:4]), reads=[rn.b], writes=[rn.b])
                    kb.ts(kb.dve, rn[:, 0:2], rn[:, 0:2], 2.0 / N, None, ALU.mult)
                    kb.barrier()
                with contextlib.ExitStack() as f2:
                    tmpg = [kb.sb("tmpg%d" % i, [128, 256], stack=f2) for i in range(2)]

                    def epi_filter(kt, aC, aS):
                        kb.cp(kb.act, G[:, kt, 0:256], aC[:, 0:256])
                        kb.cp(kb.dve, G[:, kt, 256:512], aS[:, 0:256])

                    self.dft_passes(f2, self.din["dftC1_" + tag], self.din["dftS1_" + tag], nt,
                                    lambda n_t: (hsd[:, n_t, 0:256], hsd[:, n_t, 256:512]), 256, epi_filter)
            kb.barrier()
            cw = kb.sb("hcw", [128, 6, 3], stack=ph)
            kb.ld(kb.sp, cw[:], self.din["hy_cw"][li])
            cb = kb.sb("hcb", [128, 6], stack=ph)
            kb.ld(kb.sp, cb[:], self.din["hy_cb"][li])
            skip = kb.sb("hskip", [128, 2], stack=ph)
            kb.ld(kb.sp, skip[:], self.din["hy_skip"][li])
            ng = kb.sb("hng", [128, 2], stack=ph)
            kb.ld(kb.sp, ng[:], self.din["hy_ng"][li])
            zfm = [kb.sb("zfm%d" % i, [128, L], stack=ph) for i in range(2)]
            zT = kb.sb("zT", [128, nt, 256], BF16, stack=ph)
            Y = kb.sb("Y", [128, nt, 512], BF16, stack=ph)
            with contextlib.ExitStack() as z1:
                pin1 = kb.sb("pin1", [128, L + 2], stack=z1)
                pin2 = kb.sb("pin2", [128, L + 2], stack=z1)
                u1 = kb.sb("u1", [128, L], stack=z1)
                ptz = [kb.ps("ptz%d" % i, [128, 256], stack=z1) for i in range(2)]
                for pin in (pin1, pin2):
                    kb.memset(kb.dve, pin[:, 0:1], 0.0)
                    kb.memset(kb.dve, pin[:, L + 1:L + 2], 0.0)
                for ct in range(2):
                    kb.ld(kb.sp, pin1[:, 1:L + 1], self.pT[(2 + ct) * 128:(3 + ct) * 128, tok0:tok0 + L])
                    kb.ld(kb.act, pin2[:, 1:L + 1], self.pT[(4 + ct) * 128:(5 + ct) * 128, tok0:tok0 + L])
                    self.conv3(kb.dve, u1[:], pin1, L, cw, cb, 2 + ct)
                    self.conv3(kb.dve, zfm[ct][:], pin2, L, cw, cb, 4 + ct)
                    kb.tt(kb.dve, zfm[ct][:], zfm[ct][:], u1[:], ALU.mult)
                for i in range(nt):
                    p = ptz[i % 2]
                    for ct in range(2):
                        kb.tr(p[:, ct * 128:(ct + 1) * 128], zfm[ct][:, i * 128:(i + 1) * 128], self.ident_f[:])
                    kb.cp(kb.act if i % 2 else kb.dve, zT[:, i, :], p[:])
                kb.barrier()
            with contextlib.ExitStack() as z2:
                tq = [kb.sb("tq%d" % i, [128, 256], stack=z2) for i in range(4)]

                def epi_fwd(kt, aC, aS):
                    Gr = G[:, kt, 0:256]
                    Gi = G[:, kt, 256:512]
                    kb.tt(kb.dve, tq[0][:], aC[:, 0:256], Gr, ALU.mult)
                    kb.tt(kb.dve, tq[1][:], aS[:, 0:256], Gi, ALU.mult)
                    kb.tt(kb.pool, Y[:, kt, 0:256], tq[0][:], tq[1][:], ALU.add)
                    kb.tt(kb.dve, tq[2][:], aS[:, 0:256], Gr, ALU.mult)
                    kb.tt(kb.dve, tq[3][:], aC[:, 0:256], Gi, ALU.mult)
                    kb.tt(kb.pool, Y[:, kt, 256:512], tq[2][:], tq[3][:], ALU.subtract)

                self.dft_passes(z2, C, S, nt, lambda n_t: zT[:, n_t, :], 256, epi_fwd)
            kb.barrier()
            with contextlib.ExitStack() as z3:
                acc = [[kb.ps("iacc%d_%d" % (ct, cj), [128, 512], stack=z3) for cj in range(3)] for ct in range(2)]
                prs = kb.ps("prs", [128, 4], stack=z3)
                Cp = [kb.sb("iCp%d" % j, [128, 1536], BF16, stack=z3) for j in range(2)]
                Sp = [kb.sb("iSp%d" % j, [128, 1536], BF16, stack=z3) for j in range(2)]
                pin = [kb.sb("ipin%d" % j, [128, 514], stack=z3) for j in range(2)]
                x0 = kb.sb("x0", [128, 512], stack=z3)
                tt_ = kb.sb("itt", [128, 512], stack=z3)
                sq = [kb.sb("isq%d" % j, [128, 512], stack=z3) for j in range(2)]
                ob = [kb.sb("iob%d" % j, [128, 512], BF16, stack=z3) for j in range(2)]
                r4 = kb.sb("ir4", [128, 8], stack=z3)
                chunks = chunks_of(0, L, 512)
                n = 0
                for g0 in range(0, len(chunks), 3):
                    grp = chunks[g0:g0 + 3]
                    cA = grp[0][0]
                    cB = grp[-1][0] + grp[-1][1]
                    for kt in range(nt):
                        cp_, sp_ = Cp[n % 2], Sp[n % 2]
                        n += 1
                        kb.ld(kb.sp, cp_[:, :cB - cA], C[kt * 128:(kt + 1) * 128, cA:cB])
                        kb.ld(kb.act, sp_[:, :cB - cA], S[kt * 128:(kt + 1) * 128, cA:cB])
                        for cj, (c0, W) in enumerate(grp):
                            for ct in range(2):
                                kb.mm(acc[ct][cj][:, :W], Y[:, kt, ct * 128:(ct + 1) * 128], cp_[:, c0 - cA:c0 - cA + W],
                                      start=(kt == 0), stop=False)
                                kb.mm(acc[ct][cj][:, :W], Y[:, kt, 256 + ct * 128:256 + (ct + 1) * 128], sp_[:, c0 - cA:c0 - cA + W],
                                      start=False, stop=(kt == nt - 1))
                    for cj, (c0, W) in enumerate(grp):
                        nsub = W // 128
                        for ct in range(2):
                            pi = pin[ct]
                            lo = c0 - 1
                            hi = c0 + W + 1
                            dlo = 0
                            dhi = W + 2
                            if c0 == 0:
                                kb.memset(kb.dve, pi[:, 0:1], 0.0)
                                lo = 0
                                dlo = 1
                            if c0 + W == L:
                                kb.memset(kb.dve, pi[:, W + 1:W + 2], 0.0)
                                hi = L
                                dhi = W + 1
                            kb.ld(kb.sp, pi[:, dlo:dhi], self.pT[ct * 128:(ct + 1) * 128, tok0 + lo:tok0 + hi])
                            self.conv3(kb.dve, x0[:, :W], pi, W, cw, cb, ct)
                            kb.ts(kb.dve, tt_[:, :W], acc[ct][cj][:, :W], rn[:, ct:ct + 1], None, ALU.mult)
                            kb.stt(kb.dve, tt_[:, :W], zfm[ct][:, c0:c0 + W], skip[:, ct:ct + 1], tt_[:, :W], ALU.mult, ALU.add)
                            kb.tt(kb.dve, tt_[:, :W], tt_[:, :W], x0[:, :W], ALU.mult)
                            kb.act_(sq[ct][:, :W], tt_[:, :W], AF.Square)
                            kb.act_(ob[ct][:, :W], tt_[:, :W], AF.Copy, scale=ng[:, ct:ct + 1])
                            kb.st(kb.sp, self.mixT[ct * 128:(ct + 1) * 128, tok0 + c0:tok0 + c0 + W], ob[ct][:, :W])
                        kb.tt(kb.dve, sq[0][:, :W], sq[0][:, :W], sq[1][:, :W], ALU.add)
                        for sub in range(nsub):
                            kb.mm(prs[:, sub:sub + 1], sq[0][:, sub * 128:(sub + 1) * 128], self.ones_f[:, 0:1])
                        ti0 = (tok0 + c0) // 128
                        kb.ts(kb.dve, r4[:, 0:nsub], prs[:, 0:nsub], 1.0 / 256.0, EPS, ALU.mult, ALU.add)
                        kb.act_(r4[:, 4:4 + nsub], r4[:, 0:nsub], AF.Sqrt)
                        kb.op(kb.dve, lambda h: h.reciprocal(self.r_hy.t[:, ti0:ti0 + nsub], r4.t[:, 4:4 + nsub]),
                              reads=[r4.b], writes=[self.r_hy.b])
                kb.barrier()

    S5W = 256

    def s5_alloc(self, li, st):
        kb = self.kb
        W5 = self.S5W
        tl = {}
        tl["yT"] = [kb.sb("yT%d" % h, [128, T], stack=st) for h in range(2)]
        tl["uT"] = [kb.sb("uT%d" % h, [128, T], BF16, stack=st) for h in range(2)]
        tl["dn"] = kb.sb("s5dn", [128, 2, 2], stack=st)
        tl["uf"] = kb.sb("uf", [128, 1088], stack=st)
        tl["Bm"] = [[kb.sb("Bm%d%d" % (h, r), [128, 512], BF16, stack=st) for r in range(2)] for h in range(2)]
        tl["col"] = kb.sb("s5col", [128, 3, 8], stack=st)
        tl["thc"] = kb.sb("thc", [128, 8], stack=st)
        tl["rhoc"] = kb.sb("rhoc", [128, 8], stack=st)
        tl["rowb"] = kb.sb("rowb", [128, 3, 1024], stack=st)
        tl["a8"] = kb.sb("s5a8", [128, 8], stack=st)
        tl["k8"] = kb.sb("s5k8", [128, 8], I32, stack=st)
        tl["f8"] = kb.sb("s5f8", [128, 8], stack=st)
        tl["cs256"] = kb.sb("cs256", [128, 8], stack=st)
        tl["sn256"] = kb.sb("sn256", [128, 8], stack=st)
        tl["pt"] = [kb.sb("ppt%d" % i, [128, 512], stack=st) for i in range(8)]
        tl["pki"] = kb.sb("ppki", [128, 512], I32, stack=st)
        tl["braw"] = [kb.sb("braw%d" % r, [128, 512], stack=st) for r in range(2)]
        names = ["ang", "kf", "sinT", "cosT", "t1", "t2", "t3", "t4", "bre", "bim", "gre", "gim"]
        d = {nm: [kb.sb("s5%s%d" % (nm, i), [128, W5], stack=st) for i in range(2)] for nm in names}
        d["ki"] = [kb.sb("s5ki%d" % i, [128, W5], I32, stack=st) for i in range(2)]
        d["hre"] = [kb.sb("hre%d" % i, [128, W5], BF16, stack=st) for i in range(3)]
        d["him"] = [kb.sb("him%d" % i, [128, W5], BF16, stack=st) for i in range(3)]
        d["car"] = kb.sb("car", [128, 2], stack=st)
        d["th0"] = kb.sb("th0", [128, 1], stack=st)
        d["rhoT"] = kb.sb("rhoT", [128, W5], stack=st)
        d["Cm"] = [[kb.sb("Cm%d%d" % (r, i), [128, 128], BF16, stack=st) for r in range(2)] for i in range(2)]
        d["Cf"] = [kb.sb("Cf%d" % r, [128, 128], stack=st) for r in range(2)]
        d["pp"] = [kb.ps("s5pp%d" % i, [128, 512], stack=st) for i in range(2)]
        d["ppb"] = [[Buf(), Buf()] for i in range(2)]
        tl["d"] = d
        return tl

    def s5_scan_gen(self, li, tl):
        kb = self.kb
        W5 = self.S5W
        yT, uT, dn, uf, Bm = tl["yT"], tl["uT"], tl["dn"], tl["uf"], tl["Bm"]
        col, thc, rhoc, rowb, pt, pki, braw, d = tl["col"], tl["thc"], tl["rhoc"], tl["rowb"], tl["pt"], tl["pki"], tl["braw"], tl["d"]
        self._s5step = 0
        self._s5pend = []
        kb.ld(kb.sp, dn[:], self.din["s5_dn"][li])
        for half in range(2):
            for q in range(4):
                kb.ld(kb.sp, uf[:], self.pT[768 + half * 128:768 + (half + 1) * 128, q * 1088:(q + 1) * 1088])
                kb.ts(kb.dve, yT[half][:, q * 1088:(q + 1) * 1088], uf[:], dn[:, 0, half:half + 1], None, ALU.mult)
                kb.cp(kb.pool, uT[half][:, q * 1088:(q + 1) * 1088], uf[:])
                yield
        seq = [(0, CTX)] + chunks_of(CTX, T, W5)
        for d_ in range(2):
            kb.ld(kb.sp, V(rowb.t[:].rearrange("p a n -> p (a n)"), rowb.b), self.din["s5_row"][li, d_].partition_broadcast(128))
            for half in range(2):
                hs_ = slice(half * 512, (half + 1) * 512)
                a_re = rowb[:, 0, hs_]
                a_im = rowb[:, 1, hs_]
                dt, dre, dim, rho, sn, cs, x1, x2 = [p[:] for p in pt]
                kb.act_(dt, rowb[:, 2, hs_], AF.Exp)
                kb.tt(kb.dve, dre, dt, a_re, ALU.mult)
                kb.tt(kb.dve, dim, dt, a_im, ALU.mult)
                kb.act_(rho, dre, AF.Exp)
                kb.ts(kb.dve, pki[:], dim, 1.0 / TWO_PI, None, ALU.mult)
                kb.cp(kb.dve, x2, pki[:])
                kb.stt(kb.dve, x1, x2, -TWO_PI, dim, ALU.mult, ALU.add)
                kb.ts(kb.dve, x1, x1, -3.141592, 3.141592, ALU.max, ALU.min)
                kb.act_(sn, x1, AF.Sin)
                kb.stt(kb.dve, x2, x1, -1.0, x1, ALU.mult, ALU.max)
                kb.act_(cs, x2, AF.Sin, scale=-1.0, bias=self.halfpi[:])
                kb.tt(kb.dve, cs, cs, rho, ALU.mult)
                kb.ts(kb.dve, cs, cs, -1.0, None, ALU.add)
                kb.tt(kb.dve, sn, sn, rho, ALU.mult)
                kb.tt(kb.dve, dt, a_re, a_re, ALU.mult)
                kb.tt(kb.dve, dre, a_im, a_im, ALU.mult)
                kb.tt(kb.dve, dt, dt, dre, ALU.add)
                kb.op(kb.dve, lambda h: h.reciprocal(pt[0].t[:], pt[0].t[:]), reads=[pt[0].b], writes=[pt[0].b])
                kb.tt(kb.dve, x1, cs, a_re, ALU.mult)
                kb.tt(kb.dve, dre, sn, a_im, ALU.mult)
                kb.tt(kb.dve, x1, x1, dre, ALU.add)
                kb.tt(kb.dve, x1, x1, dt, ALU.mult)
                kb.tt(kb.dve, x2, sn, a_re, ALU.mult)
                kb.tt(kb.dve, dre, cs, a_im, ALU.mult)
                kb.tt(kb.dve, x2, x2, dre, ALU.subtract)
                kb.tt(kb.dve, x2, x2, dt, ALU.mult)
                for r in range(2):
                    kb.ld(kb.sp, braw[r][:], self.din["s5_bT"][li, d_, r, half])
                ta = pt[1][:]
                tb_ = pt[2][:]
                kb.tt(kb.dve, ta, braw[0][:], x1, ALU.mult)
                kb.tt(kb.dve, tb_, braw[1][:], x2, ALU.mult)
                kb.tt(kb.dve, Bm[half][0][:], ta, tb_, ALU.subtract)
                kb.tt(kb.dve, ta, braw[1][:], x1, ALU.mult)
                kb.tt(kb.dve, tb_, braw[0][:], x2, ALU.mult)
                kb.tt(kb.dve, Bm[half][1][:], ta, tb_, ALU.add)
                yield
            kb.ld(kb.sp, col[:], self.din["s5_col"][li, d_])
            kb.act_(col[:, 2, :], col[:, 2, :], AF.Exp)
            kb.tt(kb.dve, thc[:], col[:, 2, :], col[:, 1, :], ALU.mult)
            kb.tt(kb.dve, rhoc[:], col[:, 2, :], col[:, 0, :], ALU.mult)
            kb.act_(rhoc[:], rhoc[:], AF.Exp)
            a8, k8, f8 = tl["a8"], tl["k8"], tl["f8"]
            cs256, sn256 = tl["cs256"], tl["sn256"]
            kb.ts(kb.dve, a8[:], thc[:], float(W5), None, ALU.mult)
            kb.ts(kb.dve, k8[:], a8[:], 1.0 / TWO_PI, None, ALU.mult)
            kb.cp(kb.dve, f8[:], k8[:])
            kb.stt(kb.dve, a8[:], f8[:], -TWO_PI, a8[:], ALU.mult, ALU.add)
            kb.ts(kb.dve, a8[:], a8[:], -3.141592, 3.141592, ALU.max, ALU.min)
            kb.act_(sn256[:], a8[:], AF.Sin)
            kb.stt(kb.dve, f8[:], a8[:], -1.0, a8[:], ALU.mult, ALU.max)
            kb.act_(cs256[:], f8[:], AF.Sin, scale=-1.0, bias=self.halfpi[:])
            if d_ == 0:
                order = [(t0, W, False) for (t0, W) in seq]
            else:
                order = [(0, CTX, True)] + [(t0, W, True) for (t0, W) in reversed(seq[1:])]
            hreL, himL, car, th0, rhoT, CmL, Cf, pp, ppb = d["hre"], d["him"], d["car"], d["th0"], d["rhoT"], d["Cm"], d["Cf"], d["pp"], d["ppb"]
            for pair in range(8):
                half = pair // 4
                c0 = (pair % 4) * 128
                Cm = CmL[pair % 2]
                for r in range(2):
                    kb.ld(kb.sp, Cf[r][:], self.din["s5_cT"][li, d_, r, pair])
                kb.cp(kb.dve, Cm[0][:], Cf[0][:])
                kb.ts(kb.dve, Cm[1][:], Cf[1][:], -1.0, None, ALU.mult)
                kb.ts(kb.dve, rhoT[:], self.iota512[:, :W5], 0.0, rhoc[:, pair:pair + 1], ALU.mult, ALU.add)
                tau0 = 0
                for ci, (t0, W, rev) in enumerate(order):
                    def view(tile):
                        ap = tile.t[:, t0:t0 + W]
                        if rev:
                            ap = ap[:, ::-1]
                        return V(ap, tile.b)
                    stepno = self._s5step
                    self._s5step += 1
                    sp_ = stepno % 2
                    pbr = V(pp[sp_].t[:, 0:W], ppb[sp_][0])
                    pbi = V(pp[sp_].t[:, 256:256 + W], ppb[sp_][1])
                    py = V(pp[1 - sp_].t[:, 0:W], ppb[1 - sp_][0])
                    hre = hreL[stepno % 3]
                    him = himL[stepno % 3]
                    ang, ki, kf, sinT, cosT = d["ang"][sp_], d["ki"][sp_], d["kf"][sp_], d["sinT"][sp_], d["cosT"][sp_]
                    t1, t2, t3, t4 = d["t1"][sp_], d["t2"][sp_], d["t3"][sp_], d["t4"][sp_]
                    bre, bim, gre, gim = d["bre"][sp_], d["bim"][sp_], d["gre"][sp_], d["gim"][sp_]
                    uv = view(uT[half])
                    kb.mm(pbr, Bm[half][0][:, c0:c0 + 128], uv)
                    kb.mm(pbi, Bm[half][1][:, c0:c0 + 128], uv)
                    if ci == 0:
                        kb.ts(kb.dve, ang[:, :W], self.iota512[:, :W], thc[:, pair:pair + 1], None, ALU.mult)
                        kb.ts(kb.dve, ki[:, :W], ang[:, :W], 1.0 / TWO_PI, None, ALU.mult)
                        kb.cp(kb.pool, kf[:, :W], ki[:, :W])
                        kb.stt(kb.dve, ang[:, :W], kf[:, :W], -TWO_PI, ang[:, :W], ALU.mult, ALU.add)
                        kb.ts(kb.dve, ang[:, :W], ang[:, :W], -3.141592, 3.141592, ALU.max, ALU.min)
                        kb.act_(sinT[:, :W], ang[:, :W], AF.Sin)
                        kb.stt(kb.dve, kf[:, :W], ang[:, :W], -1.0, ang[:, :W], ALU.mult, ALU.max)
                        kb.act_(cosT[:, :W], kf[:, :W], AF.Sin, scale=-1.0, bias=self.halfpi[:])
                    else:
                        sinP, cosP = d["sinT"][1 - sp_], d["cosT"][1 - sp_]
                        cD = cs256[:, pair:pair + 1]
                        sD = sn256[:, pair:pair + 1]
                        kb.act_(ang[:, :W], sinP[:, :W], AF.Copy, scale=sD)
                        kb.stt(kb.dve, cosT[:, :W], cosP[:, :W], cD, ang[:, :W], ALU.mult, ALU.subtract)
                        kb.act_(kf[:, :W], cosP[:, :W], AF.Copy, scale=sD)
                        kb.stt(kb.dve, sinT[:, :W], sinP[:, :W], cD, kf[:, :W], ALU.mult, ALU.add)
                    kb.tt(kb.dve, t1[:, :W], pbr, cosT[:, :W], ALU.mult)
                    kb.tt(kb.dve, t2[:, :W], pbi, sinT[:, :W], ALU.mult)
                    kb.tt(kb.pool, bre[:, :W], t1[:, :W], t2[:, :W], ALU.add)
                    kb.tt(kb.dve, t3[:, :W], pbi, cosT[:, :W], ALU.mult)
                    kb.tt(kb.dve, t4[:, :W], pbr, sinT[:, :W], ALU.mult)
                    kb.tt(kb.pool, bim[:, :W], t3[:, :W], t4[:, :W], ALU.subtract)
                    for (g_, b_, cc) in ((gre, bre, 0), (gim, bim, 1)):
                        init = 0.0 if ci == 0 else car.t[:, cc:cc + 1]
                        rd = [rhoT.b, b_.b] + ([] if ci == 0 else [car.b])
                        kb.op(kb.dve, lambda h: h.tensor_tensor_scan(g_.t[:, :W], rhoT.t[:, :W], b_.t[:, :W], init, ALU.mult, ALU.add),
                              reads=rd, writes=[g_.b])
                    kb.cp(kb.act, car[:, 0:1], gre[:, W - 1:W])
                    kb.cp(kb.act, car[:, 1:2], gim[:, W - 1:W])
                    kb.tt(kb.pool, t1[:, :W], gre[:, :W], cosT[:, :W], ALU.mult)
                    kb.tt(kb.pool, t2[:, :W], gim[:, :W], sinT[:, :W], ALU.mult)
                    kb.tt(kb.pool, hre[:, :W], t1[:, :W], t2[:, :W], ALU.subtract)
                    kb.tt(kb.pool, t3[:, :W], gre[:, :W], sinT[:, :W], ALU.mult)
                    kb.tt(kb.pool, t4[:, :W], gim[:, :W], cosT[:, :W], ALU.mult)
                    kb.tt(kb.pool, him[:, :W], t3[:, :W], t4[:, :W], ALU.add)
                    def readout(py=py, Cm=Cm, hre=hre, him=him, W=W, yv=view(yT[half])):
                        kb.mm(py, Cm[0][:], hre[:, :W], start=True, stop=False)
                        kb.mm(py, Cm[1][:], him[:, :W], start=False, stop=True)
                        kb.tt(kb.dve, yv, yv, py, ALU.add)
                    self._s5pend.append(readout)
                    if len(self._s5pend) > 2:
                        self._s5pend.pop(0)()
                    tau0 += W
                    yield
        while self._s5pend:
            self._s5pend.pop(0)()

    def s5_glu(self, li, tl):
        kb = self.kb
        yT, dn = tl["yT"], tl["dn"]
        if "dbg_y" in self.debug:
            dy = self.nc.dram_tensor("dbg_y", [256, T], F32, kind="ExternalOutput").ap()
            for half in range(2):
                kb.st(kb.sp, dy[half * 128:(half + 1) * 128, :], yT[half][:])
        with contextlib.ExitStack() as gl:
            glu = kb.sb("glu", [128, 2, 256], BF16, stack=gl)
            kb.ld(kb.pool, glu[:], self.din["s5_glu"][li].rearrange("(kt p) n -> p kt n", p=128))
            gf = [kb.sb("gf%d" % i, [128, 512], stack=gl) for i in range(2)]
            gb = [kb.sb("gb%d" % i, [128, 512], BF16, stack=gl) for i in range(2)]
            pg = [kb.ps("pg%d" % i, [128, 512], stack=gl) for i in range(2)]
            prs = kb.ps("prs5", [128, 4], stack=gl)
            sg = kb.sb("sg", [128, 512], stack=gl)
            o_ = [kb.sb("s5o%d" % i, [128, 512], stack=gl) for i in range(2)]
            ob = [kb.sb("s5ob%d" % i, [128, 512], BF16, stack=gl) for i in range(2)]
            r4 = kb.sb("s5r4", [128, 8], stack=gl)
            for (t0, W) in TOK_CHUNKS:
                nsub = W // 128
                for half in range(2):
                    kb.act_(gf[half][:, :W], yT[half][:, t0:t0 + W], AF.Gelu)
                    kb.cp(kb.pool, gb[half][:, :W], gf[half][:, :W])
                for mo in range(2):
                    for kt in range(2):
                        kb.mm(pg[mo][:, :W], glu[:, kt, mo * 128:(mo + 1) * 128], gb[kt][:, :W], start=(kt == 0), stop=(kt == 1))
                    kb.act_(sg[:, :W], pg[mo][:, :W], AF.Sigmoid)
                    kb.tt(kb.dve, o_[mo][:, :W], sg[:, :W], gf[mo][:, :W], ALU.mult)
                    kb.act_(ob[mo][:, :W], o_[mo][:, :W], AF.Copy, scale=dn[:, 1, mo:mo + 1])
                    kb.st(kb.sp, self.mixT[256 + mo * 128:256 + (mo + 1) * 128, t0:t0 + W], ob[mo][:, :W])
                    kb.tt(kb.dve, o_[mo][:, :W], o_[mo][:, :W], o_[mo][:, :W], ALU.mult)
                kb.tt(kb.dve, o_[0][:, :W], o_[0][:, :W], o_[1][:, :W], ALU.add)
                for sub in range(nsub):
                    kb.mm(prs[:, sub:sub + 1], o_[0][:, sub * 128:(sub + 1) * 128], self.ones_f[:, 0:1])
                ti0 = t0 // 128
                kb.ts(kb.dve, r4[:, 0:nsub], prs[:, 0:nsub], 1.0 / 256.0, EPS, ALU.mult, ALU.add)
                kb.act_(r4[:, 4:4 + nsub], r4[:, 0:nsub], AF.Sqrt)
                kb.op(kb.dve, lambda h: h.reciprocal(self.r_s5.t[:, ti0:ti0 + nsub], r4.t[:, 4:4 + nsub]),
                      reads=[r4.b], writes=[self.r_s5.b])
            kb.barrier()

    def s5_attn(self, li):
        kb = self.kb
        do_s5 = self.ph("s5")
        do_at = self.ph("attn")
        with contextlib.ExitStack() as st:
            tl = self.s5_alloc(li, st) if do_s5 else None
            gen = self.s5_scan_gen(li, tl) if do_s5 else None
            state = {"n": 0, "done": gen is None}

            def tick():
                if state["done"]:
                    return
                state["n"] += 1
                if state["n"] % self.S5_TICK == 0:
                    try:
                        next(gen)
                    except StopIteration:
                        state["done"] = True

            if do_at:
                self.attn(li, tick)
            if gen is not None:
                for _ in gen:
                    pass
            kb.barrier()
            if do_s5:
                self.s5_glu(li, tl)

    S5_TICK = 7

    def attn(self, li, tick=lambda: None):
        kb = self.kb
        lid = self.layer_ids[li]
        lam_init = 0.8 - 0.6 * math.exp(-0.3 * lid)
        need_ctx = lid < DEPTH - 1
        with contextlib.ExitStack() as ph:
            al = kb.sb("al", [128, 256], stack=ph)
            kb.ld(kb.sp, al[:], self.din["att_l"][li].partition_broadcast(128))
            prod = kb.sb("alp", [128, 128], stack=ph)
            kb.tt(kb.dve, prod[:, 0:64], al[:, 0:64], al[:, 64:128], ALU.mult)
            kb.tt(kb.dve, prod[:, 64:128], al[:, 128:192], al[:, 192:256], ALU.mult)
            s12 = kb.sb("s12", [128, 4], stack=ph)
            for i in range(2):
                kb.op(kb.dve, lambda h: h.reduce_sum(s12.t[:, i:i + 1], prod.t[:, i * 64:(i + 1) * 64], AX.X),
                      reads=[prod.b], writes=[s12.b])
            kb.act_(s12[:, 0:2], s12[:, 0:2], AF.Exp)
            kb.tt(kb.dve, s12[:, 2:3], s12[:, 1:2], s12[:, 0:1], ALU.subtract)
            kb.ts(kb.dve, s12[:, 3:4], s12[:, 2:3], -lam_init, None, ALU.add)
            neglam = s12[:, 3:4]
            gsub = kb.sb("gsub", [128, 128], stack=ph)
            kb.ld(kb.sp, gsub[:], self.din["att_g"][li].partition_broadcast(128))
            kb.ts(kb.dve, gsub[:], gsub[:], 1.0 - lam_init, None, ALU.mult)
            negM = kb.sb("negM", [128, 8], stack=ph)
            kb.tt(kb.dve, negM[:], self.qkmax[:, 0:8], self.qkmax[:, 8:16], ALU.mult)
            kb.act_(negM[:], negM[:], AF.Sqrt)
            kb.ts(kb.dve, negM[:], negM[:], -1.0, None, ALU.mult)
            QTh = [kb.sb("QTh%d" % i, [128, T], BF16, stack=ph) for i in range(1)]
            QrTh = [kb.sb("QrTh%d" % i, [128, LAT], BF16, stack=ph) for i in range(1)]
            KcTh = [kb.sb("KcTh%d" % i, [128, T], BF16, stack=ph) for i in range(1)]
            Vh = [kb.sb("Vh%d" % i, [128, NT, 129], BF16, stack=ph) for i in range(1)]
            pS = [kb.ps("pS%d" % i, [128, 512], stack=ph) for i in range(2)]
            acc = [kb.ps("aacc%d" % i, [128, 512], stack=ph) for i in range(4)]
            ptr = pS[0]
            Pb = [kb.sb("Pb%d" % i, [128, 512], BF16, stack=ph) for i in range(3)]
            om = [kb.sb("om%d" % i, [128, 4, 128], stack=ph) for i in range(2)]
            o_ = kb.sb("ao", [128, 4, 128], stack=ph)
            junk = kb.sb("ajunk", [128, 128], stack=ph)
            ssq = kb.sb("assq", [128, 12], stack=ph)
            rl = kb.sb("arl", [128, 4], stack=ph)
            att = kb.sb("att", [128, 4, 128], stack=ph)
            attT = [kb.sb("attT%d" % i, [128, 512], BF16, stack=ph) for i in range(2)]
            accS = [kb.sb("accS%d" % i, [128, 4, 129], stack=ph) for i in range(2)]
            n = 0
            nchunk = 0
            pend = []
            tk = {"n": 0}

            def tick2(free0):
                tk["n"] += 1
                while pend and pend[0][0] <= tk["n"] and (free0 or not pend[0][2]):
                    pend.pop(0)[1]()
                tick()

            def flush():
                while pend:
                    pend.pop(0)[1]()

            for hh in range(4):
                b_ = 0
                flush()
                kb.ld(kb.sp, QTh[b_][:], self.QT[hh * 128:(hh + 1) * 128, :])
                kb.ld(kb.act, QrTh[b_][:], self.QrT[hh * 128:(hh + 1) * 128, :])
                kb.ld(kb.sp, KcTh[b_][:], self.KcT[hh * 128:(hh + 1) * 128, :])
                kb.ld(kb.act, Vh[b_][:], self.Vaug.rearrange("t p c -> p t c")[:, :, hh * 129:(hh + 1) * 129])
                qchunks = [(CTX + qc * 512, 512, False) for qc in range(8)]
                if need_ctx:
                    qchunks.append((0, CTX, True))
                for (q0, W, isctx) in qchunks:
                    nsub = W // 128
                    kts = [0, 1] if isctx else list(range(NT))
                    for m in range(2):
                        r0 = m * 64

                        def qk(kk):
                            kt = kts[kk]
                            if kt < 2:
                                qv = QTh[b_][r0:r0 + 64, q0:q0 + W]
                            else:
                                qv = QrTh[b_][r0:r0 + 64, q0 - CTX:q0 - CTX + W]
                            kb.mm(pS[(n + kk) % 2][:, :W], KcTh[b_][r0:r0 + 64, kt * 128:(kt + 1) * 128], qv)
                        qk(0)
                        for kk, kt in enumerate(kts):
                            if kk + 1 < len(kts):
                                qk(kk + 1)
                            p = pS[(n + kk) % 2]
                            pb_ = Pb[(n + kk) % 3]
                            kb.act_(pb_[:, :W], p[:, :W], AF.Exp, bias=negM[:, hh * 2 + m:hh * 2 + m + 1])
                            tick2((n + kk + 1) % 2 == 1 or kk + 1 >= len(kts))
                            for sub in range(nsub):
                                kb.mm(acc[sub][:, 0:129], pb_[:, sub * 128:(sub + 1) * 128], Vh[b_][:, kt, :],
                                      start=(kk == 0), stop=(kk == len(kts) - 1))
                        n += len(kts)
                        flush()
                        for sub in range(nsub):
                            kb.cp(kb.act, accS[m][:, sub, :], acc[sub][:, 0:129])

                        def fin_m(m=m, nsub=nsub):
                            for sub in range(nsub):
                                kb.op(kb.dve, lambda h: h.reciprocal(rl.t[:, sub:sub + 1], accS[m].t[:, sub, 128:129]),
                                      reads=[accS[m].b], writes=[rl.b])
                                kb.ts(kb.dve, om[m][:, sub, :], accS[m][:, sub, 0:128], rl[:, sub:sub + 1], None, ALU.mult)
                        pend.append([tk["n"] + 8, fin_m, False])

                    def fin_chunk(nsub=nsub, W=W, q0=q0, hh=hh):
                        nonlocal nchunk
                        for sub in range(nsub):
                            kb.stt(kb.dve, o_[:, sub, :], om[1][:, sub, :], neglam, om[0][:, sub, :], ALU.mult, ALU.add)
                            kb.act_(junk[:], o_[:, sub, :], AF.Square, accum=ssq[:, sub:sub + 1])
                        kb.ts(kb.dve, ssq[:, 4:4 + nsub], ssq[:, 0:nsub], 1.0 / 128.0, EPS, ALU.mult, ALU.add)
                        kb.act_(ssq[:, 8:8 + nsub], ssq[:, 4:4 + nsub], AF.Sqrt)
                        kb.op(kb.dve, lambda h: h.reciprocal(ssq.t[:, 4:4 + nsub], ssq.t[:, 8:8 + nsub]), reads=[ssq.b], writes=[ssq.b])
                        for sub in range(nsub):
                            kb.stt(kb.dve, att[:, sub, :], o_[:, sub, :], ssq[:, 4 + sub:5 + sub], gsub[:], ALU.mult, ALU.mult)
                            kb.tr(ptr[:, sub * 128:(sub + 1) * 128], att[:, sub, :], self.ident_f[:])
                        at = attT[nchunk % 2]
                        nchunk += 1
                        kb.cp(kb.act, at[:, :W], ptr[:, :W])
                        kb.st(kb.sp, self.mixT[512 + hh * 128:512 + (hh + 1) * 128, q0:q0 + W], at[:, :W])
                    pend.append([tk["n"] + 16, fin_chunk, True])
            flush()
            kb.barrier()

    def outproj(self, li):
        kb = self.kb
        lid = self.layer_ids[li]
        need_ctx = lid < DEPTH - 1
        src = self.xsrc(li)
        with contextlib.ExitStack() as ph:
            wo = kb.sb("wo", [128, 8, D], BF16, stack=ph)
            for kt in range(8):
                kb.ld(kb.pool, wo[:, kt, :], self.din["w_out"][li][kt * 128:(kt + 1) * 128, :])
            g1b = [self.load_mod_b(li, which, 2, ph, "g1b%d" % which) for which in range(2)]
            mx = [kb.sb("mx%d" % i, [128, 8, 512], BF16, stack=ph) for i in range(2)]
            xt = [kb.sb("oxt%d" % i, [128, D], stack=ph) for i in range(2)]
            tt_ = [kb.sb("ott%d" % i, [128, D], stack=ph) for i in range(2)]
            pA = kb.ps("pA", [128, D], stack=ph)
            pB = kb.ps("pB", [128, D], stack=ph)
            pC = kb.ps("pC", [128, D], stack=ph)
            n = 0
            for ci, (t0, W) in enumerate(TOK_CHUNKS):
                if ci == 0 and not need_ctx:
                    continue
                which = 1 if ci == 0 else 0
                m_ = mx[ci % 2]
                kb.ld(kb.sp, m_[:, :, :W], self.mixT[:, t0:t0 + W].rearrange("(kt p) t -> p kt t", p=128))
                for ti in range(W // 128):
                    gi = t0 // 128 + ti
                    x = xt[n % 2]
                    t = tt_[n % 2]
                    n += 1
                    kb.ld(kb.act, x[:], src[gi * 128:(gi + 1) * 128, :])
                    for (ps_, kts) in ((pA, (0, 1)), (pB, (2, 3)), (pC, (4, 5, 6, 7))):
                        for hf in range(2):
                            for kt in kts:
                                kb.mm(ps_[:, hf * 512:(hf + 1) * 512], m_[:, kt, ti * 128:(ti + 1) * 128], wo[:, kt, hf * 512:(hf + 1) * 512],
                                      start=(kt == kts[0]), stop=(kt == kts[-1]))
                    kb.ts(kb.dve, t[:], pA[:], self.r_hy[:, gi:gi + 1], None, ALU.mult)
                    kb.stt(kb.dve, t[:], pB[:], self.r_s5[:, gi:gi + 1], t[:], ALU.mult, ALU.add)
                    kb.tt(kb.dve, t[:], t[:], pC[:], ALU.add)
                    kb.tt(kb.pool, t[:], t[:], g1b[which][:], ALU.mult)
                    kb.tt(kb.pool, t[:], t[:], x[:], ALU.add)
                    kb.st(kb.sp, self.Xres[gi * 128:(gi + 1) * 128, :], t[:])
            kb.barrier()

    def moe(self, li):
        kb = self.kb
        nc = self.nc
        lid = self.layer_ids[li]
        need_ctx = lid < DEPTH - 1
        tiles = list(range(NT)) if need_ctx else list(range(2, NT))
        NB = NBLK
        with contextlib.ExitStack() as ph:
            eid = kb.sb("eid", [128, NT, 2], stack=ph)
            gate = kb.sb("gate", [128, NT, 2], stack=ph)
            rsel = kb.sb("rsel", [128, NT, 2], stack=ph)
            dest_i = kb.sb("dest_i", [128, NT, 2], I32, stack=ph)
            cnt_b = kb.sb("cnt_b", [128, 32], stack=ph)
            widx = kb.sb("widx", [128, NB], I32, stack=ph)
            kb.memset(kb.dve, cnt_b[:], 0.0)
            e_iota = kb.sb("e_iota", [128, 32], stack=ph)
            kb.op(kb.pool, lambda h: h.iota(e_iota.t[:], [[1, 32]], base=0, channel_multiplier=0, allow_small_or_imprecise_dtypes=True),
                  writes=[e_iota.b])
            g2b = [self.load_mod_b(li, which, 5, ph, "g2b%d" % which) for which in range(2)]
            bslot = Buf()
            bHs = Buf()
            with contextlib.ExitStack() as sa:
                ab = self.norm_mod_tiles(li, 4 + lid, 3, 4, sa)
                wr = kb.sb("wr", [128, 8, 36], stack=sa)
                kb.ld(kb.sp, wr[:], self.din["moe_wr"][li].rearrange("(kt p) n -> p kt n", p=128))
                brb = kb.sb("brb", [128, 36], stack=sa)
                kb.ld(kb.sp, brb[:], self.din["moe_br"][li].partition_broadcast(128))
                t_lo = tiles[0]
                n_ = NT - t_lo
                lg_all = kb.sb("lg_all", [128, NT, 36], stack=sa)
                zrow = kb.sb("zrow", [1, D], BF16, stack=sa)
                kb.memset(kb.dve, zrow[:], 0.0)
                kb.st(kb.sp, self.Hs[T:T + 1, :], zrow[:], writes=[bHs])
                with contextlib.ExitStack() as p1:
                    xt = [kb.sb("mxt%d" % i, [128, D], stack=p1) for i in range(2)]
                    hn = [kb.sb("mhn%d" % i, [128, D], stack=p1) for i in range(2)]
                    hb = [kb.sb("mhb%d" % i, [128, D], BF16, stack=p1) for i in range(2)]
                    junk = kb.sb("mjunk", [128, D], stack=p1)
                    ss = [kb.sb("mss%d" % i, [128, 4], stack=p1) for i in range(2)]
                    hT = [kb.sb("mhT%d" % i, [128, 8, 128], stack=p1) for i in range(2)]
                    trp = [kb.ps("mtrp%d" % i, [128, D], stack=p1) for i in range(2)]
                    pl = [kb.ps("mpl%d" % i, [128, 64], stack=p1) for i in range(2)]
                    for n, ti in enumerate(tiles):
                        which = 1 if ti < 2 else 0
                        x = xt[n % 2]
                        kb.ld(kb.sp, x[:], self.Xres[ti * 128:(ti + 1) * 128, :])
                        self.norm_tile(x, ab[which], hn[n % 2], ss[n % 2], junk)
                        h16 = hb[n % 2]
                        kb.cp(kb.act, h16[:], hn[n % 2][:])
                        kb.st(kb.sp, self.Hs[ti * 128:(ti + 1) * 128, :], h16[:], writes=[bHs])
                        for kt in range(8):
                            kb.tr(trp[n % 2][:, kt * 128:(kt + 1) * 128], hn[n % 2][:, kt * 128:(kt + 1) * 128], self.ident_f[:])
                        kb.cp(kb.pool if False else kb.dve, V(hT[n % 2].t[:].rearrange("p k t -> p (k t)"), hT[n % 2].b), trp[n % 2][:])
                        for kt in range(8):
                            kb.mm(pl[n % 2][:, 0:36], hT[n % 2][:, kt, :], wr[:, kt, :], start=(kt == 0), stop=(kt == 7))
                        kb.tt(kb.dve, lg_all[:, ti, :], pl[n % 2][:, 0:36], brb[:], ALU.add)
                    kb.barrier()
                TS = slice(t_lo, NT)
                gmax = kb.sb("gmax", [128, NT, 1], stack=sa)
                ohg = kb.sb("ohg", [128, NT, 4], stack=sa)
                gidx = kb.sb("gidx", [128, NT, 1], stack=sa)
                E4 = kb.sb("E4", [128, NT, 4], stack=sa)
                sg = kb.sb("sg", [128, NT, 1], stack=sa)
                pgr = kb.sb("pgr", [128, NT, 1], stack=sa)
                esel = kb.sb("esel", [128, NT, 8], stack=sa)
                etmp = kb.sb("etmp", [128, NT, 8], stack=sa)
                mx8 = kb.sb("mx8", [128, NT, 8], stack=sa)
                ix8 = kb.sb("ix8", [128, NT, 8], U32, stack=sa)
                i12 = kb.sb("i12", [128, NT, 2], stack=sa)
                sm = kb.sb("msm", [128, NT, 4], stack=sa)
                oh = [kb.sb("oh%d" % i, [128, NT, 32], stack=sa) for i in range(2)]
                ohs = kb.sb("ohs", [128, NT, 32], stack=sa)
                cnt_all = kb.sb("cnt_all", [128, NT, 32], stack=sa)
                rk = kb.sb("rk", [128, NT, 32], stack=sa)
                e_io3 = kb.sb("e_io3", [128, 1, 32], stack=sa)
                kb.cp(kb.dve, e_io3[:, 0, :], e_iota[:])

                def B(v, shape):
                    return V(v.ap.to_broadcast(list(shape)), v.buf)

                kb.op(kb.dve, lambda h: h.tensor_reduce(gmax.t[:, TS, 0], lg_all.t[:, TS, 0:4], AX.X, ALU.max), reads=[lg_all.b], writes=[gmax.b])
                kb.tt(kb.dve, ohg[:, TS, :], lg_all[:, TS, 0:4], B(gmax[:, TS, 0:1], (128, n_, 4)), ALU.is_equal)
                kb.ts(kb.dve, gidx[:, TS, :], ohg[:, TS, 3:4], 3.0, None, ALU.mult)
                kb.stt(kb.dve, gidx[:, TS, :], ohg[:, TS, 2:3], 2.0, gidx[:, TS, :], ALU.mult, ALU.add)
                kb.tt(kb.dve, gidx[:, TS, :], gidx[:, TS, :], ohg[:, TS, 1:2], ALU.add)
                kb.tt(kb.dve, E4[:, TS, :], lg_all[:, TS, 0:4], B(gmax[:, TS, 0:1], (128, n_, 4)), ALU.subtract)
                kb.act_(E4[:, TS, :], E4[:, TS, :], AF.Exp)
                kb.op(kb.dve, lambda h: h.tensor_reduce(sg.t[:, TS, 0], E4.t[:, TS, :], AX.X, ALU.add), reads=[E4.b], writes=[sg.b])
                kb.op(kb.dve, lambda h: h.reciprocal(pgr.t[:, TS, :], sg.t[:, TS, :]), reads=[sg.b], writes=[pgr.b])
                for g in range(4):
                    dst = esel if g == 0 else etmp
                    kb.tt(kb.dve, dst[:, TS, :], lg_all[:, TS, 4 + 8 * g:12 + 8 * g], B(ohg[:, TS, g:g + 1], (128, n_, 8)), ALU.mult)
                    if g > 0:
                        kb.tt(kb.dve, esel[:, TS, :], esel[:, TS, :], etmp[:, TS, :], ALU.add)
                for ti in tiles:
                    kb.op(kb.dve, lambda h: h.max(mx8.t[:, ti, :], esel.t[:, ti, :]), reads=[esel.b], writes=[mx8.b])
                for ti in tiles:
                    kb.op(kb.dve, lambda h: h.max_index(ix8.t[:, ti, :], mx8.t[:, ti, :], esel.t[:, ti, :]), reads=[esel.b, mx8.b], writes=[ix8.b])
                kb.cp(kb.dve, i12[:, TS, :], ix8[:, TS, 0:2])
                kb.stt(kb.dve, eid[:, TS, :], B(gidx[:, TS, 0:1], (128, n_, 2)), 8.0, i12[:, TS, :], ALU.mult, ALU.add)
                kb.tt(kb.dve, sm[:, TS, 0:1], mx8[:, TS, 1:2], mx8[:, TS, 0:1], ALU.subtract)
                kb.act_(sm[:, TS, 1:2], sm[:, TS, 0:1], AF.Exp)
                kb.ts(kb.dve, sm[:, TS, 2:3], sm[:, TS, 1:2], 1.0, None, ALU.add)
                kb.op(kb.dve, lambda h: h.reciprocal(sm.t[:, TS, 3:4], sm.t[:, TS, 2:3]), reads=[sm.b], writes=[sm.b])
                kb.tt(kb.dve, gate[:, TS, 0:1], sm[:, TS, 3:4], pgr[:, TS, :], ALU.mult)
                kb.tt(kb.dve, gate[:, TS, 1:2], gate[:, TS, 0:1], sm[:, TS, 1:2], ALU.mult)
                for k in range(2):
                    kb.tt(kb.dve, oh[k][:, TS, :], B(e_io3[:, 0:1, :], (128, n_, 32)), B(eid[:, TS, k:k + 1], (128, n_, 32)), ALU.is_equal)
                kb.tt(kb.dve, ohs[:, TS, :], oh[0][:, TS, :], oh[1][:, TS, :], ALU.add)
                with contextlib.ExitStack() as p2:
                    pr = kb.ps("mpr", [128, 3, 512], stack=p2)
                    pc = kb.ps("mpc", [128, 3, 512], stack=p2)
                    ncols = n_ * 32
                    ohs_f = ohs.t[:, TS, :].rearrange("p t e -> p (t e)")
                    for c3 in range(3):
                        a = c3 * 512
                        b = min(ncols, a + 512)
                        if a >= b:
                            break
                        kb.mm(pr[:, c3, 0:b - a], self.tri[:], V(ohs_f[:, a:b], ohs.b))
                        kb.mm(pc[:, c3, 0:b - a], self.ones_f[:], V(ohs_f[:, a:b], ohs.b))
                    pr_f = pr.t[:].rearrange("p c n -> p (c n)")
                    pc_f = pc.t[:].rearrange("p c n -> p (c n)")
                    kb.memset(kb.dve, cnt_all[:, t_lo, :], 0.0)
                    for ti in range(t_lo + 1, NT):
                        o0 = (ti - 1 - t_lo) * 32
                        kb.tt(kb.dve, cnt_all[:, ti, :], cnt_all[:, ti - 1, :], V(pc_f[:, o0:o0 + 32], pc.b), ALU.add)
                    o0 = (NT - 1 - t_lo) * 32
                    kb.tt(kb.dve, cnt_b[:], cnt_all[:, NT - 1, :], V(pc_f[:, o0:o0 + 32], pc.b), ALU.add)
                    kb.tt(kb.dve, V(rk.t[:, TS, :].rearrange("p t e -> p (t e)"), rk.b), V(cnt_all.t[:, TS, :].rearrange("p t e -> p (t e)"), cnt_all.b),
                          V(pr_f[:, 0:ncols], pr.b), ALU.add)
                    kb.barrier()
                for k in range(2):
                    kb.tt(kb.dve, ohs[:, TS, :], oh[k][:, TS, :], rk[:, TS, :], ALU.mult)
                    kb.op(kb.dve, lambda h: h.tensor_reduce(rsel.t[:, TS, k], ohs.t[:, TS, :], AX.X, ALU.add), reads=[ohs.b], writes=[rsel.b])
                pad = kb.sb("pad", [128, 32], stack=sa)
                pend = kb.sb("pend", [128, 32], stack=sa)
                pstart = kb.sb("pstart", [128, 1, 32], stack=sa)
                padi = kb.sb("padi", [128, 32], I32, stack=sa)
                kb.ts(kb.dve, pad[:], cnt_b[:], float(NSLOT_BLK - 1), 1.0 / NSLOT_BLK, ALU.add, ALU.mult)
                kb.ts(kb.dve, padi[:], pad[:], -(0.5 - 1.0 / 512.0), None, ALU.add)
                kb.cp(kb.dve, pad[:], padi[:])
                kb.ts(kb.dve, pad[:], pad[:], float(NSLOT_BLK), None, ALU.mult)
                kb.op(kb.dve, lambda h: h.tensor_tensor_scan(pend.t[:], self.ones_f.t[:, 0:32], pad.t[:], 0.0, ALU.mult, ALU.add),
                      reads=[pad.b, self.ones_f.b], writes=[pend.b])
                kb.tt(kb.dve, pstart[:, 0, :], pend[:], pad[:], ALU.subtract)
                dsf = kb.sb("dsf", [128, NT, 2], stack=sa)
                for k in range(2):
                    kb.tt(kb.dve, ohs[:, TS, :], oh[k][:, TS, :], B(pstart[:, 0:1, :], (128, n_, 32)), ALU.mult)
                    kb.op(kb.dve, lambda h: h.tensor_reduce(dsf.t[:, TS, k], ohs.t[:, TS, :], AX.X, ALU.add), reads=[ohs.b], writes=[dsf.b])
                kb.tt(kb.dve, dsf[:, TS, :], dsf[:, TS, :], rsel[:, TS, :], ALU.add)
                kb.cp(kb.dve, dest_i[:, TS, :], dsf[:, TS, :])
                NSC = NPAD // 128
                inif = kb.sb("inif", [128, NSC], stack=sa)
                kb.memset(kb.dve, inif[:], float(T))
                inii = kb.sb("inii", [128, NSC], I32, stack=sa)
                kb.cp(kb.dve, inii[:], inif[:])
                kb.st(kb.sp, self.slot_tok.rearrange("(p a) o -> p (a o)", p=128), inii[:], writes=[bslot])
                tokf = kb.sb("tokf", [128, NT], stack=sa)
                kb.op(kb.pool, lambda h: h.iota(tokf.t[:], [[128, NT]], base=0, channel_multiplier=1, allow_small_or_imprecise_dtypes=True),
                      writes=[tokf.b])
                toki = kb.sb("toki", [128, NT], I32, stack=sa)
                kb.cp(kb.dve, toki[:], tokf[:])
                for ti in tiles:
                    for k in range(2):
                        kb.dma(kb.pool, lambda h: h.indirect_dma_start(
                            out=self.slot_tok, out_offset=bass.IndirectOffsetOnAxis(ap=dest_i.t[:, ti, k:k + 1], axis=0),
                            in_=toki.t[:, ti:ti + 1], in_offset=None), reads=[dest_i.b, toki.b, bslot], writes=[bslot])
                jb = kb.sb("jb", [128, NB], stack=sa)
                kb.op(kb.pool, lambda h: h.iota(jb.t[:], [[NSLOT_BLK, NB]], base=0, channel_multiplier=0, allow_small_or_imprecise_dtypes=True),
                      writes=[jb.b])
                be = kb.sb("be", [128, NB], stack=sa)
                cmp_ = kb.sb("cmp", [128, NB], stack=sa)
                kb.memset(kb.dve, be[:], 0.0)
                for e in range(32):
                    kb.ts(kb.dve, cmp_[:], jb[:], pend[:, e:e + 1], None, ALU.is_ge)
                    kb.tt(kb.dve, be[:], be[:], cmp_[:], ALU.add)
                kb.ts(kb.dve, be[:], be[:], 31.0, 128.0, ALU.min, ALU.mult)
                pcol = kb.sb("pcol", [128, 1], stack=sa)
                kb.op(kb.pool, lambda h: h.iota(pcol.t[:], [[1, 1]], base=0, channel_multiplier=1, allow_small_or_imprecise_dtypes=True),
                      writes=[pcol.b])
                kb.ts(kb.dve, be[:], be[:], pcol[:], None, ALU.add)
                kb.cp(kb.dve, widx[:], be[:])
                kb.barrier()
            with contextlib.ExitStack() as sb_:
                W13 = [kb.sb("W13_%d" % i, [128, 2, 8, 512], BF16, stack=sb_) for i in range(2)]
                W2 = [kb.sb("W2_%d" % i, [128, 4, D], BF16, stack=sb_) for i in range(2)]
                sti = [kb.sb("sti%d" % i, [128, 2], I32, stack=sb_) for i in range(2)]
                X = [kb.sb("mX%d" % i, [128, 2, D], BF16, stack=sb_) for i in range(2)]
                XT = kb.sb("mXT", [128, 8, 256], BF16, stack=sb_)
                ptx = [kb.ps("ptx%d" % i, [128, D], BF16, stack=sb_) for i in range(2)]
                pa = kb.ps("mpa", [128, 256], stack=sb_)
                pb = kb.ps("mpb", [128, 256], stack=sb_)
                pd = kb.ps("mpd", [128, D], stack=sb_)
                sa_ = kb.sb("msa", [128, 256], stack=sb_)
                hid = kb.sb("hid", [128, 4, 256], BF16, stack=sb_)
                yo = [kb.sb("yo%d" % i, [128, D], stack=sb_) for i in range(2)]
                w13src = self.din["moe_w13h_%d" % li]
                w2src = self.din["moe_w2h_%d" % li]
                for j in range(NB):
                    w13 = W13[j % 2]
                    w2 = W2[j % 2]
                    kb.dma(kb.pool, lambda h: h.indirect_dma_start(
                        out=w13.t[:].rearrange("p a k f -> p (a k f)"), out_offset=None, in_=w13src,
                        in_offset=bass.IndirectOffsetOnAxis(ap=widx.t[:, j:j + 1], axis=0)), reads=[widx.b], writes=[w13.b])
                    kb.dma(kb.pool, lambda h: h.indirect_dma_start(
                        out=w2.t[:].rearrange("p k d -> p (k d)"), out_offset=None, in_=w2src,
                        in_offset=bass.IndirectOffsetOnAxis(ap=widx.t[:, j:j + 1], axis=0)), reads=[widx.b], writes=[w2.b])
                    si = sti[j % 2]
                    kb.dma(kb.sp, lambda h: h.dma_start(out=si.t[:], in_=self.slot_tok[j * 256:(j + 1) * 256, :].rearrange("(s p) o -> p (s o)", p=128),
                                                         allow_slow_non_contiguous=True),
                           reads=[bslot], writes=[si.b])
                    x_ = X[j % 2]
                    for s_ in range(2):
                        kb.dma(kb.pool, lambda h: h.indirect_dma_start(
                            out=x_.t[:, s_, :], out_offset=None, in_=self.Hs,
                            in_offset=bass.IndirectOffsetOnAxis(ap=si.t[:, s_:s_ + 1], axis=0)), reads=[si.b, bHs], writes=[x_.b])
                    for s_ in range(2):
                        p = ptx[s_]
                        for kt in range(8):
                            kb.tr(p[:, kt * 128:(kt + 1) * 128], x_[:, s_, kt * 128:(kt + 1) * 128], self.ident_b[:])
                        kb.cp(kb.act if s_ else kb.dve, XT[:, :, s_ * 128:(s_ + 1) * 128], V(p.t[:].rearrange("p (k t) -> p k t", k=8), p.b))
                    for ft in range(4):
                        for kt in range(8):
                            kb.mm(pa[:], w13[:, 0, kt, ft * 128:(ft + 1) * 128], XT[:, kt, :], start=(kt == 0), stop=(kt == 7))
                        for kt in range(8):
                            kb.mm(pb[:], w13[:, 1, kt, ft * 128:(ft + 1) * 128], XT[:, kt, :], start=(kt == 0), stop=(kt == 7))
                        kb.act_(sa_[:], pa[:], AF.Silu)
                        kb.tt(kb.dve, hid[:, ft, :], sa_[:], pb[:], ALU.mult)
                    for s_ in range(2):
                        for hf in range(2):
                            for ft in range(4):
                                kb.mm(pd[:, hf * 512:(hf + 1) * 512], hid[:, ft, s_ * 128:(s_ + 1) * 128], w2[:, ft, hf * 512:(hf + 1) * 512],
                                      start=(ft == 0), stop=(ft == 3))
                        y_ = yo[s_]
                        kb.cp(kb.act if s_ else kb.dve, y_[:], pd[:])
                        kb.st(kb.sp, self.yb[j * 256 + s_ * 128:j * 256 + (s_ + 1) * 128, :], y_[:])
                kb.barrier()
            with contextlib.ExitStack() as sc_:
                Y = [[kb.sb("mY%d%d" % (i, k), [128, D], stack=sc_) for k in range(2)] for i in range(2)]
                xt = [kb.sb("cxt%d" % i, [128, D], stack=sc_) for i in range(2)]
                t_ = [kb.sb("ct%d" % i, [128, D], stack=sc_) for i in range(2)]
                for n, ti in enumerate(tiles):
                    which = 1 if ti < 2 else 0
                    x = xt[n % 2]
                    kb.ld(kb.sp, x[:], self.Xres[ti * 128:(ti + 1) * 128, :])
                    for k in range(2):
                        y_ = Y[n % 2][k]
                        kb.dma(kb.pool, lambda h: h.indirect_dma_start(
                            out=y_.t[:], out_offset=None, in_=self.yb,
                            in_offset=bass.IndirectOffsetOnAxis(ap=dest_i.t[:, ti, k:k + 1], axis=0)), reads=[dest_i.b], writes=[y_.b])
                    t = t_[n % 2]
                    kb.ts(kb.dve, t[:], Y[n % 2][0][:], gate[:, ti, 0:1], None, ALU.mult)
                    kb.stt(kb.dve, t[:], Y[n % 2][1][:], gate[:, ti, 1:2], t[:], ALU.mult, ALU.add)
                    kb.tt(kb.pool, t[:], t[:], g2b[which][:], ALU.mult)
                    kb.tt(kb.pool, t[:], t[:], x[:], ALU.add)
                    kb.st(kb.sp, self.Xres[ti * 128:(ti + 1) * 128, :], t[:])
                kb.barrier()

    def final_norm(self):
        kb = self.kb
        with contextlib.ExitStack() as ph:
            gb = kb.sb("fgb", [128, D], stack=ph)
            kb.ld(kb.sp, gb[:], self.din["gn_rows"][8].partition_broadcast(128))
            xt = [kb.sb("fxt%d" % i, [128, D], stack=ph) for i in range(2)]
            xn = [kb.sb("fxn%d" % i, [128, D], stack=ph) for i in range(2)]
            junk = kb.sb("fjunk", [128, D], stack=ph)
            ss = kb.sb("fss", [128, 4], stack=ph)
            for n in range(LAT // 128):
                x = xt[n % 2]
                o = xn[n % 2]
                kb.ld(kb.sp, x[:], self.Xres[CTX + n * 128:CTX + (n + 1) * 128, :])
                kb.act_(junk[:], x[:], AF.Square, accum=ss[:, 0:1])
                kb.ts(kb.dve, ss[:, 1:2], ss[:, 0:1], 1.0 / D, EPS, ALU.mult, ALU.add)
                kb.act_(ss[:, 2:3], ss[:, 1:2], AF.Sqrt)
                kb.op(kb.dve, lambda h: h.reciprocal(ss.t[:, 3:4], ss.t[:, 2:3]), reads=[ss.b], writes=[ss.b])
                kb.stt(kb.dve, o[:], x[:], ss[:, 3:4], gb[:], ALU.mult, ALU.mult)
                kb.st(kb.sp, self.out[n * 128:(n + 1) * 128, :], o[:])
            kb.barrier()


LAYERED = ("w_mod", "b_mod", "w_in", "w_out", "hy_cw", "hy_cb", "hy_skip", "hy_ng", "hy_w1", "hy_w2", "hy_w3", "hy_bf",
           "s5_row", "s5_col", "s5_bT", "s5_cT", "s5_dn", "s5_glu", "att_l", "att_g", "moe_wr", "moe_br", "moe_w13h", "moe_w2h")


def split_layers(a):
    for k in ("moe_w13h", "moe_w2h"):
        v = a.pop(k)
        for i in range(v.shape[0]):
            a["%s_%d" % (k, i)] = v[i]
    return a


def make_core_arrays(common, core, layer_ids):
    a = {}
    for k, v in common.items():
        if k in LAYERED:
            a[k] = np.ascontiguousarray(v[list(layer_ids)])
        else:
            a[k] = v
    a.update(core)
    return split_layers(a)


def kernel(**inputs):
    common = split_layers(host_common(inputs))
    arrs = []
    for b in range(8):
        a = dict(common)
        a.update(host_core(inputs, b))
        arrs.append(a)
    p = Prog(arrs[0], list(range(DEPTH)))
    nc = p.build()
    res = run_bass_kernel_spmd(nc, arrs, core_ids=list(range(8)))
    out = np.stack([np.asarray(res.results[b]["out"]) for b in range(8)], axis=0)
    return out.astype(np.float32)
```

```python
import contextlib
import math
import numpy as np
import ml_dtypes
import concourse.bass as bass
import concourse.mybir as mybir
from concourse.bass_utils import run_bass_kernel_spmd

F32 = mybir.dt.float32
BF16 = mybir.dt.bfloat16
I32 = mybir.dt.int32
U32 = mybir.dt.uint32
AF = mybir.ActivationFunctionType
ALU = mybir.AluOpType
AX = mybir.AxisListType

DEPTH = 4
D = 1024
LAT = 4096
CTX = 256
T = LAT + CTX
NT = T // 128
EPS = 1e-6
TWO_PI = 2.0 * math.pi
NSLOT_BLK = 256
NBLK = (2 * T) // NSLOT_BLK + 32
NPAD = NBLK * NSLOT_BLK


class Buf:
    __slots__ = ("writers", "readers")

    def __init__(self):
        self.writers = {}
        self.readers = {}


class V:
    __slots__ = ("ap", "buf")

    def __init__(self, ap, buf):
        self.ap = ap
        self.buf = buf


class Tile:
    def __init__(self, t, buf=None):
        self.t = t
        self.b = buf or Buf()

    def __getitem__(self, idx):
        return V(self.t[idx], self.b)

    def v(self, ap):
        return V(ap, self.b)


class Eng:
    def __init__(self, name, h, is_pe=False):
        self.name = name
        self.h = h
        self.sem = None
        self.count = 0
        self.seen = {}
        self.is_pe = is_pe
        self.dq = []
        self.dqi = 0


EPOCH = 16000
NDQ = 6


class KB:
    def __init__(self, nc, stack):
        self.nc = nc
        self.stack = stack
        self.pe = Eng("pe", nc.tensor, True)
        self.act = Eng("act", nc.scalar)
        self.dve = Eng("dve", nc.vector)
        self.pool = Eng("pool", nc.gpsimd)
        self.sp = Eng("sp", nc.sync)
        self.nsem = 0
        self.ninst = 0
        self.uid = 0
        for e in (self.pe, self.act, self.dve, self.pool):
            e.sem = self.newsem()
        for e in (self.sp, self.act, self.pool):
            e.dq = [[self.newsem(), 0] for _ in range(NDQ)]
        self.engs = (self.pe, self.act, self.dve, self.pool, self.sp)

    def newsem(self):
        self.nsem += 1
        return self.stack.enter_context(self.nc.semaphore("s%d" % self.nsem))

    def name(self, n):
        self.uid += 1
        return "%s_%d" % (n, self.uid)

    def sb(self, name, shape, dt=F32, stack=None):
        st = stack or self.stack
        return Tile(st.enter_context(self.nc.sbuf_tensor(self.name(name), list(shape), dt)))

    def ps(self, name, shape, dt=F32, stack=None):
        st = stack or self.stack
        return Tile(st.enter_context(self.nc.psum_tensor(self.name(name), list(shape), dt)))

    def _deps(self, reads, writes):
        deps = {}
        for b in reads:
            for s, v in b.writers.items():
                if deps.get(s, 0) < v:
                    deps[s] = v
        for b in writes:
            for d in (b.writers, b.readers):
                for s, v in d.items():
                    if deps.get(s, 0) < v:
                        deps[s] = v
        return deps

    def _wait(self, eng, deps):
        for s, v in deps.items():
            if eng.is_pe and s is eng.sem:
                continue
            if eng.seen.get(s, 0) < v:
                eng.h.wait_ge(s, v)
                eng.seen[s] = v

    def _mark(self, tok, reads, writes):
        s, v = tok
        for b in reads:
            if b.readers.get(s, 0) < v:
                b.readers[s] = v
        for b in writes:
            b.writers = {s: v}
            b.readers = {}

    def op(self, eng, fn, reads=(), writes=()):
        reads = [r.buf if isinstance(r, V) else r for r in reads]
        writes = [w.buf if isinstance(w, V) else w for w in writes]
        self._wait(eng, self._deps(reads, writes))
        if eng.count >= EPOCH:
            eng.sem = self.newsem()
            eng.count = 0
        inst = fn(eng.h)
        eng.count += 1
        inst.then_inc(eng.sem, 1)
        self.ninst += 1
        self._mark((eng.sem, eng.count), reads, writes)
        return inst

    def dma(self, eng, fn, reads=(), writes=()):
        reads = [r.buf if isinstance(r, V) else r for r in reads]
        writes = [w.buf if isinstance(w, V) else w for w in writes]
        self._wait(eng, self._deps(reads, writes))
        slot = eng.dq[eng.dqi % NDQ]
        eng.dqi += 1
        if slot[1] >= 30000:
            slot[0] = self.newsem()
            slot[1] = 0
        s, v = slot
        if v > 0 and eng.seen.get(s, 0) < v:
            eng.h.wait_ge(s, v)
            eng.seen[s] = v
        inst = fn(eng.h)
        inst.then_inc(s, 16)
        slot[1] = v + 16
        self.ninst += 1
        self._mark((s, v + 16), reads, writes)
        return inst

    def barrier(self):
        toks = {}
        for e in (self.pe, self.act, self.dve, self.pool):
            if e.count > 0:
                toks[e.sem] = e.count
        for e in (self.sp, self.act, self.pool):
            for s, v in e.dq:
                if v > 0:
                    toks[s] = v
        for e in self.engs:
            for s, v in toks.items():
                if s is e.sem:
                    continue
                if e.seen.get(s, 0) < v:
                    e.h.wait_ge(s, v)
                    e.seen[s] = v

    def mm(self, o, lhsT, rhs, start=True, stop=True):
        return self.op(self.pe, lambda h: h.matmul(o.ap, lhsT.ap, rhs.ap, start=start, stop=stop),
                       reads=[lhsT, rhs], writes=[o])

    def tr(self, o, i, ident):
        return self.op(self.pe, lambda h: h.transpose(o.ap, i.ap, ident.ap), reads=[i, ident], writes=[o])

    def act_(self, o, i, func, bias=None, scale=None, accum=None, eng=None):
        reads = [i]
        kw = {}
        if bias is not None:
            if isinstance(bias, V):
                reads.append(bias)
                kw["bias"] = bias.ap
            else:
                kw["bias"] = bias
        if scale is not None:
            if isinstance(scale, V):
                reads.append(scale)
                kw["scale"] = scale.ap
            else:
                kw["scale"] = scale
        writes = [o]
        if accum is not None:
            kw["accum_out"] = accum.ap
            writes.append(accum)
        return self.op(self.act, lambda h: h.activation(out=o.ap, in_=i.ap, func=func, **kw), reads=reads, writes=writes)

    def tt(self, eng, o, a, b, op):
        return self.op(eng, lambda h: h.tensor_tensor(o.ap, a.ap, b.ap, op), reads=[a, b], writes=[o])

    def ts(self, eng, o, a, s1, s2, op0, op1=None):
        reads = [a]
        x1 = s1.ap if isinstance(s1, V) else s1
        x2 = s2.ap if isinstance(s2, V) else s2
        if isinstance(s1, V):
            reads.append(s1)
        if isinstance(s2, V):
            reads.append(s2)
        if op1 is None:
            return self.op(eng, lambda h: h.tensor_scalar(o.ap, a.ap, x1, None, op0), reads=reads, writes=[o])
        return self.op(eng, lambda h: h.tensor_scalar(o.ap, a.ap, x1, x2, op0, op1), reads=reads, writes=[o])

    def stt(self, eng, o, a, s, b, op0, op1):
        reads = [a, b]
        xs = s.ap if isinstance(s, V) else s
        if isinstance(s, V):
            reads.append(s)
        return self.op(eng, lambda h: h.scalar_tensor_tensor(o.ap, a.ap, xs, b.ap, op0, op1), reads=reads, writes=[o])

    def cp(self, eng, o, i):
        if eng is self.act:
            return self.op(eng, lambda h: h.copy(o.ap, i.ap), reads=[i], writes=[o])
        return self.op(eng, lambda h: h.tensor_copy(o.ap, i.ap), reads=[i], writes=[o])

    def memset(self, eng, o, val):
        return self.op(eng, lambda h: h.memset(o.ap, val), writes=[o])

    def ld(self, q, o, src_ap, reads=()):
        return self.dma(q, lambda h: h.dma_start(out=o.ap, in_=src_ap), reads=list(reads), writes=[o])

    def st(self, q, dst_ap, i, writes=()):
        return self.dma(q, lambda h: h.dma_start(out=dst_ap, in_=i.ap), reads=[i], writes=list(writes))


def _col(v, nt):
    return np.ascontiguousarray(np.asarray(v, np.float32).reshape(nt, 128).T)


def host_constants():
    c = {}
    f32 = np.float32
    for tag, L in (("L", LAT), ("C", CTX)):
        t = (np.arange(L, dtype=f32) / f32(L)).astype(f32)
        ang = (f32(2.0 * math.pi) * t[:, None] * np.arange(1, 17, dtype=f32)).astype(f32)
        feat = np.concatenate([t[:, None], np.cos(ang), np.sin(ang)], axis=-1).astype(f32)
        c["featT_" + tag] = np.ascontiguousarray(feat.T)
        c["negt_" + tag] = _col(-t, L // 128)
        N = 2 * L
        k = np.arange(L, dtype=np.float64)
        th = 2.0 * np.pi * np.outer(k + 0.5, k + 0.5) / N
        c["dftC_" + tag] = np.cos(th).astype(ml_dtypes.bfloat16)
        c["dftS_" + tag] = np.sin(th).astype(ml_dtypes.bfloat16)
        th1 = 2.0 * np.pi * np.outer(k, k + 0.5) / N
        c["dftC1_" + tag] = np.cos(th1).astype(ml_dtypes.bfloat16)
        c["dftS1_" + tag] = np.sin(th1).astype(ml_dtypes.bfloat16)
        ph = np.pi * (k + 0.5) / N
        c["ab_" + tag] = np.ascontiguousarray(np.stack([_col(np.cos(ph), L // 128), _col(np.sin(ph), L // 128)], axis=1))
    dmin = -math.log(1e-2) / 1.5
    dmax = -math.log(1e-2) / 0.3
    c["decay_b"] = np.ascontiguousarray(np.broadcast_to(np.linspace(dmin, dmax, 256, dtype=f32)[None, :], (128, 256)))
    rows = LAT // 64
    row = np.repeat(np.arange(rows, dtype=f32), 64)
    colv = np.tile(np.arange(64, dtype=f32), rows)
    inv = (f32(10000.0) ** (-np.arange(16, dtype=f32) / f32(16))).astype(f32)
    ang = np.concatenate([row[:, None] * inv, colv[:, None] * inv], axis=-1).astype(f32)
    j = (np.arange(128) % 64) % 32
    c["ropeT"] = np.ascontiguousarray(np.stack([np.cos(ang)[:, j].T, np.sin(ang)[:, j].T], axis=1).astype(f32))
    return c


def host_common(inp):
    f32 = np.float32
    g = {k: np.asarray(v) for k, v in inp.items()}
    o = {}
    o["w_mod"] = g["w_mod"]
    o["b_mod"] = g["b_mod"].reshape(DEPTH, 1, 6 * D)
    o["gn_rows"] = np.concatenate([g["norm1_g"], g["norm2_g"], g["final_g"][None]], axis=0).reshape(9, 1, D)
    o["w_in"] = g["w_in"]
    o["w_out"] = g["w_out"]
    o["hy_cw"] = np.ascontiguousarray(g["hy_conv_w"].reshape(DEPTH, 3, 6, 128).transpose(0, 3, 2, 1))
    o["hy_cb"] = np.ascontiguousarray(g["hy_conv_b"].reshape(DEPTH, 6, 128).transpose(0, 2, 1))
    o["hy_skip"] = np.ascontiguousarray(g["hy_skip"].reshape(DEPTH, 2, 128).transpose(0, 2, 1))
    o["hy_ng"] = np.ascontiguousarray(g["hy_norm_g"].reshape(DEPTH, 2, 128).transpose(0, 2, 1))
    o["hy_w1"] = g["hy_ffn_w1"]
    o["hy_w2"] = g["hy_ffn_w2"]
    o["hy_w3"] = g["hy_ffn_w3"]
    o["hy_bf"] = np.ascontiguousarray(np.stack([g["hy_ffn_b1"], g["hy_ffn_b2"], g["hy_freq"]], axis=-1))
    G, P, H = 16, 64, 16
    rows = np.stack([g["s5_a_re"].reshape(DEPTH, 2, G * P), g["s5_a_im"].reshape(DEPTH, 2, G * P),
                     np.repeat(g["s5_log_dt"], P, axis=-1)], axis=2)
    o["s5_row"] = np.ascontiguousarray(rows.reshape(DEPTH, 2, 1, 3 * G * P))
    cols = rows.reshape(DEPTH, 2, 3, 8, 128).transpose(0, 1, 4, 2, 3)
    o["s5_col"] = np.ascontiguousarray(cols)
    bT = np.zeros((DEPTH, 2, 2, 2, 128, 512), f32)
    cT = np.zeros((DEPTH, 2, 2, 8, 128, 128), f32)
    for ri, (bsrc, csrc) in enumerate(((g["s5_b_re"], g["s5_c_re"]), (g["s5_b_im"], g["s5_c_im"]))):
        for gg in range(G):
            half, gm = gg // 8, gg % 8
            bT[:, :, ri, half, gm * 16:(gm + 1) * 16, gm * 64:(gm + 1) * 64] = bsrc[:, :, gg].transpose(0, 1, 3, 2)
            pair, gl = gg // 2, gg % 2
            cT[:, :, ri, pair, gl * 64:(gl + 1) * 64, gm * 16:(gm + 1) * 16] = csrc[:, :, gg].transpose(0, 1, 3, 2)
    o["s5_bT"] = bT
    o["s5_cT"] = cT
    o["s5_dn"] = np.ascontiguousarray(np.stack([g["s5_d"].reshape(DEPTH, 2, 128).transpose(0, 2, 1),
                                                 g["s5_norm_g"].reshape(DEPTH, 2, 128).transpose(0, 2, 1)], axis=2))
    o["s5_glu"] = g["s5_glu_w"]
    o["att_l"] = np.concatenate([g["att_lq1"], g["att_lk1"], g["att_lq2"], g["att_lk2"]], axis=-1).reshape(DEPTH, 1, 256)
    o["att_g"] = g["att_subln_g"].reshape(DEPTH, 1, 128)
    o["moe_wr"] = np.ascontiguousarray(np.concatenate([g["moe_wg"], g["moe_we"]], axis=-1))
    o["moe_br"] = np.concatenate([g["moe_bg"], g["moe_be"]], axis=-1).reshape(DEPTH, 1, 36)
    w1 = g["moe_w1"].reshape(DEPTH, 32, 8, 128, 512).transpose(0, 1, 3, 2, 4)
    w3 = g["moe_w3"].reshape(DEPTH, 32, 8, 128, 512).transpose(0, 1, 3, 2, 4)
    o["moe_w13h"] = np.ascontiguousarray(np.stack([w1, w3], axis=3)).reshape(DEPTH, 32 * 128, 2 * 8 * 512)
    o["moe_w2h"] = np.ascontiguousarray(g["moe_w2"].reshape(DEPTH, 32, 4, 128, 1024).transpose(0, 1, 3, 2, 4)).reshape(DEPTH, 32 * 128, 4 * 1024)
    o.update(host_constants())
    return {k: np.ascontiguousarray(v) for k, v in o.items()}


def host_core(inp, b):
    x = np.asarray(inp["x"])[b]
    ctx = np.asarray(inp["ctx"])[b]
    cc = np.stack([_col(np.asarray(inp["c"])[b], 8), _col(np.asarray(inp["c_ctx"]), 8)], axis=-1)
    return {"xin": np.ascontiguousarray(np.concatenate([ctx, x], axis=0)), "cT": np.ascontiguousarray(cc)}


IN_SHAPES = None


def chunks_of(T0, Ttot, W):
    out = []
    t = T0
    while t < Ttot:
        w = min(W, Ttot - t)
        out.append((t, w))
        t += w
    return out


TOK_CHUNKS = [(0, CTX)] + chunks_of(CTX, T, 512)


class Prog:
    def __init__(self, arrays, layer_ids, debug=(), phases=None):
        self.layer_ids = list(layer_ids)
        self.NL = len(self.layer_ids)
        self.debug = set(debug)
        self.phases = phases
        nc = bass.Bass("TRN2", target_bir_lowering=False)
        self.nc = nc
        self.din = {}
        for k, v in arrays.items():
            dt = {np.dtype(np.float32): F32, np.dtype(ml_dtypes.bfloat16): BF16, np.dtype(np.int32): I32}[v.dtype]
            self.din[k] = nc.dram_tensor(k, list(v.shape), dt, kind="ExternalInput").ap()
        self.out = nc.dram_tensor("out", [LAT, D], F32, kind="ExternalOutput").ap()

        def scratch(name, shape, dt):
            kind = "ExternalOutput" if name in self.debug else "Internal"
            return nc.dram_tensor(name, list(shape), dt, kind=kind).ap()

        self.Xres = scratch("Xres", [T, D], F32)
        self.modrow = scratch("modrow", [self.NL, 2, 6 * D], F32)
        self.pT = scratch("pT", [1024, T], F32)
        self.QT = scratch("QT", [512, T], BF16)
        self.QrT = scratch("QrT", [512, LAT], BF16)
        self.KcT = scratch("KcT", [512, T], BF16)
        self.Vaug = scratch("Vaug", [NT, 128, 4 * 129], BF16)
        self.mixT = scratch("mixT", [1024, T], BF16)
        self.Hs = scratch("Hs", [T + 1, D], BF16)
        self.slot_tok = scratch("slot_tok", [NPAD, 1], I32)
        self.yb = scratch("yb", [NPAD, D], F32)

    def build(self):
        nc = self.nc
        with contextlib.ExitStack() as st:
            kb = KB(nc, st)
            self.kb = kb
            self.setup()
            self.modulation()
            kb.barrier()
            for li in range(self.NL):
                self.layer(li)
            self.final_norm()
            kb.barrier()
        return nc

    def ph(self, name):
        return self.phases is None or name in self.phases

    def setup(self):
        kb = self.kb
        self.ident_f = kb.sb("ident_f", [128, 128])
        kb.memset(kb.pool, self.ident_f[:], 0.0)
        kb.op(kb.pool, lambda h: h.affine_select(self.ident_f.t[:], self.ident_f.t[:], [[-1, 128]], ALU.not_equal, 1.0,
                                                 base=0, channel_multiplier=1), reads=[self.ident_f.b], writes=[self.ident_f.b])
        self.ident_b = kb.sb("ident_b", [128, 128], BF16)
        kb.cp(kb.dve, self.ident_b[:], self.ident_f[:])
        self.ones_f = kb.sb("ones_f", [128, 128])
        kb.memset(kb.dve, self.ones_f[:], 1.0)
        self.tri = kb.sb("tri", [128, 128])
        kb.memset(kb.pool, self.tri[:], 1.0)
        kb.op(kb.pool, lambda h: h.affine_select(self.tri.t[:], self.tri.t[:], [[1, 128]], ALU.is_ge, 0.0,
                                                 base=-1, channel_multiplier=-1), reads=[self.tri.b], writes=[self.tri.b])
        self.iota512 = kb.sb("iota512", [128, 512])
        kb.op(kb.pool, lambda h: h.iota(self.iota512.t[:], [[1, 512]], base=0, channel_multiplier=0,
                                        allow_small_or_imprecise_dtypes=True), writes=[self.iota512.b])
        self.blk = []
        for m in range(2):
            b = kb.sb("blk%d" % m, [128, 128], BF16)
            kb.memset(kb.dve, b[:], 0.0)
            kb.memset(kb.dve, b[m * 64:(m + 1) * 64, :], 1.0)
            self.blk.append(b)
        self.r_hy = kb.sb("r_hy", [128, NT])
        self.r_s5 = kb.sb("r_s5", [128, NT])
        self.qkmax = kb.sb("qkmax", [128, 16])
        self.halfpi = kb.sb("halfpi", [128, 1])
        kb.memset(kb.dve, self.halfpi[:], math.pi / 2.0)

    def modulation(self):
        kb = self.kb
        with contextlib.ExitStack() as ph:
            cT = kb.sb("cT", [128, 8, 2], stack=ph)
            kb.ld(kb.sp, cT[:], self.din["cT"])
            sc = kb.sb("sc", [128, 8, 2], stack=ph)
            kb.act_(sc[:], cT[:], AF.Silu)
            wm = [kb.sb("wm%d" % i, [128, 8, 512], stack=ph) for i in range(2)]
            pm = [kb.ps("pm%d" % i, [128, 512], stack=ph) for i in range(2)]
            rows = [kb.sb("mrow%d" % i, [1, 6 * D], stack=ph) for i in range(2)]
            brow = kb.sb("brow", [1, 6 * D], stack=ph)
            n = 0
            for li in range(self.NL):
                kb.ld(kb.sp, brow[:], self.din["b_mod"][li])
                for ch in range(12):
                    w = wm[n % 2]
                    n += 1
                    kb.ld(kb.sp, w[:], self.din["w_mod"][li][:, ch * 512:(ch + 1) * 512].rearrange("(kt p) n -> p kt n", p=128))
                    for which in range(2):
                        p = pm[which]
                        for kt in range(8):
                            kb.mm(p[0:1, :], sc[:, kt, which:which + 1], w[:, kt, :], start=(kt == 0), stop=(kt == 7))
                        kb.tt(kb.dve, rows[which][:, ch * 512:(ch + 1) * 512], p[0:1, :], brow[:, ch * 512:(ch + 1) * 512], ALU.add)
                for which in range(2):
                    kb.st(kb.sp, self.modrow[li, which:which + 1, :], rows[which][:])
            kb.barrier()

    def load_mod_b(self, li, which, seg, stack, name):
        t = self.kb.sb(name, [128, D], stack=stack)
        self.kb.ld(self.kb.sp, t[:], self.modrow[li, which:which + 1, seg * D:(seg + 1) * D].partition_broadcast(128))
        return t

    def norm_mod_tiles(self, li, gidx, seg_sh, seg_sc, stack):
        kb = self.kb
        gb = kb.sb("gnb", [128, D], stack=stack)
        kb.ld(kb.sp, gb[:], self.din["gn_rows"][gidx].partition_broadcast(128))
        res = []
        for which in range(2):
            scb = self.load_mod_b(li, which, seg_sc, stack, "scb%d" % which)
            shb = self.load_mod_b(li, which, seg_sh, stack, "shb%d" % which)
            kb.stt(kb.dve, scb[:], scb[:], 1.0, gb[:], ALU.add, ALU.mult)
            res.append((scb, shb))
        return res

    def norm_tile(self, xt, ab, xn, tmp_ss, tmp_junk):
        kb = self.kb
        A, B = ab
        kb.act_(tmp_junk[:], xt[:], AF.Square, accum=tmp_ss[:, 0:1])
        kb.ts(kb.dve, tmp_ss[:, 1:2], tmp_ss[:, 0:1], 1.0 / D, EPS, ALU.mult, ALU.add)
        kb.act_(tmp_ss[:, 2:3], tmp_ss[:, 1:2], AF.Sqrt)
        kb.op(kb.dve, lambda h: h.reciprocal(tmp_ss.t[:, 3:4], tmp_ss.t[:, 2:3]), reads=[tmp_ss.b], writes=[tmp_ss.b])
        kb.stt(kb.dve, xn[:], xt[:], tmp_ss[:, 3:4], A[:], ALU.mult, ALU.mult)
        kb.tt(kb.dve, xn[:], xn[:], B[:], ALU.add)

    def layer(self, li):
        kb = self.kb
        if self.ph("inproj"):
            self.inproj(li)
            kb.barrier()
        if self.ph("hyena"):
            self.hyena(li, LAT, CTX, "L")
            kb.barrier()
            if self.layer_ids[li] < DEPTH - 1:
                self.hyena(li, CTX, 0, "C")
                kb.barrier()
        if self.ph("s5") or self.ph("attn"):
            self.s5_attn(li)
            kb.barrier()
        if self.ph("outproj"):
            self.outproj(li)
            kb.barrier()
        if self.ph("moe"):
            self.moe(li)
            kb.barrier()

    def xsrc(self, li):
        return self.din["xin"] if li == 0 else self.Xres

    def inproj(self, li):
        kb = self.kb
        src = self.xsrc(li)
        with contextlib.ExitStack() as ph:
            w_in = kb.sb("w_in", [128, 8, 2560], BF16, stack=ph)
            for kt in range(8):
                kb.ld(kb.pool, w_in[:, kt, :], self.din["w_in"][li][kt * 128:(kt + 1) * 128, :])
            wrot = kb.sb("wrot", [128, 8, 1024], BF16, stack=ph)
            wrot5 = wrot.t[:].rearrange("p k (b two j) -> p k b two j", two=2, j=32)
            for kt in range(8):
                srcv = self.din["w_in"][li][kt * 128:(kt + 1) * 128, 1024:2048].rearrange("p (b two j) -> p b two j", two=2, j=32)
                kb.dma(kb.pool, lambda h: h.dma_start(out=wrot5[:, kt, :, 0, :], in_=srcv[:, :, 1, :]), writes=[wrot.b])
                kb.dma(kb.pool, lambda h: h.dma_start(out=wrot5[:, kt, :, 1, :], in_=srcv[:, :, 0, :]), writes=[wrot.b])
            for kt in range(8):
                kb.op(kb.dve, lambda h: h.tensor_scalar(wrot5[:, kt, :, 0, :], wrot5[:, kt, :, 0, :], -1.0, None, ALU.mult),
                      reads=[wrot.b], writes=[wrot.b])
            rope = kb.sb("rope", [128, 2, LAT], stack=ph)
            kb.ld(kb.sp, rope[:], self.din["ropeT"])
            ab = self.norm_mod_tiles(li, self.layer_ids[li], 0, 1, ph)
            kb.memset(kb.dve, self.qkmax[:], 0.0)
            xt = [kb.sb("xt%d" % i, [128, D], stack=ph) for i in range(2)]
            xn = kb.sb("xn", [128, D], stack=ph)
            junk = kb.sb("junk", [128, D], stack=ph)
            ss = kb.sb("ss", [128, 4], stack=ph)
            xnT = [kb.sb("xnT%d" % i, [128, 8, 512], BF16, stack=ph) for i in range(2)]
            trp = kb.ps("trp", [128, D], stack=ph)
            pj = [kb.ps("pj%d" % i, [128, 512], stack=ph) for i in range(4)]
            pv = kb.ps("pv", [128, 512], stack=ph)
            nb = kb.ps("nb", [128, 512], stack=ph)
            stg = [kb.sb("stg%d" % i, [128, 512], stack=ph) for i in range(2)]
            stb = [kb.sb("stb%d" % i, [128, 512], BF16, stack=ph) for i in range(3)]
            sqb = kb.sb("sqb", [128, 512], BF16, stack=ph)
            red = kb.sb("red", [128, 1], stack=ph)
            t1 = kb.sb("t1", [128, 512], stack=ph)
            t2 = kb.sb("t2", [128, 512], stack=ph)
            vst = [kb.sb("vst%d" % i, [128, 4, 129], BF16, stack=ph) for i in range(2)]
            for v in vst:
                kb.memset(kb.dve, v[:], 1.0)
            cnt = {"ev": 0, "stg": 0, "stb": 0, "pj": 0, "v": 0, "x": 0}

            def evac_engine():
                cnt["ev"] += 1
                return kb.act if cnt["ev"] % 2 == 0 else kb.dve

            def next_pj():
                cnt["pj"] += 1
                return pj[cnt["pj"] % 4]

            def proj_fm(wv, c0, xc, W):
                p = next_pj()
                for kt in range(8):
                    kb.mm(p[:, :W], V(wv(kt, c0), wv.buf), xc[:, kt, :W], start=(kt == 0), stop=(kt == 7))
                return p

            def w_in_cols(kt, c0):
                return w_in.t[:, kt, c0:c0 + 128]
            w_in_cols.buf = w_in.b

            def w_rot_cols(kt, c0):
                return wrot.t[:, kt, c0:c0 + 128]
            w_rot_cols.buf = wrot.b

            def norm_stat(sv, W, col):
                kb.tt(kb.dve, sqb[:, :W], sv, sv, ALU.mult)
                for m in range(2):
                    kb.mm(nb[:, :W], self.blk[m][:], sqb[:, :W])
                    kb.op(kb.dve, lambda h: h.reduce_max(red.t[:], nb.t[:, :W], AX.X), reads=[nb.b], writes=[red.b])
                    kb.tt(kb.dve, self.qkmax[:, col + m:col + m + 1], self.qkmax[:, col + m:col + m + 1], red[:], ALU.max)

            for ci, (t0, W) in enumerate(TOK_CHUNKS):
                which = 1 if ci == 0 else 0
                xc = xnT[ci % 2]
                ntile = W // 128
                for ti in range(ntile):
                    x = xt[cnt["x"] % 2]
                    cnt["x"] += 1
                    kb.ld(kb.sp, x[:], src[t0 + ti * 128:t0 + (ti + 1) * 128, :])
                    self.norm_tile(x, ab[which], xn, ss, junk)
                    for kt in range(8):
                        kb.tr(trp[:, kt * 128:(kt + 1) * 128], xn[:, kt * 128:(kt + 1) * 128], self.ident_f[:])
                    for hf in range(2):
                        e = evac_engine()
                        kb.cp(e, V(xc.t[:, hf * 4:(hf + 1) * 4, ti * 128:(ti + 1) * 128], xc.b),
                              V(trp.t[:, hf * 512:(hf + 1) * 512].rearrange("p (k t) -> p k t", k=4), trp.b))
                for j in range(8):
                    p = proj_fm(w_in_cols, j * 128, xc, W)
                    s = stg[cnt["stg"] % 2]
                    cnt["stg"] += 1
                    kb.cp(evac_engine(), s[:, :W], p[:, :W])
                    kb.st(kb.sp, self.pT[j * 128:(j + 1) * 128, t0:t0 + W], s[:, :W])
                lat = ci > 0
                tl = t0 - CTX
                for kind in range(2):
                    for hh in range(4):
                        c0 = 1024 + kind * 512 + hh * 128
                        pa = proj_fm(w_in_cols, c0, xc, W)
                        col = kind * 8 + hh * 2
                        if (kind == 0) or (not lat):
                            sbt = stb[cnt["stb"] % 3]
                            cnt["stb"] += 1
                            kb.act_(sbt[:, :W], pa[:, :W], AF.Copy, scale=(0.125 if kind == 0 else 1.0))
                            dst = self.QT if kind == 0 else self.KcT
                            kb.st(kb.sp, dst[hh * 128:(hh + 1) * 128, t0:t0 + W], sbt[:, :W])
                            if not lat or kind == 0:
                                norm_stat(sbt[:, :W], W, col)
                        if lat:
                            pb = proj_fm(w_rot_cols, kind * 512 + hh * 128, xc, W)
                            kb.tt(kb.dve, t1[:, :W], pa[:, :W], rope[:, 0, tl:tl + W], ALU.mult)
                            kb.tt(kb.dve, t2[:, :W], pb[:, :W], rope[:, 1, tl:tl + W], ALU.mult)
                            sbt = stb[cnt["stb"] % 3]
                            cnt["stb"] += 1
                            kb.stt(kb.dve, sbt[:, :W], t1[:, :W], (0.125 if kind == 0 else 1.0), t2[:, :W], ALU.mult, ALU.add) \
                                if kind == 1 else None
                            if kind == 0:
                                kb.tt(kb.dve, t1[:, :W], t1[:, :W], t2[:, :W], ALU.add)
                                kb.act_(sbt[:, :W], t1[:, :W], AF.Copy, scale=0.125)
                                kb.st(kb.sp, self.QrT[hh * 128:(hh + 1) * 128, tl:tl + W], sbt[:, :W])
                            else:
                                kb.st(kb.sp, self.KcT[hh * 128:(hh + 1) * 128, t0:t0 + W], sbt[:, :W])
                                norm_stat(sbt[:, :W], W, col)
                for ti in range(ntile):
                    for kt in range(8):
                        kb.mm(pv[:], xc[:, kt, ti * 128:(ti + 1) * 128], w_in[:, kt, 2048:2560], start=(kt == 0), stop=(kt == 7))
                    vs = vst[cnt["v"] % 2]
                    cnt["v"] += 1
                    kb.cp(evac_engine(), V(vs.t[:, :, 0:128], vs.b), V(pv.t[:].rearrange("p (h d) -> p h d", h=4), pv.b))
                    kb.st(kb.sp, self.Vaug[(t0 // 128) + ti], V(vs.t[:].rearrange("p h d -> p (h d)"), vs.b))

    def range_reduce_sin(self, out, arg, ki, kf, W, rows):
        kb = self.kb
        a = arg[0:rows, :W]
        kb.ts(kb.dve, ki[0:rows, :W], a, 1.0 / TWO_PI, None, ALU.mult)
        kb.cp(kb.dve, kf[0:rows, :W], ki[0:rows, :W])
        kb.stt(kb.dve, a, kf[0:rows, :W], -TWO_PI, a, ALU.mult, ALU.add)
        kb.ts(kb.dve, a, a, -3.141592, 3.141592, ALU.max, ALU.min)
        kb.act_(out, a, AF.Sin)

    def conv3(self, eng, u, pin, W, cw, cb, j):
        kb = self.kb
        kb.ts(eng, u, pin[:, 1:W + 1], cw[:, j, 1:2], cb[:, j:j + 1], ALU.mult, ALU.add)
        kb.stt(eng, u, pin[:, 0:W], cw[:, j, 0:1], u, ALU.mult, ALU.add)
        kb.stt(eng, u, pin[:, 2:W + 2], cw[:, j, 2:3], u, ALU.mult, ALU.add)

    def dft_passes(self, ph, C, S, nt, rhs_fn, ncols, epilogue):
        kb = self.kb
        with contextlib.ExitStack() as sc:
            accC = [kb.ps("accC%d" % j, [128, 512], stack=sc) for j in range(4)]
            accS = [kb.ps("accS%d" % j, [128, 512], stack=sc) for j in range(4)]
            NG = 4 if nt % 4 == 0 else 2
            Cp = [kb.sb("Cp%d" % j, [128, NG, 512], BF16, stack=sc) for j in range(3)]
            Sp = [kb.sb("Sp%d" % j, [128, NG, 512], BF16, stack=sc) for j in range(3)]
            n = 0
            for p0 in range(0, nt, 4):
                kts = min(4, nt - p0)
                for ng in range(0, nt, NG):
                    cp_, sp_ = Cp[n % 3], Sp[n % 3]
                    n += 1
                    kb.ld(kb.sp, cp_[:, :, :kts * 128],
                          C[ng * 128:(ng + NG) * 128, p0 * 128:(p0 + kts) * 128].rearrange("(a p) k -> p a k", p=128))
                    kb.ld(kb.act, sp_[:, :, :kts * 128],
                          S[ng * 128:(ng + NG) * 128, p0 * 128:(p0 + kts) * 128].rearrange("(a p) k -> p a k", p=128))
                    for a_ in range(NG):
                        n_t = ng + a_
                        r = rhs_fn(n_t)
                        rC, rS = r if isinstance(r, tuple) else (r, r)
                        for j in range(kts):
                            kb.mm(accC[j][:, :ncols], cp_[:, a_, j * 128:(j + 1) * 128], rC, start=(n_t == 0), stop=(n_t == nt - 1))
                            kb.mm(accS[j][:, :ncols], sp_[:, a_, j * 128:(j + 1) * 128], rS, start=(n_t == 0), stop=(n_t == nt - 1))
                for j in range(kts):
                    epilogue(p0 + j, accC[j], accS[j])
            kb.barrier()

    def hyena(self, li, L, tok0, tag):
        kb = self.kb
        nt = L // 128
        N = 2 * L
        C = self.din["dftC_" + tag]
        S = self.din["dftS_" + tag]
        with contextlib.ExitStack() as ph:
            G = kb.sb("G", [128, nt, 512], BF16, stack=ph)
            rn = kb.sb("rn", [128, 4], stack=ph)
            ab = kb.sb("ab", [128, 2, nt], stack=ph)
            kb.ld(kb.sp, ab[:], self.din["ab_" + tag])
            with contextlib.ExitStack() as fa:
                hsd = kb.sb("hsd", [128, nt, 512], BF16, stack=fa)
                with contextlib.ExitStack() as f:
                    featT = kb.sb("featT", [33, L], stack=f)
                    kb.ld(kb.sp, featT[:], self.din["featT_" + tag])
                    w1 = kb.sb("hw1", [33, 64], stack=f)
                    kb.ld(kb.sp, w1[:], self.din["hy_w1"][li])
                    w2 = kb.sb("hw2", [64, 64], stack=f)
                    kb.ld(kb.sp, w2[:], self.din["hy_w2"][li])
                    w3 = kb.sb("hw3", [64, 512], stack=f)
                    kb.ld(kb.sp, w3[:], self.din["hy_w3"][li])
                    bf_ = kb.sb("hbf", [64, 3], stack=f)
                    kb.ld(kb.sp, bf_[:], self.din["hy_bf"][li])
                    fb = kb.sb("hfb", [64, 2], stack=f)
                    kb.tt(kb.dve, fb[:, 0:1], bf_[:, 0:1], bf_[:, 2:3], ALU.mult)
                    kb.tt(kb.dve, fb[:, 1:2], bf_[:, 1:2], bf_[:, 2:3], ALU.mult)
                    negt = kb.sb("negt", [128, nt], stack=f)
                    kb.ld(kb.sp, negt[:], self.din["negt_" + tag])
                    decay = kb.sb("decay", [128, 256], stack=f)
                    kb.ld(kb.sp, decay[:], self.din["decay_b"])
                    h1T = kb.sb("h1T", [64, L], stack=f)
                    h2T = kb.sb("h2T", [64, L], stack=f)
                    arg = kb.sb("harg", [64, 512], stack=f)
                    ki = kb.sb("hki", [64, 512], I32, stack=f)
                    kf = kb.sb("hkf", [64, 512], stack=f)
                    pf = [kb.ps("pf%d" % i, [128, 512], stack=f) for i in range(2)]
                    pn = kb.ps("pn", [128, 2], stack=f)
                    n = 0
                    for layer_i, (wv, inT, outT) in enumerate(((w1, featT, h1T), (w2, h1T, h2T))):
                        for (c0, W) in chunks_of(0, L, 512):
                            p = pf[n % 2]
                            n += 1
                            kb.mm(p[0:64, :W], wv[:], inT[:, c0:c0 + W])
                            kb.ts(kb.dve, arg[:, :W], p[0:64, :W], bf_[:, 2:3], fb[:, layer_i:layer_i + 1], ALU.mult, ALU.add)
                            self.range_reduce_sin(outT[:, c0:c0 + W], arg, ki, kf, W, 64)
                    win = kb.sb("win", [128, 256], stack=f)
                    tf = kb.sb("tf", [128, 256], stack=f)
                    tb = kb.sb("tb", [128, 256], stack=f)
                    absacc = kb.sb("absacc", [128, 256], stack=f)
                    for i in range(nt):
                        p = pf[n % 2]
                        n += 1
                        kb.mm(p[:], h2T[:, i * 128:(i + 1) * 128], w3[:])
                        kb.act_(win[:], decay[:], AF.Exp, scale=negt[:, i:i + 1])
                        kb.tt(kb.dve, tf[:], p[:, 0:256], win[:], ALU.mult)
                        kb.tt(kb.dve, tb[:], p[:, 256:512], win[:], ALU.mult)
                        if i == 0:
                            kb.memset(kb.dve, tb[0:1, :], 0.0)
                        kb.tt(kb.pool, hsd[:, i, 0:256], tf[:], tb[:], ALU.add)
                        kb.tt(kb.pool, hsd[:, i, 256:512], tb[:], tf[:], ALU.subtract)
                        kb.stt(kb.dve, tf[:], tf[:], -1.0, tf[:], ALU.mult, ALU.max)
                        kb.stt(kb.dve, tb[:], tb[:], -1.0, tb[:], ALU.mult, ALU.max)
                        kb.tt(kb.dve, tf[:], tf[:], tb[:], ALU.add)
                        if i == 0:
                            kb.cp(kb.dve, absacc[:], tf[:])
                        else:
                            kb.tt(kb.dve, absacc[:], absacc[:], tf[:], ALU.add)
                    for ct in range(2):
                        kb.mm(pn[:, ct:ct + 1], absacc[:, ct * 128:(ct + 1) * 128], self.ones_f[:, 0:1])
                    kb.ts(kb.dve, rn[:, 2:4], pn[:, 0:2], EPS, None, ALU.add)
                    kb.op(kb.dve, lambda h: h.reciprocal(rn.t[:, 0:2], rn.t[:, 2:4]), reads=[rn.b], writes=[rn.b])
                    kb.ts(kb.dve, rn[:, 0:2], rn[:, 0:2], 2.0 / N, None, ALU.mult)
                    kb.barrier()
                with contextlib.ExitStack() as f2:
                    tmpg = [kb.sb("tmpg%d" % i, [128, 256], stack=f2) for i in range(2)]

                    def epi_filter(kt, aC, aS):
                        kb.cp(kb.act, G[:, kt, 0:256], aC[:, 0:256])
                        kb.cp(kb.dve, G[:, kt, 256:512], aS[:, 0:256])

                    self.dft_passes(f2, self.din["dftC1_" + tag], self.din["dftS1_" + tag], nt,
                                    lambda n_t: (hsd[:, n_t, 0:256], hsd[:, n_t, 256:512]), 256, epi_filter)
            kb.barrier()
            cw = kb.sb("hcw", [128, 6, 3], stack=ph)
            kb.ld(kb.sp, cw[:], self.din["hy_cw"][li])
            cb = kb.sb("hcb", [128, 6], stack=ph)
            kb.ld(kb.sp, cb[:], self.din["hy_cb"][li])
            skip = kb.sb("hskip", [128, 2], stack=ph)
            kb.ld(kb.sp, skip[:], self.din["hy_skip"][li])
            ng = kb.sb("hng", [128, 2], stack=ph)
            kb.ld(kb.sp, ng[:], self.din["hy_ng"][li])
            zfm = [kb.sb("zfm%d" % i, [128, L], stack=ph) for i in range(2)]
            zT = kb.sb("zT", [128, nt, 256], BF16, stack=ph)
            Y = kb.sb("Y", [128, nt, 512], BF16, stack=ph)
            with contextlib.ExitStack() as z1:
                pin1 = kb.sb("pin1", [128, L + 2], stack=z1)
                pin2 = kb.sb("pin2", [128, L + 2], stack=z1)
                u1 = kb.sb("u1", [128, L], stack=z1)
                ptz = [kb.ps("ptz%d" % i, [128, 256], stack=z1) for i in range(2)]
                for pin in (pin1, pin2):
                    kb.memset(kb.dve, pin[:, 0:1], 0.0)
                    kb.memset(kb.dve, pin[:, L + 1:L + 2], 0.0)
                for ct in range(2):
                    kb.ld(kb.sp, pin1[:, 1:L + 1], self.pT[(2 + ct) * 128:(3 + ct) * 128, tok0:tok0 + L])
                    kb.ld(kb.act, pin2[:, 1:L + 1], self.pT[(4 + ct) * 128:(5 + ct) * 128, tok0:tok0 + L])
                    self.conv3(kb.dve, u1[:], pin1, L, cw, cb, 2 + ct)
                    self.conv3(kb.dve, zfm[ct][:], pin2, L, cw, cb, 4 + ct)
                    kb.tt(kb.dve, zfm[ct][:], zfm[ct][:], u1[:], ALU.mult)
                for i in range(nt):
                    p = ptz[i % 2]
                    for ct in range(2):
                        kb.tr(p[:, ct * 128:(ct + 1) * 128], zfm[ct][:, i * 128:(i + 1) * 128], self.ident_f[:])
                    kb.cp(kb.act if i % 2 else kb.dve, zT[:, i, :], p[:])
                kb.barrier()
            with contextlib.ExitStack() as z2:
                tq = [kb.sb("tq%d" % i, [128, 256], stack=z2) for i in range(4)]

                def epi_fwd(kt, aC, aS):
                    Gr = G[:, kt, 0:256]
                    Gi = G[:, kt, 256:512]
                    kb.tt(kb.dve, tq[0][:], aC[:, 0:256], Gr, ALU.mult)
                    kb.tt(kb.dve, tq[1][:], aS[:, 0:256], Gi, ALU.mult)
                    kb.tt(kb.pool, Y[:, kt, 0:256], tq[0][:], tq[1][:], ALU.add)
                    kb.tt(kb.dve, tq[2][:], aS[:, 0:256], Gr, ALU.mult)
                    kb.tt(kb.dve, tq[3][:], aC[:, 0:256], Gi, ALU.mult)
                    kb.tt(kb.pool, Y[:, kt, 256:512], tq[2][:], tq[3][:], ALU.subtract)

                self.dft_passes(z2, C, S, nt, lambda n_t: zT[:, n_t, :], 256, epi_fwd)
            kb.barrier()
            with contextlib.ExitStack() as z3:
                acc = [[kb.ps("iacc%d_%d" % (ct, cj), [128, 512], stack=z3) for cj in range(3)] for ct in range(2)]
                prs = kb.ps("prs", [128, 4], stack=z3)
                Cp = [kb.sb("iCp%d" % j, [128, 1536], BF16, stack=z3) for j in range(2)]
                Sp = [kb.sb("iSp%d" % j, [128, 1536], BF16, stack=z3) for j in range(2)]
                pin = [kb.sb("ipin%d" % j, [128, 514], stack=z3) for j in range(2)]
                x0 = kb.sb("x0", [128, 512], stack=z3)
                tt_ = kb.sb("itt", [128, 512], stack=z3)
                sq = [kb.sb("isq%d" % j, [128, 512], stack=z3) for j in range(2)]
                ob = [kb.sb("iob%d" % j, [128, 512], BF16, stack=z3) for j in range(2)]
                r4 = kb.sb("ir4", [128, 8], stack=z3)
                chunks = chunks_of(0, L, 512)
                n = 0
                for g0 in range(0, len(chunks), 3):
                    grp = chunks[g0:g0 + 3]
                    cA = grp[0][0]
                    cB = grp[-1][0] + grp[-1][1]
                    for kt in range(nt):
                        cp_, sp_ = Cp[n % 2], Sp[n % 2]
                        n += 1
                        kb.ld(kb.sp, cp_[:, :cB - cA], C[kt * 128:(kt + 1) * 128, cA:cB])
                        kb.ld(kb.act, sp_[:, :cB - cA], S[kt * 128:(kt + 1) * 128, cA:cB])
                        for cj, (c0, W) in enumerate(grp):
                            for ct in range(2):
                                kb.mm(acc[ct][cj][:, :W], Y[:, kt, ct * 128:(ct + 1) * 128], cp_[:, c0 - cA:c0 - cA + W],
                                      start=(kt == 0), stop=False)
                                kb.mm(acc[ct][cj][:, :W], Y[:, kt, 256 + ct * 128:256 + (ct + 1) * 128], sp_[:, c0 - cA:c0 - cA + W],
                                      start=False, stop=(kt == nt - 1))
                    for cj, (c0, W) in enumerate(grp):
                        nsub = W // 128
                        for ct in range(2):
                            pi = pin[ct]
                            lo = c0 - 1
                            hi = c0 + W + 1
                            dlo = 0
                            dhi = W + 2
                            if c0 == 0:
                                kb.memset(kb.dve, pi[:, 0:1], 0.0)
                                lo = 0
                                dlo = 1
                            if c0 + W == L:
                                kb.memset(kb.dve, pi[:, W + 1:W + 2], 0.0)
                                hi = L
                                dhi = W + 1
                            kb.ld(kb.sp, pi[:, dlo:dhi], self.pT[ct * 128:(ct + 1) * 128, tok0 + lo:tok0 + hi])
                            self.conv3(kb.dve, x0[:, :W], pi, W, cw, cb, ct)
                            kb.ts(kb.dve, tt_[:, :W], acc[ct][cj][:, :W], rn[:, ct:ct + 1], None, ALU.mult)
                            kb.stt(kb.dve, tt_[:, :W], zfm[ct][:, c0:c0 + W], skip[:, ct:ct + 1], tt_[:, :W], ALU.mult, ALU.add)
                            kb.tt(kb.dve, tt_[:, :W], tt_[:, :W], x0[:, :W], ALU.mult)
                            kb.act_(sq[ct][:, :W], tt_[:, :W], AF.Square)
                            kb.act_(ob[ct][:, :W], tt_[:, :W], AF.Copy, scale=ng[:, ct:ct + 1])
                            kb.st(kb.sp, self.mixT[ct * 128:(ct + 1) * 128, tok0 + c0:tok0 + c0 + W], ob[ct][:, :W])
                        kb.tt(kb.dve, sq[0][:, :W], sq[0][:, :W], sq[1][:, :W], ALU.add)
                        for sub in range(nsub):
                            kb.mm(prs[:, sub:sub + 1], sq[0][:, sub * 128:(sub + 1) * 128], self.ones_f[:, 0:1])
                        ti0 = (tok0 + c0) // 128
                        kb.ts(kb.dve, r4[:, 0:nsub], prs[:, 0:nsub], 1.0 / 256.0, EPS, ALU.mult, ALU.add)
                        kb.act_(r4[:, 4:4 + nsub], r4[:, 0:nsub], AF.Sqrt)
                        kb.op(kb.dve, lambda h: h.reciprocal(self.r_hy.t[:, ti0:ti0 + nsub], r4.t[:, 4:4 + nsub]),
                              reads=[r4.b], writes=[self.r_hy.b])
                kb.barrier()

    S5W = 256

    def s5_alloc(self, li, st):
        kb = self.kb
        W5 = self.S5W
        tl = {}
        tl["yT"] = [kb.sb("yT%d" % h, [128, T], stack=st) for h in range(2)]
        tl["uT"] = [kb.sb("uT%d" % h, [128, T], BF16, stack=st) for h in range(2)]
        tl["dn"] = kb.sb("s5dn", [128, 2, 2], stack=st)
        tl["uf"] = kb.sb("uf", [128, 1088], stack=st)
        tl["Bm"] = [[kb.sb("Bm%d%d" % (h, r), [128, 512], BF16, stack=st) for r in range(2)] for h in range(2)]
        tl["col"] = kb.sb("s5col", [128, 3, 8], stack=st)
        tl["thc"] = kb.sb("thc", [128, 8], stack=st)
        tl["rhoc"] = kb.sb("rhoc", [128, 8], stack=st)
        tl["rowb"] = kb.sb("rowb", [128, 3, 1024], stack=st)
        tl["a8"] = kb.sb("s5a8", [128, 8], stack=st)
        tl["k8"] = kb.sb("s5k8", [128, 8], I32, stack=st)
        tl["f8"] = kb.sb("s5f8", [128, 8], stack=st)
        tl["cs256"] = kb.sb("cs256", [128, 8], stack=st)
        tl["sn256"] = kb.sb("sn256", [128, 8], stack=st)
        tl["pt"] = [kb.sb("ppt%d" % i, [128, 512], stack=st) for i in range(8)]
        tl["pki"] = kb.sb("ppki", [128, 512], I32, stack=st)
        tl["braw"] = [kb.sb("braw%d" % r, [128, 512], stack=st) for r in range(2)]
        names = ["ang", "kf", "sinT", "cosT", "t1", "t2", "t3", "t4", "bre", "bim", "gre", "gim"]
        d = {nm: [kb.sb("s5%s%d" % (nm, i), [128, W5], stack=st) for i in range(2)] for nm in names}
        d["ki"] = [kb.sb("s5ki%d" % i, [128, W5], I32, stack=st) for i in range(2)]
        d["hre"] = [kb.sb("hre%d" % i, [128, W5], BF16, stack=st) for i in range(3)]
        d["him"] = [kb.sb("him%d" % i, [128, W5], BF16, stack=st) for i in range(3)]
        d["car"] = kb.sb("car", [128, 2], stack=st)
        d["th0"] = kb.sb("th0", [128, 1], stack=st)
        d["rhoT"] = kb.sb("rhoT", [128, W5], stack=st)
        d["Cm"] = [[kb.sb("Cm%d%d" % (r, i), [128, 128], BF16, stack=st) for r in range(2)] for i in range(2)]
        d["Cf"] = [kb.sb("Cf%d" % r, [128, 128], stack=st) for r in range(2)]
        d["pp"] = [kb.ps("s5pp%d" % i, [128, 512], stack=st) for i in range(2)]
        d["ppb"] = [[Buf(), Buf()] for i in range(2)]
        tl["d"] = d
        return tl

    def s5_scan_gen(self, li, tl):
        kb = self.kb
        W5 = self.S5W
        yT, uT, dn, uf, Bm = tl["yT"], tl["uT"], tl["dn"], tl["uf"], tl["Bm"]
        col, thc, rhoc, rowb, pt, pki, braw, d = tl["col"], tl["thc"], tl["rhoc"], tl["rowb"], tl["pt"], tl["pki"], tl["braw"], tl["d"]
        self._s5step = 0
        self._s5pend = []
        kb.ld(kb.sp, dn[:], self.din["s5_dn"][li])
        for half in range(2):
            for q in range(4):
                kb.ld(kb.sp, uf[:], self.pT[768 + half * 128:768 + (half + 1) * 128, q * 1088:(q + 1) * 1088])
                kb.ts(kb.dve, yT[half][:, q * 1088:(q + 1) * 1088], uf[:], dn[:, 0, half:half + 1], None, ALU.mult)
                kb.cp(kb.pool, uT[half][:, q * 1088:(q + 1) * 1088], uf[:])
                yield
        seq = [(0, CTX)] + chunks_of(CTX, T, W5)
        for d_ in range(2):
            kb.ld(kb.sp, V(rowb.t[:].rearrange("p a n -> p (a n)"), rowb.b), self.din["s5_row"][li, d_].partition_broadcast(128))
            for half in range(2):
                hs_ = slice(half * 512, (half + 1) * 512)
                a_re = rowb[:, 0, hs_]
                a_im = rowb[:, 1, hs_]
                dt, dre, dim, rho, sn, cs, x1, x2 = [p[:] for p in pt]
                kb.act_(dt, rowb[:, 2, hs_], AF.Exp)
                kb.tt(kb.dve, dre, dt, a_re, ALU.mult)
                kb.tt(kb.dve, dim, dt, a_im, ALU.mult)
                kb.act_(rho, dre, AF.Exp)
                kb.ts(kb.dve, pki[:], dim, 1.0 / TWO_PI, None, ALU.mult)
                kb.cp(kb.dve, x2, pki[:])
                kb.stt(kb.dve, x1, x2, -TWO_PI, dim, ALU.mult, ALU.add)
                kb.ts(kb.dve, x1, x1, -3.141592, 3.141592, ALU.max, ALU.min)
                kb.act_(sn, x1, AF.Sin)
                kb.stt(kb.dve, x2, x1, -1.0, x1, ALU.mult, ALU.max)
                kb.act_(cs, x2, AF.Sin, scale=-1.0, bias=self.halfpi[:])
                kb.tt(kb.dve, cs, cs, rho, ALU.mult)
                kb.ts(kb.dve, cs, cs, -1.0, None, ALU.add)
                kb.tt(kb.dve, sn, sn, rho, ALU.mult)
                kb.tt(kb.dve, dt, a_re, a_re, ALU.mult)
                kb.tt(kb.dve, dre, a_im, a_im, ALU.mult)
                kb.tt(kb.dve, dt, dt, dre, ALU.add)
                kb.op(kb.dve, lambda h: h.reciprocal(pt[0].t[:], pt[0].t[:]), reads=[pt[0].b], writes=[pt[0].b])
                kb.tt(kb.dve, x1, cs, a_re, ALU.mult)
                kb.tt(kb.dve, dre, sn, a_im, ALU.mult)
                kb.tt(kb.dve, x1, x1, dre, ALU.add)
                kb.tt(kb.dve, x1, x1, dt, ALU.mult)
                kb.tt(kb.dve, x2, sn, a_re, ALU.mult)
                kb.tt(kb.dve, dre, cs, a_im, ALU.mult)
                kb.tt(kb.dve, x2, x2, dre, ALU.subtract)
                kb.tt(kb.dve, x2, x2, dt, ALU.mult)
                for r in range(2):
                    kb.ld(kb.sp, braw[r][:], self.din["s5_bT"][li, d_, r, half])
                ta = pt[1][:]
                tb_ = pt[2][:]
                kb.tt(kb.dve, ta, braw[0][:], x1, ALU.mult)
                kb.tt(kb.dve, tb_, braw[1][:], x2, ALU.mult)
                kb.tt(kb.dve, Bm[half][0][:], ta, tb_, ALU.subtract)
                kb.tt(kb.dve, ta, braw[1][:], x1, ALU.mult)
                kb.tt(kb.dve, tb_, braw[0][:], x2, ALU.mult)
                kb.tt(kb.dve, Bm[half][1][:], ta, tb_, ALU.add)
                yield
            kb.ld(kb.sp, col[:], self.din["s5_col"][li, d_])
            kb.act_(col[:, 2, :], col[:, 2, :], AF.Exp)
            kb.tt(kb.dve, thc[:], col[:, 2, :], col[:, 1, :], ALU.mult)
            kb.tt(kb.dve, rhoc[:], col[:, 2, :], col[:, 0, :], ALU.mult)
            kb.act_(rhoc[:], rhoc[:], AF.Exp)
            a8, k8, f8 = tl["a8"], tl["k8"], tl["f8"]
            cs256, sn256 = tl["cs256"], tl["sn256"]
            kb.ts(kb.dve, a8[:], thc[:], float(W5), None, ALU.mult)
            kb.ts(kb.dve, k8[:], a8[:], 1.0 / TWO_PI, None, ALU.mult)
            kb.cp(kb.dve, f8[:], k8[:])
            kb.stt(kb.dve, a8[:], f8[:], -TWO_PI, a8[:], ALU.mult, ALU.add)
            kb.ts(kb.dve, a8[:], a8[:], -3.141592, 3.141592, ALU.max, ALU.min)
            kb.act_(sn256[:], a8[:], AF.Sin)
            kb.stt(kb.dve, f8[:], a8[:], -1.0, a8[:], ALU.mult, ALU.max)
            kb.act_(cs256[:], f8[:], AF.Sin, scale=-1.0, bias=self.halfpi[:])
            if d_ == 0:
                order = [(t0, W, False) for (t0, W) in seq]
            else:
                order = [(0, CTX, True)] + [(t0, W, True) for (t0, W) in reversed(seq[1:])]
            hreL, himL, car, th0, rhoT, CmL, Cf, pp, ppb = d["hre"], d["him"], d["car"], d["th0"], d["rhoT"], d["Cm"], d["Cf"], d["pp"], d["ppb"]
            for pair in range(8):
                half = pair // 4
                c0 = (pair % 4) * 128
                Cm = CmL[pair % 2]
                for r in range(2):
                    kb.ld(kb.sp, Cf[r][:], self.din["s5_cT"][li, d_, r, pair])
                kb.cp(kb.dve, Cm[0][:], Cf[0][:])
                kb.ts(kb.dve, Cm[1][:], Cf[1][:], -1.0, None, ALU.mult)
                kb.ts(kb.dve, rhoT[:], self.iota512[:, :W5], 0.0, rhoc[:, pair:pair + 1], ALU.mult, ALU.add)
                tau0 = 0
                for ci, (t0, W, rev) in enumerate(order):
                    def view(tile):
                        ap = tile.t[:, t0:t0 + W]
                        if rev:
                            ap = ap[:, ::-1]
                        return V(ap, tile.b)
                    stepno = self._s5step
                    self._s5step += 1
                    sp_ = stepno % 2
                    pbr = V(pp[sp_].t[:, 0:W], ppb[sp_][0])
                    pbi = V(pp[sp_].t[:, 256:256 + W], ppb[sp_][1])
                    py = V(pp[1 - sp_].t[:, 0:W], ppb[1 - sp_][0])
                    hre = hreL[stepno % 3]
                    him = himL[stepno % 3]
                    ang, ki, kf, sinT, cosT = d["ang"][sp_], d["ki"][sp_], d["kf"][sp_], d["sinT"][sp_], d["cosT"][sp_]
                    t1, t2, t3, t4 = d["t1"][sp_], d["t2"][sp_], d["t3"][sp_], d["t4"][sp_]
                    bre, bim, gre, gim = d["bre"][sp_], d["bim"][sp_], d["gre"][sp_], d["gim"][sp_]
                    uv = view(uT[half])
                    kb.mm(pbr, Bm[half][0][:, c0:c0 + 128], uv)
                    kb.mm(pbi, Bm[half][1][:, c0:c0 + 128], uv)
                    if ci == 0:
                        kb.ts(kb.dve, ang[:, :W], self.iota512[:, :W], thc[:, pair:pair + 1], None, ALU.mult)
                        kb.ts(kb.dve, ki[:, :W], ang[:, :W], 1.0 / TWO_PI, None, ALU.mult)
                        kb.cp(kb.pool, kf[:, :W], ki[:, :W])
                        kb.stt(kb.dve, ang[:, :W], kf[:, :W], -TWO_PI, ang[:, :W], ALU.mult, ALU.add)
                        kb.ts(kb.dve, ang[:, :W], ang[:, :W], -3.141592, 3.141592, ALU.max, ALU.min)
                        kb.act_(sinT[:, :W], ang[:, :W], AF.Sin)
                        kb.stt(kb.dve, kf[:, :W], ang[:, :W], -1.0, ang[:, :W], ALU.mult, ALU.max)
                        kb.act_(cosT[:, :W], kf[:, :W], AF.Sin, scale=-1.0, bias=self.halfpi[:])
                    else:
                        sinP, cosP = d["sinT"][1 - sp_], d["cosT"][1 - sp_]
                        cD = cs256[:, pair:pair + 1]
                        sD = sn256[:, pair:pair + 1]
                        kb.act_(ang[:, :W], sinP[:, :W], AF.Copy, scale=sD)
                        kb.stt(kb.dve, cosT[:, :W], cosP[:, :W], cD, ang[:, :W], ALU.mult, ALU.subtract)
                        kb.act_(kf[:, :W], cosP[:, :W], AF.Copy, scale=sD)
                        kb.stt(kb.dve, sinT[:, :W], sinP[:, :W], cD, kf[:, :W], ALU.mult, ALU.add)
                    kb.tt(kb.dve, t1[:, :W], pbr, cosT[:, :W], ALU.mult)
                    kb.tt(kb.dve, t2[:, :W], pbi, sinT[:, :W], ALU.mult)
                    kb.tt(kb.pool, bre[:, :W], t1[:, :W], t2[:, :W], ALU.add)
                    kb.tt(kb.dve, t3[:, :W], pbi, cosT[:, :W], ALU.mult)
                    kb.tt(kb.dve, t4[:, :W], pbr, sinT[:, :W], ALU.mult)
                    kb.tt(kb.pool, bim[:, :W], t3[:, :W], t4[:, :W], ALU.subtract)
                    for (g_, b_, cc) in ((gre, bre, 0), (gim, bim, 1)):
                        init = 0.0 if ci == 0 else car.t[:, cc:cc + 1]
                        rd = [rhoT.b, b_.b] + ([] if ci == 0 else [car.b])
                        kb.op(kb.dve, lambda h: h.tensor_tensor_scan(g_.t[:, :W], rhoT.t[:, :W], b_.t[:, :W], init, ALU.mult, ALU.add),
                              reads=rd, writes=[g_.b])
                    kb.cp(kb.act, car[:, 0:1], gre[:, W - 1:W])
                    kb.cp(kb.act, car[:, 1:2], gim[:, W - 1:W])
                    kb.tt(kb.pool, t1[:, :W], gre[:, :W], cosT[:, :W], ALU.mult)
                    kb.tt(kb.pool, t2[:, :W], gim[:, :W], sinT[:, :W], ALU.mult)
                    kb.tt(kb.pool, hre[:, :W], t1[:, :W], t2[:, :W], ALU.subtract)
                    kb.tt(kb.pool, t3[:, :W], gre[:, :W], sinT[:, :W], ALU.mult)
                    kb.tt(kb.pool, t4[:, :W], gim[:, :W], cosT[:, :W], ALU.mult)
                    kb.tt(kb.pool, him[:, :W], t3[:, :W], t4[:, :W], ALU.add)
                    def readout(py=py, Cm=Cm, hre=hre, him=him, W=W, yv=view(yT[half])):
                        kb.mm(py, Cm[0][:], hre[:, :W], start=True, stop=False)
                        kb.mm(py, Cm[1][:], him[:, :W], start=False, stop=True)
                        kb.tt(kb.dve, yv, yv, py, ALU.add)
                    self._s5pend.append(readout)
                    if len(self._s5pend) > 2:
                        self._s5pend.pop(0)()
                    tau0 += W
                    yield
        while self._s5pend:
            self._s5pend.pop(0)()

    def s5_glu(self, li, tl):
        kb = self.kb
        yT, dn = tl["yT"], tl["dn"]
        if "dbg_y" in self.debug:
            dy = self.nc.dram_tensor("dbg_y", [256, T], F32, kind="ExternalOutput").ap()
            for half in range(2):
                kb.st(kb.sp, dy[half * 128:(half + 1) * 128, :], yT[half][:])
        with contextlib.ExitStack() as gl:
            glu = kb.sb("glu", [128, 2, 256], BF16, stack=gl)
            kb.ld(kb.pool, glu[:], self.din["s5_glu"][li].rearrange("(kt p) n -> p kt n", p=128))
            gf = [kb.sb("gf%d" % i, [128, 512], stack=gl) for i in range(2)]
            gb = [kb.sb("gb%d" % i, [128, 512], BF16, stack=gl) for i in range(2)]
            pg = [kb.ps("pg%d" % i, [128, 512], stack=gl) for i in range(2)]
            prs = kb.ps("prs5", [128, 4], stack=gl)
            sg = kb.sb("sg", [128, 512], stack=gl)
            o_ = [kb.sb("s5o%d" % i, [128, 512], stack=gl) for i in range(2)]
            ob = [kb.sb("s5ob%d" % i, [128, 512], BF16, stack=gl) for i in range(2)]
            r4 = kb.sb("s5r4", [128, 8], stack=gl)
            for (t0, W) in TOK_CHUNKS:
                nsub = W // 128
                for half in range(2):
                    kb.act_(gf[half][:, :W], yT[half][:, t0:t0 + W], AF.Gelu)
                    kb.cp(kb.pool, gb[half][:, :W], gf[half][:, :W])
                for mo in range(2):
                    for kt in range(2):
                        kb.mm(pg[mo][:, :W], glu[:, kt, mo * 128:(mo + 1) * 128], gb[kt][:, :W], start=(kt == 0), stop=(kt == 1))
                    kb.act_(sg[:, :W], pg[mo][:, :W], AF.Sigmoid)
                    kb.tt(kb.dve, o_[mo][:, :W], sg[:, :W], gf[mo][:, :W], ALU.mult)
                    kb.act_(ob[mo][:, :W], o_[mo][:, :W], AF.Copy, scale=dn[:, 1, mo:mo + 1])
                    kb.st(kb.sp, self.mixT[256 + mo * 128:256 + (mo + 1) * 128, t0:t0 + W], ob[mo][:, :W])
                    kb.tt(kb.dve, o_[mo][:, :W], o_[mo][:, :W], o_[mo][:, :W], ALU.mult)
                kb.tt(kb.dve, o_[0][:, :W], o_[0][:, :W], o_[1][:, :W], ALU.add)
                for sub in range(nsub):
                    kb.mm(prs[:, sub:sub + 1], o_[0][:, sub * 128:(sub + 1) * 128], self.ones_f[:, 0:1])
                ti0 = t0 // 128
                kb.ts(kb.dve, r4[:, 0:nsub], prs[:, 0:nsub], 1.0 / 256.0, EPS, ALU.mult, ALU.add)
                kb.act_(r4[:, 4:4 + nsub], r4[:, 0:nsub], AF.Sqrt)
                kb.op(kb.dve, lambda h: h.reciprocal(self.r_s5.t[:, ti0:ti0 + nsub], r4.t[:, 4:4 + nsub]),
                      reads=[r4.b], writes=[self.r_s5.b])
            kb.barrier()

    def s5_attn(self, li):
        kb = self.kb
        do_s5 = self.ph("s5")
        do_at = self.ph("attn")
        with contextlib.ExitStack() as st:
            tl = self.s5_alloc(li, st) if do_s5 else None
            gen = self.s5_scan_gen(li, tl) if do_s5 else None
            state = {"n": 0, "done": gen is None}

            def tick():
                if state["done"]:
                    return
                state["n"] += 1
                if state["n"] % self.S5_TICK == 0:
                    try:
                        next(gen)
                    except StopIteration:
                        state["done"] = True

            if do_at:
                self.attn(li, tick)
            if gen is not None:
                for _ in gen:
                    pass
            kb.barrier()
            if do_s5:
                self.s5_glu(li, tl)

    S5_TICK = 7

    def attn(self, li, tick=lambda: None):
        kb = self.kb
        lid = self.layer_ids[li]
        lam_init = 0.8 - 0.6 * math.exp(-0.3 * lid)
        need_ctx = lid < DEPTH - 1
        with contextlib.ExitStack() as ph:
            al = kb.sb("al", [128, 256], stack=ph)
            kb.ld(kb.sp, al[:], self.din["att_l"][li].partition_broadcast(128))
            prod = kb.sb("alp", [128, 128], stack=ph)
            kb.tt(kb.dve, prod[:, 0:64], al[:, 0:64], al[:, 64:128], ALU.mult)
            kb.tt(kb.dve, prod[:, 64:128], al[:, 128:192], al[:, 192:256], ALU.mult)
            s12 = kb.sb("s12", [128, 4], stack=ph)
            for i in range(2):
                kb.op(kb.dve, lambda h: h.reduce_sum(s12.t[:, i:i + 1], prod.t[:, i * 64:(i + 1) * 64], AX.X),
                      reads=[prod.b], writes=[s12.b])
            kb.act_(s12[:, 0:2], s12[:, 0:2], AF.Exp)
            kb.tt(kb.dve, s12[:, 2:3], s12[:, 1:2], s12[:, 0:1], ALU.subtract)
            kb.ts(kb.dve, s12[:, 3:4], s12[:, 2:3], -lam_init, None, ALU.add)
            neglam = s12[:, 3:4]
            gsub = kb.sb("gsub", [128, 128], stack=ph)
            kb.ld(kb.sp, gsub[:], self.din["att_g"][li].partition_broadcast(128))
            kb.ts(kb.dve, gsub[:], gsub[:], 1.0 - lam_init, None, ALU.mult)
            negM = kb.sb("negM", [128, 8], stack=ph)
            kb.tt(kb.dve, negM[:], self.qkmax[:, 0:8], self.qkmax[:, 8:16], ALU.mult)
            kb.act_(negM[:], negM[:], AF.Sqrt)
            kb.ts(kb.dve, negM[:], negM[:], -1.0, None, ALU.mult)
            QTh = [kb.sb("QTh%d" % i, [128, T], BF16, stack=ph) for i in range(1)]
            QrTh = [kb.sb("QrTh%d" % i, [128, LAT], BF16, stack=ph) for i in range(1)]
            KcTh = [kb.sb("KcTh%d" % i, [128, T], BF16, stack=ph) for i in range(1)]
            Vh = [kb.sb("Vh%d" % i, [128, NT, 129], BF16, stack=ph) for i in range(1)]
            pS = [kb.ps("pS%d" % i, [128, 512], stack=ph) for i in range(2)]
            acc = [kb.ps("aacc%d" % i, [128, 512], stack=ph) for i in range(4)]
            ptr = pS[0]
            Pb = [kb.sb("Pb%d" % i, [128, 512], BF16, stack=ph) for i in range(3)]
            om = [kb.sb("om%d" % i, [128, 4, 128], stack=ph) for i in range(2)]
            o_ = kb.sb("ao", [128, 4, 128], stack=ph)
            junk = kb.sb("ajunk", [128, 128], stack=ph)
            ssq = kb.sb("assq", [128, 12], stack=ph)
            rl = kb.sb("arl", [128, 4], stack=ph)
            att = kb.sb("att", [128, 4, 128], stack=ph)
            attT = [kb.sb("attT%d" % i, [128, 512], BF16, stack=ph) for i in range(2)]
            accS = [kb.sb("accS%d" % i, [128, 4, 129], stack=ph) for i in range(2)]
            n = 0
            nchunk = 0
            pend = []
            tk = {"n": 0}

            def tick2(free0):
                tk["n"] += 1
                while pend and pend[0][0] <= tk["n"] and (free0 or not pend[0][2]):
                    pend.pop(0)[1]()
                tick()

            def flush():
                while pend:
                    pend.pop(0)[1]()

            for hh in range(4):
                b_ = 0
                flush()
                kb.ld(kb.sp, QTh[b_][:], self.QT[hh * 128:(hh + 1) * 128, :])
                kb.ld(kb.act, QrTh[b_][:], self.QrT[hh * 128:(hh + 1) * 128, :])
                kb.ld(kb.sp, KcTh[b_][:], self.KcT[hh * 128:(hh + 1) * 128, :])
                kb.ld(kb.act, Vh[b_][:], self.Vaug.rearrange("t p c -> p t c")[:, :, hh * 129:(hh + 1) * 129])
                qchunks = [(CTX + qc * 512, 512, False) for qc in range(8)]
                if need_ctx:
                    qchunks.append((0, CTX, True))
                for (q0, W, isctx) in qchunks:
                    nsub = W // 128
                    kts = [0, 1] if isctx else list(range(NT))
                    for m in range(2):
                        r0 = m * 64

                        def qk(kk):
                            kt = kts[kk]
                            if kt < 2:
                                qv = QTh[b_][r0:r0 + 64, q0:q0 + W]
                            else:
                                qv = QrTh[b_][r0:r0 + 64, q0 - CTX:q0 - CTX + W]
                            kb.mm(pS[(n + kk) % 2][:, :W], KcTh[b_][r0:r0 + 64, kt * 128:(kt + 1) * 128], qv)
                        qk(0)
                        for kk, kt in enumerate(kts):
                            if kk + 1 < len(kts):
                                qk(kk + 1)
                            p = pS[(n + kk) % 2]
                            pb_ = Pb[(n + kk) % 3]
                            kb.act_(pb_[:, :W], p[:, :W], AF.Exp, bias=negM[:, hh * 2 + m:hh * 2 + m + 1])
                            tick2((n + kk + 1) % 2 == 1 or kk + 1 >= len(kts))
                            for sub in range(nsub):
                                kb.mm(acc[sub][:, 0:129], pb_[:, sub * 128:(sub + 1) * 128], Vh[b_][:, kt, :],
                                      start=(kk == 0), stop=(kk == len(kts) - 1))
                        n += len(kts)
                        flush()
                        for sub in range(nsub):
                            kb.cp(kb.act, accS[m][:, sub, :], acc[sub][:, 0:129])

                        def fin_m(m=m, nsub=nsub):
                            for sub in range(nsub):
                                kb.op(kb.dve, lambda h: h.reciprocal(rl.t[:, sub:sub + 1], accS[m].t[:, sub, 128:129]),
                                      reads=[accS[m].b], writes=[rl.b])
                                kb.ts(kb.dve, om[m][:, sub, :], accS[m][:, sub, 0:128], rl[:, sub:sub + 1], None, ALU.mult)
                        pend.append([tk["n"] + 8, fin_m, False])

                    def fin_chunk(nsub=nsub, W=W, q0=q0, hh=hh):
                        nonlocal nchunk
                        for sub in range(nsub):
                            kb.stt(kb.dve, o_[:, sub, :], om[1][:, sub, :], neglam, om[0][:, sub, :], ALU.mult, ALU.add)
                            kb.act_(junk[:], o_[:, sub, :], AF.Square, accum=ssq[:, sub:sub + 1])
                        kb.ts(kb.dve, ssq[:, 4:4 + nsub], ssq[:, 0:nsub], 1.0 / 128.0, EPS, ALU.mult, ALU.add)
                        kb.act_(ssq[:, 8:8 + nsub], ssq[:, 4:4 + nsub], AF.Sqrt)
                        kb.op(kb.dve, lambda h: h.reciprocal(ssq.t[:, 4:4 + nsub], ssq.t[:, 8:8 + nsub]), reads=[ssq.b], writes=[ssq.b])
                        for sub in range(nsub):
                            kb.stt(kb.dve, att[:, sub, :], o_[:, sub, :], ssq[:, 4 + sub:5 + sub], gsub[:], ALU.mult, ALU.mult)
                            kb.tr(ptr[:, sub * 128:(sub + 1) * 128], att[:, sub, :], self.ident_f[:])
                        at = attT[nchunk % 2]
                        nchunk += 1
                        kb.cp(kb.act, at[:, :W], ptr[:, :W])
                        kb.st(kb.sp, self.mixT[512 + hh * 128:512 + (hh + 1) * 128, q0:q0 + W], at[:, :W])
                    pend.append([tk["n"] + 16, fin_chunk, True])
            flush()
            kb.barrier()

    def outproj(self, li):
        kb = self.kb
        lid = self.layer_ids[li]
        need_ctx = lid < DEPTH - 1
        src = self.xsrc(li)
        with contextlib.ExitStack() as ph:
            wo = kb.sb("wo", [128, 8, D], BF16, stack=ph)
            for kt in range(8):
                kb.ld(kb.pool, wo[:, kt, :], self.din["w_out"][li][kt * 128:(kt + 1) * 128, :])
            g1b = [self.load_mod_b(li, which, 2, ph, "g1b%d" % which) for which in range(2)]
            mx = [kb.sb("mx%d" % i, [128, 8, 512], BF16, stack=ph) for i in range(2)]
            xt = [kb.sb("oxt%d" % i, [128, D], stack=ph) for i in range(2)]
            tt_ = [kb.sb("ott%d" % i, [128, D], stack=ph) for i in range(2)]
            pA = kb.ps("pA", [128, D], stack=ph)
            pB = kb.ps("pB", [128, D], stack=ph)
            pC = kb.ps("pC", [128, D], stack=ph)
            n = 0
            for ci, (t0, W) in enumerate(TOK_CHUNKS):
                if ci == 0 and not need_ctx:
                    continue
                which = 1 if ci == 0 else 0
                m_ = mx[ci % 2]
                kb.ld(kb.sp, m_[:, :, :W], self.mixT[:, t0:t0 + W].rearrange("(kt p) t -> p kt t", p=128))
                for ti in range(W // 128):
                    gi = t0 // 128 + ti
                    x = xt[n % 2]
                    t = tt_[n % 2]
                    n += 1
                    kb.ld(kb.act, x[:], src[gi * 128:(gi + 1) * 128, :])
                    for (ps_, kts) in ((pA, (0, 1)), (pB, (2, 3)), (pC, (4, 5, 6, 7))):
                        for hf in range(2):
                            for kt in kts:
                                kb.mm(ps_[:, hf * 512:(hf + 1) * 512], m_[:, kt, ti * 128:(ti + 1) * 128], wo[:, kt, hf * 512:(hf + 1) * 512],
                                      start=(kt == kts[0]), stop=(kt == kts[-1]))
                    kb.ts(kb.dve, t[:], pA[:], self.r_hy[:, gi:gi + 1], None, ALU.mult)
                    kb.stt(kb.dve, t[:], pB[:], self.r_s5[:, gi:gi + 1], t[:], ALU.mult, ALU.add)
                    kb.tt(kb.dve, t[:], t[:], pC[:], ALU.add)
                    kb.tt(kb.pool, t[:], t[:], g1b[which][:], ALU.mult)
                    kb.tt(kb.pool, t[:], t[:], x[:], ALU.add)
                    kb.st(kb.sp, self.Xres[gi * 128:(gi + 1) * 128, :], t[:])
            kb.barrier()

    def moe(self, li):
        kb = self.kb
        nc = self.nc
        lid = self.layer_ids[li]
        need_ctx = lid < DEPTH - 1
        tiles = list(range(NT)) if need_ctx else list(range(2, NT))
        NB = NBLK
        with contextlib.ExitStack() as ph:
            eid = kb.sb("eid", [128, NT, 2], stack=ph)
            gate = kb.sb("gate", [128, NT, 2], stack=ph)
            rsel = kb.sb("rsel", [128, NT, 2], stack=ph)
            dest_i = kb.sb("dest_i", [128, NT, 2], I32, stack=ph)
            cnt_b = kb.sb("cnt_b", [128, 32], stack=ph)
            widx = kb.sb("widx", [128, NB], I32, stack=ph)
            kb.memset(kb.dve, cnt_b[:], 0.0)
            e_iota = kb.sb("e_iota", [128, 32], stack=ph)
            kb.op(kb.pool, lambda h: h.iota(e_iota.t[:], [[1, 32]], base=0, channel_multiplier=0, allow_small_or_imprecise_dtypes=True),
                  writes=[e_iota.b])
            g2b = [self.load_mod_b(li, which, 5, ph, "g2b%d" % which) for which in range(2)]
            bslot = Buf()
            bHs = Buf()
            with contextlib.ExitStack() as sa:
                ab = self.norm_mod_tiles(li, 4 + lid, 3, 4, sa)
                wr = kb.sb("wr", [128, 8, 36], stack=sa)
                kb.ld(kb.sp, wr[:], self.din["moe_wr"][li].rearrange("(kt p) n -> p kt n", p=128))
                brb = kb.sb("brb", [128, 36], stack=sa)
                kb.ld(kb.sp, brb[:], self.din["moe_br"][li].partition_broadcast(128))
                t_lo = tiles[0]
                n_ = NT - t_lo
                lg_all = kb.sb("lg_all", [128, NT, 36], stack=sa)
                zrow = kb.sb("zrow", [1, D], BF16, stack=sa)
                kb.memset(kb.dve, zrow[:], 0.0)
                kb.st(kb.sp, self.Hs[T:T + 1, :], zrow[:], writes=[bHs])
                with contextlib.ExitStack() as p1:
                    xt = [kb.sb("mxt%d" % i, [128, D], stack=p1) for i in range(2)]
                    hn = [kb.sb("mhn%d" % i, [128, D], stack=p1) for i in range(2)]
                    hb = [kb.sb("mhb%d" % i, [128, D], BF16, stack=p1) for i in range(2)]
                    junk = kb.sb("mjunk", [128, D], stack=p1)
                    ss = [kb.sb("mss%d" % i, [128, 4], stack=p1) for i in range(2)]
                    hT = [kb.sb("mhT%d" % i, [128, 8, 128], stack=p1) for i in range(2)]
                    trp = [kb.ps("mtrp%d" % i, [128, D], stack=p1) for i in range(2)]
                    pl = [kb.ps("mpl%d" % i, [128, 64], stack=p1) for i in range(2)]
                    for n, ti in enumerate(tiles):
                        which = 1 if ti < 2 else 0
                        x = xt[n % 2]
                        kb.ld(kb.sp, x[:], self.Xres[ti * 128:(ti + 1) * 128, :])
                        self.norm_tile(x, ab[which], hn[n % 2], ss[n % 2], junk)
                        h16 = hb[n % 2]
                        kb.cp(kb.act, h16[:], hn[n % 2][:])
                        kb.st(kb.sp, self.Hs[ti * 128:(ti + 1) * 128, :], h16[:], writes=[bHs])
                        for kt in range(8):
                            kb.tr(trp[n % 2][:, kt * 128:(kt + 1) * 128], hn[n % 2][:, kt * 128:(kt + 1) * 128], self.ident_f[:])
                        kb.cp(kb.pool if False else kb.dve, V(hT[n % 2].t[:].rearrange("p k t -> p (k t)"), hT[n % 2].b), trp[n % 2][:])
                        for kt in range(8):
                            kb.mm(pl[n % 2][:, 0:36], hT[n % 2][:, kt, :], wr[:, kt, :], start=(kt == 0), stop=(kt == 7))
                        kb.tt(kb.dve, lg_all[:, ti, :], pl[n % 2][:, 0:36], brb[:], ALU.add)
                    kb.barrier()
                TS = slice(t_lo, NT)
                gmax = kb.sb("gmax", [128, NT, 1], stack=sa)
                ohg = kb.sb("ohg", [128, NT, 4], stack=sa)
                gidx = kb.sb("gidx", [128, NT, 1], stack=sa)
                E4 = kb.sb("E4", [128, NT, 4], stack=sa)
                sg = kb.sb("sg", [128, NT, 1], stack=sa)
                pgr = kb.sb("pgr", [128, NT, 1], stack=sa)
                esel = kb.sb("esel", [128, NT, 8], stack=sa)
                etmp = kb.sb("etmp", [128, NT, 8], stack=sa)
                mx8 = kb.sb("mx8", [128, NT, 8], stack=sa)
                ix8 = kb.sb("ix8", [128, NT, 8], U32, stack=sa)
                i12 = kb.sb("i12", [128, NT, 2], stack=sa)
                sm = kb.sb("msm", [128, NT, 4], stack=sa)
                oh = [kb.sb("oh%d" % i, [128, NT, 32], stack=sa) for i in range(2)]
                ohs = kb.sb("ohs", [128, NT, 32], stack=sa)
                cnt_all = kb.sb("cnt_all", [128, NT, 32], stack=sa)
                rk = kb.sb("rk", [128, NT, 32], stack=sa)
                e_io3 = kb.sb("e_io3", [128, 1, 32], stack=sa)
                kb.cp(kb.dve, e_io3[:, 0, :], e_iota[:])

                def B(v, shape):
                    return V(v.ap.to_broadcast(list(shape)), v.buf)

                kb.op(kb.dve, lambda h: h.tensor_reduce(gmax.t[:, TS, 0], lg_all.t[:, TS, 0:4], AX.X, ALU.max), reads=[lg_all.b], writes=[gmax.b])
                kb.tt(kb.dve, ohg[:, TS, :], lg_all[:, TS, 0:4], B(gmax[:, TS, 0:1], (128, n_, 4)), ALU.is_equal)
                kb.ts(kb.dve, gidx[:, TS, :], ohg[:, TS, 3:4], 3.0, None, ALU.mult)
                kb.stt(kb.dve, gidx[:, TS, :], ohg[:, TS, 2:3], 2.0, gidx[:, TS, :], ALU.mult, ALU.add)
                kb.tt(kb.dve, gidx[:, TS, :], gidx[:, TS, :], ohg[:, TS, 1:2], ALU.add)
                kb.tt(kb.dve, E4[:, TS, :], lg_all[:, TS, 0:4], B(gmax[:, TS, 0:1], (128, n_, 4)), ALU.subtract)
                kb.act_(E4[:, TS, :], E4[:, TS, :], AF.Exp)
                kb.op(kb.dve, lambda h: h.tensor_reduce(sg.t[:, TS, 0], E4.t[:, TS, :], AX.X, ALU.add), reads=[E4.b], writes=[sg.b])
                kb.op(kb.dve, lambda h: h.reciprocal(pgr.t[:, TS, :], sg.t[:, TS, :]), reads=[sg.b], writes=[pgr.b])
                for g in range(4):
                    dst = esel if g == 0 else etmp
                    kb.tt(kb.dve, dst[:, TS, :], lg_all[:, TS, 4 + 8 * g:12 + 8 * g], B(ohg[:, TS, g:g + 1], (128, n_, 8)), ALU.mult)
                    if g > 0:
                        kb.tt(kb.dve, esel[:, TS, :], esel[:, TS, :], etmp[:, TS, :], ALU.add)
                for ti in tiles:
                    kb.op(kb.dve, lambda h: h.max(mx8.t[:, ti, :], esel.t[:, ti, :]), reads=[esel.b], writes=[mx8.b])
                for ti in tiles:
                    kb.op(kb.dve, lambda h: h.max_index(ix8.t[:, ti, :], mx8.t[:, ti, :], esel.t[:, ti, :]), reads=[esel.b, mx8.b], writes=[ix8.b])
                kb.cp(kb.dve, i12[:, TS, :], ix8[:, TS, 0:2])
                kb.stt(kb.dve, eid[:, TS, :], B(gidx[:, TS, 0:1], (128, n_, 2)), 8.0, i12[:, TS, :], ALU.mult, ALU.add)
                kb.tt(kb.dve, sm[:, TS, 0:1], mx8[:, TS, 1:2], mx8[:, TS, 0:1], ALU.subtract)
                kb.act_(sm[:, TS, 1:2], sm[:, TS, 0:1], AF.Exp)
                kb.ts(kb.dve, sm[:, TS, 2:3], sm[:, TS, 1:2], 1.0, None, ALU.add)
                kb.op(kb.dve, lambda h: h.reciprocal(sm.t[:, TS, 3:4], sm.t[:, TS, 2:3]), reads=[sm.b], writes=[sm.b])
                kb.tt(kb.dve, gate[:, TS, 0:1], sm[:, TS, 3:4], pgr[:, TS, :], ALU.mult)
                kb.tt(kb.dve, gate[:, TS, 1:2], gate[:, TS, 0:1], sm[:, TS, 1:2], ALU.mult)
                for k in range(2):
                    kb.tt(kb.dve, oh[k][:, TS, :], B(e_io3[:, 0:1, :], (128, n_, 32)), B(eid[:, TS, k:k + 1], (128, n_, 32)), ALU.is_equal)
                kb.tt(kb.dve, ohs[:, TS, :], oh[0][:, TS, :], oh[1][:, TS, :], ALU.add)
                with contextlib.ExitStack() as p2:
                    pr = kb.ps("mpr", [128, 3, 512], stack=p2)
                    pc = kb.ps("mpc", [128, 3, 512], stack=p2)
                    ncols = n_ * 32
                    ohs_f = ohs.t[:, TS, :].rearrange("p t e -> p (t e)")
                    for c3 in range(3):
                        a = c3 * 512
                        b = min(ncols, a + 512)
                        if a >= b:
                            break
                        kb.mm(pr[:, c3, 0:b - a], self.tri[:], V(ohs_f[:, a:b], ohs.b))
                        kb.mm(pc[:, c3, 0:b - a], self.ones_f[:], V(ohs_f[:, a:b], ohs.b))
                    pr_f = pr.t[:].rearrange("p c n -> p (c n)")
                    pc_f = pc.t[:].rearrange("p c n -> p (c n)")
                    kb.memset(kb.dve, cnt_all[:, t_lo, :], 0.0)
                    for ti in range(t_lo + 1, NT):
                        o0 = (ti - 1 - t_lo) * 32
                        kb.tt(kb.dve, cnt_all[:, ti, :], cnt_all[:, ti - 1, :], V(pc_f[:, o0:o0 + 32], pc.b), ALU.add)
                    o0 = (NT - 1 - t_lo) * 32
                    kb.tt(kb.dve, cnt_b[:], cnt_all[:, NT - 1, :], V(pc_f[:, o0:o0 + 32], pc.b), ALU.add)
                    kb.tt(kb.dve, V(rk.t[:, TS, :].rearrange("p t e -> p (t e)"), rk.b), V(cnt_all.t[:, TS, :].rearrange("p t e -> p (t e)"), cnt_all.b),
                          V(pr_f[:, 0:ncols], pr.b), ALU.add)
                    kb.barrier()
                for k in range(2):
                    kb.tt(kb.dve, ohs[:, TS, :], oh[k][:, TS, :], rk[:, TS, :], ALU.mult)
                    kb.op(kb.dve, lambda h: h.tensor_reduce(rsel.t[:, TS, k], ohs.t[:, TS, :], AX.X, ALU.add), reads=[ohs.b], writes=[rsel.b])
                pad = kb.sb("pad", [128, 32], stack=sa)
                pend = kb.sb("pend", [128, 32], stack=sa)
                pstart = kb.sb("pstart", [128, 1, 32], stack=sa)
                padi = kb.sb("padi", [128, 32], I32, stack=sa)
                kb.ts(kb.dve, pad[:], cnt_b[:], float(NSLOT_BLK - 1), 1.0 / NSLOT_BLK, ALU.add, ALU.mult)
                kb.ts(kb.dve, padi[:], pad[:], -(0.5 - 1.0 / 512.0), None, ALU.add)
                kb.cp(kb.dve, pad[:], padi[:])
                kb.ts(kb.dve, pad[:], pad[:], float(NSLOT_BLK), None, ALU.mult)
                kb.op(kb.dve, lambda h: h.tensor_tensor_scan(pend.t[:], self.ones_f.t[:, 0:32], pad.t[:], 0.0, ALU.mult, ALU.add),
                      reads=[pad.b, self.ones_f.b], writes=[pend.b])
                kb.tt(kb.dve, pstart[:, 0, :], pend[:], pad[:], ALU.subtract)
                dsf = kb.sb("dsf", [128, NT, 2], stack=sa)
                for k in range(2):
                    kb.tt(kb.dve, ohs[:, TS, :], oh[k][:, TS, :], B(pstart[:, 0:1, :], (128, n_, 32)), ALU.mult)
                    kb.op(kb.dve, lambda h: h.tensor_reduce(dsf.t[:, TS, k], ohs.t[:, TS, :], AX.X, ALU.add), reads=[ohs.b], writes=[dsf.b])
                kb.tt(kb.dve, dsf[:, TS, :], dsf[:, TS, :], rsel[:, TS, :], ALU.add)
                kb.cp(kb.dve, dest_i[:, TS, :], dsf[:, TS, :])
                NSC = NPAD // 128
                inif = kb.sb("inif", [128, NSC], stack=sa)
                kb.memset(kb.dve, inif[:], float(T))
                inii = kb.sb("inii", [128, NSC], I32, stack=sa)
                kb.cp(kb.dve, inii[:], inif[:])
                kb.st(kb.sp, self.slot_tok.rearrange("(p a) o -> p (a o)", p=128), inii[:], writes=[bslot])
                tokf = kb.sb("tokf", [128, NT], stack=sa)
                kb.op(kb.pool, lambda h: h.iota(tokf.t[:], [[128, NT]], base=0, channel_multiplier=1, allow_small_or_imprecise_dtypes=True),
                      writes=[tokf.b])
                toki = kb.sb("toki", [128, NT], I32, stack=sa)
                kb.cp(kb.dve, toki[:], tokf[:])
                for ti in tiles:
                    for k in range(2):
                        kb.dma(kb.pool, lambda h: h.indirect_dma_start(
                            out=self.slot_tok, out_offset=bass.IndirectOffsetOnAxis(ap=dest_i.t[:, ti, k:k + 1], axis=0),
                            in_=toki.t[:, ti:ti + 1], in_offset=None), reads=[dest_i.b, toki.b, bslot], writes=[bslot])
                jb = kb.sb("jb", [128, NB], stack=sa)
                kb.op(kb.pool, lambda h: h.iota(jb.t[:], [[NSLOT_BLK, NB]], base=0, channel_multiplier=0, allow_small_or_imprecise_dtypes=True),
                      writes=[jb.b])
                be = kb.sb("be", [128, NB], stack=sa)
                cmp_ = kb.sb("cmp", [128, NB], stack=sa)
                kb.memset(kb.dve, be[:], 0.0)
                for e in range(32):
                    kb.ts(kb.dve, cmp_[:], jb[:], pend[:, e:e + 1], None, ALU.is_ge)
                    kb.tt(kb.dve, be[:], be[:], cmp_[:], ALU.add)
                kb.ts(kb.dve, be[:], be[:], 31.0, 128.0, ALU.min, ALU.mult)
                pcol = kb.sb("pcol", [128, 1], stack=sa)
                kb.op(kb.pool, lambda h: h.iota(pcol.t[:], [[1, 1]], base=0, channel_multiplier=1, allow_small_or_imprecise_dtypes=True),
                      writes=[pcol.b])
                kb.ts(kb.dve, be[:], be[:], pcol[:], None, ALU.add)
                kb.cp(kb.dve, widx[:], be[:])
                kb.barrier()
            with contextlib.ExitStack() as sb_:
                W13 = [kb.sb("W13_%d" % i, [128, 2, 8, 512], BF16, stack=sb_) for i in range(2)]
                W2 = [kb.sb("W2_%d" % i, [128, 4, D], BF16, stack=sb_) for i in range(2)]
                sti = [kb.sb("sti%d" % i, [128, 2], I32, stack=sb_) for i in range(2)]
                X = [kb.sb("mX%d" % i, [128, 2, D], BF16, stack=sb_) for i in range(2)]
                XT = kb.sb("mXT", [128, 8, 256], BF16, stack=sb_)
                ptx = [kb.ps("ptx%d" % i, [128, D], BF16, stack=sb_) for i in range(2)]
                pab = [kb.ps("mpab%d" % i, [128, 512], stack=sb_) for i in range(2)]
                pdL = [kb.ps("mpd%d" % i, [128, D], stack=sb_) for i in range(2)]
                saL = [kb.sb("msa%d" % i, [128, 256], stack=sb_) for i in range(2)]
                hid = kb.sb("hid", [128, 4, 256], BF16, stack=sb_)
                yo = [kb.sb("yo%d" % i, [128, D], stack=sb_) for i in range(2)]
                w13src = self.din["moe_w13h_%d" % li]
                w2src = self.din["moe_w2h_%d" % li]
                for j in range(NB):
                    w13 = W13[j % 2]
                    w2 = W2[j % 2]
                    kb.dma(kb.pool, lambda h: h.indirect_dma_start(
                        out=w13.t[:].rearrange("p a k f -> p (a k f)"), out_offset=None, in_=w13src,
                        in_offset=bass.IndirectOffsetOnAxis(ap=widx.t[:, j:j + 1], axis=0)), reads=[widx.b], writes=[w13.b])
                    kb.dma(kb.pool, lambda h: h.indirect_dma_start(
                        out=w2.t[:].rearrange("p k d -> p (k d)"), out_offset=None, in_=w2src,
                        in_offset=bass.IndirectOffsetOnAxis(ap=widx.t[:, j:j + 1], axis=0)), reads=[widx.b], writes=[w2.b])
                    si = sti[j % 2]
                    kb.dma(kb.sp, lambda h: h.dma_start(out=si.t[:], in_=self.slot_tok[j * 256:(j + 1) * 256, :].rearrange("(s p) o -> p (s o)", p=128),
                                                         allow_slow_non_contiguous=True),
                           reads=[bslot], writes=[si.b])
                    x_ = X[j % 2]
                    for s_ in range(2):
                        kb.dma(kb.pool, lambda h: h.indirect_dma_start(
                            out=x_.t[:, s_, :], out_offset=None, in_=self.Hs,
                            in_offset=bass.IndirectOffsetOnAxis(ap=si.t[:, s_:s_ + 1], axis=0)), reads=[si.b, bHs], writes=[x_.b])
                    for s_ in range(2):
                        p = ptx[s_]
                        for kt in range(8):
                            kb.tr(p[:, kt * 128:(kt + 1) * 128], x_[:, s_, kt * 128:(kt + 1) * 128], self.ident_b[:])
                        kb.cp(kb.act if s_ else kb.dve, XT[:, :, s_ * 128:(s_ + 1) * 128], V(p.t[:].rearrange("p (k t) -> p k t", k=8), p.b))
                    for ft in range(4):
                        pa = pab[ft % 2][:, 0:256]
                        pb = pab[ft % 2][:, 256:512]
                        sa_ = saL[ft % 2]
                        for kt in range(8):
                            kb.mm(pa, w13[:, 0, kt, ft * 128:(ft + 1) * 128], XT[:, kt, :], start=(kt == 0), stop=(kt == 7))
                        for kt in range(8):
                            kb.mm(pb, w13[:, 1, kt, ft * 128:(ft + 1) * 128], XT[:, kt, :], start=(kt == 0), stop=(kt == 7))
                        kb.act_(sa_[:], pa, AF.Silu)
                        kb.tt(kb.dve, hid[:, ft, :], sa_[:], pb, ALU.mult)
                    for s_ in range(2):
                        pd = pdL[s_]
                        for hf in range(2):
                            for ft in range(4):
                                kb.mm(pd[:, hf * 512:(hf + 1) * 512], hid[:, ft, s_ * 128:(s_ + 1) * 128], w2[:, ft, hf * 512:(hf + 1) * 512],
                                      start=(ft == 0), stop=(ft == 3))
                        y_ = yo[s_]
                        kb.cp(kb.act if s_ else kb.dve, y_[:], pd[:])
                        kb.st(kb.sp, self.yb[j * 256 + s_ * 128:j * 256 + (s_ + 1) * 128, :], y_[:])
                kb.barrier()
            with contextlib.ExitStack() as sc_:
                Y = [[kb.sb("mY%d%d" % (i, k), [128, D], stack=sc_) for k in range(2)] for i in range(2)]
                xt = [kb.sb("cxt%d" % i, [128, D], stack=sc_) for i in range(2)]
                t_ = [kb.sb("ct%d" % i, [128, D], stack=sc_) for i in range(2)]
                for n, ti in enumerate(tiles):
                    which = 1 if ti < 2 else 0
                    x = xt[n % 2]
                    kb.ld(kb.sp, x[:], self.Xres[ti * 128:(ti + 1) * 128, :])
                    for k in range(2):
                        y_ = Y[n % 2][k]
                        kb.dma(kb.pool, lambda h: h.indirect_dma_start(
                            out=y_.t[:], out_offset=None, in_=self.yb,
                            in_offset=bass.IndirectOffsetOnAxis(ap=dest_i.t[:, ti, k:k + 1], axis=0)), reads=[dest_i.b], writes=[y_.b])
                    t = t_[n % 2]
                    kb.ts(kb.dve, t[:], Y[n % 2][0][:], gate[:, ti, 0:1], None, ALU.mult)
                    kb.stt(kb.dve, t[:], Y[n % 2][1][:], gate[:, ti, 1:2], t[:], ALU.mult, ALU.add)
                    kb.tt(kb.dve, t[:], t[:], g2b[which][:], ALU.mult)
                    kb.tt(kb.dve, t[:], t[:], x[:], ALU.add)
                    kb.st(kb.sp, self.Xres[ti * 128:(ti + 1) * 128, :], t[:])
                kb.barrier()

    def final_norm(self):
        kb = self.kb
        with contextlib.ExitStack() as ph:
            gb = kb.sb("fgb", [128, D], stack=ph)
            kb.ld(kb.sp, gb[:], self.din["gn_rows"][8].partition_broadcast(128))
            xt = [kb.sb("fxt%d" % i, [128, D], stack=ph) for i in range(2)]
            xn = [kb.sb("fxn%d" % i, [128, D], stack=ph) for i in range(2)]
            junk = kb.sb("fjunk", [128, D], stack=ph)
            ss = kb.sb("fss", [128, 4], stack=ph)
            for n in range(LAT // 128):
                x = xt[n % 2]
                o = xn[n % 2]
                kb.ld(kb.sp, x[:], self.Xres[CTX + n * 128:CTX + (n + 1) * 128, :])
                kb.act_(junk[:], x[:], AF.Square, accum=ss[:, 0:1])
                kb.ts(kb.dve, ss[:, 1:2], ss[:, 0:1], 1.0 / D, EPS, ALU.mult, ALU.add)
                kb.act_(ss[:, 2:3], ss[:, 1:2], AF.Sqrt)
                kb.op(kb.dve, lambda h: h.reciprocal(ss.t[:, 3:4], ss.t[:, 2:3]), reads=[ss.b], writes=[ss.b])
                kb.stt(kb.dve, o[:], x[:], ss[:, 3:4], gb[:], ALU.mult, ALU.mult)
                kb.st(kb.sp, self.out[n * 128:(n + 1) * 128, :], o[:])
            kb.barrier()


LAYERED = ("w_mod", "b_mod", "w_in", "w_out", "hy_cw", "hy_cb", "hy_skip", "hy_ng", "hy_w1", "hy_w2", "hy_w3", "hy_bf",
           "s5_row", "s5_col", "s5_bT", "s5_cT", "s5_dn", "s5_glu", "att_l", "att_g", "moe_wr", "moe_br", "moe_w13h", "moe_w2h")


def split_layers(a):
    for k in ("moe_w13h", "moe_w2h"):
        v = a.pop(k)
        for i in range(v.shape[0]):
            a["%s_%d" % (k, i)] = v[i]
    return a


def make_core_arrays(common, core, layer_ids):
    a = {}
    for k, v in common.items():
        if k in LAYERED:
            a[k] = np.ascontiguousarray(v[list(layer_ids)])
        else:
            a[k] = v
    a.update(core)
    return split_layers(a)


def kernel(**inputs):
    common = split_layers(host_common(inputs))
    arrs = []
    for b in range(8):
        a = dict(common)
        a.update(host_core(inputs, b))
        arrs.append(a)
    p = Prog(arrs[0], list(range(DEPTH)))
    nc = p.build()
    res = run_bass_kernel_spmd(nc, arrs, core_ids=list(range(8)))
    out = np.stack([np.asarray(res.results[b]["out"]) for b in range(8)], axis=0)
    return out.astype(np.float32)
```

```python
import contextlib
import math
import numpy as np
import ml_dtypes
import concourse.bass as bass
import concourse.mybir as mybir
from concourse.bass_utils import run_bass_kernel_spmd

F32 = mybir.dt.float32
BF16 = mybir.dt.bfloat16
I32 = mybir.dt.int32
U32 = mybir.dt.uint32
AF = mybir.ActivationFunctionType
ALU = mybir.AluOpType
AX = mybir.AxisListType

DEPTH = 4
D = 1024
LAT = 4096
CTX = 256
T = LAT + CTX
NT = T // 128
EPS = 1e-6
TWO_PI = 2.0 * math.pi
NSLOT_BLK = 256
NBLK = (2 * T) // NSLOT_BLK + 32
NPAD = NBLK * NSLOT_BLK


class Buf:
    __slots__ = ("writers", "readers")

    def __init__(self):
        self.writers = {}
        self.readers = {}


class V:
    __slots__ = ("ap", "buf")

    def __init__(self, ap, buf):
        self.ap = ap
        self.buf = buf


class Tile:
    def __init__(self, t, buf=None):
        self.t = t
        self.b = buf or Buf()

    def __getitem__(self, idx):
        return V(self.t[idx], self.b)

    def v(self, ap):
        return V(ap, self.b)


class Eng:
    def __init__(self, name, h, is_pe=False):
        self.name = name
        self.h = h
        self.sem = None
        self.count = 0
        self.seen = {}
        self.is_pe = is_pe
        self.dq = []
        self.dqi = 0


EPOCH = 16000
NDQ = 6


class KB:
    def __init__(self, nc, stack):
        self.nc = nc
        self.stack = stack
        self.pe = Eng("pe", nc.tensor, True)
        self.act = Eng("act", nc.scalar)
        self.dve = Eng("dve", nc.vector)
        self.pool = Eng("pool", nc.gpsimd)
        self.sp = Eng("sp", nc.sync)
        self.nsem = 0
        self.ninst = 0
        self.uid = 0
        for e in (self.pe, self.act, self.dve, self.pool):
            e.sem = self.newsem()
        for e in (self.sp, self.act, self.pool):
            e.dq = [[self.newsem(), 0] for _ in range(NDQ)]
        self.engs = (self.pe, self.act, self.dve, self.pool, self.sp)

    def newsem(self):
        self.nsem += 1
        return self.stack.enter_context(self.nc.semaphore("s%d" % self.nsem))

    def name(self, n):
        self.uid += 1
        return "%s_%d" % (n, self.uid)

    def sb(self, name, shape, dt=F32, stack=None):
        st = stack or self.stack
        return Tile(st.enter_context(self.nc.sbuf_tensor(self.name(name), list(shape), dt)))

    def ps(self, name, shape, dt=F32, stack=None):
        st = stack or self.stack
        return Tile(st.enter_context(self.nc.psum_tensor(self.name(name), list(shape), dt)))

    def _deps(self, reads, writes):
        deps = {}
        for b in reads:
            for s, v in b.writers.items():
                if deps.get(s, 0) < v:
                    deps[s] = v
        for b in writes:
            for d in (b.writers, b.readers):
                for s, v in d.items():
                    if deps.get(s, 0) < v:
                        deps[s] = v
        return deps

    def _wait(self, eng, deps):
        for s, v in deps.items():
            if eng.is_pe and s is eng.sem:
                continue
            if eng.seen.get(s, 0) < v:
                eng.h.wait_ge(s, v)
                eng.seen[s] = v

    def _mark(self, tok, reads, writes):
        s, v = tok
        for b in reads:
            if b.readers.get(s, 0) < v:
                b.readers[s] = v
        for b in writes:
            b.writers = {s: v}
            b.readers = {}

    def op(self, eng, fn, reads=(), writes=()):
        reads = [r.buf if isinstance(r, V) else r for r in reads]
        writes = [w.buf if isinstance(w, V) else w for w in writes]
        self._wait(eng, self._deps(reads, writes))
        if eng.count >= EPOCH:
            eng.sem = self.newsem()
            eng.count = 0
        inst = fn(eng.h)
        eng.count += 1
        inst.then_inc(eng.sem, 1)
        self.ninst += 1
        self._mark((eng.sem, eng.count), reads, writes)
        return inst

    def dma(self, eng, fn, reads=(), writes=()):
        reads = [r.buf if isinstance(r, V) else r for r in reads]
        writes = [w.buf if isinstance(w, V) else w for w in writes]
        self._wait(eng, self._deps(reads, writes))
        slot = eng.dq[eng.dqi % NDQ]
        eng.dqi += 1
        if slot[1] >= 30000:
            slot[0] = self.newsem()
            slot[1] = 0
        s, v = slot
        if v > 0 and eng.seen.get(s, 0) < v:
            eng.h.wait_ge(s, v)
            eng.seen[s] = v
        inst = fn(eng.h)
        inst.then_inc(s, 16)
        slot[1] = v + 16
        self.ninst += 1
        self._mark((s, v + 16), reads, writes)
        return inst

    def barrier(self):
        toks = {}
        for e in (self.pe, self.act, self.dve, self.pool):
            if e.count > 0:
                toks[e.sem] = e.count
        for e in (self.sp, self.act, self.pool):
            for s, v in e.dq:
                if v > 0:
                    toks[s] = v
        for e in self.engs:
            for s, v in toks.items():
                if s is e.sem:
                    continue
                if e.seen.get(s, 0) < v:
                    e.h.wait_ge(s, v)
                    e.seen[s] = v

    def mm(self, o, lhsT, rhs, start=True, stop=True):
        return self.op(self.pe, lambda h: h.matmul(o.ap, lhsT.ap, rhs.ap, start=start, stop=stop),
                       reads=[lhsT, rhs], writes=[o])

    def tr(self, o, i, ident):
        return self.op(self.pe, lambda h: h.transpose(o.ap, i.ap, ident.ap), reads=[i, ident], writes=[o])

    def act_(self, o, i, func, bias=None, scale=None, accum=None, eng=None):
        reads = [i]
        kw = {}
        if bias is not None:
            if isinstance(bias, V):
                reads.append(bias)
                kw["bias"] = bias.ap
            else:
                kw["bias"] = bias
        if scale is not None:
            if isinstance(scale, V):
                reads.append(scale)
                kw["scale"] = scale.ap
            else:
                kw["scale"] = scale
        writes = [o]
        if accum is not None:
            kw["accum_out"] = accum.ap
            writes.append(accum)
        return self.op(self.act, lambda h: h.activation(out=o.ap, in_=i.ap, func=func, **kw), reads=reads, writes=writes)

    def tt(self, eng, o, a, b, op):
        return self.op(eng, lambda h: h.tensor_tensor(o.ap, a.ap, b.ap, op), reads=[a, b], writes=[o])

    def ts(self, eng, o, a, s1, s2, op0, op1=None):
        reads = [a]
        x1 = s1.ap if isinstance(s1, V) else s1
        x2 = s2.ap if isinstance(s2, V) else s2
        if isinstance(s1, V):
            reads.append(s1)
        if isinstance(s2, V):
            reads.append(s2)
        if op1 is None:
            return self.op(eng, lambda h: h.tensor_scalar(o.ap, a.ap, x1, None, op0), reads=reads, writes=[o])
        return self.op(eng, lambda h: h.tensor_scalar(o.ap, a.ap, x1, x2, op0, op1), reads=reads, writes=[o])

    def stt(self, eng, o, a, s, b, op0, op1):
        reads = [a, b]
        xs = s.ap if isinstance(s, V) else s
        if isinstance(s, V):
            reads.append(s)
        return self.op(eng, lambda h: h.scalar_tensor_tensor(o.ap, a.ap, xs, b.ap, op0, op1), reads=reads, writes=[o])

    def cp(self, eng, o, i):
        if eng is self.act:
            return self.op(eng, lambda h: h.copy(o.ap, i.ap), reads=[i], writes=[o])
        return self.op(eng, lambda h: h.tensor_copy(o.ap, i.ap), reads=[i], writes=[o])

    def memset(self, eng, o, val):
        return self.op(eng, lambda h: h.memset(o.ap, val), writes=[o])

    def ld(self, q, o, src_ap, reads=()):
        return self.dma(q, lambda h: h.dma_start(out=o.ap, in_=src_ap), reads=list(reads), writes=[o])

    def st(self, q, dst_ap, i, writes=()):
        return self.dma(q, lambda h: h.dma_start(out=dst_ap, in_=i.ap), reads=[i], writes=list(writes))


def _col(v, nt):
    return np.ascontiguousarray(np.asarray(v, np.float32).reshape(nt, 128).T)


def host_constants():
    c = {}
    f32 = np.float32
    for tag, L in (("L", LAT), ("C", CTX)):
        t = (np.arange(L, dtype=f32) / f32(L)).astype(f32)
        ang = (f32(2.0 * math.pi) * t[:, None] * np.arange(1, 17, dtype=f32)).astype(f32)
        feat = np.concatenate([t[:, None], np.cos(ang), np.sin(ang)], axis=-1).astype(f32)
        c["featT_" + tag] = np.ascontiguousarray(feat.T)
        c["negt_" + tag] = _col(-t, L // 128)
        N = 2 * L
        k = np.arange(L, dtype=np.float64)
        th = 2.0 * np.pi * np.outer(k + 0.5, k + 0.5) / N
        c["dftC_" + tag] = np.cos(th).astype(ml_dtypes.bfloat16)
        c["dftS_" + tag] = np.sin(th).astype(ml_dtypes.bfloat16)
        th1 = 2.0 * np.pi * np.outer(k, k + 0.5) / N
        c["dftC1_" + tag] = np.cos(th1).astype(ml_dtypes.bfloat16)
        c["dftS1_" + tag] = np.sin(th1).astype(ml_dtypes.bfloat16)
        ph = np.pi * (k + 0.5) / N
        c["ab_" + tag] = np.ascontiguousarray(np.stack([_col(np.cos(ph), L // 128), _col(np.sin(ph), L // 128)], axis=1))
    dmin = -math.log(1e-2) / 1.5
    dmax = -math.log(1e-2) / 0.3
    c["decay_b"] = np.ascontiguousarray(np.broadcast_to(np.linspace(dmin, dmax, 256, dtype=f32)[None, :], (128, 256)))
    rows = LAT // 64
    row = np.repeat(np.arange(rows, dtype=f32), 64)
    colv = np.tile(np.arange(64, dtype=f32), rows)
    inv = (f32(10000.0) ** (-np.arange(16, dtype=f32) / f32(16))).astype(f32)
    ang = np.concatenate([row[:, None] * inv, colv[:, None] * inv], axis=-1).astype(f32)
    j = (np.arange(128) % 64) % 32
    c["ropeT"] = np.ascontiguousarray(np.stack([np.cos(ang)[:, j].T, np.sin(ang)[:, j].T], axis=1).astype(f32))
    return c


def host_common(inp):
    f32 = np.float32
    g = {k: np.asarray(v) for k, v in inp.items()}
    o = {}
    o["w_mod"] = g["w_mod"]
    o["b_mod"] = g["b_mod"].reshape(DEPTH, 1, 6 * D)
    o["gn_rows"] = np.concatenate([g["norm1_g"], g["norm2_g"], g["final_g"][None]], axis=0).reshape(9, 1, D)
    o["w_in"] = g["w_in"]
    o["w_out"] = g["w_out"]
    o["hy_cw"] = np.ascontiguousarray(g["hy_conv_w"].reshape(DEPTH, 3, 6, 128).transpose(0, 3, 2, 1))
    o["hy_cb"] = np.ascontiguousarray(g["hy_conv_b"].reshape(DEPTH, 6, 128).transpose(0, 2, 1))
    o["hy_skip"] = np.ascontiguousarray(g["hy_skip"].reshape(DEPTH, 2, 128).transpose(0, 2, 1))
    o["hy_ng"] = np.ascontiguousarray(g["hy_norm_g"].reshape(DEPTH, 2, 128).transpose(0, 2, 1))
    o["hy_w1"] = g["hy_ffn_w1"]
    o["hy_w2"] = g["hy_ffn_w2"]
    o["hy_w3"] = g["hy_ffn_w3"]
    o["hy_bf"] = np.ascontiguousarray(np.stack([g["hy_ffn_b1"], g["hy_ffn_b2"], g["hy_freq"]], axis=-1))
    G, P, H = 16, 64, 16
    rows = np.stack([g["s5_a_re"].reshape(DEPTH, 2, G * P), g["s5_a_im"].reshape(DEPTH, 2, G * P),
                     np.repeat(g["s5_log_dt"], P, axis=-1)], axis=2)
    o["s5_row"] = np.ascontiguousarray(rows.reshape(DEPTH, 2, 1, 3 * G * P))
    cols = rows.reshape(DEPTH, 2, 3, 8, 128).transpose(0, 1, 4, 2, 3)
    o["s5_col"] = np.ascontiguousarray(cols)
    bT = np.zeros((DEPTH, 2, 2, 2, 128, 512), f32)
    cT = np.zeros((DEPTH, 2, 2, 8, 128, 128), f32)
    for ri, (bsrc, csrc) in enumerate(((g["s5_b_re"], g["s5_c_re"]), (g["s5_b_im"], g["s5_c_im"]))):
        for gg in range(G):
            half, gm = gg // 8, gg % 8
            bT[:, :, ri, half, gm * 16:(gm + 1) * 16, gm * 64:(gm + 1) * 64] = bsrc[:, :, gg].transpose(0, 1, 3, 2)
            pair, gl = gg // 2, gg % 2
            cT[:, :, ri, pair, gl * 64:(gl + 1) * 64, gm * 16:(gm + 1) * 16] = csrc[:, :, gg].transpose(0, 1, 3, 2)
    o["s5_bT"] = bT
    o["s5_cT"] = cT
    o["s5_dn"] = np.ascontiguousarray(np.stack([g["s5_d"].reshape(DEPTH, 2, 128).transpose(0, 2, 1),
                                                 g["s5_norm_g"].reshape(DEPTH, 2, 128).transpose(0, 2, 1)], axis=2))
    o["s5_glu"] = g["s5_glu_w"]
    o["att_l"] = np.concatenate([g["att_lq1"], g["att_lk1"], g["att_lq2"], g["att_lk2"]], axis=-1).reshape(DEPTH, 1, 256)
    o["att_g"] = g["att_subln_g"].reshape(DEPTH, 1, 128)
    o["moe_wr"] = np.ascontiguousarray(np.concatenate([g["moe_wg"], g["moe_we"]], axis=-1))
    o["moe_br"] = np.concatenate([g["moe_bg"], g["moe_be"]], axis=-1).reshape(DEPTH, 1, 36)
    w1 = g["moe_w1"].reshape(DEPTH, 32, 8, 128, 512).transpose(0, 1, 3, 2, 4)
    w3 = g["moe_w3"].reshape(DEPTH, 32, 8, 128, 512).transpose(0, 1, 3, 2, 4)
    o["moe_w13h"] = np.ascontiguousarray(np.stack([w1, w3], axis=3)).reshape(DEPTH, 32 * 128, 2 * 8 * 512)
    o["moe_w2h"] = np.ascontiguousarray(g["moe_w2"].reshape(DEPTH, 32, 4, 128, 1024).transpose(0, 1, 3, 2, 4)).reshape(DEPTH, 32 * 128, 4 * 1024)
    o.update(host_constants())
    return {k: np.ascontiguousarray(v) for k, v in o.items()}


def host_core(inp, b):
    x = np.asarray(inp["x"])[b]
    ctx = np.asarray(inp["ctx"])[b]
    cc = np.stack([_col(np.asarray(inp["c"])[b], 8), _col(np.asarray(inp["c_ctx"]), 8)], axis=-1)
    return {"xin": np.ascontiguousarray(np.concatenate([ctx, x], axis=0)), "cT": np.ascontiguousarray(cc)}


IN_SHAPES = None


def chunks_of(T0, Ttot, W):
    out = []
    t = T0
    while t < Ttot:
        w = min(W, Ttot - t)
        out.append((t, w))
        t += w
    return out


TOK_CHUNKS = [(0, CTX)] + chunks_of(CTX, T, 512)


class Prog:
    def __init__(self, arrays, layer_ids, debug=(), phases=None):
        self.layer_ids = list(layer_ids)
        self.NL = len(self.layer_ids)
        self.debug = set(debug)
        self.phases = phases
        nc = bass.Bass("TRN2", target_bir_lowering=False)
        self.nc = nc
        self.din = {}
        for k, v in arrays.items():
            dt = {np.dtype(np.float32): F32, np.dtype(ml_dtypes.bfloat16): BF16, np.dtype(np.int32): I32}[v.dtype]
            self.din[k] = nc.dram_tensor(k, list(v.shape), dt, kind="ExternalInput").ap()
        self.out = nc.dram_tensor("out", [LAT, D], F32, kind="ExternalOutput").ap()

        def scratch(name, shape, dt):
            kind = "ExternalOutput" if name in self.debug else "Internal"
            return nc.dram_tensor(name, list(shape), dt, kind=kind).ap()

        self.Xres = scratch("Xres", [T, D], F32)
        self.modrow = scratch("modrow", [self.NL, 2, 6 * D], F32)
        self.pT = scratch("pT", [1024, T], F32)
        self.QT = scratch("QT", [512, T], BF16)
        self.QrT = scratch("QrT", [512, LAT], BF16)
        self.KcT = scratch("KcT", [512, T], BF16)
        self.Vaug = scratch("Vaug", [NT, 128, 4 * 129], BF16)
        self.mixT = scratch("mixT", [1024, T], BF16)
        self.Hs = scratch("Hs", [T + 1, D], BF16)
        self.slot_tok = scratch("slot_tok", [NPAD, 1], I32)
        self.yb = scratch("yb", [NPAD, D], F32)

    def build(self):
        nc = self.nc
        with contextlib.ExitStack() as st:
            kb = KB(nc, st)
            self.kb = kb
            self.setup()
            self.modulation()
            kb.barrier()
            for li in range(self.NL):
                self.layer(li)
            self.final_norm()
            kb.barrier()
        return nc

    def ph(self, name):
        return self.phases is None or name in self.phases

    def setup(self):
        kb = self.kb
        self.ident_f = kb.sb("ident_f", [128, 128])
        kb.memset(kb.pool, self.ident_f[:], 0.0)
        kb.op(kb.pool, lambda h: h.affine_select(self.ident_f.t[:], self.ident_f.t[:], [[-1, 128]], ALU.not_equal, 1.0,
                                                 base=0, channel_multiplier=1), reads=[self.ident_f.b], writes=[self.ident_f.b])
        self.ident_b = kb.sb("ident_b", [128, 128], BF16)
        kb.cp(kb.dve, self.ident_b[:], self.ident_f[:])
        self.ones_f = kb.sb("ones_f", [128, 128])
        kb.memset(kb.dve, self.ones_f[:], 1.0)
        self.tri = kb.sb("tri", [128, 128])
        kb.memset(kb.pool, self.tri[:], 1.0)
        kb.op(kb.pool, lambda h: h.affine_select(self.tri.t[:], self.tri.t[:], [[1, 128]], ALU.is_ge, 0.0,
                                                 base=-1, channel_multiplier=-1), reads=[self.tri.b], writes=[self.tri.b])
        self.iota512 = kb.sb("iota512", [128, 512])
        kb.op(kb.pool, lambda h: h.iota(self.iota512.t[:], [[1, 512]], base=0, channel_multiplier=0,
                                        allow_small_or_imprecise_dtypes=True), writes=[self.iota512.b])
        self.blk = []
        for m in range(2):
            b = kb.sb("blk%d" % m, [128, 128], BF16)
            kb.memset(kb.dve, b[:], 0.0)
            kb.memset(kb.dve, b[m * 64:(m + 1) * 64, :], 1.0)
            self.blk.append(b)
        self.r_hy = kb.sb("r_hy", [128, NT])
        self.r_s5 = kb.sb("r_s5", [128, NT])
        self.qkmax = kb.sb("qkmax", [128, 16])
        self.halfpi = kb.sb("halfpi", [128, 1])
        kb.memset(kb.dve, self.halfpi[:], math.pi / 2.0)

    def modulation(self):
        kb = self.kb
        with contextlib.ExitStack() as ph:
            cT = kb.sb("cT", [128, 8, 2], stack=ph)
            kb.ld(kb.sp, cT[:], self.din["cT"])
            sc = kb.sb("sc", [128, 8, 2], stack=ph)
            kb.act_(sc[:], cT[:], AF.Silu)
            wm = [kb.sb("wm%d" % i, [128, 8, 512], stack=ph) for i in range(2)]
            pm = [kb.ps("pm%d" % i, [128, 512], stack=ph) for i in range(2)]
            rows = [kb.sb("mrow%d" % i, [1, 6 * D], stack=ph) for i in range(2)]
            brow = kb.sb("brow", [1, 6 * D], stack=ph)
            n = 0
            for li in range(self.NL):
                kb.ld(kb.sp, brow[:], self.din["b_mod"][li])
                for ch in range(12):
                    w = wm[n % 2]
                    n += 1
                    kb.ld(kb.sp if n % 2 else kb.act, w[:], self.din["w_mod"][li][:, ch * 512:(ch + 1) * 512].rearrange("(kt p) n -> p kt n", p=128))
                    for which in range(2):
                        p = pm[which]
                        for kt in range(8):
                            kb.mm(p[0:1, :], sc[:, kt, which:which + 1], w[:, kt, :], start=(kt == 0), stop=(kt == 7))
                        kb.tt(kb.dve, rows[which][:, ch * 512:(ch + 1) * 512], p[0:1, :], brow[:, ch * 512:(ch + 1) * 512], ALU.add)
                for which in range(2):
                    kb.st(kb.sp, self.modrow[li, which:which + 1, :], rows[which][:])
            kb.barrier()

    def load_mod_b(self, li, which, seg, stack, name):
        t = self.kb.sb(name, [128, D], stack=stack)
        self.kb.ld(self.kb.sp, t[:], self.modrow[li, which:which + 1, seg * D:(seg + 1) * D].partition_broadcast(128))
        return t

    def norm_mod_tiles(self, li, gidx, seg_sh, seg_sc, stack):
        kb = self.kb
        gb = kb.sb("gnb", [128, D], stack=stack)
        kb.ld(kb.sp, gb[:], self.din["gn_rows"][gidx].partition_broadcast(128))
        res = []
        for which in range(2):
            scb = self.load_mod_b(li, which, seg_sc, stack, "scb%d" % which)
            shb = self.load_mod_b(li, which, seg_sh, stack, "shb%d" % which)
            kb.stt(kb.dve, scb[:], scb[:], 1.0, gb[:], ALU.add, ALU.mult)
            res.append((scb, shb))
        return res

    def norm_tile(self, xt, ab, xn, tmp_ss, tmp_junk):
        kb = self.kb
        A, B = ab
        kb.act_(tmp_junk[:], xt[:], AF.Square, accum=tmp_ss[:, 0:1])
        kb.ts(kb.dve, tmp_ss[:, 1:2], tmp_ss[:, 0:1], 1.0 / D, EPS, ALU.mult, ALU.add)
        kb.act_(tmp_ss[:, 2:3], tmp_ss[:, 1:2], AF.Sqrt)
        kb.op(kb.dve, lambda h: h.reciprocal(tmp_ss.t[:, 3:4], tmp_ss.t[:, 2:3]), reads=[tmp_ss.b], writes=[tmp_ss.b])
        kb.stt(kb.dve, xn[:], xt[:], tmp_ss[:, 3:4], A[:], ALU.mult, ALU.mult)
        kb.tt(kb.dve, xn[:], xn[:], B[:], ALU.add)

    def layer(self, li):
        kb = self.kb
        if self.ph("inproj"):
            self.inproj(li)
            kb.barrier()
        if self.ph("hyena"):
            self.hyena(li, LAT, CTX, "L")
            kb.barrier()
            if self.layer_ids[li] < DEPTH - 1:
                self.hyena(li, CTX, 0, "C")
                kb.barrier()
        if self.ph("s5") or self.ph("attn"):
            self.s5_attn(li)
            kb.barrier()
        if self.ph("outproj"):
            self.outproj(li)
            kb.barrier()
        if self.ph("moe"):
            self.moe(li)
            kb.barrier()

    def xsrc(self, li):
        return self.din["xin"] if li == 0 else self.Xres

    def inproj(self, li):
        kb = self.kb
        src = self.xsrc(li)
        with contextlib.ExitStack() as ph:
            w_in = kb.sb("w_in", [128, 8, 2560], BF16, stack=ph)
            for kt in range(8):
                kb.ld(kb.pool, w_in[:, kt, :], self.din["w_in"][li][kt * 128:(kt + 1) * 128, :])
            wrot = kb.sb("wrot", [128, 8, 1024], BF16, stack=ph)
            wrot5 = wrot.t[:].rearrange("p k (b two j) -> p k b two j", two=2, j=32)
            for kt in range(8):
                srcv = self.din["w_in"][li][kt * 128:(kt + 1) * 128, 1024:2048].rearrange("p (b two j) -> p b two j", two=2, j=32)
                kb.dma(kb.pool, lambda h: h.dma_start(out=wrot5[:, kt, :, 0, :], in_=srcv[:, :, 1, :]), writes=[wrot.b])
                kb.dma(kb.pool, lambda h: h.dma_start(out=wrot5[:, kt, :, 1, :], in_=srcv[:, :, 0, :]), writes=[wrot.b])
            for kt in range(8):
                kb.op(kb.dve, lambda h: h.tensor_scalar(wrot5[:, kt, :, 0, :], wrot5[:, kt, :, 0, :], -1.0, None, ALU.mult),
                      reads=[wrot.b], writes=[wrot.b])
            rope = kb.sb("rope", [128, 2, LAT], stack=ph)
            kb.ld(kb.sp, rope[:], self.din["ropeT"])
            ab = self.norm_mod_tiles(li, self.layer_ids[li], 0, 1, ph)
            kb.memset(kb.dve, self.qkmax[:], 0.0)
            xt = [kb.sb("xt%d" % i, [128, D], stack=ph) for i in range(2)]
            xn = kb.sb("xn", [128, D], stack=ph)
            junk = kb.sb("junk", [128, D], stack=ph)
            ss = kb.sb("ss", [128, 4], stack=ph)
            xnT = [kb.sb("xnT%d" % i, [128, 8, 512], BF16, stack=ph) for i in range(2)]
            trp = kb.ps("trp", [128, D], stack=ph)
            pj = [kb.ps("pj%d" % i, [128, 512], stack=ph) for i in range(4)]
            pv = kb.ps("pv", [128, 512], stack=ph)
            nb = kb.ps("nb", [128, 512], stack=ph)
            stg = [kb.sb("stg%d" % i, [128, 512], stack=ph) for i in range(2)]
            stb = [kb.sb("stb%d" % i, [128, 512], BF16, stack=ph) for i in range(3)]
            sqb = kb.sb("sqb", [128, 512], BF16, stack=ph)
            red = kb.sb("red", [128, 1], stack=ph)
            t1 = kb.sb("t1", [128, 512], stack=ph)
            t2 = kb.sb("t2", [128, 512], stack=ph)
            vst = [kb.sb("vst%d" % i, [128, 4, 129], BF16, stack=ph) for i in range(2)]
            for v in vst:
                kb.memset(kb.dve, v[:], 1.0)
            cnt = {"ev": 0, "stg": 0, "stb": 0, "pj": 0, "v": 0, "x": 0}

            def evac_engine():
                cnt["ev"] += 1
                return kb.act if cnt["ev"] % 2 == 0 else kb.dve

            def next_pj():
                cnt["pj"] += 1
                return pj[cnt["pj"] % 4]

            def proj_fm(wv, c0, xc, W):
                p = next_pj()
                for kt in range(8):
                    kb.mm(p[:, :W], V(wv(kt, c0), wv.buf), xc[:, kt, :W], start=(kt == 0), stop=(kt == 7))
                return p

            def w_in_cols(kt, c0):
                return w_in.t[:, kt, c0:c0 + 128]
            w_in_cols.buf = w_in.b

            def w_rot_cols(kt, c0):
                return wrot.t[:, kt, c0:c0 + 128]
            w_rot_cols.buf = wrot.b

            def norm_stat(sv, W, col):
                kb.tt(kb.dve, sqb[:, :W], sv, sv, ALU.mult)
                for m in range(2):
                    kb.mm(nb[:, :W], self.blk[m][:], sqb[:, :W])
                    kb.op(kb.dve, lambda h: h.reduce_max(red.t[:], nb.t[:, :W], AX.X), reads=[nb.b], writes=[red.b])
                    kb.tt(kb.dve, self.qkmax[:, col + m:col + m + 1], self.qkmax[:, col + m:col + m + 1], red[:], ALU.max)

            for ci, (t0, W) in enumerate(TOK_CHUNKS):
                which = 1 if ci == 0 else 0
                xc = xnT[ci % 2]
                ntile = W // 128
                for ti in range(ntile):
                    x = xt[cnt["x"] % 2]
                    cnt["x"] += 1
                    kb.ld(kb.sp, x[:], src[t0 + ti * 128:t0 + (ti + 1) * 128, :])
                    self.norm_tile(x, ab[which], xn, ss, junk)
                    for kt in range(8):
                        kb.tr(trp[:, kt * 128:(kt + 1) * 128], xn[:, kt * 128:(kt + 1) * 128], self.ident_f[:])
                    for hf in range(2):
                        e = evac_engine()
                        kb.cp(e, V(xc.t[:, hf * 4:(hf + 1) * 4, ti * 128:(ti + 1) * 128], xc.b),
                              V(trp.t[:, hf * 512:(hf + 1) * 512].rearrange("p (k t) -> p k t", k=4), trp.b))
                for j in range(8):
                    p = proj_fm(w_in_cols, j * 128, xc, W)
                    s = stg[cnt["stg"] % 2]
                    cnt["stg"] += 1
                    kb.cp(evac_engine(), s[:, :W], p[:, :W])
                    kb.st(kb.sp, self.pT[j * 128:(j + 1) * 128, t0:t0 + W], s[:, :W])
                lat = ci > 0
                tl = t0 - CTX
                for kind in range(2):
                    for hh in range(4):
                        c0 = 1024 + kind * 512 + hh * 128
                        pa = proj_fm(w_in_cols, c0, xc, W)
                        col = kind * 8 + hh * 2
                        if (kind == 0) or (not lat):
                            sbt = stb[cnt["stb"] % 3]
                            cnt["stb"] += 1
                            kb.act_(sbt[:, :W], pa[:, :W], AF.Copy, scale=(0.125 if kind == 0 else 1.0))
                            dst = self.QT if kind == 0 else self.KcT
                            kb.st(kb.sp, dst[hh * 128:(hh + 1) * 128, t0:t0 + W], sbt[:, :W])
                            if not lat or kind == 0:
                                norm_stat(sbt[:, :W], W, col)
                        if lat:
                            pb = proj_fm(w_rot_cols, kind * 512 + hh * 128, xc, W)
                            kb.tt(kb.dve, t1[:, :W], pa[:, :W], rope[:, 0, tl:tl + W], ALU.mult)
                            kb.tt(kb.dve, t2[:, :W], pb[:, :W], rope[:, 1, tl:tl + W], ALU.mult)
                            sbt = stb[cnt["stb"] % 3]
                            cnt["stb"] += 1
                            kb.stt(kb.dve, sbt[:, :W], t1[:, :W], (0.125 if kind == 0 else 1.0), t2[:, :W], ALU.mult, ALU.add) \
                                if kind == 1 else None
                            if kind == 0:
                                kb.tt(kb.dve, t1[:, :W], t1[:, :W], t2[:, :W], ALU.add)
                                kb.act_(sbt[:, :W], t1[:, :W], AF.Copy, scale=0.125)
                                kb.st(kb.sp, self.QrT[hh * 128:(hh + 1) * 128, tl:tl + W], sbt[:, :W])
                            else:
                                kb.st(kb.sp, self.KcT[hh * 128:(hh + 1) * 128, t0:t0 + W], sbt[:, :W])
                                norm_stat(sbt[:, :W], W, col)
                for ti in range(ntile):
                    for kt in range(8):
                        kb.mm(pv[:], xc[:, kt, ti * 128:(ti + 1) * 128], w_in[:, kt, 2048:2560], start=(kt == 0), stop=(kt == 7))
                    vs = vst[cnt["v"] % 2]
                    cnt["v"] += 1
                    kb.cp(evac_engine(), V(vs.t[:, :, 0:128], vs.b), V(pv.t[:].rearrange("p (h d) -> p h d", h=4), pv.b))
                    kb.st(kb.sp, self.Vaug[(t0 // 128) + ti], V(vs.t[:].rearrange("p h d -> p (h d)"), vs.b))

    def range_reduce_sin(self, out, arg, ki, kf, W, rows):
        kb = self.kb
        a = arg[0:rows, :W]
        kb.ts(kb.dve, ki[0:rows, :W], a, 1.0 / TWO_PI, None, ALU.mult)
        kb.cp(kb.dve, kf[0:rows, :W], ki[0:rows, :W])
        kb.stt(kb.dve, a, kf[0:rows, :W], -TWO_PI, a, ALU.mult, ALU.add)
        kb.ts(kb.dve, a, a, -3.141592, 3.141592, ALU.max, ALU.min)
        kb.act_(out, a, AF.Sin)

    def conv3(self, eng, u, pin, W, cw, cb, j):
        kb = self.kb
        kb.ts(eng, u, pin[:, 1:W + 1], cw[:, j, 1:2], cb[:, j:j + 1], ALU.mult, ALU.add)
        kb.stt(eng, u, pin[:, 0:W], cw[:, j, 0:1], u, ALU.mult, ALU.add)
        kb.stt(eng, u, pin[:, 2:W + 2], cw[:, j, 2:3], u, ALU.mult, ALU.add)

    def dft_passes(self, ph, C, S, nt, rhs_fn, ncols, epilogue):
        kb = self.kb
        with contextlib.ExitStack() as sc:
            accC = [kb.ps("accC%d" % j, [128, 512], stack=sc) for j in range(4)]
            accS = [kb.ps("accS%d" % j, [128, 512], stack=sc) for j in range(4)]
            NG = 4 if nt % 4 == 0 else 2
            Cp = [kb.sb("Cp%d" % j, [128, NG, 512], BF16, stack=sc) for j in range(3)]
            Sp = [kb.sb("Sp%d" % j, [128, NG, 512], BF16, stack=sc) for j in range(3)]
            n = 0
            for p0 in range(0, nt, 4):
                kts = min(4, nt - p0)
                for ng in range(0, nt, NG):
                    cp_, sp_ = Cp[n % 3], Sp[n % 3]
                    n += 1
                    kb.ld(kb.sp, cp_[:, :, :kts * 128],
                          C[ng * 128:(ng + NG) * 128, p0 * 128:(p0 + kts) * 128].rearrange("(a p) k -> p a k", p=128))
                    kb.ld(kb.act, sp_[:, :, :kts * 128],
                          S[ng * 128:(ng + NG) * 128, p0 * 128:(p0 + kts) * 128].rearrange("(a p) k -> p a k", p=128))
                    for a_ in range(NG):
                        n_t = ng + a_
                        r = rhs_fn(n_t)
                        rC, rS = r if isinstance(r, tuple) else (r, r)
                        for j in range(kts):
                            kb.mm(accC[j][:, :ncols], cp_[:, a_, j * 128:(j + 1) * 128], rC, start=(n_t == 0), stop=(n_t == nt - 1))
                            kb.mm(accS[j][:, :ncols], sp_[:, a_, j * 128:(j + 1) * 128], rS, start=(n_t == 0), stop=(n_t == nt - 1))
                for j in range(kts):
                    epilogue(p0 + j, accC[j], accS[j])
            kb.barrier()

    def hyena(self, li, L, tok0, tag):
        kb = self.kb
        nt = L // 128
        N = 2 * L
        C = self.din["dftC_" + tag]
        S = self.din["dftS_" + tag]
        with contextlib.ExitStack() as ph:
            G = kb.sb("G", [128, nt, 512], BF16, stack=ph)
            rn = kb.sb("rn", [128, 4], stack=ph)
            ab = kb.sb("ab", [128, 2, nt], stack=ph)
            kb.ld(kb.sp, ab[:], self.din["ab_" + tag])
            with contextlib.ExitStack() as fa:
                hsd = kb.sb("hsd", [128, nt, 512], BF16, stack=fa)
                with contextlib.ExitStack() as f:
                    featT = kb.sb("featT", [33, L], stack=f)
                    kb.ld(kb.sp, featT[:], self.din["featT_" + tag])
                    w1 = kb.sb("hw1", [33, 64], stack=f)
                    kb.ld(kb.sp, w1[:], self.din["hy_w1"][li])
                    w2 = kb.sb("hw2", [64, 64], stack=f)
                    kb.ld(kb.sp, w2[:], self.din["hy_w2"][li])
                    w3 = kb.sb("hw3", [64, 512], stack=f)
                    kb.ld(kb.sp, w3[:], self.din["hy_w3"][li])
                    bf_ = kb.sb("hbf", [64, 3], stack=f)
                    kb.ld(kb.sp, bf_[:], self.din["hy_bf"][li])
                    fb = kb.sb("hfb", [64, 2], stack=f)
                    kb.tt(kb.dve, fb[:, 0:1], bf_[:, 0:1], bf_[:, 2:3], ALU.mult)
                    kb.tt(kb.dve, fb[:, 1:2], bf_[:, 1:2], bf_[:, 2:3], ALU.mult)
                    negt = kb.sb("negt", [128, nt], stack=f)
                    kb.ld(kb.sp, negt[:], self.din["negt_" + tag])
                    decay = kb.sb("decay", [128, 256], stack=f)
                    kb.ld(kb.sp, decay[:], self.din["decay_b"])
                    h1T = kb.sb("h1T", [64, L], stack=f)
                    h2T = kb.sb("h2T", [64, L], stack=f)
                    arg = kb.sb("harg", [64, 512], stack=f)
                    ki = kb.sb("hki", [64, 512], I32, stack=f)
                    kf = kb.sb("hkf", [64, 512], stack=f)
                    pf = [kb.ps("pf%d" % i, [128, 512], stack=f) for i in range(2)]
                    pn = kb.ps("pn", [128, 2], stack=f)
                    n = 0
                    for layer_i, (wv, inT, outT) in enumerate(((w1, featT, h1T), (w2, h1T, h2T))):
                        for (c0, W) in chunks_of(0, L, 512):
                            p = pf[n % 2]
                            n += 1
                            kb.mm(p[0:64, :W], wv[:], inT[:, c0:c0 + W])
                            kb.ts(kb.dve, arg[:, :W], p[0:64, :W], bf_[:, 2:3], fb[:, layer_i:layer_i + 1], ALU.mult, ALU.add)
                            self.range_reduce_sin(outT[:, c0:c0 + W], arg, ki, kf, W, 64)
                    win = kb.sb("win", [128, 256], stack=f)
                    tf = kb.sb("tf", [128, 256], stack=f)
                    tb = kb.sb("tb", [128, 256], stack=f)
                    absacc = kb.sb("absacc", [128, 256], stack=f)
                    for i in range(nt):
                        p = pf[n % 2]
                        n += 1
                        kb.mm(p[:], h2T[:, i * 128:(i + 1) * 128], w3[:])
                        kb.act_(win[:], decay[:], AF.Exp, scale=negt[:, i:i + 1])
                        kb.tt(kb.dve, tf[:], p[:, 0:256], win[:], ALU.mult)
                        kb.tt(kb.dve, tb[:], p[:, 256:512], win[:], ALU.mult)
                        if i == 0:
                            kb.memset(kb.dve, tb[0:1, :], 0.0)
                        kb.tt(kb.pool, hsd[:, i, 0:256], tf[:], tb[:], ALU.add)
                        kb.tt(kb.pool, hsd[:, i, 256:512], tb[:], tf[:], ALU.subtract)
                        kb.stt(kb.dve, tf[:], tf[:], -1.0, tf[:], ALU.mult, ALU.max)
                        kb.stt(kb.dve, tb[:], tb[:], -1.0, tb[:], ALU.mult, ALU.max)
                        kb.tt(kb.dve, tf[:], tf[:], tb[:], ALU.add)
                        if i == 0:
                            kb.cp(kb.dve, absacc[:], tf[:])
                        else:
                            kb.tt(kb.dve, absacc[:], absacc[:], tf[:], ALU.add)
                    for ct in range(2):
                        kb.mm(pn[:, ct:ct + 1], absacc[:, ct * 128:(ct + 1) * 128], self.ones_f[:, 0:1])
                    kb.ts(kb.dve, rn[:, 2:4], pn[:, 0:2], EPS, None, ALU.add)
                    kb.op(kb.dve, lambda h: h.reciprocal(rn.t[:, 0:2], rn.t[:, 2:4]), reads=[rn.b], writes=[rn.b])
                    kb.ts(kb.dve, rn[:, 0:2], rn[:, 0:2], 2.0 / N, None, ALU.mult)
                    kb.barrier()
                with contextlib.ExitStack() as f2:
                    tmpg = [kb.sb("tmpg%d" % i, [128, 256], stack=f2) for i in range(2)]

                    def epi_filter(kt, aC, aS):
                        kb.cp(kb.act, G[:, kt, 0:256], aC[:, 0:256])
                        kb.cp(kb.dve, G[:, kt, 256:512], aS[:, 0:256])

                    self.dft_passes(f2, self.din["dftC1_" + tag], self.din["dftS1_" + tag], nt,
                                    lambda n_t: (hsd[:, n_t, 0:256], hsd[:, n_t, 256:512]), 256, epi_filter)
            kb.barrier()
            cw = kb.sb("hcw", [128, 6, 3], stack=ph)
            kb.ld(kb.sp, cw[:], self.din["hy_cw"][li])
            cb = kb.sb("hcb", [128, 6], stack=ph)
            kb.ld(kb.sp, cb[:], self.din["hy_cb"][li])
            skip = kb.sb("hskip", [128, 2], stack=ph)
            kb.ld(kb.sp, skip[:], self.din["hy_skip"][li])
            ng = kb.sb("hng", [128, 2], stack=ph)
            kb.ld(kb.sp, ng[:], self.din["hy_ng"][li])
            zfm = [kb.sb("zfm%d" % i, [128, L], stack=ph) for i in range(2)]
            zT = kb.sb("zT", [128, nt, 256], BF16, stack=ph)
            Y = kb.sb("Y", [128, nt, 512], BF16, stack=ph)
            with contextlib.ExitStack() as z1:
                pin1 = kb.sb("pin1", [128, L + 2], stack=z1)
                pin2 = kb.sb("pin2", [128, L + 2], stack=z1)
                u1 = kb.sb("u1", [128, L], stack=z1)
                ptz = [kb.ps("ptz%d" % i, [128, 256], stack=z1) for i in range(2)]
                for pin in (pin1, pin2):
                    kb.memset(kb.dve, pin[:, 0:1], 0.0)
                    kb.memset(kb.dve, pin[:, L + 1:L + 2], 0.0)
                for ct in range(2):
                    kb.ld(kb.sp, pin1[:, 1:L + 1], self.pT[(2 + ct) * 128:(3 + ct) * 128, tok0:tok0 + L])
                    kb.ld(kb.act, pin2[:, 1:L + 1], self.pT[(4 + ct) * 128:(5 + ct) * 128, tok0:tok0 + L])
                    self.conv3(kb.dve, u1[:], pin1, L, cw, cb, 2 + ct)
                    self.conv3(kb.dve, zfm[ct][:], pin2, L, cw, cb, 4 + ct)
                    kb.tt(kb.dve, zfm[ct][:], zfm[ct][:], u1[:], ALU.mult)
                for i in range(nt):
                    p = ptz[i % 2]
                    for ct in range(2):
                        kb.tr(p[:, ct * 128:(ct + 1) * 128], zfm[ct][:, i * 128:(i + 1) * 128], self.ident_f[:])
                    kb.cp(kb.act if i % 2 else kb.dve, zT[:, i, :], p[:])
                kb.barrier()
            with contextlib.ExitStack() as z2:
                tq = [kb.sb("tq%d" % i, [128, 256], stack=z2) for i in range(4)]

                def epi_fwd(kt, aC, aS):
                    Gr = G[:, kt, 0:256]
                    Gi = G[:, kt, 256:512]
                    kb.tt(kb.dve, tq[0][:], aC[:, 0:256], Gr, ALU.mult)
                    kb.tt(kb.dve, tq[1][:], aS[:, 0:256], Gi, ALU.mult)
                    kb.tt(kb.pool, Y[:, kt, 0:256], tq[0][:], tq[1][:], ALU.add)
                    kb.tt(kb.dve, tq[2][:], aS[:, 0:256], Gr, ALU.mult)
                    kb.tt(kb.dve, tq[3][:], aC[:, 0:256], Gi, ALU.mult)
                    kb.tt(kb.pool, Y[:, kt, 256:512], tq[2][:], tq[3][:], ALU.subtract)

                self.dft_passes(z2, C, S, nt, lambda n_t: zT[:, n_t, :], 256, epi_fwd)
            kb.barrier()
            with contextlib.ExitStack() as z3:
                acc = [[kb.ps("iacc%d_%d" % (ct, cj), [128, 512], stack=z3) for cj in range(3)] for ct in range(2)]
                prs = kb.ps("prs", [128, 4], stack=z3)
                Cp = [kb.sb("iCp%d" % j, [128, 1536], BF16, stack=z3) for j in range(2)]
                Sp = [kb.sb("iSp%d" % j, [128, 1536], BF16, stack=z3) for j in range(2)]
                pin = [kb.sb("ipin%d" % j, [128, 514], stack=z3) for j in range(2)]
                x0 = kb.sb("x0", [128, 512], stack=z3)
                tt_ = kb.sb("itt", [128, 512], stack=z3)
                sq = [kb.sb("isq%d" % j, [128, 512], stack=z3) for j in range(2)]
                ob = [kb.sb("iob%d" % j, [128, 512], BF16, stack=z3) for j in range(2)]
                r4 = kb.sb("ir4", [128, 8], stack=z3)
                chunks = chunks_of(0, L, 512)
                n = 0
                for g0 in range(0, len(chunks), 3):
                    grp = chunks[g0:g0 + 3]
                    cA = grp[0][0]
                    cB = grp[-1][0] + grp[-1][1]
                    for kt in range(nt):
                        cp_, sp_ = Cp[n % 2], Sp[n % 2]
                        n += 1
                        kb.ld(kb.sp, cp_[:, :cB - cA], C[kt * 128:(kt + 1) * 128, cA:cB])
                        kb.ld(kb.act, sp_[:, :cB - cA], S[kt * 128:(kt + 1) * 128, cA:cB])
                        for cj, (c0, W) in enumerate(grp):
                            for ct in range(2):
                                kb.mm(acc[ct][cj][:, :W], Y[:, kt, ct * 128:(ct + 1) * 128], cp_[:, c0 - cA:c0 - cA + W],
                                      start=(kt == 0), stop=False)
                                kb.mm(acc[ct][cj][:, :W], Y[:, kt, 256 + ct * 128:256 + (ct + 1) * 128], sp_[:, c0 - cA:c0 - cA + W],
                                      start=False, stop=(kt == nt - 1))
                    for cj, (c0, W) in enumerate(grp):
                        nsub = W // 128
                        for ct in range(2):
                            pi = pin[ct]
                            lo = c0 - 1
                            hi = c0 + W + 1
                            dlo = 0
                            dhi = W + 2
                            if c0 == 0:
                                kb.memset(kb.dve, pi[:, 0:1], 0.0)
                                lo = 0
                                dlo = 1
                            if c0 + W == L:
                                kb.memset(kb.dve, pi[:, W + 1:W + 2], 0.0)
                                hi = L
                                dhi = W + 1
                            kb.ld(kb.sp, pi[:, dlo:dhi], self.pT[ct * 128:(ct + 1) * 128, tok0 + lo:tok0 + hi])
                            self.conv3(kb.dve, x0[:, :W], pi, W, cw, cb, ct)
                            kb.ts(kb.dve, tt_[:, :W], acc[ct][cj][:, :W], rn[:, ct:ct + 1], None, ALU.mult)
                            kb.stt(kb.dve, tt_[:, :W], zfm[ct][:, c0:c0 + W], skip[:, ct:ct + 1], tt_[:, :W], ALU.mult, ALU.add)
                            kb.tt(kb.dve, tt_[:, :W], tt_[:, :W], x0[:, :W], ALU.mult)
                            kb.act_(sq[ct][:, :W], tt_[:, :W], AF.Square)
                            kb.act_(ob[ct][:, :W], tt_[:, :W], AF.Copy, scale=ng[:, ct:ct + 1])
                            kb.st(kb.sp, self.mixT[ct * 128:(ct + 1) * 128, tok0 + c0:tok0 + c0 + W], ob[ct][:, :W])
                        kb.tt(kb.dve, sq[0][:, :W], sq[0][:, :W], sq[1][:, :W], ALU.add)
                        for sub in range(nsub):
                            kb.mm(prs[:, sub:sub + 1], sq[0][:, sub * 128:(sub + 1) * 128], self.ones_f[:, 0:1])
                        ti0 = (tok0 + c0) // 128
                        kb.ts(kb.dve, r4[:, 0:nsub], prs[:, 0:nsub], 1.0 / 256.0, EPS, ALU.mult, ALU.add)
                        kb.act_(r4[:, 4:4 + nsub], r4[:, 0:nsub], AF.Sqrt)
                        kb.op(kb.dve, lambda h: h.reciprocal(self.r_hy.t[:, ti0:ti0 + nsub], r4.t[:, 4:4 + nsub]),
                              reads=[r4.b], writes=[self.r_hy.b])
                kb.barrier()

    S5W = 256

    def s5_alloc(self, li, st):
        kb = self.kb
        W5 = self.S5W
        tl = {}
        tl["yT"] = [kb.sb("yT%d" % h, [128, T], stack=st) for h in range(2)]
        tl["uT"] = [kb.sb("uT%d" % h, [128, T], BF16, stack=st) for h in range(2)]
        tl["dn"] = kb.sb("s5dn", [128, 2, 2], stack=st)
        tl["uf"] = kb.sb("uf", [128, 1088], stack=st)
        tl["Bm"] = [[kb.sb("Bm%d%d" % (h, r), [128, 512], BF16, stack=st) for r in range(2)] for h in range(2)]
        tl["col"] = kb.sb("s5col", [128, 3, 8], stack=st)
        tl["thc"] = kb.sb("thc", [128, 8], stack=st)
        tl["rhoc"] = kb.sb("rhoc", [128, 8], stack=st)
        tl["rowb"] = kb.sb("rowb", [128, 3, 1024], stack=st)
        tl["a8"] = kb.sb("s5a8", [128, 8], stack=st)
        tl["k8"] = kb.sb("s5k8", [128, 8], I32, stack=st)
        tl["f8"] = kb.sb("s5f8", [128, 8], stack=st)
        tl["cs256"] = kb.sb("cs256", [128, 8], stack=st)
        tl["sn256"] = kb.sb("sn256", [128, 8], stack=st)
        tl["pt"] = [kb.sb("ppt%d" % i, [128, 512], stack=st) for i in range(8)]
        tl["pki"] = kb.sb("ppki", [128, 512], I32, stack=st)
        tl["braw"] = [kb.sb("braw%d" % r, [128, 512], stack=st) for r in range(2)]
        names = ["ang", "kf", "sinT", "cosT", "t1", "t2", "t3", "t4", "bre", "bim", "gre", "gim"]
        d = {nm: [kb.sb("s5%s%d" % (nm, i), [128, W5], stack=st) for i in range(2)] for nm in names}
        d["ki"] = [kb.sb("s5ki%d" % i, [128, W5], I32, stack=st) for i in range(2)]
        d["hre"] = [kb.sb("hre%d" % i, [128, W5], BF16, stack=st) for i in range(3)]
        d["him"] = [kb.sb("him%d" % i, [128, W5], BF16, stack=st) for i in range(3)]
        d["car"] = kb.sb("car", [128, 2], stack=st)
        d["th0"] = kb.sb("th0", [128, 1], stack=st)
        d["rhoT"] = kb.sb("rhoT", [128, W5], stack=st)
        d["Cm"] = [[kb.sb("Cm%d%d" % (r, i), [128, 128], BF16, stack=st) for r in range(2)] for i in range(2)]
        d["Cf"] = [kb.sb("Cf%d" % r, [128, 128], stack=st) for r in range(2)]
        d["pp"] = [kb.ps("s5pp%d" % i, [128, 512], stack=st) for i in range(2)]
        d["ppb"] = [[Buf(), Buf()] for i in range(2)]
        tl["d"] = d
        return tl

    def s5_scan_gen(self, li, tl):
        kb = self.kb
        W5 = self.S5W
        yT, uT, dn, uf, Bm = tl["yT"], tl["uT"], tl["dn"], tl["uf"], tl["Bm"]
        col, thc, rhoc, rowb, pt, pki, braw, d = tl["col"], tl["thc"], tl["rhoc"], tl["rowb"], tl["pt"], tl["pki"], tl["braw"], tl["d"]
        self._s5step = 0
        self._s5pend = []
        kb.ld(kb.sp, dn[:], self.din["s5_dn"][li])
        for half in range(2):
            for q in range(4):
                kb.ld(kb.sp, uf[:], self.pT[768 + half * 128:768 + (half + 1) * 128, q * 1088:(q + 1) * 1088])
                kb.ts(kb.dve, yT[half][:, q * 1088:(q + 1) * 1088], uf[:], dn[:, 0, half:half + 1], None, ALU.mult)
                kb.cp(kb.pool, uT[half][:, q * 1088:(q + 1) * 1088], uf[:])
                yield
        seq = [(0, CTX)] + chunks_of(CTX, T, W5)
        for d_ in range(2):
            kb.ld(kb.sp, V(rowb.t[:].rearrange("p a n -> p (a n)"), rowb.b), self.din["s5_row"][li, d_].partition_broadcast(128))
            for half in range(2):
                hs_ = slice(half * 512, (half + 1) * 512)
                a_re = rowb[:, 0, hs_]
                a_im = rowb[:, 1, hs_]
                dt, dre, dim, rho, sn, cs, x1, x2 = [p[:] for p in pt]
                kb.act_(dt, rowb[:, 2, hs_], AF.Exp)
                kb.tt(kb.dve, dre, dt, a_re, ALU.mult)
                kb.tt(kb.dve, dim, dt, a_im, ALU.mult)
                kb.act_(rho, dre, AF.Exp)
                kb.ts(kb.dve, pki[:], dim, 1.0 / TWO_PI, None, ALU.mult)
                kb.cp(kb.dve, x2, pki[:])
                kb.stt(kb.dve, x1, x2, -TWO_PI, dim, ALU.mult, ALU.add)
                kb.ts(kb.dve, x1, x1, -3.141592, 3.141592, ALU.max, ALU.min)
                kb.act_(sn, x1, AF.Sin)
                kb.stt(kb.dve, x2, x1, -1.0, x1, ALU.mult, ALU.max)
                kb.act_(cs, x2, AF.Sin, scale=-1.0, bias=self.halfpi[:])
                kb.tt(kb.dve, cs, cs, rho, ALU.mult)
                kb.ts(kb.dve, cs, cs, -1.0, None, ALU.add)
                kb.tt(kb.dve, sn, sn, rho, ALU.mult)
                kb.tt(kb.dve, dt, a_re, a_re, ALU.mult)
                kb.tt(kb.dve, dre, a_im, a_im, ALU.mult)
                kb.tt(kb.dve, dt, dt, dre, ALU.add)
                kb.op(kb.dve, lambda h: h.reciprocal(pt[0].t[:], pt[0].t[:]), reads=[pt[0].b], writes=[pt[0].b])
                kb.tt(kb.dve, x1, cs, a_re, ALU.mult)
                kb.tt(kb.dve, dre, sn, a_im, ALU.mult)
                kb.tt(kb.dve, x1, x1, dre, ALU.add)
                kb.tt(kb.dve, x1, x1, dt, ALU.mult)
                kb.tt(kb.dve, x2, sn, a_re, ALU.mult)
                kb.tt(kb.dve, dre, cs, a_im, ALU.mult)
                kb.tt(kb.dve, x2, x2, dre, ALU.subtract)
                kb.tt(kb.dve, x2, x2, dt, ALU.mult)
                for r in range(2):
                    kb.ld(kb.sp, braw[r][:], self.din["s5_bT"][li, d_, r, half])
                ta = pt[1][:]
                tb_ = pt[2][:]
                kb.tt(kb.dve, ta, braw[0][:], x1, ALU.mult)
                kb.tt(kb.dve, tb_, braw[1][:], x2, ALU.mult)
                kb.tt(kb.dve, Bm[half][0][:], ta, tb_, ALU.subtract)
                kb.tt(kb.dve, ta, braw[1][:], x1, ALU.mult)
                kb.tt(kb.dve, tb_, braw[0][:], x2, ALU.mult)
                kb.tt(kb.dve, Bm[half][1][:], ta, tb_, ALU.add)
                yield
            kb.ld(kb.sp, col[:], self.din["s5_col"][li, d_])
            kb.act_(col[:, 2, :], col[:, 2, :], AF.Exp)
            kb.tt(kb.dve, thc[:], col[:, 2, :], col[:, 1, :], ALU.mult)
            kb.tt(kb.dve, rhoc[:], col[:, 2, :], col[:, 0, :], ALU.mult)
            kb.act_(rhoc[:], rhoc[:], AF.Exp)
            a8, k8, f8 = tl["a8"], tl["k8"], tl["f8"]
            cs256, sn256 = tl["cs256"], tl["sn256"]
            kb.ts(kb.dve, a8[:], thc[:], float(W5), None, ALU.mult)
            kb.ts(kb.dve, k8[:], a8[:], 1.0 / TWO_PI, None, ALU.mult)
            kb.cp(kb.dve, f8[:], k8[:])
            kb.stt(kb.dve, a8[:], f8[:], -TWO_PI, a8[:], ALU.mult, ALU.add)
            kb.ts(kb.dve, a8[:], a8[:], -3.141592, 3.141592, ALU.max, ALU.min)
            kb.act_(sn256[:], a8[:], AF.Sin)
            kb.stt(kb.dve, f8[:], a8[:], -1.0, a8[:], ALU.mult, ALU.max)
            kb.act_(cs256[:], f8[:], AF.Sin, scale=-1.0, bias=self.halfpi[:])
            if d_ == 0:
                order = [(t0, W, False) for (t0, W) in seq]
            else:
                order = [(0, CTX, True)] + [(t0, W, True) for (t0, W) in reversed(seq[1:])]
            hreL, himL, car, th0, rhoT, CmL, Cf, pp, ppb = d["hre"], d["him"], d["car"], d["th0"], d["rhoT"], d["Cm"], d["Cf"], d["pp"], d["ppb"]
            for pair in range(8):
                half = pair // 4
                c0 = (pair % 4) * 128
                Cm = CmL[pair % 2]
                for r in range(2):
                    kb.ld(kb.sp, Cf[r][:], self.din["s5_cT"][li, d_, r, pair])
                kb.cp(kb.dve, Cm[0][:], Cf[0][:])
                kb.ts(kb.dve, Cm[1][:], Cf[1][:], -1.0, None, ALU.mult)
                kb.ts(kb.dve, rhoT[:], self.iota512[:, :W5], 0.0, rhoc[:, pair:pair + 1], ALU.mult, ALU.add)
                tau0 = 0
                for ci, (t0, W, rev) in enumerate(order):
                    def view(tile):
                        ap = tile.t[:, t0:t0 + W]
                        if rev:
                            ap = ap[:, ::-1]
                        return V(ap, tile.b)
                    stepno = self._s5step
                    self._s5step += 1
                    sp_ = stepno % 2
                    pbr = V(pp[sp_].t[:, 0:W], ppb[sp_][0])
                    pbi = V(pp[sp_].t[:, 256:256 + W], ppb[sp_][1])
                    py = V(pp[1 - sp_].t[:, 0:W], ppb[1 - sp_][0])
                    hre = hreL[stepno % 3]
                    him = himL[stepno % 3]
                    ang, ki, kf, sinT, cosT = d["ang"][sp_], d["ki"][sp_], d["kf"][sp_], d["sinT"][sp_], d["cosT"][sp_]
                    t1, t2, t3, t4 = d["t1"][sp_], d["t2"][sp_], d["t3"][sp_], d["t4"][sp_]
                    bre, bim, gre, gim = d["bre"][sp_], d["bim"][sp_], d["gre"][sp_], d["gim"][sp_]
                    uv = view(uT[half])
                    kb.mm(pbr, Bm[half][0][:, c0:c0 + 128], uv)
                    kb.mm(pbi, Bm[half][1][:, c0:c0 + 128], uv)
                    if ci == 0:
                        kb.ts(kb.dve, ang[:, :W], self.iota512[:, :W], thc[:, pair:pair + 1], None, ALU.mult)
                        kb.ts(kb.dve, ki[:, :W], ang[:, :W], 1.0 / TWO_PI, None, ALU.mult)
                        kb.cp(kb.pool, kf[:, :W], ki[:, :W])
                        kb.stt(kb.dve, ang[:, :W], kf[:, :W], -TWO_PI, ang[:, :W], ALU.mult, ALU.add)
                        kb.ts(kb.dve, ang[:, :W], ang[:, :W], -3.141592, 3.141592, ALU.max, ALU.min)
                        kb.act_(sinT[:, :W], ang[:, :W], AF.Sin)
                        kb.stt(kb.dve, kf[:, :W], ang[:, :W], -1.0, ang[:, :W], ALU.mult, ALU.max)
                        kb.act_(cosT[:, :W], kf[:, :W], AF.Sin, scale=-1.0, bias=self.halfpi[:])
                    else:
                        sinP, cosP = d["sinT"][1 - sp_], d["cosT"][1 - sp_]
                        cD = cs256[:, pair:pair + 1]
                        sD = sn256[:, pair:pair + 1]
                        kb.act_(ang[:, :W], sinP[:, :W], AF.Copy, scale=sD)
                        kb.stt(kb.dve, cosT[:, :W], cosP[:, :W], cD, ang[:, :W], ALU.mult, ALU.subtract)
                        kb.act_(kf[:, :W], cosP[:, :W], AF.Copy, scale=sD)
                        kb.stt(kb.dve, sinT[:, :W], sinP[:, :W], cD, kf[:, :W], ALU.mult, ALU.add)
                    kb.tt(kb.dve, t1[:, :W], pbr, cosT[:, :W], ALU.mult)
                    kb.tt(kb.dve, t2[:, :W], pbi, sinT[:, :W], ALU.mult)
                    kb.tt(kb.pool, bre[:, :W], t1[:, :W], t2[:, :W], ALU.add)
                    kb.tt(kb.dve, t3[:, :W], pbi, cosT[:, :W], ALU.mult)
                    kb.tt(kb.dve, t4[:, :W], pbr, sinT[:, :W], ALU.mult)
                    kb.tt(kb.pool, bim[:, :W], t3[:, :W], t4[:, :W], ALU.subtract)
                    for (g_, b_, cc) in ((gre, bre, 0), (gim, bim, 1)):
                        init = 0.0 if ci == 0 else car.t[:, cc:cc + 1]
                        rd = [rhoT.b, b_.b] + ([] if ci == 0 else [car.b])
                        kb.op(kb.dve, lambda h: h.tensor_tensor_scan(g_.t[:, :W], rhoT.t[:, :W], b_.t[:, :W], init, ALU.mult, ALU.add),
                              reads=rd, writes=[g_.b])
                    kb.cp(kb.act, car[:, 0:1], gre[:, W - 1:W])
                    kb.cp(kb.act, car[:, 1:2], gim[:, W - 1:W])
                    kb.tt(kb.pool, t1[:, :W], gre[:, :W], cosT[:, :W], ALU.mult)
                    kb.tt(kb.pool, t2[:, :W], gim[:, :W], sinT[:, :W], ALU.mult)
                    kb.tt(kb.pool, hre[:, :W], t1[:, :W], t2[:, :W], ALU.subtract)
                    kb.tt(kb.pool, t3[:, :W], gre[:, :W], sinT[:, :W], ALU.mult)
                    kb.tt(kb.pool, t4[:, :W], gim[:, :W], cosT[:, :W], ALU.mult)
                    kb.tt(kb.pool, him[:, :W], t3[:, :W], t4[:, :W], ALU.add)
                    def readout(py=py, Cm=Cm, hre=hre, him=him, W=W, yv=view(yT[half])):
                        kb.mm(py, Cm[0][:], hre[:, :W], start=True, stop=False)
                        kb.mm(py, Cm[1][:], him[:, :W], start=False, stop=True)
                        kb.tt(kb.dve, yv, yv, py, ALU.add)
                    self._s5pend.append(readout)
                    if len(self._s5pend) > 2:
                        self._s5pend.pop(0)()
                    tau0 += W
                    yield
        while self._s5pend:
            self._s5pend.pop(0)()

    def s5_glu(self, li, tl):
        kb = self.kb
        yT, dn = tl["yT"], tl["dn"]
        if "dbg_y" in self.debug:
            dy = self.nc.dram_tensor("dbg_y", [256, T], F32, kind="ExternalOutput").ap()
            for half in range(2):
                kb.st(kb.sp, dy[half * 128:(half + 1) * 128, :], yT[half][:])
        with contextlib.ExitStack() as gl:
            glu = kb.sb("glu", [128, 2, 256], BF16, stack=gl)
            kb.ld(kb.pool, glu[:], self.din["s5_glu"][li].rearrange("(kt p) n -> p kt n", p=128))
            gf = [kb.sb("gf%d" % i, [128, 512], stack=gl) for i in range(2)]
            gb = [kb.sb("gb%d" % i, [128, 512], BF16, stack=gl) for i in range(2)]
            pg = [kb.ps("pg%d" % i, [128, 512], stack=gl) for i in range(2)]
            prs = kb.ps("prs5", [128, 4], stack=gl)
            sg = kb.sb("sg", [128, 512], stack=gl)
            o_ = [kb.sb("s5o%d" % i, [128, 512], stack=gl) for i in range(2)]
            ob = [kb.sb("s5ob%d" % i, [128, 512], BF16, stack=gl) for i in range(2)]
            r4 = kb.sb("s5r4", [128, 8], stack=gl)
            for (t0, W) in TOK_CHUNKS:
                nsub = W // 128
                for half in range(2):
                    kb.act_(gf[half][:, :W], yT[half][:, t0:t0 + W], AF.Gelu)
                    kb.cp(kb.pool, gb[half][:, :W], gf[half][:, :W])
                for mo in range(2):
                    for kt in range(2):
                        kb.mm(pg[mo][:, :W], glu[:, kt, mo * 128:(mo + 1) * 128], gb[kt][:, :W], start=(kt == 0), stop=(kt == 1))
                    kb.act_(sg[:, :W], pg[mo][:, :W], AF.Sigmoid)
                    kb.tt(kb.dve, o_[mo][:, :W], sg[:, :W], gf[mo][:, :W], ALU.mult)
                    kb.act_(ob[mo][:, :W], o_[mo][:, :W], AF.Copy, scale=dn[:, 1, mo:mo + 1])
                    kb.st(kb.sp, self.mixT[256 + mo * 128:256 + (mo + 1) * 128, t0:t0 + W], ob[mo][:, :W])
                    kb.tt(kb.dve, o_[mo][:, :W], o_[mo][:, :W], o_[mo][:, :W], ALU.mult)
                kb.tt(kb.dve, o_[0][:, :W], o_[0][:, :W], o_[1][:, :W], ALU.add)
                for sub in range(nsub):
                    kb.mm(prs[:, sub:sub + 1], o_[0][:, sub * 128:(sub + 1) * 128], self.ones_f[:, 0:1])
                ti0 = t0 // 128
                kb.ts(kb.dve, r4[:, 0:nsub], prs[:, 0:nsub], 1.0 / 256.0, EPS, ALU.mult, ALU.add)
                kb.act_(r4[:, 4:4 + nsub], r4[:, 0:nsub], AF.Sqrt)
                kb.op(kb.dve, lambda h: h.reciprocal(self.r_s5.t[:, ti0:ti0 + nsub], r4.t[:, 4:4 + nsub]),
                      reads=[r4.b], writes=[self.r_s5.b])
            kb.barrier()

    def s5_attn(self, li):
        kb = self.kb
        do_s5 = self.ph("s5")
        do_at = self.ph("attn")
        with contextlib.ExitStack() as st:
            tl = self.s5_alloc(li, st) if do_s5 else None
            gen = self.s5_scan_gen(li, tl) if do_s5 else None
            state = {"n": 0, "done": gen is None}

            def tick():
                if state["done"]:
                    return
                state["n"] += 1
                if state["n"] % self.S5_TICK == 0:
                    try:
                        next(gen)
                    except StopIteration:
                        state["done"] = True

            if do_at:
                self.attn(li, tick)
            if gen is not None:
                for _ in gen:
                    pass
            kb.barrier()
            if do_s5:
                self.s5_glu(li, tl)

    S5_TICK = 7

    def attn(self, li, tick=lambda: None):
        kb = self.kb
        lid = self.layer_ids[li]
        lam_init = 0.8 - 0.6 * math.exp(-0.3 * lid)
        need_ctx = lid < DEPTH - 1
        with contextlib.ExitStack() as ph:
            al = kb.sb("al", [128, 256], stack=ph)
            kb.ld(kb.sp, al[:], self.din["att_l"][li].partition_broadcast(128))
            prod = kb.sb("alp", [128, 128], stack=ph)
            kb.tt(kb.dve, prod[:, 0:64], al[:, 0:64], al[:, 64:128], ALU.mult)
            kb.tt(kb.dve, prod[:, 64:128], al[:, 128:192], al[:, 192:256], ALU.mult)
            s12 = kb.sb("s12", [128, 4], stack=ph)
            for i in range(2):
                kb.op(kb.dve, lambda h: h.reduce_sum(s12.t[:, i:i + 1], prod.t[:, i * 64:(i + 1) * 64], AX.X),
                      reads=[prod.b], writes=[s12.b])
            kb.act_(s12[:, 0:2], s12[:, 0:2], AF.Exp)
            kb.tt(kb.dve, s12[:, 2:3], s12[:, 1:2], s12[:, 0:1], ALU.subtract)
            kb.ts(kb.dve, s12[:, 3:4], s12[:, 2:3], -lam_init, None, ALU.add)
            neglam = s12[:, 3:4]
            gsub = kb.sb("gsub", [128, 128], stack=ph)
            kb.ld(kb.sp, gsub[:], self.din["att_g"][li].partition_broadcast(128))
            kb.ts(kb.dve, gsub[:], gsub[:], 1.0 - lam_init, None, ALU.mult)
            negM = kb.sb("negM", [128, 8], stack=ph)
            kb.tt(kb.dve, negM[:], self.qkmax[:, 0:8], self.qkmax[:, 8:16], ALU.mult)
            kb.act_(negM[:], negM[:], AF.Sqrt)
            kb.ts(kb.dve, negM[:], negM[:], -1.0, None, ALU.mult)
            QTh = [kb.sb("QTh%d" % i, [128, T], BF16, stack=ph) for i in range(1)]
            QrTh = [kb.sb("QrTh%d" % i, [128, LAT], BF16, stack=ph) for i in range(1)]
            KcTh = [kb.sb("KcTh%d" % i, [128, T], BF16, stack=ph) for i in range(1)]
            Vh = [kb.sb("Vh%d" % i, [128, NT, 129], BF16, stack=ph) for i in range(1)]
            pS = [kb.ps("pS%d" % i, [128, 512], stack=ph) for i in range(2)]
            acc = [kb.ps("aacc%d" % i, [128, 512], stack=ph) for i in range(4)]
            ptr = pS[0]
            Pb = [kb.sb("Pb%d" % i, [128, 512], BF16, stack=ph) for i in range(3)]
            om = [kb.sb("om%d" % i, [128, 4, 128], stack=ph) for i in range(2)]
            o_ = kb.sb("ao", [128, 4, 128], stack=ph)
            junk = kb.sb("ajunk", [128, 128], stack=ph)
            ssq = kb.sb("assq", [128, 12], stack=ph)
            rl = kb.sb("arl", [128, 4], stack=ph)
            att = kb.sb("att", [128, 4, 128], stack=ph)
            attT = [kb.sb("attT%d" % i, [128, 512], BF16, stack=ph) for i in range(2)]
            accS = [kb.sb("accS%d" % i, [128, 4, 129], stack=ph) for i in range(2)]
            n = 0
            nchunk = 0
            pend = []
            tk = {"n": 0}

            def tick2(free0):
                tk["n"] += 1
                while pend and pend[0][0] <= tk["n"] and (free0 or not pend[0][2]):
                    pend.pop(0)[1]()
                tick()

            def flush():
                while pend:
                    pend.pop(0)[1]()

            for hh in range(4):
                b_ = 0
                flush()
                kb.ld(kb.sp, QTh[b_][:], self.QT[hh * 128:(hh + 1) * 128, :])
                kb.ld(kb.act, QrTh[b_][:], self.QrT[hh * 128:(hh + 1) * 128, :])
                kb.ld(kb.sp, KcTh[b_][:], self.KcT[hh * 128:(hh + 1) * 128, :])
                kb.ld(kb.act, Vh[b_][:], self.Vaug.rearrange("t p c -> p t c")[:, :, hh * 129:(hh + 1) * 129])
                qchunks = [(CTX + qc * 512, 512, False) for qc in range(8)]
                if need_ctx:
                    qchunks.append((0, CTX, True))
                for (q0, W, isctx) in qchunks:
                    nsub = W // 128
                    kts = [0, 1] if isctx else list(range(NT))
                    for m in range(2):
                        r0 = m * 64

                        def qk(kk):
                            kt = kts[kk]
                            if kt < 2:
                                qv = QTh[b_][r0:r0 + 64, q0:q0 + W]
                            else:
                                qv = QrTh[b_][r0:r0 + 64, q0 - CTX:q0 - CTX + W]
                            kb.mm(pS[(n + kk) % 2][:, :W], KcTh[b_][r0:r0 + 64, kt * 128:(kt + 1) * 128], qv)
                        qk(0)
                        for kk, kt in enumerate(kts):
                            if kk + 1 < len(kts):
                                qk(kk + 1)
                            p = pS[(n + kk) % 2]
                            pb_ = Pb[(n + kk) % 3]
                            kb.act_(pb_[:, :W], p[:, :W], AF.Exp, bias=negM[:, hh * 2 + m:hh * 2 + m + 1])
                            tick2((n + kk + 1) % 2 == 1 or kk + 1 >= len(kts))
                            for sub in range(nsub):
                                kb.mm(acc[sub][:, 0:129], pb_[:, sub * 128:(sub + 1) * 128], Vh[b_][:, kt, :],
                                      start=(kk == 0), stop=(kk == len(kts) - 1))
                        n += len(kts)
                        flush()
                        for sub in range(nsub):
                            kb.cp(kb.act, accS[m][:, sub, :], acc[sub][:, 0:129])

                        def fin_m(m=m, nsub=nsub):
                            for sub in range(nsub):
                                kb.op(kb.dve, lambda h: h.reciprocal(rl.t[:, sub:sub + 1], accS[m].t[:, sub, 128:129]),
                                      reads=[accS[m].b], writes=[rl.b])
                                kb.ts(kb.dve, om[m][:, sub, :], accS[m][:, sub, 0:128], rl[:, sub:sub + 1], None, ALU.mult)
                        pend.append([tk["n"] + 8, fin_m, False])

                    def fin_chunk(nsub=nsub, W=W, q0=q0, hh=hh):
                        nonlocal nchunk
                        for sub in range(nsub):
                            kb.stt(kb.dve, o_[:, sub, :], om[1][:, sub, :], neglam, om[0][:, sub, :], ALU.mult, ALU.add)
                            kb.act_(junk[:], o_[:, sub, :], AF.Square, accum=ssq[:, sub:sub + 1])
                        kb.ts(kb.dve, ssq[:, 4:4 + nsub], ssq[:, 0:nsub], 1.0 / 128.0, EPS, ALU.mult, ALU.add)
                        kb.act_(ssq[:, 8:8 + nsub], ssq[:, 4:4 + nsub], AF.Sqrt)
                        kb.op(kb.dve, lambda h: h.reciprocal(ssq.t[:, 4:4 + nsub], ssq.t[:, 8:8 + nsub]), reads=[ssq.b], writes=[ssq.b])
                        for sub in range(nsub):
                            kb.stt(kb.dve, att[:, sub, :], o_[:, sub, :], ssq[:, 4 + sub:5 + sub], gsub[:], ALU.mult, ALU.mult)
                            kb.tr(ptr[:, sub * 128:(sub + 1) * 128], att[:, sub, :], self.ident_f[:])
                        at = attT[nchunk % 2]
                        nchunk += 1
                        kb.cp(kb.act, at[:, :W], ptr[:, :W])
                        kb.st(kb.sp, self.mixT[512 + hh * 128:512 + (hh + 1) * 128, q0:q0 + W], at[:, :W])
                    pend.append([tk["n"] + 16, fin_chunk, True])
            flush()
            kb.barrier()

    def outproj(self, li):
        kb = self.kb
        lid = self.layer_ids[li]
        need_ctx = lid < DEPTH - 1
        src = self.xsrc(li)
        with contextlib.ExitStack() as ph:
            wo = kb.sb("wo", [128, 8, D], BF16, stack=ph)
            for kt in range(8):
                kb.ld(kb.pool, wo[:, kt, :], self.din["w_out"][li][kt * 128:(kt + 1) * 128, :])
            g1b = [self.load_mod_b(li, which, 2, ph, "g1b%d" % which) for which in range(2)]
            mx = [kb.sb("mx%d" % i, [128, 8, 512], BF16, stack=ph) for i in range(2)]
            xt = [kb.sb("oxt%d" % i, [128, D], stack=ph) for i in range(2)]
            tt_ = [kb.sb("ott%d" % i, [128, D], stack=ph) for i in range(2)]
            pA = kb.ps("pA", [128, D], stack=ph)
            pB = kb.ps("pB", [128, D], stack=ph)
            pC = kb.ps("pC", [128, D], stack=ph)
            n = 0
            for ci, (t0, W) in enumerate(TOK_CHUNKS):
                if ci == 0 and not need_ctx:
                    continue
                which = 1 if ci == 0 else 0
                m_ = mx[ci % 2]
                kb.ld(kb.sp, m_[:, :, :W], self.mixT[:, t0:t0 + W].rearrange("(kt p) t -> p kt t", p=128))
                for ti in range(W // 128):
                    gi = t0 // 128 + ti
                    x = xt[n % 2]
                    t = tt_[n % 2]
                    n += 1
                    kb.ld(kb.act, x[:], src[gi * 128:(gi + 1) * 128, :])
                    for (ps_, kts) in ((pA, (0, 1)), (pB, (2, 3)), (pC, (4, 5, 6, 7))):
                        for hf in range(2):
                            for kt in kts:
                                kb.mm(ps_[:, hf * 512:(hf + 1) * 512], m_[:, kt, ti * 128:(ti + 1) * 128], wo[:, kt, hf * 512:(hf + 1) * 512],
                                      start=(kt == kts[0]), stop=(kt == kts[-1]))
                    kb.ts(kb.dve, t[:], pA[:], self.r_hy[:, gi:gi + 1], None, ALU.mult)
                    kb.stt(kb.dve, t[:], pB[:], self.r_s5[:, gi:gi + 1], t[:], ALU.mult, ALU.add)
                    kb.tt(kb.dve, t[:], t[:], pC[:], ALU.add)
                    kb.tt(kb.dve, t[:], t[:], g1b[which][:], ALU.mult)
                    kb.tt(kb.pool, t[:], t[:], x[:], ALU.add)
                    kb.st(kb.sp, self.Xres[gi * 128:(gi + 1) * 128, :], t[:])
            kb.barrier()

    def moe(self, li):
        kb = self.kb
        nc = self.nc
        lid = self.layer_ids[li]
        need_ctx = lid < DEPTH - 1
        tiles = list(range(NT)) if need_ctx else list(range(2, NT))
        NB = NBLK
        with contextlib.ExitStack() as ph:
            eid = kb.sb("eid", [128, NT, 2], stack=ph)
            gate = kb.sb("gate", [128, NT, 2], stack=ph)
            rsel = kb.sb("rsel", [128, NT, 2], stack=ph)
            dest_i = kb.sb("dest_i", [128, NT, 2], I32, stack=ph)
            cnt_b = kb.sb("cnt_b", [128, 32], stack=ph)
            widx = kb.sb("widx", [128, NB], I32, stack=ph)
            kb.memset(kb.dve, cnt_b[:], 0.0)
            e_iota = kb.sb("e_iota", [128, 32], stack=ph)
            kb.op(kb.pool, lambda h: h.iota(e_iota.t[:], [[1, 32]], base=0, channel_multiplier=0, allow_small_or_imprecise_dtypes=True),
                  writes=[e_iota.b])
            g2b = [self.load_mod_b(li, which, 5, ph, "g2b%d" % which) for which in range(2)]
            bslot = Buf()
            bHs = Buf()
            with contextlib.ExitStack() as sa:
                ab = self.norm_mod_tiles(li, 4 + lid, 3, 4, sa)
                wr = kb.sb("wr", [128, 8, 36], stack=sa)
                kb.ld(kb.sp, wr[:], self.din["moe_wr"][li].rearrange("(kt p) n -> p kt n", p=128))
                brb = kb.sb("brb", [128, 36], stack=sa)
                kb.ld(kb.sp, brb[:], self.din["moe_br"][li].partition_broadcast(128))
                t_lo = tiles[0]
                n_ = NT - t_lo
                lg_all = kb.sb("lg_all", [128, NT, 36], stack=sa)
                zrow = kb.sb("zrow", [1, D], BF16, stack=sa)
                kb.memset(kb.dve, zrow[:], 0.0)
                kb.st(kb.sp, self.Hs[T:T + 1, :], zrow[:], writes=[bHs])
                with contextlib.ExitStack() as p1:
                    xt = [kb.sb("mxt%d" % i, [128, D], stack=p1) for i in range(2)]
                    hn = [kb.sb("mhn%d" % i, [128, D], stack=p1) for i in range(2)]
                    hb = [kb.sb("mhb%d" % i, [128, D], BF16, stack=p1) for i in range(2)]
                    junk = kb.sb("mjunk", [128, D], stack=p1)
                    ss = [kb.sb("mss%d" % i, [128, 4], stack=p1) for i in range(2)]
                    hT = [kb.sb("mhT%d" % i, [128, 8, 128], stack=p1) for i in range(2)]
                    trp = [kb.ps("mtrp%d" % i, [128, D], stack=p1) for i in range(2)]
                    pl = [kb.ps("mpl%d" % i, [128, 64], stack=p1) for i in range(2)]
                    for n, ti in enumerate(tiles):
                        which = 1 if ti < 2 else 0
                        x = xt[n % 2]
                        kb.ld(kb.sp, x[:], self.Xres[ti * 128:(ti + 1) * 128, :])
                        self.norm_tile(x, ab[which], hn[n % 2], ss[n % 2], junk)
                        h16 = hb[n % 2]
                        kb.cp(kb.act, h16[:], hn[n % 2][:])
                        kb.st(kb.sp, self.Hs[ti * 128:(ti + 1) * 128, :], h16[:], writes=[bHs])
                        for kt in range(8):
                            kb.tr(trp[n % 2][:, kt * 128:(kt + 1) * 128], hn[n % 2][:, kt * 128:(kt + 1) * 128], self.ident_f[:])
                        kb.cp(kb.pool if False else kb.dve, V(hT[n % 2].t[:].rearrange("p k t -> p (k t)"), hT[n % 2].b), trp[n % 2][:])
                        for kt in range(8):
                            kb.mm(pl[n % 2][:, 0:36], hT[n % 2][:, kt, :], wr[:, kt, :], start=(kt == 0), stop=(kt == 7))
                        kb.tt(kb.dve, lg_all[:, ti, :], pl[n % 2][:, 0:36], brb[:], ALU.add)
                    kb.barrier()
                TS = slice(t_lo, NT)
                gmax = kb.sb("gmax", [128, NT, 1], stack=sa)
                ohg = kb.sb("ohg", [128, NT, 4], stack=sa)
                gidx = kb.sb("gidx", [128, NT, 1], stack=sa)
                E4 = kb.sb("E4", [128, NT, 4], stack=sa)
                sg = kb.sb("sg", [128, NT, 1], stack=sa)
                pgr = kb.sb("pgr", [128, NT, 1], stack=sa)
                esel = kb.sb("esel", [128, NT, 8], stack=sa)
                etmp = kb.sb("etmp", [128, NT, 8], stack=sa)
                mx8 = kb.sb("mx8", [128, NT, 8], stack=sa)
                ix8 = kb.sb("ix8", [128, NT, 8], U32, stack=sa)
                i12 = kb.sb("i12", [128, NT, 2], stack=sa)
                sm = kb.sb("msm", [128, NT, 4], stack=sa)
                oh = [kb.sb("oh%d" % i, [128, NT, 32], stack=sa) for i in range(2)]
                ohs = kb.sb("ohs", [128, NT, 32], stack=sa)
                cnt_all = kb.sb("cnt_all", [128, NT, 32], stack=sa)
                rk = kb.sb("rk", [128, NT, 32], stack=sa)
                e_io3 = kb.sb("e_io3", [128, 1, 32], stack=sa)
                kb.cp(kb.dve, e_io3[:, 0, :], e_iota[:])

                def B(v, shape):
                    return V(v.ap.to_broadcast(list(shape)), v.buf)

                kb.op(kb.dve, lambda h: h.tensor_reduce(gmax.t[:, TS, 0], lg_all.t[:, TS, 0:4], AX.X, ALU.max), reads=[lg_all.b], writes=[gmax.b])
                kb.tt(kb.dve, ohg[:, TS, :], lg_all[:, TS, 0:4], B(gmax[:, TS, 0:1], (128, n_, 4)), ALU.is_equal)
                kb.ts(kb.dve, gidx[:, TS, :], ohg[:, TS, 3:4], 3.0, None, ALU.mult)
                kb.stt(kb.dve, gidx[:, TS, :], ohg[:, TS, 2:3], 2.0, gidx[:, TS, :], ALU.mult, ALU.add)
                kb.tt(kb.dve, gidx[:, TS, :], gidx[:, TS, :], ohg[:, TS, 1:2], ALU.add)
                kb.tt(kb.dve, E4[:, TS, :], lg_all[:, TS, 0:4], B(gmax[:, TS, 0:1], (128, n_, 4)), ALU.subtract)
                kb.act_(E4[:, TS, :], E4[:, TS, :], AF.Exp)
                kb.op(kb.dve, lambda h: h.tensor_reduce(sg.t[:, TS, 0], E4.t[:, TS, :], AX.X, ALU.add), reads=[E4.b], writes=[sg.b])
                kb.op(kb.dve, lambda h: h.reciprocal(pgr.t[:, TS, :], sg.t[:, TS, :]), reads=[sg.b], writes=[pgr.b])
                for g in range(4):
                    dst = esel if g == 0 else etmp
                    kb.tt(kb.dve, dst[:, TS, :], lg_all[:, TS, 4 + 8 * g:12 + 8 * g], B(ohg[:, TS, g:g + 1], (128, n_, 8)), ALU.mult)
                    if g > 0:
                        kb.tt(kb.dve, esel[:, TS, :], esel[:, TS, :], etmp[:, TS, :], ALU.add)
                for ti in tiles:
                    kb.op(kb.dve, lambda h: h.max(mx8.t[:, ti, :], esel.t[:, ti, :]), reads=[esel.b], writes=[mx8.b])
                for ti in tiles:
                    kb.op(kb.dve, lambda h: h.max_index(ix8.t[:, ti, :], mx8.t[:, ti, :], esel.t[:, ti, :]), reads=[esel.b, mx8.b], writes=[ix8.b])
                kb.cp(kb.dve, i12[:, TS, :], ix8[:, TS, 0:2])
                kb.stt(kb.dve, eid[:, TS, :], B(gidx[:, TS, 0:1], (128, n_, 2)), 8.0, i12[:, TS, :], ALU.mult, ALU.add)
                kb.tt(kb.dve, sm[:, TS, 0:1], mx8[:, TS, 1:2], mx8[:, TS, 0:1], ALU.subtract)
                kb.act_(sm[:, TS, 1:2], sm[:, TS, 0:1], AF.Exp)
                kb.ts(kb.dve, sm[:, TS, 2:3], sm[:, TS, 1:2], 1.0, None, ALU.add)
                kb.op(kb.dve, lambda h: h.reciprocal(sm.t[:, TS, 3:4], sm.t[:, TS, 2:3]), reads=[sm.b], writes=[sm.b])
                kb.tt(kb.dve, gate[:, TS, 0:1], sm[:, TS, 3:4], pgr[:, TS, :], ALU.mult)
                kb.tt(kb.dve, gate[:, TS, 1:2], gate[:, TS, 0:1], sm[:, TS, 1:2], ALU.mult)
                for k in range(2):
                    kb.tt(kb.dve, oh[k][:, TS, :], B(e_io3[:, 0:1, :], (128, n_, 32)), B(eid[:, TS, k:k + 1], (128, n_, 32)), ALU.is_equal)
                kb.tt(kb.dve, ohs[:, TS, :], oh[0][:, TS, :], oh[1][:, TS, :], ALU.add)
                with contextlib.ExitStack() as p2:
                    pr = kb.ps("mpr", [128, 3, 512], stack=p2)
                    pc = kb.ps("mpc", [128, 3, 512], stack=p2)
                    ncols = n_ * 32
                    ohs_f = ohs.t[:, TS, :].rearrange("p t e -> p (t e)")
                    for c3 in range(3):
                        a = c3 * 512
                        b = min(ncols, a + 512)
                        if a >= b:
                            break
                        kb.mm(pr[:, c3, 0:b - a], self.tri[:], V(ohs_f[:, a:b], ohs.b))
                        kb.mm(pc[:, c3, 0:b - a], self.ones_f[:], V(ohs_f[:, a:b], ohs.b))
                    pr_f = pr.t[:].rearrange("p c n -> p (c n)")
                    pc_f = pc.t[:].rearrange("p c n -> p (c n)")
                    kb.memset(kb.dve, cnt_all[:, t_lo, :], 0.0)
                    for ti in range(t_lo + 1, NT):
                        o0 = (ti - 1 - t_lo) * 32
                        kb.tt(kb.dve, cnt_all[:, ti, :], cnt_all[:, ti - 1, :], V(pc_f[:, o0:o0 + 32], pc.b), ALU.add)
                    o0 = (NT - 1 - t_lo) * 32
                    kb.tt(kb.dve, cnt_b[:], cnt_all[:, NT - 1, :], V(pc_f[:, o0:o0 + 32], pc.b), ALU.add)
                    kb.tt(kb.dve, V(rk.t[:, TS, :].rearrange("p t e -> p (t e)"), rk.b), V(cnt_all.t[:, TS, :].rearrange("p t e -> p (t e)"), cnt_all.b),
                          V(pr_f[:, 0:ncols], pr.b), ALU.add)
                    kb.barrier()
                for k in range(2):
                    kb.tt(kb.dve, ohs[:, TS, :], oh[k][:, TS, :], rk[:, TS, :], ALU.mult)
                    kb.op(kb.dve, lambda h: h.tensor_reduce(rsel.t[:, TS, k], ohs.t[:, TS, :], AX.X, ALU.add), reads=[ohs.b], writes=[rsel.b])
                pad = kb.sb("pad", [128, 32], stack=sa)
                pend = kb.sb("pend", [128, 32], stack=sa)
                pstart = kb.sb("pstart", [128, 1, 32], stack=sa)
                padi = kb.sb("padi", [128, 32], I32, stack=sa)
                kb.ts(kb.dve, pad[:], cnt_b[:], float(NSLOT_BLK - 1), 1.0 / NSLOT_BLK, ALU.add, ALU.mult)
                kb.ts(kb.dve, padi[:], pad[:], -(0.5 - 1.0 / 512.0), None, ALU.add)
                kb.cp(kb.dve, pad[:], padi[:])
                kb.ts(kb.dve, pad[:], pad[:], float(NSLOT_BLK), None, ALU.mult)
                kb.op(kb.dve, lambda h: h.tensor_tensor_scan(pend.t[:], self.ones_f.t[:, 0:32], pad.t[:], 0.0, ALU.mult, ALU.add),
                      reads=[pad.b, self.ones_f.b], writes=[pend.b])
                kb.tt(kb.dve, pstart[:, 0, :], pend[:], pad[:], ALU.subtract)
                dsf = kb.sb("dsf", [128, NT, 2], stack=sa)
                for k in range(2):
                    kb.tt(kb.dve, ohs[:, TS, :], oh[k][:, TS, :], B(pstart[:, 0:1, :], (128, n_, 32)), ALU.mult)
                    kb.op(kb.dve, lambda h: h.tensor_reduce(dsf.t[:, TS, k], ohs.t[:, TS, :], AX.X, ALU.add), reads=[ohs.b], writes=[dsf.b])
                kb.tt(kb.dve, dsf[:, TS, :], dsf[:, TS, :], rsel[:, TS, :], ALU.add)
                kb.cp(kb.dve, dest_i[:, TS, :], dsf[:, TS, :])
                NSC = NPAD // 128
                inif = kb.sb("inif", [128, NSC], stack=sa)
                kb.memset(kb.dve, inif[:], float(T))
                inii = kb.sb("inii", [128, NSC], I32, stack=sa)
                kb.cp(kb.dve, inii[:], inif[:])
                kb.st(kb.sp, self.slot_tok.rearrange("(p a) o -> p (a o)", p=128), inii[:], writes=[bslot])
                tokf = kb.sb("tokf", [128, NT], stack=sa)
                kb.op(kb.pool, lambda h: h.iota(tokf.t[:], [[128, NT]], base=0, channel_multiplier=1, allow_small_or_imprecise_dtypes=True),
                      writes=[tokf.b])
                toki = kb.sb("toki", [128, NT], I32, stack=sa)
                kb.cp(kb.dve, toki[:], tokf[:])
                for ti in tiles:
                    for k in range(2):
                        kb.dma(kb.pool, lambda h: h.indirect_dma_start(
                            out=self.slot_tok, out_offset=bass.IndirectOffsetOnAxis(ap=dest_i.t[:, ti, k:k + 1], axis=0),
                            in_=toki.t[:, ti:ti + 1], in_offset=None), reads=[dest_i.b, toki.b, bslot], writes=[bslot])
                jb = kb.sb("jb", [128, NB], stack=sa)
                kb.op(kb.pool, lambda h: h.iota(jb.t[:], [[NSLOT_BLK, NB]], base=0, channel_multiplier=0, allow_small_or_imprecise_dtypes=True),
                      writes=[jb.b])
                be = kb.sb("be", [128, NB], stack=sa)
                cmp_ = kb.sb("cmp", [128, NB], stack=sa)
                kb.memset(kb.dve, be[:], 0.0)
                for e in range(32):
                    kb.ts(kb.dve, cmp_[:], jb[:], pend[:, e:e + 1], None, ALU.is_ge)
                    kb.tt(kb.dve, be[:], be[:], cmp_[:], ALU.add)
                kb.ts(kb.dve, be[:], be[:], 31.0, 128.0, ALU.min, ALU.mult)
                pcol = kb.sb("pcol", [128, 1], stack=sa)
                kb.op(kb.pool, lambda h: h.iota(pcol.t[:], [[1, 1]], base=0, channel_multiplier=1, allow_small_or_imprecise_dtypes=True),
                      writes=[pcol.b])
                kb.ts(kb.dve, be[:], be[:], pcol[:], None, ALU.add)
                kb.cp(kb.dve, widx[:], be[:])
                kb.barrier()
            with contextlib.ExitStack() as sb_:
                W13 = [kb.sb("W13_%d" % i, [128, 2, 8, 512], BF16, stack=sb_) for i in range(2)]
                W2 = [kb.sb("W2_%d" % i, [128, 4, D], BF16, stack=sb_) for i in range(2)]
                sti = [kb.sb("sti%d" % i, [128, 2], I32, stack=sb_) for i in range(2)]
                X = [kb.sb("mX%d" % i, [128, 2, D], BF16, stack=sb_) for i in range(2)]
                XT = kb.sb("mXT", [128, 8, 256], BF16, stack=sb_)
                ptx = [kb.ps("ptx%d" % i, [128, D], BF16, stack=sb_) for i in range(2)]
                pab = [kb.ps("mpab%d" % i, [128, 512], stack=sb_) for i in range(2)]
                pdL = [kb.ps("mpd%d" % i, [128, D], stack=sb_) for i in range(2)]
                saL = [kb.sb("msa%d" % i, [128, 256], stack=sb_) for i in range(2)]
                hid = kb.sb("hid", [128, 4, 256], BF16, stack=sb_)
                yo = [kb.sb("yo%d" % i, [128, D], stack=sb_) for i in range(2)]
                w13src = self.din["moe_w13h_%d" % li]
                w2src = self.din["moe_w2h_%d" % li]
                for j in range(NB):
                    w13 = W13[j % 2]
                    w2 = W2[j % 2]
                    kb.dma(kb.pool, lambda h: h.indirect_dma_start(
                        out=w13.t[:].rearrange("p a k f -> p (a k f)"), out_offset=None, in_=w13src,
                        in_offset=bass.IndirectOffsetOnAxis(ap=widx.t[:, j:j + 1], axis=0)), reads=[widx.b], writes=[w13.b])
                    kb.dma(kb.pool, lambda h: h.indirect_dma_start(
                        out=w2.t[:].rearrange("p k d -> p (k d)"), out_offset=None, in_=w2src,
                        in_offset=bass.IndirectOffsetOnAxis(ap=widx.t[:, j:j + 1], axis=0)), reads=[widx.b], writes=[w2.b])
                    si = sti[j % 2]
                    kb.dma(kb.sp, lambda h: h.dma_start(out=si.t[:], in_=self.slot_tok[j * 256:(j + 1) * 256, :].rearrange("(s p) o -> p (s o)", p=128),
                                                         allow_slow_non_contiguous=True),
                           reads=[bslot], writes=[si.b])
                    x_ = X[j % 2]
                    for s_ in range(2):
                        kb.dma(kb.pool, lambda h: h.indirect_dma_start(
                            out=x_.t[:, s_, :], out_offset=None, in_=self.Hs,
                            in_offset=bass.IndirectOffsetOnAxis(ap=si.t[:, s_:s_ + 1], axis=0)), reads=[si.b, bHs], writes=[x_.b])
                    for s_ in range(2):
                        p = ptx[s_]
                        for kt in range(8):
                            kb.tr(p[:, kt * 128:(kt + 1) * 128], x_[:, s_, kt * 128:(kt + 1) * 128], self.ident_b[:])
                        kb.cp(kb.act if s_ else kb.dve, XT[:, :, s_ * 128:(s_ + 1) * 128], V(p.t[:].rearrange("p (k t) -> p k t", k=8), p.b))
                    for ft in range(4):
                        pa = pab[ft % 2][:, 0:256]
                        pb = pab[ft % 2][:, 256:512]
                        sa_ = saL[ft % 2]
                        for kt in range(8):
                            kb.mm(pa, w13[:, 0, kt, ft * 128:(ft + 1) * 128], XT[:, kt, :], start=(kt == 0), stop=(kt == 7))
                        for kt in range(8):
                            kb.mm(pb, w13[:, 1, kt, ft * 128:(ft + 1) * 128], XT[:, kt, :], start=(kt == 0), stop=(kt == 7))
                        kb.act_(sa_[:], pa, AF.Silu)
                        kb.tt(kb.dve, hid[:, ft, :], sa_[:], pb, ALU.mult)
                    for s_ in range(2):
                        pd = pdL[s_]
                        for hf in range(2):
                            for ft in range(4):
                                kb.mm(pd[:, hf * 512:(hf + 1) * 512], hid[:, ft, s_ * 128:(s_ + 1) * 128], w2[:, ft, hf * 512:(hf + 1) * 512],
                                      start=(ft == 0), stop=(ft == 3))
                        y_ = yo[s_]
                        kb.cp(kb.act if s_ else kb.dve, y_[:], pd[:])
                        kb.st(kb.sp, self.yb[j * 256 + s_ * 128:j * 256 + (s_ + 1) * 128, :], y_[:])
                kb.barrier()
            with contextlib.ExitStack() as sc_:
                Y = [[kb.sb("mY%d%d" % (i, k), [128, D], stack=sc_) for k in range(2)] for i in range(2)]
                xt = [kb.sb("cxt%d" % i, [128, D], stack=sc_) for i in range(2)]
                t_ = [kb.sb("ct%d" % i, [128, D], stack=sc_) for i in range(2)]
                for n, ti in enumerate(tiles):
                    which = 1 if ti < 2 else 0
                    x = xt[n % 2]
                    kb.ld(kb.sp, x[:], self.Xres[ti * 128:(ti + 1) * 128, :])
                    for k in range(2):
                        y_ = Y[n % 2][k]
                        kb.dma(kb.pool, lambda h: h.indirect_dma_start(
                            out=y_.t[:], out_offset=None, in_=self.yb,
                            in_offset=bass.IndirectOffsetOnAxis(ap=dest_i.t[:, ti, k:k + 1], axis=0)), reads=[dest_i.b], writes=[y_.b])
                    t = t_[n % 2]
                    kb.ts(kb.dve, t[:], Y[n % 2][0][:], gate[:, ti, 0:1], None, ALU.mult)
                    kb.stt(kb.dve, t[:], Y[n % 2][1][:], gate[:, ti, 1:2], t[:], ALU.mult, ALU.add)
                    kb.tt(kb.dve, t[:], t[:], g2b[which][:], ALU.mult)
                    kb.tt(kb.dve, t[:], t[:], x[:], ALU.add)
                    kb.st(kb.sp, self.Xres[ti * 128:(ti + 1) * 128, :], t[:])
                kb.barrier()

    def final_norm(self):
        kb = self.kb
        with contextlib.ExitStack() as ph:
            gb = kb.sb("fgb", [128, D], stack=ph)
            kb.ld(kb.sp, gb[:], self.din["gn_rows"][8].partition_broadcast(128))
            xt = [kb.sb("fxt%d" % i, [128, D], stack=ph) for i in range(2)]
            xn = [kb.sb("fxn%d" % i, [128, D], stack=ph) for i in range(2)]
            junk = kb.sb("fjunk", [128, D], stack=ph)
            ss = kb.sb("fss", [128, 4], stack=ph)
            for n in range(LAT // 128):
                x = xt[n % 2]
                o = xn[n % 2]
                kb.ld(kb.sp, x[:], self.Xres[CTX + n * 128:CTX + (n + 1) * 128, :])
                kb.act_(junk[:], x[:], AF.Square, accum=ss[:, 0:1])
                kb.ts(kb.dve, ss[:, 1:2], ss[:, 0:1], 1.0 / D, EPS, ALU.mult, ALU.add)
                kb.act_(ss[:, 2:3], ss[:, 1:2], AF.Sqrt)
                kb.op(kb.dve, lambda h: h.reciprocal(ss.t[:, 3:4], ss.t[:, 2:3]), reads=[ss.b], writes=[ss.b])
                kb.stt(kb.dve, o[:], x[:], ss[:, 3:4], gb[:], ALU.mult, ALU.mult)
                kb.st(kb.sp, self.out[n * 128:(n + 1) * 128, :], o[:])
            kb.barrier()


LAYERED = ("w_mod", "b_mod", "w_in", "w_out", "hy_cw", "hy_cb", "hy_skip", "hy_ng", "hy_w1", "hy_w2", "hy_w3", "hy_bf",
           "s5_row", "s5_col", "s5_bT", "s5_cT", "s5_dn", "s5_glu", "att_l", "att_g", "moe_wr", "moe_br", "moe_w13h", "moe_w2h")


def split_layers(a):
    for k in ("moe_w13h", "moe_w2h"):
        v = a.pop(k)
        for i in range(v.shape[0]):
            a["%s_%d" % (k, i)] = v[i]
    return a


def make_core_arrays(common, core, layer_ids):
    a = {}
    for k, v in common.items():
        if k in LAYERED:
            a[k] = np.ascontiguousarray(v[list(layer_ids)])
        else:
            a[k] = v
    a.update(core)
    return split_layers(a)


def kernel(**inputs):
    common = split_layers(host_common(inputs))
    arrs = []
    for b in range(8):
        a = dict(common)
        a.update(host_core(inputs, b))
        arrs.append(a)
    p = Prog(arrs[0], list(range(DEPTH)))
    nc = p.build()
    res = run_bass_kernel_spmd(nc, arrs, core_ids=list(range(8)))
    out = np.stack([np.asarray(res.results[b]["out"]) for b in range(8)], axis=0)
    return out.astype(np.float32)
```
